# Optimizing a Trainium2 kernel written in Bass

```python
import jax
import jax.numpy as jnp
from jax import lax
import numpy as np

D_MODEL = 2048
BATCH = 1
SEQ = 16384
DEPTH = 2

GRID_W = 64
CTX_LEN = 256
EPS = 1e-6

GLA_HEADS = 4
GLA_DK = 64
GLA_DV = 128
GLA_GATE_RANK = 16
GLA_TAU = 16.0
GLA_CHUNK = 64
ROPE_BASE = 10000.0

NA_HEADS = 8
NA_HD = 128
NA_KH = 8
NA_KW = 16

LRU_WIDTH = 512
LRU_BLOCKS = 4
LRU_CONV = 4
LRU_C = 8.0

N_EXPERTS = 16
EXPERT_FF = 2048
CAPACITY = 2

GLA_QK = GLA_HEADS * GLA_DK
GLA_V = GLA_HEADS * GLA_DV
NA_W = NA_HEADS * NA_HD
MIX_W = GLA_V + NA_W + LRU_WIDTH
IN_SIZES = (GLA_QK, GLA_QK, GLA_V, GLA_V, GLA_GATE_RANK, GLA_GATE_RANK, NA_W, NA_W, NA_W, LRU_WIDTH, LRU_WIDTH)
D_IN = sum(IN_SIZES)

kernel_name = "hybrid_gla_natten_rglru_ecmoe_dit"


def rms_norm(x, gain):
    xf = x.astype(jnp.float32)
    y = xf * lax.rsqrt(jnp.mean(xf * xf, axis=-1, keepdims=True) + EPS)
    return (y * gain.astype(jnp.float32)).astype(x.dtype)


def modulate(h, shift, scale):
    return h * (1 + scale) + shift


def split_in(z):
    out, o = [], 0
    for s in IN_SIZES:
        out.append(z[..., o:o + s])
        o += s
    return out


def to_heads(t, n):
    b, T, _ = t.shape
    return t.reshape(b, T, n, -1).transpose(0, 2, 1, 3)


def from_heads(t):
    b, h, T, d = t.shape
    return t.transpose(0, 2, 1, 3).reshape(b, T, h * d)


def axial_rope(n_tok, dim):
    pos = jnp.arange(n_tok)
    row = (pos // GRID_W).astype(jnp.float32)
    col = (pos % GRID_W).astype(jnp.float32)
    nf = dim // 4
    inv = ROPE_BASE ** (-jnp.arange(nf, dtype=jnp.float32) / nf)
    ang = jnp.concatenate([row[:, None] * inv, col[:, None] * inv], axis=-1)
    return jnp.cos(ang), jnp.sin(ang)


def apply_rope(t, cos, sin):
    t1, t2 = jnp.split(t, 2, axis=-1)
    return jnp.concatenate([t1 * cos - t2 * sin, t1 * sin + t2 * cos], axis=-1)


def gla_decay(lr, w_g, b_g):
    z = (lr @ w_g + b_g).astype(jnp.float32)
    return to_heads(jax.nn.log_sigmoid(z) / GLA_TAU, GLA_HEADS)


def gla_chunked(q, k, v, g, s0):
    B, H, T, dk = q.shape
    dv = v.shape[-1]
    C = GLA_CHUNK
    n = T // C
    q, k, g = (a.reshape(B, H, n, C, dk) for a in (q, k, g))
    v = v.reshape(B, H, n, C, dv)
    gc = jnp.cumsum(g, axis=3)
    g_last = gc[:, :, :, -1:, :]
    qe = q * jnp.exp(gc)
    ke = k * jnp.exp(-gc)
    kd = k * jnp.exp(g_last - gc)
    mask = jnp.tril(jnp.ones((C, C), dtype=bool))
    att = jnp.where(mask, jnp.einsum('bhnid,bhnjd->bhnij', qe, ke), 0.0)
    o_intra = jnp.einsum('bhnij,bhnjv->bhniv', att, v)
    ds = jnp.einsum('bhncd,bhncv->bhndv', kd, v)
    decay = jnp.exp(g_last[:, :, :, 0, :])

    def step(s, inp):
        dec, d = inp
        return dec[..., None] * s + d, s

    s_fin, s_prev = lax.scan(step, s0, (jnp.moveaxis(decay, 2, 0), jnp.moveaxis(ds, 2, 0)))
    s_prev = jnp.moveaxis(s_prev, 0, 2)
    o = o_intra + jnp.einsum('bhncd,bhndv->bhncv', qe, s_prev)
    return o.reshape(B, H, T, dv), s_fin


def gla_state(k, v, g):
    G = jnp.cumsum(g, axis=2)
    w = jnp.exp(G[:, :, -1:, :] - G)
    return jnp.einsum('bhtd,bhtv->bhdv', k * w, v)


def gla_mixer(q, k, v, r, lf, lb, cq, ck, cv, cr, clf, clb, w_gate, b_gate, norm_g, cos, sin, ctx_out):
    f32 = jnp.float32
    scale = GLA_DK ** -0.5
    ql = apply_rope(to_heads(q, GLA_HEADS).astype(f32), cos, sin) * scale
    kl = apply_rope(to_heads(k, GLA_HEADS).astype(f32), cos, sin)
    vl = to_heads(v, GLA_HEADS).astype(f32)
    kc = to_heads(ck, GLA_HEADS).astype(f32)
    vc = to_heads(cv, GLA_HEADS).astype(f32)
    dec_l = (gla_decay(lf, w_gate[0], b_gate[0]), gla_decay(lb, w_gate[1], b_gate[1]))
    dec_c = (gla_decay(clf, w_gate[0], b_gate[0]), gla_decay(clb, w_gate[1], b_gate[1]))
    qc = to_heads(cq, GLA_HEADS).astype(f32) * scale if ctx_out else None
    zeros = jnp.zeros(kc.shape[:2] + (GLA_DK, GLA_DV), f32)
    o_l, o_c = 0.0, 0.0
    for d in range(2):
        fl = (lambda t: jnp.flip(t, axis=2)) if d else (lambda t: t)
        if ctx_out:
            oc_d, s_c = gla_chunked(fl(qc), fl(kc), fl(vc), fl(dec_c[d]), zeros)
            o_c = o_c + fl(oc_d)
        else:
            s_c = gla_state(fl(kc), fl(vc), fl(dec_c[d]))
        ol_d, _ = gla_chunked(fl(ql), fl(kl), fl(vl), fl(dec_l[d]), s_c)
        o_l = o_l + fl(ol_d)
    out_l = from_heads(rms_norm(o_l, norm_g)) * jax.nn.silu(r.astype(f32))
    out_c = from_heads(rms_norm(o_c, norm_g)) * jax.nn.silu(cr.astype(f32)) if ctx_out else None
    return out_l, out_c


def na_latent(q, k, v, kc, vc, rpb):
    B, T, H, hd = q.shape
    rows = T // GRID_W
    kh = min(NA_KH, rows)
    kw = NA_KW
    qg = q.reshape(B, rows, GRID_W, H, hd)
    kg = k.reshape(B, rows, GRID_W, H, hd)
    vg = v.reshape(B, rows, GRID_W, H, hd)
    cols = jnp.arange(GRID_W)
    col_start = jnp.clip(cols - kw // 2, 0, GRID_W - kw)
    col_idx = col_start[:, None] + jnp.arange(kw)
    col_bias_idx = col_idx - cols[:, None] + (NA_KW - 1)
    scale = hd ** -0.5

    def row_block(r):
        rs = jnp.clip(r - kh // 2, 0, rows - kh)
        kb = lax.dynamic_slice_in_dim(kg, rs, kh, axis=1)[:, :, col_idx]
        vb = lax.dynamic_slice_in_dim(vg, rs, kh, axis=1)[:, :, col_idx]
        qb = lax.dynamic_index_in_dim(qg, r, axis=1, keepdims=False)
        row_bias_idx = rs + jnp.arange(kh) - r + (NA_KH - 1)
        bias = rpb[:, row_bias_idx][:, :, col_bias_idx]
        bias = bias.transpose(0, 2, 1, 3).reshape(H, GRID_W, kh * kw)
        s_loc = jnp.einsum('bqhd,bkqwhd->bhqkw', qb, kb).reshape(B, H, GRID_W, kh * kw) * scale + bias
        s_ctx = jnp.einsum('bqhd,bchd->bhqc', qb, kc) * scale
        p = jax.nn.softmax(jnp.concatenate([s_loc, s_ctx], axis=-1).astype(jnp.float32), axis=-1).astype(v.dtype)
        p_loc = p[..., :kh * kw].reshape(B, H, GRID_W, kh, kw)
        p_ctx = p[..., kh * kw:]
        return (jnp.einsum('bhqkw,bkqwhd->bqhd', p_loc, vb)
                + jnp.einsum('bhqc,bchd->bqhd', p_ctx, vc))

    o = lax.map(row_block, jnp.arange(rows))
    return o.transpose(1, 0, 2, 3, 4).reshape(B, T, H * hd)


def na_mixer(q, k, v, cq, ck, cv, qn, kn, rpb, ctx_out):
    heads = lambda t: t.reshape(t.shape[0], t.shape[1], NA_HEADS, NA_HD)
    ql, kl, vl = rms_norm(heads(q), qn), rms_norm(heads(k), kn), heads(v)
    kc, vc = rms_norm(heads(ck), kn), heads(cv)
    out_l = na_latent(ql, kl, vl, kc, vc, rpb)
    out_c = None
    if ctx_out:
        qc = rms_norm(heads(cq), qn)
        s = jnp.einsum('bqhd,bkhd->bhqk', qc, kc) * NA_HD ** -0.5
        p = jax.nn.softmax(s.astype(jnp.float32), axis=-1).astype(vc.dtype)
        out_c = jnp.einsum('bhqk,bkhd->bqhd', p, vc).reshape(cq.shape[0], cq.shape[1], NA_W)
    return out_l, out_c


def centred_conv(x, w, b):
    C = x.shape[-1]
    y = lax.conv_general_dilated(x, w[:, None, :].astype(x.dtype), window_strides=(1,),
                                 padding=[((LRU_CONV - 1) // 2, LRU_CONV // 2)],
                                 dimension_numbers=('NWC', 'WIO', 'NWC'), feature_group_count=C)
    return y + b


def rglru_coeffs(xc, w_a, b_a, w_i, b_i, lam):
    B, T, C = xc.shape
    xb = xc.reshape(B, T, LRU_BLOCKS, C // LRU_BLOCKS)
    r = jax.nn.sigmoid(jnp.einsum('btnc,ncd->btnd', xb, w_a).reshape(B, T, C) + b_a)
    i = jax.nn.sigmoid(jnp.einsum('btnc,ncd->btnd', xb, w_i).reshape(B, T, C) + b_i)
    log_a = LRU_C * r * jax.nn.log_sigmoid(lam)
    a = jnp.exp(log_a)
    bterm = jnp.sqrt(-jnp.expm1(2.0 * log_a)) * (i * xc)
    return a, bterm


def linear_scan(a, b, h0):
    b = b.at[:, 0].add(a[:, 0] * h0)

    def comb(lhs, rhs):
        al, bl = lhs
        ar, br = rhs
        return al * ar, ar * bl + br

    _, h = lax.associative_scan(comb, (a, b), axis=1)
    return h


def lru_mixer(xl, yl, xc, yc, conv_w, conv_b, w_a, b_a, w_i, b_i, lam, ctx_out):
    f32 = jnp.float32
    xl = centred_conv(xl, conv_w, conv_b).astype(f32)
    xc = centred_conv(xc, conv_w, conv_b).astype(f32)
    hl, hc = 0.0, 0.0
    for d in range(2):
        fl = (lambda t: jnp.flip(t, axis=1)) if d else (lambda t: t)
        ac, bc = rglru_coeffs(fl(xc), w_a[d], b_a[d], w_i[d], b_i[d], lam[d])
        h_c = linear_scan(ac, bc, jnp.zeros_like(bc[:, 0]))
        al, bl = rglru_coeffs(fl(xl), w_a[d], b_a[d], w_i[d], b_i[d], lam[d])
        h_l = linear_scan(al, bl, h_c[:, -1])
        hl = hl + fl(h_l)
        if ctx_out:
            hc = hc + fl(h_c)
    out_l = hl * jax.nn.gelu(yl.astype(f32))
    out_c = hc * jax.nn.gelu(yc.astype(f32)) if ctx_out else None
    return out_l, out_c


def expert_choice(h, w_router, w_gate, w_up, w_down):
    B, N, D = h.shape
    cap = CAPACITY * N // N_EXPERTS
    aff = jax.nn.softmax((h @ w_router).astype(jnp.float32), axis=-1)
    top_w, top_idx = lax.top_k(jnp.swapaxes(aff, 1, 2), cap)
    xs = jax.vmap(lambda hb, ib: hb[ib])(h, top_idx)
    hid = jax.nn.silu(jnp.einsum('becd,edf->becf', xs, w_gate)) * jnp.einsum('becd,edf->becf', xs, w_up)
    ye = jnp.einsum('becf,efd->becd', hid, w_down) * top_w[..., None].astype(h.dtype)
    return jax.vmap(lambda yb, ib: jnp.zeros((N, D), yb.dtype).at[ib.reshape(-1)].add(yb.reshape(-1, D)))(ye, top_idx)


def setup_inputs(seed: int = 0) -> dict:
    key = jax.random.key(seed)
    counter = [0]

    def nrm(shape, scale):
        counter[0] += 1
        return jax.random.normal(jax.random.fold_in(key, counter[0]), shape, jnp.float32) * scale

    L, D = DEPTH, D_MODEL
    bw = LRU_WIDTH // LRU_BLOCKS
    counter[0] += 1
    u = jax.random.uniform(jax.random.fold_in(key, counter[0]), (L, 2, LRU_WIDTH), jnp.float32, 0.9, 0.999)
    s = u ** (1.0 / LRU_C)
    return {
        "x": nrm((BATCH, SEQ, D), 1.0),
        "c": nrm((BATCH, D), 1.0),
        "ctx": nrm((BATCH, CTX_LEN, D), 1.0),
        "c_ctx": nrm((D,), 1.0),
        "norm1_g": 1.0 + nrm((L, D), 0.02),
        "norm2_g": 1.0 + nrm((L, D), 0.02),
        "w_mod": nrm((L, D, 6 * D), 0.5 * D ** -0.5),
        "b_mod": nrm((L, 6 * D), 0.02),
        "w_in": nrm((L, D, D_IN), D ** -0.5),
        "gla_w_gate": nrm((L, 2, GLA_GATE_RANK, GLA_QK), GLA_GATE_RANK ** -0.5),
        "gla_b_gate": nrm((L, 2, GLA_QK), 0.5),
        "gla_norm_g": 1.0 + nrm((L, GLA_DV), 0.02),
        "na_q_norm_g": 1.0 + nrm((L, NA_HD), 0.02),
        "na_k_norm_g": 1.0 + nrm((L, NA_HD), 0.02),
        "na_rpb": nrm((L, NA_HEADS, 2 * NA_KH - 1, 2 * NA_KW - 1), 0.1),
        "lru_conv_w": nrm((L, LRU_CONV, LRU_WIDTH), LRU_CONV ** -0.5),
        "lru_conv_b": nrm((L, LRU_WIDTH), 0.02),
        "lru_w_a": nrm((L, 2, LRU_BLOCKS, bw, bw), bw ** -0.5),
        "lru_b_a": nrm((L, 2, LRU_WIDTH), 0.1),
        "lru_w_i": nrm((L, 2, LRU_BLOCKS, bw, bw), bw ** -0.5),
        "lru_b_i": nrm((L, 2, LRU_WIDTH), 0.1),
        "lru_lambda": jnp.log(s) - jnp.log1p(-s),
        "w_out": nrm((L, MIX_W, D), MIX_W ** -0.5),
        "w_router": nrm((L, D, N_EXPERTS), D ** -0.5),
        "w_exp_gate": nrm((L, N_EXPERTS, D, EXPERT_FF), D ** -0.5),
        "w_exp_up": nrm((L, N_EXPERTS, D, EXPERT_FF), D ** -0.5),
        "w_exp_down": nrm((L, N_EXPERTS, EXPERT_FF, D), EXPERT_FF ** -0.5),
    }


def reference(x, c, ctx, c_ctx, norm1_g, norm2_g, w_mod, b_mod, w_in, gla_w_gate, gla_b_gate, gla_norm_g,
              na_q_norm_g, na_k_norm_g, na_rpb, lru_conv_w, lru_conv_b, lru_w_a, lru_b_a, lru_w_i, lru_b_i,
              lru_lambda, w_out, w_router, w_exp_gate, w_exp_up, w_exp_down):
    cos, sin = axial_rope(x.shape[1], GLA_DK)
    for l in range(DEPTH):
        ctx_out = l < DEPTH - 1
        mx = jnp.split((jax.nn.silu(c) @ w_mod[l] + b_mod[l])[:, None, :], 6, axis=-1)
        mc = jnp.split((jax.nn.silu(c_ctx) @ w_mod[l] + b_mod[l])[None, None, :], 6, axis=-1)
        zx = split_in(modulate(rms_norm(x, norm1_g[l]), mx[0], mx[1]) @ w_in[l])
        zc = split_in(modulate(rms_norm(ctx, norm1_g[l]), mc[0], mc[1]) @ w_in[l])
        gla_x, gla_c = gla_mixer(*zx[0:6], *zc[0:6], gla_w_gate[l], gla_b_gate[l], gla_norm_g[l], cos, sin, ctx_out)
        na_x, na_c = na_mixer(*zx[6:9], *zc[6:9], na_q_norm_g[l], na_k_norm_g[l], na_rpb[l], ctx_out)
        lru_x, lru_c = lru_mixer(*zx[9:11], *zc[9:11], lru_conv_w[l], lru_conv_b[l], lru_w_a[l], lru_b_a[l],
                                 lru_w_i[l], lru_b_i[l], lru_lambda[l], ctx_out)
        mix_x = jnp.concatenate([gla_x, na_x.astype(jnp.float32), lru_x], axis=-1).astype(x.dtype)
        x = x + mx[2] * (mix_x @ w_out[l])
        x = x + mx[5] * expert_choice(modulate(rms_norm(x, norm2_g[l]), mx[3], mx[4]),
                                      w_router[l], w_exp_gate[l], w_exp_up[l], w_exp_down[l])
        if ctx_out:
            mix_c = jnp.concatenate([gla_c, na_c.astype(jnp.float32), lru_c], axis=-1).astype(ctx.dtype)
            ctx = ctx + mc[2] * (mix_c @ w_out[l])
            ctx = ctx + mc[5] * expert_choice(modulate(rms_norm(ctx, norm2_g[l]), mc[3], mc[4]),
                                              w_router[l], w_exp_gate[l], w_exp_up[l], w_exp_down[l])
    return x
```

```python
import numpy as np
import concourse.bass as bass
import concourse.mybir as mybir
from concourse.bass_utils import run_bass_kernel_spmd

F32 = mybir.dt.float32
BF16 = mybir.dt.bfloat16
U32 = mybir.dt.uint32
AF = mybir.ActivationFunctionType
ALU = mybir.AluOpType
AX = mybir.AxisListType

D = 2048
KD = D // 128
EPS = 1e-6
GRID_W = 64
D_IN = 5664
C_Q, C_K, C_V, C_R, C_LF, C_LB, C_NQ, C_NK, C_NV, C_LX, C_LY = 0, 256, 512, 1024, 1536, 1552, 1568, 2592, 3616, 4640, 5152


class Cfg:
    def __init__(self, seq=16384, ctx=256, depth=2, ne=16):
        self.seq, self.ctx, self.depth, self.ne = seq, ctx, depth, ne
        self.nt = seq + ctx
        self.ntl = self.nt // 128
        self.rows = seq // GRID_W
        self.cap_l = 2 * seq // ne
        self.cap_c = 2 * ctx // ne


class P:
    def __init__(self, nc):
        self.nc = nc
        self.eng = {"pe": nc.tensor, "dve": nc.vector, "act": nc.scalar, "pool": nc.gpsimd, "sp": nc.sync}
        self.sem = {}
        self.cnt = {e: 0 for e in self.eng}
        self.waited = {e: {} for e in self.eng}
        self.res = {}
        self.dq = {e: 0 for e in self.eng}
        self.NDS = 8
        for e in self.eng:
            self.sem["c_" + e] = nc.semaphore("c_" + e).__enter__()
            for i in range(self.NDS):
                self.sem[f"d_{e}_{i}"] = nc.semaphore(f"d_{e}_{i}").__enter__()

    def _need(self, eng, tok, waits):
        if tok is None:
            return
        name, val = tok
        if self.waited[eng].get(name, 0) >= val:
            return
        self.waited[eng][name] = val
        waits.append(tok)

    def op(self, eng, fn, r=(), w=(), dma=False):
        waits = []
        for k in r:
            st = self.res.get(k)
            if st:
                self._need(eng, st[0], waits)
        for k in w:
            st = self.res.get(k)
            if st:
                self._need(eng, st[0], waits)
                for t in st[1].items():
                    self._need(eng, t, waits)
        if dma:
            j = self.dq[eng]
            self.dq[eng] += 1
            name = f"d_{eng}_{j % self.NDS}"
            val = 16 * (j // self.NDS + 1)
            if j >= self.NDS:
                self._need(eng, (name, val - 16), waits)
            inc = 16
        else:
            self.cnt[eng] += 1
            name, val, inc = "c_" + eng, self.cnt[eng], 1
        e = self.eng[eng]
        for (n, v) in waits:
            e.wait_ge(self.sem[n], v)
        ins = fn(e)
        ins.then_inc(self.sem[name], inc)
        tok = (name, val)
        for k in r:
            st = self.res.setdefault(k, [None, {}])
            st[1][name] = max(st[1].get(name, 0), val)
        for k in w:
            self.res[k] = [tok, {}]
        return tok

    def barrier(self):
        toks = []
        for e in self.eng:
            if self.cnt[e]:
                toks.append(("c_" + e, self.cnt[e]))
            j = self.dq[e]
            for i in range(self.NDS):
                n = (j - 1 - i)
                if n >= 0:
                    toks.append((f"d_{e}_{n % self.NDS}", 16 * (n // self.NDS + 1)))
        for e in self.eng:
            ws = []
            for t in toks:
                self._need(e, t, ws)
            for (n, v) in ws:
                self.eng[e].wait_ge(self.sem[n], v)

    def dma(self, out, in_, r=(), w=(), q="sp", **kw):
        return self.op(q, lambda e: e.dma_start(out=out, in_=in_, **kw), r, w, dma=True)

    def mm(self, out, lhsT, rhs, start, stop, r=(), w=()):
        return self.op("pe", lambda e: e.matmul(out, lhsT, rhs, start=start, stop=stop), r, w)

    def tr(self, out, in_, ident, r=(), w=()):
        return self.op("pe", lambda e: e.transpose(out, in_, ident), r, w)

    def act(self, out, in_, func, r=(), w=(), bias=0.0, scale=1.0, accum=None):
        if accum is None:
            return self.op("act", lambda e: e.activation(out=out, in_=in_, func=func, bias=bias, scale=scale), r, w)
        return self.op("act", lambda e: e.activation(out=out, in_=in_, func=func, bias=bias, scale=scale,
                                                       accum_out=accum), r, w)

    def ts(self, eng, out, in0, s1, s2, op0, op1=None, r=(), w=(), accum=None):
        if op1 is None:
            return self.op(eng, lambda e: e.tensor_scalar(out, in0, s1, None, op0), r, w)
        if accum is not None:
            return self.op(eng, lambda e: e.tensor_scalar(out, in0, s1, s2, op0, op1, accum), r, w)
        return self.op(eng, lambda e: e.tensor_scalar(out, in0, s1, s2, op0, op1), r, w)

    def tt(self, eng, out, in0, in1, op, r=(), w=()):
        return self.op(eng, lambda e: e.tensor_tensor(out, in0, in1, op), r, w)

    def stt(self, out, in0, scalar, in1, op0, op1, r=(), w=()):
        return self.op("dve", lambda e: e.scalar_tensor_tensor(out, in0, scalar, in1, op0, op1), r, w)

    def copy(self, eng, out, in_, r=(), w=()):
        if eng == "act":
            return self.op("act", lambda e: e.copy(out, in_), r, w)
        return self.op(eng, lambda e: e.tensor_copy(out, in_), r, w)


def _na_tile_geom(cfg):
    rows = cfg.rows
    rs = lambda r: min(max(r - 4, 0), rows - 8)
    plo, nch, cls, defs = [], [], [], {}
    for i in range(rows // 2):
        a, b = rs(2 * i), rs(2 * i + 1)
        lo, hi = a // 2, (b + 7) // 2
        key = (a - 2 * i, b - 2 * i - 1, lo - i, hi - lo + 1)
        if key not in defs:
            defs[key] = len(defs)
        plo.append(lo)
        nch.append(hi - lo + 1)
        cls.append(defs[key])
    return plo, nch, cls, defs


def _na_masks(cfg, rpb):
    plo, nch, cls, defs = _na_tile_geom(cfg)
    ncls = len(defs)
    out = np.full((ncls, 8, 5, 128, 128), -30000.0, np.float32)
    kk = np.arange(128)
    kdr, kc = kk // 64, kk % 64
    qdr, qc = kk // 64, kk % 64
    cs = np.clip(qc - 8, 0, GRID_W - 16)
    for key, ci in defs.items():
        a_rel, b_rel, lo_rel, n = key
        for c in range(n):
            krow = 2 * (lo_rel + c) + kdr
            rs_q = np.where(qdr == 0, a_rel, b_rel + 1)
            qrow = qdr
            inr = (krow[:, None] >= rs_q[None, :]) & (krow[:, None] <= rs_q[None, :] + 7)
            inc = (kc[:, None] >= cs[None, :]) & (kc[:, None] <= cs[None, :] + 15)
            ri = np.clip(krow[:, None] - qrow[None, :] + 7, 0, 14)
            cix = np.clip(kc[:, None] - qc[None, :] + 15, 0, 30)
            m = inr & inc
            for h in range(8):
                out[ci, h, c] = np.where(m, rpb[h][ri, cix], np.float32(-30000.0))
    return out, plo, nch, cls


def _rope_tables(cfg):
    pos = np.arange(cfg.seq)
    row = (pos // GRID_W).astype(np.float32)
    col = (pos % GRID_W).astype(np.float32)
    nf = 16
    inv = (np.float32(10000.0) ** (-np.arange(nf, dtype=np.float32) / np.float32(nf))).astype(np.float32)
    ang = np.concatenate([row[:, None] * inv, col[:, None] * inv], axis=-1).astype(np.float32)
    cos, sin = np.cos(ang).astype(np.float32), np.sin(ang).astype(np.float32)
    c_full = np.concatenate([np.ones((cfg.ctx, 32), np.float32), cos], 0)
    s_full = np.concatenate([np.zeros((cfg.ctx, 32), np.float32), sin], 0)
    p = np.arange(128)
    f = p % 32
    sign = np.where((p % 64) < 32, -1.0, 1.0).astype(np.float32)
    ck = c_full[:, f].T.copy()
    sk = (s_full[:, f].T * sign[:, None]).copy()
    sc = np.float32(64 ** -0.5)
    return np.stack([ck * sc, sk * sc, ck, sk]).astype(np.float32)


def build_program(cfg, dbg=()):
    nc = bass.Bass("TRN2", target_bir_lowering=False)
    NT, NTL, L, NE = cfg.nt, cfg.ntl, cfg.depth, cfg.ne
    plo_l, nch_l, cls_l, cdefs = _na_tile_geom(cfg)
    NCLS = len(cdefs)

    def din(name, shape, dt=F32):
        return nc.dram_tensor(name, list(shape), dt, kind="ExternalInput").ap()

    def dscr(name, shape, dt=F32):
        return nc.dram_tensor(name, list(shape), dt, kind="Internal").ap()

    I = dict(
        xin=din("xin", [NT, D]), cT=din("cT", [128, KD, 2]),
        w_mod=din("w_mod", [L, D, 6 * D]), b_mod=din("b_mod", [L, 1, 6 * D]),
        n1g=din("n1g", [L, 128, KD]), n2g=din("n2g", [L, 128, KD]),
        w_in=din("w_in", [L, D, D_IN]), wg=din("wg", [L, 2, 16, 256]), bg=din("bg", [L, 128, 4]),
        gng=din("gng", [L, 1, 128]), nqg=din("nqg", [L, 128, 1]), nkg=din("nkg", [L, 128, 1]),
        bt=din("bt", [L, NCLS, 8, 128, 5, 128]), cw=din("cw", [L, 128, 4, 4]), cb=din("cb", [L, 128, 4]),
        wa=din("wa", [L, 2, 4, 128, 128]), ba=din("ba", [L, 128, 8]), wi=din("wi", [L, 2, 4, 128, 128]),
        bi=din("bi", [L, 128, 8]), lam=din("lam", [L, 128, 8]),
        w_out=din("w_out", [L, D, D]), w_r=din("w_r", [L, D, NE]),
        weg=din("weg", [L, NE, D, D]), weu=din("weu", [L, NE, D, D]), wed=din("wed", [L, NE, D, D]),
        rope=din("rope", [4, 128, NT]), consts=din("consts", [6, 128, 128]),
    )
    yout = nc.dram_tensor("yout", [cfg.seq, D], F32, kind="ExternalOutput").ap()
    dbg_out = {}
    S = dict(
        xcur=dscr("xcur", [NT, D]), modsd=dscr("modsd", [2, 6 * D]), hT=dscr("hTd", [D, NT], BF16),
        gqT=dscr("gqT", [256, NT], BF16), gkT=dscr("gkT", [256, NT], BF16), gv=dscr("gv", [NT, 512], BF16),
        gr=dscr("gr", [NT, 512]), gg=dscr("gg", [2, 256, NT]), go=dscr("go", [NT, 512]),
        nqT=dscr("nqT", [1024, NT], BF16), nkT=dscr("nkT", [1024, NT], BF16), nv=dscr("nv", [NT, 1024], BF16),
        lxT=dscr("lxT", [512, NT]), lyT=dscr("lyT", [512, NT]), hf=dscr("hf", [512, NT]),
        mixT=dscr("mixT", [D, NT], BF16), xn2=dscr("xn2", [NT, D], BF16),
        idxd=dscr("idxd", [NE, cfg.cap_l], U32), wtd=dscr("wtd", [NE, cfg.cap_l]),
        idxc=dscr("idxc", [NE, cfg.cap_c], U32), wtc=dscr("wtc", [NE, cfg.cap_c]),
    )
    p = P(nc)
    from contextlib import ExitStack
    es = ExitStack()

    uid = [0]

    def sb(name, shape, dt=F32, stack=None):
        uid[0] += 1
        return (stack or es).enter_context(nc.sbuf_tensor(f"{name}_u{uid[0]}", list(shape), dt))

    psf = [es.enter_context(nc.psum_tensor(f"psf{i}", [128, 512], F32)) for i in range(6)]
    pst = [es.enter_context(nc.psum_tensor(f"pst{i}", [128, 1024], BF16)) for i in range(2)]
    cst = sb("cst", [128, 6, 128])
    cstb = sb("cstb", [128, 2, 128], BF16)
    p.dma(cst[:], I["consts"].rearrange("c p n -> p c n"), w=["cst"])
    p.copy("dve", cstb[:], cst[:, 0:2, :], r=["cst"], w=["cstb"])
    ident_f, ones_f, maskF, maskB = cst[:, 0, :], cst[:, 1, :], cst[:, 2, :], cst[:, 3, :]
    ident_b, ones_b = cstb[:, 0, :], cstb[:, 1, :]
    modp = sb("modp", [128, 6, KD, 2])
    amul = sb("amul", [128, 2, KD, 2])
    def big_copy(dst, src, rows, step=128, key="bigcopy"):
        for r0 in range(0, rows, step):
            r1 = min(rows, r0 + step)
            p.dma(dst[r0:r1], src[r0:r1], w=[key])

    def finish(names):
        for nm in names:
            o = nc.dram_tensor("dbg_" + nm, list(S[nm].shape), S[nm].dtype, kind="ExternalOutput").ap()
            big_copy(o, S[nm], S[nm].shape[0], key="dbg")
        p.barrier()
        return nc

    big_copy(S["xcur"], I["xin"], NT, key="xcur")
    p.barrier()

    def stream_of(tile):
        return 1 if tile < cfg.ctx // 128 else 0

    supers = [(0, cfg.ctx, 1)] + [(cfg.ctx + i * 512, 512, 0) for i in range(cfg.seq // 512)]

    for l in range(L):
        with ExitStack() as st:
            sc = sb("m_sc", [128, KD, 2], stack=st)
            wm = sb("m_w", [128, KD, 512], stack=st)
            brow = sb("m_b", [2, 512], stack=st)
            mrow = sb("m_r", [2, 512], stack=st)
            p.dma(sc[:], I["cT"], w=["m_sc"])
            p.act(sc[:], sc[:], AF.Silu, r=["m_sc"], w=["m_sc"])
            for cb in range(24):
                p.dma(wm[:], I["w_mod"][l].rearrange("(k p) c -> p k c", p=128)[:, :, cb * 512:(cb + 1) * 512],
                      w=["m_w"])
                p.dma(brow[:], I["b_mod"][l][:, cb * 512:(cb + 1) * 512].partition_broadcast(2), w=["m_b"])
                for k in range(KD):
                    p.mm(psf[0][0:2, :], sc[:, k, :], wm[:, k, :], k == 0, k == KD - 1, r=["m_sc", "m_w"], w=["psf0"])
                p.tt("dve", mrow[:], psf[0][0:2, :], brow[:], ALU.add, r=["psf0", "m_b"], w=["m_r"])
                p.dma(S["modsd"][:, cb * 512:(cb + 1) * 512], mrow[:], r=["m_r"], w=["modsd"])
            p.barrier()
            for j in range(6):
                for s_ in range(2):
                    p.dma(modp[:, j, :, s_], S["modsd"][s_, j * D:(j + 1) * D].rearrange("(k p) -> p k", p=128),
                          r=["modsd"], w=["modp"], allow_slow_non_contiguous=True)
            g1 = sb("m_g1", [128, KD], stack=st)
            g2 = sb("m_g2", [128, KD], stack=st)
            p.dma(g1[:], I["n1g"][l], w=["m_g1"])
            p.dma(g2[:], I["n2g"][l], w=["m_g2"])
            for s_ in range(2):
                p.stt(amul[:, 0, :, s_], modp[:, 1, :, s_], 1.0, g1[:], ALU.add, ALU.mult, r=["modp", "m_g1"], w=["amul"])
                p.stt(amul[:, 1, :, s_], modp[:, 4, :, s_], 1.0, g2[:], ALU.add, ALU.mult, r=["modp", "m_g2"], w=["amul"])
            p.barrier()

        if "mods" in dbg:
            return finish(["modsd"])

        def norm_stage(st, which, dst_hT, dst_xn=None, router=None):
            xt = [sb(f"n_x{i}", [128, D], stack=st) for i in range(2)]
            junk = sb("n_junk", [128, D], BF16, stack=st)
            xn = [sb(f"n_xn{i}", [128, D], BF16, stack=st) for i in range(2)]
            hTt = [sb(f"n_hT{i}", [128, KD, 128], BF16, stack=st) for i in range(2)]
            stat = sb("n_stat", [128, 4], stack=st)
            for t in range(NTL):
                b = t % 2
                s_ = stream_of(t)
                p.dma(xt[b][:], S["xcur"][t * 128:(t + 1) * 128, :], r=["xcur"], w=[f"n_x{b}"])
                p.act(junk[:], xt[b][:], AF.Square, r=[f"n_x{b}"], w=["n_junk", "n_ss"], accum=stat[:, 0:1])
                p.act(stat[:, 1:2], stat[:, 0:1], AF.Sqrt, r=["n_ss"], w=["n_sd"], scale=1.0 / D, bias=EPS)
                p.op("dve", lambda e: e.reciprocal(stat[:, 2:3], stat[:, 1:2]), r=["n_sd"], w=["n_rs"])
                p.ts("dve", xn[b][:], xt[b][:], stat[:, 2:3], None, ALU.mult, r=[f"n_x{b}", "n_rs"], w=[f"n_xn{b}"])
                if dst_xn is not None:
                    p.dma(dst_xn[t * 128:(t + 1) * 128, :], xn[b][:], r=[f"n_xn{b}"], w=["xn2"])
                for k in range(KD):
                    pb = pst[k // 8]
                    p.tr(pb[:, (k % 8) * 128:(k % 8 + 1) * 128], xn[b][:, k * 128:(k + 1) * 128], ident_b,
                         r=[f"n_xn{b}", "cstb"], w=[f"pst{k // 8}"])
                for k in range(KD):
                    pb = pst[k // 8]
                    src = pb[:, (k % 8) * 128:(k % 8 + 1) * 128]
                    if k % 2 == 0:
                        p.ts("dve", hTt[b][:, k, :], src, amul[:, which, k, s_:s_ + 1], modp[:, 3 * which, k, s_:s_ + 1],
                             ALU.mult, ALU.add, r=[f"pst{k // 8}", "amul", "modp"], w=[f"n_hT{b}"])
                    else:
                        p.act(hTt[b][:, k, :], src, AF.Identity, r=[f"pst{k // 8}", "amul", "modp"], w=[f"n_hT{b}"],
                              scale=amul[:, which, k, s_:s_ + 1], bias=modp[:, 3 * which, k, s_:s_ + 1])
                if dst_hT is not None:
                    p.dma(dst_hT.rearrange("(k p) n -> p k n", p=128)[:, :, t * 128:(t + 1) * 128], hTt[b][:],
                          r=[f"n_hT{b}"], w=["hTd"])
                if router is not None:
                    router(t, hTt[b], f"n_hT{b}")

        with ExitStack() as st:
            norm_stage(st, 0, S["hT"])
            p.barrier()
        if "norm" in dbg:
            return finish(["hT"])

        with ExitStack() as st:
            wb = sb("i_w", [128, KD, 1024], BF16, stack=st)
            wsw = sb("i_wsw", [128, KD, 512], BF16, stack=st)
            hs = [sb(f"i_h{i}", [128, KD, 512], BF16, stack=st) for i in range(2)]
            rp = sb("i_rope", [128, 4, 512], stack=st)
            ev = [sb(f"i_ev{i}", [128, 512], stack=st) for i in range(4)]
            evb = [sb(f"i_evb{i}", [128, 512], BF16, stack=st) for i in range(2)]
            lrt = sb("i_lr", [16, 2, 512], BF16, stack=st)
            wgs = sb("i_wg", [16, 2, 256], BF16, stack=st)
            bgs = sb("i_bg", [128, 4], stack=st)
            nbg = sb("i_nbg", [128, 4], stack=st)
            nqk = sb("i_nqk", [128, 2], stack=st)
            p.dma(wgs[:], I["wg"][l].rearrange("d r c -> r d c"), w=["i_wg"], q="pool")
            p.dma(bgs[:], I["bg"][l], w=["i_bg"])
            p.ts("dve", nbg[:], bgs[:], -1.0, None, ALU.mult, r=["i_bg"], w=["i_nbg"])
            p.dma(nqk[:, 0:1], I["nqg"][l], w=["i_nqk"])
            p.dma(nqk[:, 1:2], I["nkg"][l], w=["i_nqk"])
            p.ts("dve", nqk[:, 0:1], nqk[:, 0:1], float(128 ** -0.5), None, ALU.mult, r=["i_nqk"], w=["i_nqk"])
            wsrc = I["w_in"][l].rearrange("(k p) c -> p k c", p=128)

            def load_w(c0, n):
                for k0 in range(0, KD, 4):
                    p.dma(wb[:, k0:k0 + 4, 0:n], wsrc[:, k0:k0 + 4, c0:c0 + n], w=["i_w"], q="pool")

            def load_h(si):
                t0, ntok, s_ = supers[si]
                b = si % 2
                p.dma(hs[b][:, :, 0:ntok], S["hT"].rearrange("(k p) n -> p k n", p=128)[:, :, t0:t0 + ntok],
                      r=["hTd"], w=[f"i_h{b}"])
                return hs[b], f"i_h{b}", t0, ntok

            def fm_mm(ps, pskey, wtile, wkey, c0, m, h, hkey, ntok):
                for k in range(KD):
                    p.mm(ps[0:m, 0:ntok], wtile[:, k, c0:c0 + m], h[:, k, 0:ntok], k == 0, k == KD - 1,
                         r=[wkey, hkey], w=[pskey])

            load_w(C_Q, 512)
            p.dma(wb[:, :, 512:544], wsrc[:, :, C_LF:C_LF + 32], w=["i_w"], q="pool")
            w5 = wb[:, :, 0:512].rearrange("p k (h two f) -> p k h two f", two=2, f=32)
            s5 = wsw[:].rearrange("p k (h two f) -> p k h two f", two=2, f=32)
            p.copy("dve", s5[:, :, :, 0, :], w5[:, :, :, 1, :], r=["i_w"], w=["i_wsw"])
            p.copy("pool", s5[:, :, :, 1, :], w5[:, :, :, 0, :], r=["i_w"], w=["i_wsw"])
            for si in range(len(supers)):
                h, hkey, t0, ntok = load_h(si)
                p.dma(rp[:, :, 0:ntok], I["rope"].rearrange("c p n -> p c n")[:, :, t0:t0 + ntok], w=["i_rope"])
                for qk in range(2):
                    dst = S["gqT"] if qk == 0 else S["gkT"]
                    for hp in range(2):
                        c0 = qk * 256 + hp * 128
                        fm_mm(psf[0], "psf0", wb, "i_w", c0, 128, h, hkey, ntok)
                        fm_mm(psf[1], "psf1", wsw, "i_wsw", c0, 128, h, hkey, ntok)
                        p.tt("dve", ev[0][:, 0:ntok], psf[0][:, 0:ntok], rp[:, 2 * qk, 0:ntok], ALU.mult,
                             r=["psf0", "i_rope"], w=["i_ev0"])
                        p.tt("dve", ev[1][:, 0:ntok], psf[1][:, 0:ntok], rp[:, 2 * qk + 1, 0:ntok], ALU.mult,
                             r=["psf1", "i_rope"], w=["i_ev1"])
                        p.tt("pool", evb[0][:, 0:ntok], ev[0][:, 0:ntok], ev[1][:, 0:ntok], ALU.add,
                             r=["i_ev0", "i_ev1"], w=["i_evb0"])
                        p.dma(dst[hp * 128:(hp + 1) * 128, t0:t0 + ntok], evb[0][:, 0:ntok], r=["i_evb0"], w=["gqk"])
                for d_ in range(2):
                    fm_mm(psf[2], "psf2", wb, "i_w", 512 + 16 * d_, 16, h, hkey, ntok)
                    p.copy("act", lrt[:, d_, 0:ntok], psf[2][0:16, 0:ntok], r=["psf2"], w=["i_lr"])
                for d_ in range(2):
                    for hp in range(2):
                        p.mm(psf[3][:, 0:ntok], wgs[:, d_, hp * 128:(hp + 1) * 128], lrt[:, d_, 0:ntok], True, True,
                             r=["i_wg", "i_lr"], w=["psf3"])
                        p.act(ev[2][:, 0:ntok], psf[3][:, 0:ntok], AF.Exp, r=["psf3", "i_nbg"], w=["i_ev2"],
                              scale=-1.0, bias=nbg[:, d_ * 2 + hp:d_ * 2 + hp + 1])
                        p.act(ev[2][:, 0:ntok], ev[2][:, 0:ntok], AF.Ln, r=["i_ev2"], w=["i_ev2"], bias=1.0)
                        p.ts("dve", ev[3][:, 0:ntok], ev[2][:, 0:ntok], -1.0 / 16.0, None, ALU.mult, r=["i_ev2"], w=["i_ev3"])
                        p.dma(S["gg"][d_, hp * 128:(hp + 1) * 128, t0:t0 + ntok], ev[3][:, 0:ntok], r=["i_ev3"], w=["gg"])

            if "ip1" in dbg:
                return finish(["gqT", "gkT", "gg"])

            def tok_group(c0, ncols, dst, dst_dt_bf, silu):
                load_w(c0, ncols)
                for si in range(len(supers)):
                    h, hkey, t0, ntok = load_h(si)
                    for tt_ in range(ntok // 128):
                        for cb in range(ncols // 512):
                            ps, pk = psf[(tt_ * 2 + cb) % 4], f"psf{(tt_ * 2 + cb) % 4}"
                            for k in range(KD):
                                p.mm(ps[:, :], h[:, k, tt_ * 128:(tt_ + 1) * 128], wb[:, k, cb * 512:(cb + 1) * 512],
                                     k == 0, k == KD - 1, r=["i_w", hkey], w=[pk])
                            tok0 = t0 + tt_ * 128
                            if dst_dt_bf:
                                e_, ek = evb[cb % 2], f"i_evb{cb % 2}"
                                p.copy("act" if cb % 2 else "dve", e_[:], ps[:, :], r=[pk], w=[ek])
                            else:
                                e_, ek = ev[cb % 2], f"i_ev{cb % 2}"
                                p.act(e_[:], ps[:, :], AF.Silu if silu else AF.Copy, r=[pk], w=[ek])
                            p.dma(dst[tok0:tok0 + 128, cb * 512:(cb + 1) * 512], e_[:], r=[ek], w=["tokdst"])

            tok_group(C_V, 512, S["gv"], True, False)
            tok_group(C_R, 512, S["gr"], False, True)
            tok_group(C_NV, 1024, S["nv"], True, False)

            if "ip2" in dbg:
                return finish(["gv", "gr", "nv"])
            for qk in range(2):
                load_w(C_NQ if qk == 0 else C_NK, 1024)
                dst = S["nqT"] if qk == 0 else S["nkT"]
                for si in range(len(supers)):
                    h, hkey, t0, ntok = load_h(si)
                    for hd in range(8):
                        fm_mm(psf[0], "psf0", wb, "i_w", hd * 128, 128, h, hkey, ntok)
                        import os
                        NAV = int(os.environ.get("NAV", "9"))
                        p.copy("dve", ev[0][:, 0:ntok], psf[0][:, 0:ntok], r=["psf0"], w=["i_ev0"])
                        if NAV >= 2:
                            p.tt("dve", evb[1][:, 0:ntok], ev[0][:, 0:ntok], ev[0][:, 0:ntok], ALU.mult, r=["i_ev0"], w=["i_evb1"])
                            p.mm(psf[1][:, 0:ntok], ones_b, evb[1][:, 0:ntok], True, True, r=["cstb", "i_evb1"], w=["psf1"])
                        if NAV >= 3:
                            p.act(ev[2][:, 0:ntok], psf[1][:, 0:ntok], AF.Sqrt, r=["psf1"], w=["i_ev2"], scale=1.0 / 128, bias=EPS)
                        if NAV >= 4:
                            p.op("dve", lambda e: e.reciprocal(ev[3][:, 0:ntok], ev[2][:, 0:ntok]), r=["i_ev2"], w=["i_ev3"])
                        if NAV >= 5:
                            p.stt(evb[0][:, 0:ntok], ev[0][:, 0:ntok], nqk[:, qk:qk + 1], ev[3][:, 0:ntok], ALU.mult, ALU.mult,
                                  r=["i_ev0", "i_ev3", "i_nqk"], w=["i_evb0"])
                        else:
                            p.copy("dve", evb[0][:, 0:ntok], ev[0][:, 0:ntok], r=["i_ev0"], w=["i_evb0"])
                        p.dma(dst[hd * 128:(hd + 1) * 128, t0:t0 + ntok], evb[0][:, 0:ntok], r=["i_evb0"], w=["nqk"])

            if "ip3" in dbg:
                return finish(["nqT", "nkT"])
            load_w(C_LX, 1024)
            for si in range(len(supers)):
                h, hkey, t0, ntok = load_h(si)
                for c in range(8):
                    ps, pk = psf[c % 4], f"psf{c % 4}"
                    fm_mm(ps, pk, wb, "i_w", c * 128, 128, h, hkey, ntok)
                    e_, ek = ev[c % 4], f"i_ev{c % 4}"
                    p.copy("act" if c % 2 else "dve", e_[:, 0:ntok], ps[:, 0:ntok], r=[pk], w=[ek])
                    dst = S["lxT"] if c < 4 else S["lyT"]
                    p.dma(dst[(c % 4) * 128:(c % 4 + 1) * 128, t0:t0 + ntok], e_[:, 0:ntok], r=[ek], w=["lxy"])
            p.barrier()

        if "inproj" in dbg:
            return finish(["gqT", "gkT", "gg", "gv", "gr", "nqT", "nkT", "nv", "lxT", "lyT"])

        nct = cfg.ctx // 128
        with ExitStack() as st:
            BL = min(1024, cfg.seq)
            blocks = [(0, cfg.ctx)] + [(cfg.ctx + i * BL, BL) for i in range(cfg.seq // BL)]
            qb = sb("g_q", [128, BL], BF16, stack=st)
            kb = sb("g_k", [128, BL], BF16, stack=st)
            gb = sb("g_g", [128, BL], stack=st)
            vb = sb("g_v", [128, BL // 128, 256], BF16, stack=st)
            gc = sb("g_gc", [128, 128], stack=st)
            Hh = sb("g_H", [128, 128], stack=st)
            ex = sb("g_ex", [128, 3, 128], stack=st)
            qkb = sb("g_qkb", [128, 3, 128], BF16, stack=st)
            kdT = sb("g_kdT", [128, 128], BF16, stack=st)
            attm = [sb(f"g_att{i}", [128, 128], BF16, stack=st) for i in range(2)]
            Sst = sb("g_S", [128, 128], stack=st)
            Sbf = sb("g_Sb", [128, 128], BF16, stack=st)
            sm = sb("g_sm", [128, 8], stack=st)
            ob = sb("g_o", [128, 256], stack=st)
            of = sb("g_of", [128, 256], stack=st)
            rr = sb("g_r", [128, 256], stack=st)
            gn = sb("g_gn", [128, 128], stack=st)
            outb = sb("g_outb", [128, 256], BF16, stack=st)
            mT = sb("g_mT", [128, 256], BF16, stack=st)
            junk = sb("g_junk", [128, 128], stack=st)
            p.dma(gn[:], I["gng"][l].partition_broadcast(128), w=["g_gn"])
            for hp in range(2):
                for d_ in range(2):
                    p.op("dve", lambda e: e.memset(Sst[:], 0.0), w=["g_S"])
                    p.op("dve", lambda e: e.memset(Sbf[:], 0.0), w=["g_Sb"])
                    order = blocks if d_ == 0 else [blocks[0]] + blocks[:0:-1]
                    msk = maskF if d_ == 0 else maskB
                    for (t0, ntok) in order:
                        p.dma(qb[:, 0:ntok], S["gqT"][hp * 128:(hp + 1) * 128, t0:t0 + ntok], r=["gqk"], w=["g_q"])
                        p.dma(kb[:, 0:ntok], S["gkT"][hp * 128:(hp + 1) * 128, t0:t0 + ntok], r=["gqk"], w=["g_k"])
                        p.dma(gb[:, 0:ntok], S["gg"][d_, hp * 128:(hp + 1) * 128, t0:t0 + ntok], r=["gg"], w=["g_g"])
                        p.dma(vb[:, 0:ntok // 128, :],
                              S["gv"][t0:t0 + ntok, hp * 256:(hp + 1) * 256].rearrange("(c p) f -> p c f", p=128),
                              r=["tokdst"], w=["g_v"])
                        cs_ = list(range(ntok // 128))
                        if d_ == 1:
                            cs_ = cs_[::-1]
                        for c in cs_:
                            sl = slice(c * 128, (c + 1) * 128)
                            tok = t0 + c * 128
                            p.op("dve", lambda e: e.tensor_tensor_scan(out=gc[:], data0=ones_f, data1=gb[:, sl], initial=0.0,
                                                                         op0=ALU.mult, op1=ALU.add), r=["g_g", "cst"], w=["g_gc"])
                            gl = gc[:, 127:128]
                            p.act(sm[:, 2:3], gl, AF.Exp, r=["g_gc"], w=["g_dec"])
                            if d_ == 0:
                                p.act(ex[:, 0, :], gc[:], AF.Exp, r=["g_gc"], w=["g_ex"])
                                p.act(ex[:, 1, :], gc[:], AF.Exp, r=["g_gc"], w=["g_ex"], scale=-1.0)
                                p.act(ex[:, 2, :], gc[:], AF.Exp, r=["g_gc"], w=["g_ex"], scale=-1.0, bias=gl)
                            else:
                                p.tt("dve", Hh[:], gc[:], gb[:, sl], ALU.subtract, r=["g_gc", "g_g"], w=["g_H"])
                                p.ts("dve", sm[:, 1:2], gl, -1.0, None, ALU.mult, r=["g_gc"], w=["g_ngl"])
                                p.act(ex[:, 0, :], Hh[:], AF.Exp, r=["g_H", "g_gc"], w=["g_ex"], scale=-1.0, bias=gl)
                                p.act(ex[:, 1, :], Hh[:], AF.Exp, r=["g_H", "g_ngl"], w=["g_ex"], bias=sm[:, 1:2])
                                p.act(ex[:, 2, :], Hh[:], AF.Exp, r=["g_H"], w=["g_ex"])
                            p.tt("dve", qkb[:, 0, :], qb[:, sl], ex[:, 0, :], ALU.mult, r=["g_q", "g_ex"], w=["g_qe"])
                            p.tt("pool", qkb[:, 1, :], kb[:, sl], ex[:, 1, :], ALU.mult, r=["g_k", "g_ex"], w=["g_ke"])
                            p.tt("dve", qkb[:, 2, :], kb[:, sl], ex[:, 2, :], ALU.mult, r=["g_k", "g_ex"], w=["g_kd"])
                            p.tr(pst[0][:, 0:128], qkb[:, 2, :], ident_b, r=["g_kd", "cstb"], w=["pst0"])
                            p.copy("act", kdT[:], pst[0][:, 0:128], r=["pst0"], w=["g_kdT"])
                            for hh in range(2):
                                b0 = 64 * hh
                                p.mm(psf[0][:, hh * 128:(hh + 1) * 128], qkb[b0:b0 + 64, 1, :], qkb[b0:b0 + 64, 0, :], True, True,
                                     r=["g_ke", "g_qe"], w=["psf0"])
                                p.tt("dve", attm[hh][:], psf[0][:, hh * 128:(hh + 1) * 128], msk, ALU.mult,
                                     r=["psf0", "cst"], w=[f"g_att{hh}"])
                                p.mm(psf[1][:, hh * 128:(hh + 1) * 128], attm[hh][:], vb[:, c, hh * 128:(hh + 1) * 128], True, False,
                                     r=[f"g_att{hh}", "g_v"], w=["psf1"])
                                p.mm(psf[1][:, hh * 128:(hh + 1) * 128], qkb[b0:b0 + 64, 0, :], Sbf[b0:b0 + 64, :], False, True,
                                     r=["g_qe", "g_Sb"], w=["psf1"])
                            p.mm(psf[2][:, 0:256], kdT[:], vb[:, c, :], True, True, r=["g_kdT", "g_v"], w=["psf2"])
                            if d_ == 0:
                                p.copy("act", ob[:], psf[1][:, 0:256], r=["psf1"], w=["g_o"])
                                p.dma(S["go"][tok:tok + 128, hp * 256:(hp + 1) * 256], ob[:], r=["g_o"], w=["go"])
                            else:
                                p.dma(of[:], S["go"][tok:tok + 128, hp * 256:(hp + 1) * 256], r=["go"], w=["g_of"])
                                p.dma(rr[:], S["gr"][tok:tok + 128, hp * 256:(hp + 1) * 256], r=["tokdst"], w=["g_r"])
                                p.tt("dve", ob[:], psf[1][:, 0:256], of[:], ALU.add, r=["psf1", "g_of"], w=["g_o"])
                                for hh in range(2):
                                    oh = ob[:, hh * 128:(hh + 1) * 128]
                                    p.act(junk[:], oh, AF.Square, r=["g_o"], w=["g_junk", "g_ss"], accum=sm[:, 4 + hh:5 + hh])
                                p.act(sm[:, 6:8], sm[:, 4:6], AF.Sqrt, r=["g_ss"], w=["g_sd"], scale=1.0 / 128, bias=EPS)
                                p.op("dve", lambda e: e.reciprocal(sm[:, 4:6], sm[:, 6:8]), r=["g_sd"], w=["g_rs"])
                                for hh in range(2):
                                    oh = ob[:, hh * 128:(hh + 1) * 128]
                                    p.stt(oh, oh, sm[:, 4 + hh:5 + hh], gn[:], ALU.mult, ALU.mult, r=["g_o", "g_rs", "g_gn"], w=["g_o"])
                                p.tt("dve", outb[:], ob[:], rr[:], ALU.mult, r=["g_o", "g_r"], w=["g_outb"])
                                for hh in range(2):
                                    p.tr(pst[1][:, hh * 128:(hh + 1) * 128], outb[:, hh * 128:(hh + 1) * 128], ident_b,
                                         r=["g_outb", "cstb"], w=["pst1"])
                                p.copy("act", mT[:], pst[1][:, 0:256], r=["pst1"], w=["g_mT"])
                                for hh in range(2):
                                    f0 = (hp * 2 + hh) * 128
                                    p.dma(S["mixT"][f0:f0 + 128, tok:tok + 128], mT[:, hh * 128:(hh + 1) * 128], r=["g_mT"], w=["mixT"])
                            for hh in range(2):
                                b0 = 64 * hh
                                p.stt(Sst[b0:b0 + 64, :], Sst[b0:b0 + 64, :], sm[b0:b0 + 64, 2:3], psf[2][b0:b0 + 64, hh * 128:(hh + 1) * 128],
                                      ALU.mult, ALU.add, r=["g_S", "g_dec", "psf2"], w=["g_S"])
                            p.copy("act", Sbf[:], Sst[:], r=["g_S"], w=["g_Sb"])
            p.barrier()
        if "gla" in dbg:
            return finish(["mixT"])

        with ExitStack() as st:
            BL = min(2048, cfg.seq)
            blocks = [(0, cfg.ctx)] + [(cfg.ctx + i * BL, BL) for i in range(cfg.seq // BL)]
            xp = sb("l_xp", [128, BL + 3], stack=st)
            xc = sb("l_xc", [128, BL], stack=st)
            xcb = sb("l_xcb", [128, BL], BF16, stack=st)
            aa = sb("l_aa", [128, BL], stack=st)
            bb = sb("l_bb", [128, BL], stack=st)
            hh_ = sb("l_hh", [128, BL], stack=st)
            yb = sb("l_yb", [128, BL], stack=st)
            hfb = sb("l_hfb", [128, BL], stack=st)
            lob = sb("l_ob", [128, BL], BF16, stack=st)
            tmp = [sb(f"l_t{i}", [128, 512], stack=st) for i in range(4)]
            hprev = sb("l_hp", [128, 1], stack=st)
            cw_s = sb("l_cw", [128, 4, 4], stack=st)
            cb_s = sb("l_cb", [128, 4], stack=st)
            ba_s = sb("l_ba", [128, 8], stack=st)
            bi_s = sb("l_bi", [128, 8], stack=st)
            c8 = sb("l_c8", [128, 8], stack=st)
            wa_s = sb("l_wa", [128, 2, 4, 128], BF16, stack=st)
            wi_s = sb("l_wi", [128, 2, 4, 128], BF16, stack=st)
            p.dma(cw_s[:], I["cw"][l], w=["l_cw"])
            p.dma(cb_s[:], I["cb"][l], w=["l_cb"])
            p.dma(ba_s[:], I["ba"][l], w=["l_ba"])
            p.dma(bi_s[:], I["bi"][l], w=["l_bi"])
            p.dma(c8[:], I["lam"][l], w=["l_c8"])
            p.dma(wa_s[:], I["wa"][l].rearrange("d n c o -> c d n o"), w=["l_wa"], q="pool")
            p.dma(wi_s[:], I["wi"][l].rearrange("d n c o -> c d n o"), w=["l_wi"], q="pool")
            p.act(c8[:], c8[:], AF.Sigmoid, r=["l_c8"], w=["l_c8"])
            p.act(c8[:], c8[:], AF.Ln, r=["l_c8"], w=["l_c8"])
            p.ts("dve", c8[:], c8[:], 8.0, None, ALU.mult, r=["l_c8"], w=["l_c8"])
            for n in range(4):
                for d_ in range(2):
                    order = blocks if d_ == 0 else [blocks[0]] + blocks[:0:-1]
                    p.op("dve", lambda e: e.memset(hprev[:], 0.0), w=["l_hp"])
                    for (t0, ntok) in order:
                        seg_lo, seg_hi = (0, cfg.ctx) if t0 < cfg.ctx else (cfg.ctx, NT)
                        p.op("pool", lambda e: e.memset(xp[:], 0.0), w=["l_xp"])
                        lo, hi = max(t0 - 1, seg_lo), min(t0 + ntok + 2, seg_hi)
                        p.dma(xp[:, lo - (t0 - 1):hi - (t0 - 1)], S["lxT"][n * 128:(n + 1) * 128, lo:hi], r=["lxy"], w=["l_xp"])
                        p.ts("dve", xc[:, 0:ntok], xp[:, 0:ntok], cw_s[:, n, 0:1], cb_s[:, n:n + 1], ALU.mult, ALU.add,
                             r=["l_xp", "l_cw", "l_cb"], w=["l_xc"])
                        for j in range(1, 4):
                            p.stt(xc[:, 0:ntok], xp[:, j:j + ntok], cw_s[:, n, j:j + 1], xc[:, 0:ntok], ALU.mult, ALU.add,
                                  r=["l_xp", "l_cw", "l_xc"], w=["l_xc"])
                        p.copy("act", xcb[:, 0:ntok], xc[:, 0:ntok], r=["l_xc"], w=["l_xcb"])
                        SUB = min(512, ntok)
                        for s0 in range(0, ntok, SUB):
                            ss_ = slice(s0, s0 + SUB)
                            p.mm(psf[0][:, 0:SUB], wa_s[:, d_, n, :], xcb[:, ss_], True, True, r=["l_wa", "l_xcb"], w=["psf0"])
                            p.mm(psf[1][:, 0:SUB], wi_s[:, d_, n, :], xcb[:, ss_], True, True, r=["l_wi", "l_xcb"], w=["psf1"])
                            p.act(tmp[0][:, 0:SUB], psf[0][:, 0:SUB], AF.Sigmoid, r=["psf0", "l_ba"], w=["l_t0"],
                                  bias=ba_s[:, d_ * 4 + n:d_ * 4 + n + 1])
                            p.act(tmp[1][:, 0:SUB], psf[1][:, 0:SUB], AF.Sigmoid, r=["psf1", "l_bi"], w=["l_t1"],
                                  bias=bi_s[:, d_ * 4 + n:d_ * 4 + n + 1])
                            p.act(aa[:, ss_], tmp[0][:, 0:SUB], AF.Exp, r=["l_t0", "l_c8"], w=["l_aa"],
                                  scale=c8[:, d_ * 4 + n:d_ * 4 + n + 1])
                            p.stt(tmp[2][:, 0:SUB], aa[:, ss_], -1.0, aa[:, ss_], ALU.mult, ALU.mult, r=["l_aa"], w=["l_t2"])
                            p.act(tmp[2][:, 0:SUB], tmp[2][:, 0:SUB], AF.Sqrt, r=["l_t2"], w=["l_t2"], bias=1.0)
                            p.tt("dve", tmp[3][:, 0:SUB], tmp[1][:, 0:SUB], tmp[2][:, 0:SUB], ALU.mult, r=["l_t1", "l_t2"], w=["l_t3"])
                            p.tt("dve", bb[:, ss_], tmp[3][:, 0:SUB], xc[:, ss_], ALU.mult, r=["l_t3", "l_xc"], w=["l_bb"])
                        if d_ == 0:
                            p.op("dve", lambda e: e.tensor_tensor_scan(out=hh_[:, 0:ntok], data0=aa[:, 0:ntok], data1=bb[:, 0:ntok],
                                                                         initial=hprev[:, 0:1], op0=ALU.mult, op1=ALU.add),
                                 r=["l_aa", "l_bb", "l_hp"], w=["l_hh"])
                            p.copy("dve", hprev[:], hh_[:, ntok - 1:ntok], r=["l_hh"], w=["l_hp"])
                            p.dma(S["hf"][n * 128:(n + 1) * 128, t0:t0 + ntok], hh_[:, 0:ntok], r=["l_hh"], w=["hf"])
                        else:
                            p.op("dve", lambda e: e.tensor_tensor_scan(out=hh_[:, 0:ntok][:, ::-1], data0=aa[:, 0:ntok][:, ::-1],
                                                                         data1=bb[:, 0:ntok][:, ::-1], initial=hprev[:, 0:1],
                                                                         op0=ALU.mult, op1=ALU.add),
                                 r=["l_aa", "l_bb", "l_hp"], w=["l_hh"])
                            p.copy("dve", hprev[:], hh_[:, 0:1], r=["l_hh"], w=["l_hp"])
                            p.dma(hfb[:, 0:ntok], S["hf"][n * 128:(n + 1) * 128, t0:t0 + ntok], r=["hf"], w=["l_hfb"])
                            p.dma(yb[:, 0:ntok], S["lyT"][n * 128:(n + 1) * 128, t0:t0 + ntok], r=["lxy"], w=["l_yb"])
                            p.tt("pool", hfb[:, 0:ntok], hfb[:, 0:ntok], hh_[:, 0:ntok], ALU.add, r=["l_hfb", "l_hh"], w=["l_hfb"])
                            p.tt("dve", xc[:, 0:ntok], yb[:, 0:ntok], yb[:, 0:ntok], ALU.mult, r=["l_yb"], w=["l_xc"])
                            p.ts("dve", xc[:, 0:ntok], xc[:, 0:ntok], 0.044715, 1.0, ALU.mult, ALU.add, r=["l_xc"], w=["l_xc"])
                            p.tt("dve", xc[:, 0:ntok], xc[:, 0:ntok], yb[:, 0:ntok], ALU.mult, r=["l_xc", "l_yb"], w=["l_xc"])
                            p.act(xc[:, 0:ntok], xc[:, 0:ntok], AF.Sigmoid, r=["l_xc"], w=["l_xc"], scale=1.5957691216057308)
                            p.tt("dve", xc[:, 0:ntok], xc[:, 0:ntok], yb[:, 0:ntok], ALU.mult, r=["l_xc", "l_yb"], w=["l_xc"])
                            p.tt("dve", lob[:, 0:ntok], xc[:, 0:ntok], hfb[:, 0:ntok], ALU.mult, r=["l_xc", "l_hfb"], w=["l_ob"])
                            p.dma(S["mixT"][1536 + n * 128:1536 + (n + 1) * 128, t0:t0 + ntok], lob[:, 0:ntok], r=["l_ob"], w=["mixT"])
            p.barrier()
        if "lru" in dbg:
            return finish(["mixT"])

        with ExitStack() as st:
            kT = sb("a_k", [128, NT], BF16, stack=st)
            qT = sb("a_q", [128, NT], BF16, stack=st)
            vv = sb("a_v", [128, NTL, 128], BF16, stack=st)
            btm = sb("a_bt", [128, NCLS, 5, 128], stack=st)
            sT = sb("a_s", [128, 5 * 128], stack=st)
            pT = sb("a_p", [128, 8 * 128], BF16, stack=st)
            rs_ = sb("a_rs", [128, 128], stack=st)
            aob = [sb(f"a_ob{i}", [128, 128], BF16, stack=st) for i in range(2)]
            for hd in range(8):
                for c0 in range(0, NT, 2048):
                    c1 = min(NT, c0 + 2048)
                    p.dma(kT[:, c0:c1], S["nkT"][hd * 128:(hd + 1) * 128, c0:c1], r=["nqk"], w=["a_k"])
                    p.dma(qT[:, c0:c1], S["nqT"][hd * 128:(hd + 1) * 128, c0:c1], r=["nqk"], w=["a_q"])
                for c0 in range(0, NTL, 16):
                    c1 = min(NTL, c0 + 16)
                    p.dma(vv[:, c0:c1, :], S["nv"][c0 * 128:c1 * 128, hd * 128:(hd + 1) * 128].rearrange("(t p) f -> p t f", p=128),
                          r=["tokdst"], w=["a_v"])
                for ci in range(NCLS):
                    p.dma(btm[:, ci, :, :], I["bt"][l, ci, hd], w=["a_bt"])
                for t in range(NTL):
                    if t < nct:
                        win, ci = [], 0
                    else:
                        i = t - nct
                        win = [nct + plo_l[i] + c for c in range(nch_l[i])]
                        ci = cls_l[i]
                    keys = win + list(range(nct))
                    nw, nk = len(win), len(keys)
                    for idx, kt in enumerate(keys):
                        bk = idx // 4
                        p.mm(psf[bk][:, (idx % 4) * 128:(idx % 4 + 1) * 128], kT[:, kt * 128:(kt + 1) * 128], qT[:, t * 128:(t + 1) * 128],
                             True, True, r=["a_k", "a_q"], w=[f"psf{bk}"])
                    for bk in range(2):
                        a0, a1 = bk * 4, min(nw, bk * 4 + 4)
                        if a1 > a0:
                            n_ = (a1 - a0) * 128
                            p.tt("dve", sT[:, a0 * 128:a1 * 128], psf[bk][:, 0:n_],
                                 btm[:, ci, a0:a1, :].rearrange("p c q -> p (c q)"), ALU.add, r=[f"psf{bk}", "a_bt"], w=["a_s"])
                    if nw:
                        p.act(pT[:, 0:nw * 128], sT[:, 0:nw * 128], AF.Exp, r=["a_s"], w=["a_p"])
                    for idx in range(nw, nk):
                        bk = idx // 4
                        p.act(pT[:, idx * 128:(idx + 1) * 128], psf[bk][:, (idx % 4) * 128:(idx % 4 + 1) * 128], AF.Exp,
                              r=[f"psf{bk}"], w=["a_p"])
                    for idx, kt in enumerate(keys):
                        p.mm(psf[2][:, 0:128], vv[:, kt, :], pT[:, idx * 128:(idx + 1) * 128], idx == 0, idx == nk - 1,
                             r=["a_v", "a_p"], w=["psf2"])
                    for idx, kt in enumerate(keys):
                        p.mm(psf[3][:, 0:128], ones_b, pT[:, idx * 128:(idx + 1) * 128], idx == 0, idx == nk - 1,
                             r=["cstb", "a_p"], w=["psf3"])
                    p.op("dve", lambda e: e.reciprocal(rs_[:], psf[3][:, 0:128]), r=["psf3"], w=["a_rs"])
                    b = t % 2
                    p.tt("dve", aob[b][:], psf[2][:, 0:128], rs_[:], ALU.mult, r=["psf2", "a_rs"], w=[f"a_ob{b}"])
                    p.dma(S["mixT"][512 + hd * 128:512 + (hd + 1) * 128, t * 128:(t + 1) * 128], aob[b][:], r=[f"a_ob{b}"], w=["mixT"])
            p.barrier()
        if "mix" in dbg:
            return finish(["mixT"])

        with ExitStack() as st:
            wo = sb("o_w", [128, KD, D], BF16, stack=st)
            gbc = sb("o_g", [128, 2, D], stack=st)
            mx_ = [sb(f"o_m{i}", [128, KD, 128], BF16, stack=st) for i in range(2)]
            xt = [sb(f"o_x{i}", [128, D], stack=st) for i in range(2)]
            tq = sb("o_t", [128, 512], stack=st)
            for k0 in range(0, KD, 4):
                p.dma(wo[:, k0:k0 + 4, :], I["w_out"][l].rearrange("(k p) c -> p k c", p=128)[:, k0:k0 + 4, :], w=["o_w"], q="pool")
            for s_ in range(2):
                p.dma(gbc[:, s_, :], S["modsd"][s_:s_ + 1, 2 * D:3 * D].partition_broadcast(128), r=["modsd"], w=["o_g"])
            for t in range(NTL):
                b = t % 2
                s_ = stream_of(t)
                p.dma(mx_[b][:], S["mixT"].rearrange("(k p) n -> p k n", p=128)[:, :, t * 128:(t + 1) * 128], r=["mixT"], w=[f"o_m{b}"])
                p.dma(xt[b][:], S["xcur"][t * 128:(t + 1) * 128, :], r=["xcur"], w=[f"o_x{b}"])
                for cb in range(4):
                    ps, pk = psf[cb], f"psf{cb}"
                    for k in range(KD):
                        p.mm(ps[:, :], mx_[b][:, k, :], wo[:, k, cb * 512:(cb + 1) * 512], k == 0, k == KD - 1, r=[f"o_m{b}", "o_w"], w=[pk])
                    p.tt("dve", tq[:], ps[:, :], gbc[:, s_, cb * 512:(cb + 1) * 512], ALU.mult, r=[pk, "o_g"], w=["o_t"])
                    p.tt("pool", xt[b][:, cb * 512:(cb + 1) * 512], xt[b][:, cb * 512:(cb + 1) * 512], tq[:], ALU.add,
                         r=["o_t", f"o_x{b}"], w=[f"o_x{b}"])
                p.dma(S["xcur"][t * 128:(t + 1) * 128, :], xt[b][:], r=[f"o_x{b}"], w=["xcur"])
            p.barrier()
        if "xmid" in dbg:
            return finish(["xcur"])

        with ExitStack() as st:
            affT = sb("r_affT", [NE, NT], stack=st)
            with ExitStack() as st2:
                wr_s = sb("r_w", [128, KD, NE], BF16, stack=st2)
                lg = sb("r_lg", [128, NE], stack=st2)
                rsm = sb("r_sm", [128, 4], stack=st2)
                p.dma(wr_s[:], I["w_r"][l].rearrange("(k p) e -> p k e", p=128), w=["r_w"], q="pool")

                def router(t, hTt, hkey):
                    for k in range(KD):
                        p.mm(psf[4][:, 0:NE], hTt[:, k, :], wr_s[:, k, :], k == 0, k == KD - 1, r=[hkey, "r_w"], w=["psf4"])
                    p.op("dve", lambda e: e.reduce_max(out=rsm[:, 0:1], in_=psf[4][:, 0:NE], axis=AX.X), r=["psf4"], w=["r_mx"])
                    p.ts("dve", rsm[:, 1:2], rsm[:, 0:1], -1.0, None, ALU.mult, r=["r_mx"], w=["r_nmx"])
                    p.act(lg[:], psf[4][:, 0:NE], AF.Exp, r=["psf4", "r_nmx"], w=["r_lg", "r_sum"], bias=rsm[:, 1:2], accum=rsm[:, 2:3])
                    p.op("dve", lambda e: e.reciprocal(rsm[:, 3:4], rsm[:, 2:3]), r=["r_sum"], w=["r_rs"])
                    p.ts("dve", lg[:], lg[:], rsm[:, 3:4], None, ALU.mult, r=["r_lg", "r_rs"], w=["r_lg"])
                    p.mm(psf[5][0:NE, 0:128], lg[:], ident_f, True, True, r=["r_lg", "cst"], w=["psf5"])
                    p.copy("act", affT[:, t * 128:(t + 1) * 128], psf[5][0:NE, 0:128], r=["psf5"], w=["r_affT"])

                norm_stage(st2, 1, None, dst_xn=S["xn2"], router=router)
                p.barrier()
            with ExitStack() as st2:
                wk = sb("r_wk", [NE, cfg.seq], stack=st2)
                wts = sb("r_wts", [NE, cfg.cap_l], stack=st2)
                ixs = sb("r_ixs", [NE, cfg.cap_l], U32, stack=st2)
                for (c0, n, cap, doff, off) in [(cfg.ctx, cfg.seq, cfg.cap_l, 0, cfg.ctx), (0, cfg.ctx, cfg.cap_c, 1, 0)]:
                    p.copy("dve", wk[:, 0:n], affT[:, c0:c0 + n], r=["r_affT"], w=["r_wk"])
                    for r_ in range(cap // 8):
                        s8 = slice(r_ * 8, r_ * 8 + 8)
                        p.op("dve", lambda e: e.max(out=wts[:, s8], in_=wk[:, 0:n]), r=["r_wk"], w=["r_wts"])
                        p.op("dve", lambda e: e.max_index(out=ixs[:, s8], in_max=wts[:, s8], in_values=wk[:, 0:n]), r=["r_wk", "r_wts"], w=["r_ixs"])
                        p.op("dve", lambda e: e.match_replace(out=wk[:, 0:n], in_to_replace=wts[:, s8], in_values=wk[:, 0:n], imm_value=-1.0),
                             r=["r_wts", "r_wk"], w=["r_wk"])
                    if off:
                        p.ts("dve", ixs[:, 0:cap], ixs[:, 0:cap], float(off), None, ALU.add, r=["r_ixs"], w=["r_ixs"])
                    dI = S["idxd"] if doff == 0 else S["idxc"]
                    dW = S["wtd"] if doff == 0 else S["wtc"]
                    p.dma(dI[:, 0:cap], ixs[:, 0:cap], r=["r_ixs"], w=["idxd"])
                    p.dma(dW[:, 0:cap], wts[:, 0:cap], r=["r_wts"], w=["idxd"])
                p.barrier()
        if "route" in dbg:
            return finish(["idxd", "wtd", "idxc", "wtc"])

        with ExitStack() as st:
            GS = min(1024, cfg.cap_l)
            xsT = sb("e_xsT", [128, KD, GS], BF16, stack=st)
            hid = sb("e_hid", [128, KD, GS], BF16, stack=st)
            wd = sb("e_wd", [128, KD, D], BF16, stack=st)
            wgf = [sb(f"e_wg{i}", [128, KD, 128], BF16, stack=st) for i in range(2)]
            wuf = [sb(f"e_wu{i}", [128, KD, 128], BF16, stack=st) for i in range(2)]
            xs = sb("e_xs", [128, D], BF16, stack=st)
            xr = sb("e_xr", [128, D], stack=st)
            yt = sb("e_yt", [128, 512], stack=st)
            sg = sb("e_sg", [128, 512], stack=st)
            icol = sb("e_ic", [128, 1], U32, stack=st)
            wcol = sb("e_wc", [128, 1], stack=st)
            g2 = sb("e_g2", [128, 2, D], stack=st)
            for s_ in range(2):
                p.dma(g2[:, s_, :], S["modsd"][s_:s_ + 1, 5 * D:6 * D].partition_broadcast(128), r=["modsd"], w=["e_g2"])
            for e_ in range(NE):
                for k0 in range(0, KD, 4):
                    p.dma(wd[:, k0:k0 + 4, :], I["wed"][l, e_].rearrange("(k p) c -> p k c", p=128)[:, k0:k0 + 4, :], w=["e_wd"], q="pool")
                groups = [(0, g0, min(GS, cfg.cap_l - g0)) for g0 in range(0, cfg.cap_l, GS)] + [(1, 0, cfg.cap_c)]
                for (s_, g0, ns) in groups:
                    dI = S["idxd"] if s_ == 0 else S["idxc"]
                    dW = S["wtd"] if s_ == 0 else S["wtc"]
                    tiles = [(j0, min(128, ns - j0)) for j0 in range(0, ns, 128)]
                    for (j0, rows) in tiles:
                        p.dma(icol[0:rows, :], dI[e_, g0 + j0:g0 + j0 + rows].rearrange("(p o) -> p o", o=1), r=["idxd"], w=["e_ic"])
                        p.op("pool", lambda e: e.indirect_dma_start(out=xs[0:rows, :], out_offset=None, in_=S["xn2"][:, :],
                                                                      in_offset=bass.IndirectOffsetOnAxis(ap=icol[0:rows, 0:1], axis=0)),
                             r=["e_ic", "xn2"], w=["e_xs"], dma=True)
                        for k in range(KD):
                            pb = pst[k // 8]
                            p.tr(pb[:, (k % 8) * 128:(k % 8) * 128 + rows], xs[0:rows, k * 128:(k + 1) * 128], ident_b[0:rows, 0:rows],
                                 r=["e_xs", "cstb"], w=[f"pst{k // 8}"])
                        for k in range(KD):
                            pb = pst[k // 8]
                            src = pb[:, (k % 8) * 128:(k % 8) * 128 + rows]
                            if k % 2 == 0:
                                p.ts("dve", xsT[:, k, j0:j0 + rows], src, amul[:, 1, k, s_:s_ + 1], modp[:, 3, k, s_:s_ + 1],
                                     ALU.mult, ALU.add, r=[f"pst{k // 8}"], w=["e_xsT"])
                            else:
                                p.act(xsT[:, k, j0:j0 + rows], src, AF.Identity, r=[f"pst{k // 8}"], w=["e_xsT"],
                                      scale=amul[:, 1, k, s_:s_ + 1], bias=modp[:, 3, k, s_:s_ + 1])
                    for f in range(KD):
                        b = f % 2
                        p.dma(wgf[b][:], I["weg"][l, e_].rearrange("(k p) c -> p k c", p=128)[:, :, f * 128:(f + 1) * 128], w=[f"e_wg{b}"], q="pool")
                        p.dma(wuf[b][:], I["weu"][l, e_].rearrange("(k p) c -> p k c", p=128)[:, :, f * 128:(f + 1) * 128], w=[f"e_wu{b}"], q="pool")
                        for s0 in range(0, ns, 512):
                            n_ = min(512, ns - s0)
                            for k in range(KD):
                                p.mm(psf[0][:, 0:n_], wgf[b][:, k, :], xsT[:, k, s0:s0 + n_], k == 0, k == KD - 1, r=[f"e_wg{b}", "e_xsT"], w=["psf0"])
                            for k in range(KD):
                                p.mm(psf[1][:, 0:n_], wuf[b][:, k, :], xsT[:, k, s0:s0 + n_], k == 0, k == KD - 1, r=[f"e_wu{b}", "e_xsT"], w=["psf1"])
                            p.act(sg[:, 0:n_], psf[0][:, 0:n_], AF.Silu, r=["psf0"], w=["e_sg"])
                            p.tt("dve", hid[:, f, s0:s0 + n_], sg[:, 0:n_], psf[1][:, 0:n_], ALU.mult, r=["e_sg", "psf1"], w=["e_hid"])
                    for (j0, rows) in tiles:
                        p.dma(icol[0:rows, :], dI[e_, g0 + j0:g0 + j0 + rows].rearrange("(p o) -> p o", o=1), r=["idxd"], w=["e_ic"])
                        p.dma(wcol[0:rows, :], dW[e_, g0 + j0:g0 + j0 + rows].rearrange("(p o) -> p o", o=1), r=["idxd"], w=["e_wc"])
                        p.op("pool", lambda e: e.indirect_dma_start(out=xr[0:rows, :], out_offset=None, in_=S["xcur"][:, :],
                                                                      in_offset=bass.IndirectOffsetOnAxis(ap=icol[0:rows, 0:1], axis=0)),
                             r=["e_ic", "xcur"], w=["e_xr"], dma=True)
                        for cb in range(4):
                            ps, pk = psf[2 + cb % 2], f"psf{2 + cb % 2}"
                            for f in range(KD):
                                p.mm(ps[0:rows, :], hid[:, f, j0:j0 + rows], wd[:, f, cb * 512:(cb + 1) * 512], f == 0, f == KD - 1,
                                     r=["e_hid", "e_wd"], w=[pk])
                            p.stt(yt[0:rows, :], ps[0:rows, :], wcol[0:rows, 0:1], g2[0:rows, s_, cb * 512:(cb + 1) * 512], ALU.mult, ALU.mult,
                                  r=[pk, "e_wc", "e_g2"], w=["e_yt"])
                            p.tt("pool", xr[0:rows, cb * 512:(cb + 1) * 512], xr[0:rows, cb * 512:(cb + 1) * 512], yt[0:rows, :], ALU.add,
                                 r=["e_yt", "e_xr"], w=["e_xr"])
                        p.op("pool", lambda e: e.indirect_dma_start(out=S["xcur"][:, :],
                                                                      out_offset=bass.IndirectOffsetOnAxis(ap=icol[0:rows, 0:1], axis=0),
                                                                      in_=xr[0:rows, :], in_offset=None),
                             r=["e_ic", "e_xr"], w=["xcur"], dma=True)
            p.barrier()
        if "layer" in dbg:
            return finish(["xcur"])

    big_copy(yout, S["xcur"][cfg.ctx:, :], cfg.seq, key="yout")
    p.barrier()
    es.close()
    return nc


def prep_inputs(cfg, inp):
    L = cfg.depth
    f = lambda a: np.ascontiguousarray(np.asarray(a, dtype=np.float32))
    pk = lambda v: f(np.asarray(v).reshape(-1, 128).T)
    m = {}
    m["xin"] = f(np.concatenate([inp["ctx"][0], inp["x"][0]], 0))
    m["cT"] = f(np.stack([pk(inp["c"][0]), pk(inp["c_ctx"])], -1))
    m["w_mod"] = f(inp["w_mod"])
    m["b_mod"] = f(inp["b_mod"]).reshape(L, 1, -1)
    m["n1g"] = f(np.stack([pk(inp["norm1_g"][l]) for l in range(L)]))
    m["n2g"] = f(np.stack([pk(inp["norm2_g"][l]) for l in range(L)]))
    m["w_in"] = f(inp["w_in"])
    m["wg"] = f(inp["gla_w_gate"])
    m["bg"] = f(np.stack([np.concatenate([pk(inp["gla_b_gate"][l][d]) for d in range(2)], 1) for l in range(L)]))
    m["gng"] = f(inp["gla_norm_g"]).reshape(L, 1, 128)
    m["nqg"] = f(inp["na_q_norm_g"]).reshape(L, 128, 1)
    m["nkg"] = f(inp["na_k_norm_g"]).reshape(L, 128, 1)
    bts = []
    for l in range(L):
        bt, _, _, _ = _na_masks(cfg, np.asarray(inp["na_rpb"][l], np.float32))
        bts.append(bt.transpose(0, 1, 3, 2, 4))
    m["bt"] = f(np.stack(bts))
    m["cw"] = f(np.stack([np.asarray(inp["lru_conv_w"][l]).reshape(4, 4, 128).transpose(2, 1, 0) for l in range(L)]))
    m["cb"] = f(np.stack([pk(inp["lru_conv_b"][l]) for l in range(L)]))
    m["wa"] = f(inp["lru_w_a"])
    m["wi"] = f(inp["lru_w_i"])
    cat2 = lambda a, l: np.concatenate([pk(a[l][d]) for d in range(2)], 1)
    m["ba"] = f(np.stack([cat2(inp["lru_b_a"], l) for l in range(L)]))
    m["bi"] = f(np.stack([cat2(inp["lru_b_i"], l) for l in range(L)]))
    m["lam"] = f(np.stack([cat2(inp["lru_lambda"], l) for l in range(L)]))
    m["w_out"] = f(inp["w_out"])
    m["w_r"] = f(inp["w_router"])
    m["weg"] = f(inp["w_exp_gate"])
    m["weu"] = f(inp["w_exp_up"])
    m["wed"] = f(inp["w_exp_down"])
    m["rope"] = _rope_tables(cfg)
    tri = np.tril(np.ones((128, 128), np.float32))
    m["consts"] = f(np.stack([np.eye(128), np.ones((128, 128)), tri.T, tri, np.zeros((128, 128)), np.zeros((128, 128))]))
    return m


def kernel(**inputs):
    cfg = Cfg()
    nc = build_program(cfg)
    m = prep_inputs(cfg, inputs)
    res = run_bass_kernel_spmd(nc, [m], core_ids=[0])
    return np.asarray(res.results[0]["yout"], dtype=np.float32).reshape(1, cfg.seq, D)
```

```python
import numpy as np
import concourse.bass as bass
import concourse.mybir as mybir
from concourse.bass_utils import run_bass_kernel_spmd

F32 = mybir.dt.float32
BF16 = mybir.dt.bfloat16
U32 = mybir.dt.uint32
AF = mybir.ActivationFunctionType
ALU = mybir.AluOpType
AX = mybir.AxisListType

D = 2048
KD = D // 128
EPS = 1e-6
GRID_W = 64
D_IN = 5664
C_Q, C_K, C_V, C_R, C_LF, C_LB, C_NQ, C_NK, C_NV, C_LX, C_LY = 0, 256, 512, 1024, 1536, 1552, 1568, 2592, 3616, 4640, 5152


class Cfg:
    def __init__(self, seq=16384, ctx=256, depth=2, ne=16):
        self.seq, self.ctx, self.depth, self.ne = seq, ctx, depth, ne
        self.nt = seq + ctx
        self.ntl = self.nt // 128
        self.rows = seq // GRID_W
        self.cap_l = 2 * seq // ne
        self.cap_c = 2 * ctx // ne


class P:
    def __init__(self, nc):
        self.nc = nc
        self.eng = {"pe": nc.tensor, "dve": nc.vector, "act": nc.scalar, "pool": nc.gpsimd, "sp": nc.sync}
        self.sem = {}
        self.cnt = {e: 0 for e in self.eng}
        self.waited = {e: {} for e in self.eng}
        self.res = {}
        self.dq = {e: 0 for e in self.eng}
        self.NDS = 8
        for e in self.eng:
            self.sem["c_" + e] = nc.semaphore("c_" + e).__enter__()
            for i in range(self.NDS):
                self.sem[f"d_{e}_{i}"] = nc.semaphore(f"d_{e}_{i}").__enter__()

    def _need(self, eng, tok, waits):
        if tok is None:
            return
        name, val = tok
        if eng == "pe" and name == "c_pe":
            return
        if self.waited[eng].get(name, 0) >= val:
            return
        self.waited[eng][name] = val
        waits.append(tok)

    def op(self, eng, fn, r=(), w=(), dma=False, sig=True):
        waits = []
        for k in r:
            st = self.res.get(k)
            if st:
                self._need(eng, st[0], waits)
        for k in w:
            st = self.res.get(k)
            if st:
                self._need(eng, st[0], waits)
                for t in st[1].items():
                    self._need(eng, t, waits)
        if dma:
            j = self.dq[eng]
            self.dq[eng] += 1
            name = f"d_{eng}_{j % self.NDS}"
            val = 16 * (j // self.NDS + 1)
            if j >= self.NDS:
                self._need(eng, (name, val - 16), waits)
            inc = 16
        elif not sig:
            name, val, inc = "c_" + eng, self.cnt[eng] + 1, 0
        else:
            self.cnt[eng] += 1
            name, val, inc = "c_" + eng, self.cnt[eng], 1
        e = self.eng[eng]
        for (n, v) in waits:
            e.wait_ge(self.sem[n], v)
        ins = fn(e)
        if inc:
            ins.then_inc(self.sem[name], inc)
        tok = (name, val)
        for k in r:
            st = self.res.setdefault(k, [None, {}])
            st[1][name] = max(st[1].get(name, 0), val)
        for k in w:
            self.res[k] = [tok, {}]
        return tok

    def barrier(self):
        toks = []
        for e in self.eng:
            if self.cnt[e]:
                toks.append(("c_" + e, self.cnt[e]))
            j = self.dq[e]
            for i in range(self.NDS):
                n = (j - 1 - i)
                if n >= 0:
                    toks.append((f"d_{e}_{n % self.NDS}", 16 * (n // self.NDS + 1)))
        for e in self.eng:
            ws = []
            for t in toks:
                self._need(e, t, ws)
            for (n, v) in ws:
                self.eng[e].wait_ge(self.sem[n], v)

    def dma(self, out, in_, r=(), w=(), q="sp", **kw):
        return self.op(q, lambda e: e.dma_start(out=out, in_=in_, **kw), r, w, dma=True)

    def mm(self, out, lhsT, rhs, start, stop, r=(), w=()):
        return self.op("pe", lambda e: e.matmul(out, lhsT, rhs, start=start, stop=stop), r, w, sig=bool(stop))

    def tr(self, out, in_, ident, r=(), w=()):
        return self.op("pe", lambda e: e.transpose(out, in_, ident), r, w)

    def act(self, out, in_, func, r=(), w=(), bias=0.0, scale=1.0, accum=None):
        if accum is None:
            return self.op("act", lambda e: e.activation(out=out, in_=in_, func=func, bias=bias, scale=scale), r, w)
        return self.op("act", lambda e: e.activation(out=out, in_=in_, func=func, bias=bias, scale=scale,
                                                       accum_out=accum), r, w)

    def ts(self, eng, out, in0, s1, s2, op0, op1=None, r=(), w=(), accum=None):
        if op1 is None:
            return self.op(eng, lambda e: e.tensor_scalar(out, in0, s1, None, op0), r, w)
        if accum is not None:
            return self.op(eng, lambda e: e.tensor_scalar(out, in0, s1, s2, op0, op1, accum), r, w)
        return self.op(eng, lambda e: e.tensor_scalar(out, in0, s1, s2, op0, op1), r, w)

    def tt(self, eng, out, in0, in1, op, r=(), w=()):
        return self.op(eng, lambda e: e.tensor_tensor(out, in0, in1, op), r, w)

    def stt(self, out, in0, scalar, in1, op0, op1, r=(), w=()):
        return self.op("dve", lambda e: e.scalar_tensor_tensor(out, in0, scalar, in1, op0, op1), r, w)

    def copy(self, eng, out, in_, r=(), w=()):
        if eng == "act":
            return self.op("act", lambda e: e.copy(out, in_), r, w)
        return self.op(eng, lambda e: e.tensor_copy(out, in_), r, w)


def _na_tile_geom(cfg):
    rows = cfg.rows
    rs = lambda r: min(max(r - 4, 0), rows - 8)
    plo, nch, cls, defs = [], [], [], {}
    for i in range(rows // 2):
        a, b = rs(2 * i), rs(2 * i + 1)
        lo, hi = a // 2, (b + 7) // 2
        key = (a - 2 * i, b - 2 * i - 1, lo - i, hi - lo + 1)
        if key not in defs:
            defs[key] = len(defs)
        plo.append(lo)
        nch.append(hi - lo + 1)
        cls.append(defs[key])
    return plo, nch, cls, defs


def _na_masks(cfg, rpb):
    plo, nch, cls, defs = _na_tile_geom(cfg)
    ncls = len(defs)
    out = np.full((ncls, 8, 5, 128, 128), -30000.0, np.float32)
    kk = np.arange(128)
    kdr, kc = kk // 64, kk % 64
    qdr, qc = kk // 64, kk % 64
    cs = np.clip(qc - 8, 0, GRID_W - 16)
    for key, ci in defs.items():
        a_rel, b_rel, lo_rel, n = key
        for c in range(n):
            krow = 2 * (lo_rel + c) + kdr
            rs_q = np.where(qdr == 0, a_rel, b_rel + 1)
            qrow = qdr
            inr = (krow[:, None] >= rs_q[None, :]) & (krow[:, None] <= rs_q[None, :] + 7)
            inc = (kc[:, None] >= cs[None, :]) & (kc[:, None] <= cs[None, :] + 15)
            ri = np.clip(krow[:, None] - qrow[None, :] + 7, 0, 14)
            cix = np.clip(kc[:, None] - qc[None, :] + 15, 0, 30)
            m = inr & inc
            for h in range(8):
                out[ci, h, c] = np.where(m, rpb[h][ri, cix], np.float32(-30000.0))
    return out, plo, nch, cls


def _rope_tables(cfg):
    pos = np.arange(cfg.seq)
    row = (pos // GRID_W).astype(np.float32)
    col = (pos % GRID_W).astype(np.float32)
    nf = 16
    inv = (np.float32(10000.0) ** (-np.arange(nf, dtype=np.float32) / np.float32(nf))).astype(np.float32)
    ang = np.concatenate([row[:, None] * inv, col[:, None] * inv], axis=-1).astype(np.float32)
    cos, sin = np.cos(ang).astype(np.float32), np.sin(ang).astype(np.float32)
    c_full = np.concatenate([np.ones((cfg.ctx, 32), np.float32), cos], 0)
    s_full = np.concatenate([np.zeros((cfg.ctx, 32), np.float32), sin], 0)
    p = np.arange(128)
    f = p % 32
    sign = np.where((p % 64) < 32, -1.0, 1.0).astype(np.float32)
    ck = c_full[:, f].T.copy()
    sk = (s_full[:, f].T * sign[:, None]).copy()
    sc = np.float32(64 ** -0.5)
    return np.stack([ck * sc, sk * sc, ck, sk]).astype(np.float32)


def build_program(cfg, dbg=()):
    nc = bass.Bass("TRN2", target_bir_lowering=False)
    NT, NTL, L, NE = cfg.nt, cfg.ntl, cfg.depth, cfg.ne
    plo_l, nch_l, cls_l, cdefs = _na_tile_geom(cfg)
    NCLS = len(cdefs)

    def din(name, shape, dt=F32):
        return nc.dram_tensor(name, list(shape), dt, kind="ExternalInput").ap()

    def dscr(name, shape, dt=F32):
        return nc.dram_tensor(name, list(shape), dt, kind="Internal").ap()

    I = dict(
        xin=din("xin", [NT, D]), cT=din("cT", [128, KD, 2]),
        w_mod=din("w_mod", [L, D, 6 * D]), b_mod=din("b_mod", [L, 1, 6 * D]),
        n1g=din("n1g", [L, 128, KD]), n2g=din("n2g", [L, 128, KD]),
        w_in=din("w_in", [L, D, D_IN]), wg=din("wg", [L, 2, 16, 256]), bg=din("bg", [L, 128, 4]),
        gng=din("gng", [L, 1, 128]), nqg=din("nqg", [L, 128, 1]), nkg=din("nkg", [L, 128, 1]),
        bt=din("bt", [L, NCLS, 8, 128, 5, 128]), cw=din("cw", [L, 128, 4, 4]), cb=din("cb", [L, 128, 4]),
        wa=din("wa", [L, 2, 4, 128, 128]), ba=din("ba", [L, 128, 8]), wi=din("wi", [L, 2, 4, 128, 128]),
        bi=din("bi", [L, 128, 8]), lam=din("lam", [L, 128, 8]),
        w_out=din("w_out", [L, D, D]), w_r=din("w_r", [L, D, NE]),
        weg=din("weg", [L, NE, D, D]), weu=din("weu", [L, NE, D, D]), wed=din("wed", [L, NE, D, D]),
        rope=din("rope", [4, 128, NT]), consts=din("consts", [6, 128, 128]),
    )
    yout = nc.dram_tensor("yout", [cfg.seq, D], F32, kind="ExternalOutput").ap()
    dbg_out = {}
    S = dict(
        xcur=dscr("xcur", [NT, D]), modsd=dscr("modsd", [2, 6 * D]), hT=dscr("hTd", [D, NT], BF16),
        gqT=dscr("gqT", [256, NT], BF16), gkT=dscr("gkT", [256, NT], BF16), gv=dscr("gv", [NT, 512], BF16),
        gr=dscr("gr", [NT, 512]), gg=dscr("gg", [2, 256, NT]), go=dscr("go", [NT, 512]),
        nqT=dscr("nqT", [1024, NT], BF16), nkT=dscr("nkT", [1024, NT], BF16), nv=dscr("nv", [NT, 1024], BF16),
        lxT=dscr("lxT", [512, NT]), lyT=dscr("lyT", [512, NT]), hf=dscr("hf", [512, NT]),
        mixT=dscr("mixT", [D, NT], BF16), xn2=dscr("xn2", [NT, D], BF16),
        idxd=dscr("idxd", [NE, cfg.cap_l], U32), wtd=dscr("wtd", [NE, cfg.cap_l]),
        idxc=dscr("idxc", [NE, cfg.cap_c], U32), wtc=dscr("wtc", [NE, cfg.cap_c]),
    )
    p = P(nc)
    from contextlib import ExitStack
    es = ExitStack()

    uid = [0]

    def sb(name, shape, dt=F32, stack=None):
        uid[0] += 1
        return (stack or es).enter_context(nc.sbuf_tensor(f"{name}_u{uid[0]}", list(shape), dt))

    psf = [es.enter_context(nc.psum_tensor(f"psf{i}", [128, 512], F32)) for i in range(6)]
    pst = [es.enter_context(nc.psum_tensor(f"pst{i}", [128, 1024], BF16)) for i in range(2)]
    cst = sb("cst", [128, 6, 128])
    cstb = sb("cstb", [128, 2, 128], BF16)
    p.dma(cst[:], I["consts"].rearrange("c p n -> p c n"), w=["cst"])
    p.copy("dve", cstb[:], cst[:, 0:2, :], r=["cst"], w=["cstb"])
    ident_f, ones_f, maskF, maskB = cst[:, 0, :], cst[:, 1, :], cst[:, 2, :], cst[:, 3, :]
    ident_b, ones_b = cstb[:, 0, :], cstb[:, 1, :]
    modp = sb("modp", [128, 6, KD, 2])
    amul = sb("amul", [128, 2, KD, 2])
    def big_copy(dst, src, rows, step=128, key="bigcopy"):
        for r0 in range(0, rows, step):
            r1 = min(rows, r0 + step)
            p.dma(dst[r0:r1], src[r0:r1], w=[key])

    def finish(names):
        for nm in names:
            o = nc.dram_tensor("dbg_" + nm, list(S[nm].shape), S[nm].dtype, kind="ExternalOutput").ap()
            big_copy(o, S[nm], S[nm].shape[0], key="dbg")
        p.barrier()
        return nc

    big_copy(S["xcur"], I["xin"], NT, key="xcur")
    p.barrier()

    def stream_of(tile):
        return 1 if tile < cfg.ctx // 128 else 0

    supers = [(0, cfg.ctx, 1)] + [(cfg.ctx + i * 512, 512, 0) for i in range(cfg.seq // 512)]

    for l in range(L):
        with ExitStack() as st:
            sc = sb("m_sc", [128, KD, 2], stack=st)
            wm = sb("m_w", [128, KD, 512], stack=st)
            brow = sb("m_b", [2, 512], stack=st)
            mrow = sb("m_r", [2, 512], stack=st)
            p.dma(sc[:], I["cT"], w=["m_sc"])
            p.act(sc[:], sc[:], AF.Silu, r=["m_sc"], w=["m_sc"])
            for cb in range(24):
                p.dma(wm[:], I["w_mod"][l].rearrange("(k p) c -> p k c", p=128)[:, :, cb * 512:(cb + 1) * 512],
                      w=["m_w"])
                p.dma(brow[:], I["b_mod"][l][:, cb * 512:(cb + 1) * 512].partition_broadcast(2), w=["m_b"])
                for k in range(KD):
                    p.mm(psf[0][0:2, :], sc[:, k, :], wm[:, k, :], k == 0, k == KD - 1, r=["m_sc", "m_w"], w=["psf0"])
                p.tt("dve", mrow[:], psf[0][0:2, :], brow[:], ALU.add, r=["psf0", "m_b"], w=["m_r"])
                p.dma(S["modsd"][:, cb * 512:(cb + 1) * 512], mrow[:], r=["m_r"], w=["modsd"])
            p.barrier()
            for j in range(6):
                for s_ in range(2):
                    p.dma(modp[:, j, :, s_], S["modsd"][s_, j * D:(j + 1) * D].rearrange("(k p) -> p k", p=128),
                          r=["modsd"], w=["modp"], allow_slow_non_contiguous=True)
            g1 = sb("m_g1", [128, KD], stack=st)
            g2 = sb("m_g2", [128, KD], stack=st)
            p.dma(g1[:], I["n1g"][l], w=["m_g1"])
            p.dma(g2[:], I["n2g"][l], w=["m_g2"])
            for s_ in range(2):
                p.stt(amul[:, 0, :, s_], modp[:, 1, :, s_], 1.0, g1[:], ALU.add, ALU.mult, r=["modp", "m_g1"], w=["amul"])
                p.stt(amul[:, 1, :, s_], modp[:, 4, :, s_], 1.0, g2[:], ALU.add, ALU.mult, r=["modp", "m_g2"], w=["amul"])
            p.barrier()

        if "mods" in dbg:
            return finish(["modsd"])

        def norm_stage(st, which, dst_hT, dst_xn=None, router=None):
            xt = [sb(f"n_x{i}", [128, D], stack=st) for i in range(2)]
            junk = sb("n_junk", [128, D], BF16, stack=st)
            xn = [sb(f"n_xn{i}", [128, D], BF16, stack=st) for i in range(2)]
            hTt = [sb(f"n_hT{i}", [128, KD, 128], BF16, stack=st) for i in range(2)]
            stat = sb("n_stat", [128, 4], stack=st)
            for t in range(NTL):
                b = t % 2
                s_ = stream_of(t)
                p.dma(xt[b][:], S["xcur"][t * 128:(t + 1) * 128, :], r=["xcur"], w=[f"n_x{b}"])
                p.act(junk[:], xt[b][:], AF.Square, r=[f"n_x{b}"], w=["n_junk", "n_ss"], accum=stat[:, 0:1])
                p.act(stat[:, 1:2], stat[:, 0:1], AF.Sqrt, r=["n_ss"], w=["n_sd"], scale=1.0 / D, bias=EPS)
                p.op("dve", lambda e: e.reciprocal(stat[:, 2:3], stat[:, 1:2]), r=["n_sd"], w=["n_rs"])
                p.ts("dve", xn[b][:], xt[b][:], stat[:, 2:3], None, ALU.mult, r=[f"n_x{b}", "n_rs"], w=[f"n_xn{b}"])
                if dst_xn is not None:
                    p.dma(dst_xn[t * 128:(t + 1) * 128, :], xn[b][:], r=[f"n_xn{b}"], w=["xn2"])
                for k in range(KD):
                    pb = pst[k // 8]
                    p.tr(pb[:, (k % 8) * 128:(k % 8 + 1) * 128], xn[b][:, k * 128:(k + 1) * 128], ident_b,
                         r=[f"n_xn{b}", "cstb"], w=[f"pst{k // 8}"])
                for k in range(KD):
                    pb = pst[k // 8]
                    src = pb[:, (k % 8) * 128:(k % 8 + 1) * 128]
                    if k % 2 == 0:
                        p.ts("dve", hTt[b][:, k, :], src, amul[:, which, k, s_:s_ + 1], modp[:, 3 * which, k, s_:s_ + 1],
                             ALU.mult, ALU.add, r=[f"pst{k // 8}", "amul", "modp"], w=[f"n_hT{b}"])
                    else:
                        p.act(hTt[b][:, k, :], src, AF.Identity, r=[f"pst{k // 8}", "amul", "modp"], w=[f"n_hT{b}"],
                              scale=amul[:, which, k, s_:s_ + 1], bias=modp[:, 3 * which, k, s_:s_ + 1])
                if dst_hT is not None:
                    p.dma(dst_hT.rearrange("(k p) n -> p k n", p=128)[:, :, t * 128:(t + 1) * 128], hTt[b][:],
                          r=[f"n_hT{b}"], w=["hTd"])
                if router is not None:
                    router(t, hTt[b], f"n_hT{b}")

        with ExitStack() as st:
            norm_stage(st, 0, S["hT"])
            p.barrier()
        if "norm" in dbg:
            return finish(["hT"])

        with ExitStack() as st:
            wb = sb("i_w", [128, KD, 1024], BF16, stack=st)
            wsw = sb("i_wsw", [128, KD, 512], BF16, stack=st)
            hs = [sb(f"i_h{i}", [128, KD, 512], BF16, stack=st) for i in range(2)]
            rp = sb("i_rope", [128, 4, 512], stack=st)
            ev = [sb(f"i_ev{i}", [128, 512], stack=st) for i in range(4)]
            evb = [sb(f"i_evb{i}", [128, 512], BF16, stack=st) for i in range(2)]
            lrt = sb("i_lr", [16, 2, 512], BF16, stack=st)
            wgs = sb("i_wg", [16, 2, 256], BF16, stack=st)
            bgs = sb("i_bg", [128, 4], stack=st)
            nbg = sb("i_nbg", [128, 4], stack=st)
            nqk = sb("i_nqk", [128, 2], stack=st)
            p.dma(wgs[:], I["wg"][l].rearrange("d r c -> r d c"), w=["i_wg"], q="pool")
            p.dma(bgs[:], I["bg"][l], w=["i_bg"])
            p.ts("dve", nbg[:], bgs[:], -1.0, None, ALU.mult, r=["i_bg"], w=["i_nbg"])
            p.dma(nqk[:, 0:1], I["nqg"][l], w=["i_nqk"])
            p.dma(nqk[:, 1:2], I["nkg"][l], w=["i_nqk"])
            p.ts("dve", nqk[:, 0:1], nqk[:, 0:1], float(128 ** -0.5), None, ALU.mult, r=["i_nqk"], w=["i_nqk"])
            wsrc = I["w_in"][l].rearrange("(k p) c -> p k c", p=128)

            def load_w(c0, n):
                for k0 in range(0, KD, 4):
                    p.dma(wb[:, k0:k0 + 4, 0:n], wsrc[:, k0:k0 + 4, c0:c0 + n], w=["i_w"], q="pool")

            def load_h(si):
                t0, ntok, s_ = supers[si]
                b = si % 2
                p.dma(hs[b][:, :, 0:ntok], S["hT"].rearrange("(k p) n -> p k n", p=128)[:, :, t0:t0 + ntok],
                      r=["hTd"], w=[f"i_h{b}"])
                return hs[b], f"i_h{b}", t0, ntok

            def fm_mm(ps, pskey, wtile, wkey, c0, m, h, hkey, ntok):
                for k in range(KD):
                    p.mm(ps[0:m, 0:ntok], wtile[:, k, c0:c0 + m], h[:, k, 0:ntok], k == 0, k == KD - 1,
                         r=[wkey, hkey], w=[pskey])

            load_w(C_Q, 512)
            p.dma(wb[:, :, 512:544], wsrc[:, :, C_LF:C_LF + 32], w=["i_w"], q="pool")
            w5 = wb[:, :, 0:512].rearrange("p k (h two f) -> p k h two f", two=2, f=32)
            s5 = wsw[:].rearrange("p k (h two f) -> p k h two f", two=2, f=32)
            p.copy("dve", s5[:, :, :, 0, :], w5[:, :, :, 1, :], r=["i_w"], w=["i_wsw"])
            p.copy("pool", s5[:, :, :, 1, :], w5[:, :, :, 0, :], r=["i_w"], w=["i_wsw"])
            for si in range(len(supers)):
                h, hkey, t0, ntok = load_h(si)
                p.dma(rp[:, :, 0:ntok], I["rope"].rearrange("c p n -> p c n")[:, :, t0:t0 + ntok], w=["i_rope"])
                for qk in range(2):
                    dst = S["gqT"] if qk == 0 else S["gkT"]
                    for hp in range(2):
                        c0 = qk * 256 + hp * 128
                        fm_mm(psf[0], "psf0", wb, "i_w", c0, 128, h, hkey, ntok)
                        fm_mm(psf[1], "psf1", wsw, "i_wsw", c0, 128, h, hkey, ntok)
                        p.tt("dve", ev[0][:, 0:ntok], psf[0][:, 0:ntok], rp[:, 2 * qk, 0:ntok], ALU.mult,
                             r=["psf0", "i_rope"], w=["i_ev0"])
                        p.tt("dve", ev[1][:, 0:ntok], psf[1][:, 0:ntok], rp[:, 2 * qk + 1, 0:ntok], ALU.mult,
                             r=["psf1", "i_rope"], w=["i_ev1"])
                        p.tt("pool", evb[0][:, 0:ntok], ev[0][:, 0:ntok], ev[1][:, 0:ntok], ALU.add,
                             r=["i_ev0", "i_ev1"], w=["i_evb0"])
                        p.dma(dst[hp * 128:(hp + 1) * 128, t0:t0 + ntok], evb[0][:, 0:ntok], r=["i_evb0"], w=["gqk"])
                for d_ in range(2):
                    fm_mm(psf[2], "psf2", wb, "i_w", 512 + 16 * d_, 16, h, hkey, ntok)
                    p.copy("act", lrt[:, d_, 0:ntok], psf[2][0:16, 0:ntok], r=["psf2"], w=["i_lr"])
                for d_ in range(2):
                    for hp in range(2):
                        p.mm(psf[3][:, 0:ntok], wgs[:, d_, hp * 128:(hp + 1) * 128], lrt[:, d_, 0:ntok], True, True,
                             r=["i_wg", "i_lr"], w=["psf3"])
                        p.act(ev[2][:, 0:ntok], psf[3][:, 0:ntok], AF.Exp, r=["psf3", "i_nbg"], w=["i_ev2"],
                              scale=-1.0, bias=nbg[:, d_ * 2 + hp:d_ * 2 + hp + 1])
                        p.act(ev[2][:, 0:ntok], ev[2][:, 0:ntok], AF.Ln, r=["i_ev2"], w=["i_ev2"], bias=1.0)
                        p.ts("dve", ev[3][:, 0:ntok], ev[2][:, 0:ntok], -1.0 / 16.0, None, ALU.mult, r=["i_ev2"], w=["i_ev3"])
                        p.dma(S["gg"][d_, hp * 128:(hp + 1) * 128, t0:t0 + ntok], ev[3][:, 0:ntok], r=["i_ev3"], w=["gg"])

            if "ip1" in dbg:
                return finish(["gqT", "gkT", "gg"])

            def tok_group(c0, ncols, dst, dst_dt_bf, silu):
                load_w(c0, ncols)
                for si in range(len(supers)):
                    h, hkey, t0, ntok = load_h(si)
                    for tt_ in range(ntok // 128):
                        for cb in range(ncols // 512):
                            ps, pk = psf[(tt_ * 2 + cb) % 4], f"psf{(tt_ * 2 + cb) % 4}"
                            for k in range(KD):
                                p.mm(ps[:, :], h[:, k, tt_ * 128:(tt_ + 1) * 128], wb[:, k, cb * 512:(cb + 1) * 512],
                                     k == 0, k == KD - 1, r=["i_w", hkey], w=[pk])
                            tok0 = t0 + tt_ * 128
                            if dst_dt_bf:
                                e_, ek = evb[cb % 2], f"i_evb{cb % 2}"
                                p.copy("act" if cb % 2 else "dve", e_[:], ps[:, :], r=[pk], w=[ek])
                            else:
                                e_, ek = ev[cb % 2], f"i_ev{cb % 2}"
                                p.act(e_[:], ps[:, :], AF.Silu if silu else AF.Copy, r=[pk], w=[ek])
                            p.dma(dst[tok0:tok0 + 128, cb * 512:(cb + 1) * 512], e_[:], r=[ek], w=["tokdst"])

            tok_group(C_V, 512, S["gv"], True, False)
            tok_group(C_R, 512, S["gr"], False, True)
            tok_group(C_NV, 1024, S["nv"], True, False)

            if "ip2" in dbg:
                return finish(["gv", "gr", "nv"])
            for qk in range(2):
                load_w(C_NQ if qk == 0 else C_NK, 1024)
                dst = S["nqT"] if qk == 0 else S["nkT"]
                for si in range(len(supers)):
                    h, hkey, t0, ntok = load_h(si)
                    for hd in range(8):
                        pa_, pb_ = (0, 1) if hd % 2 == 0 else (2, 3)
                        fm_mm(psf[pa_], f"psf{pa_}", wb, "i_w", hd * 128, 128, h, hkey, ntok)
                        p.copy("dve", ev[0][:, 0:ntok], psf[pa_][:, 0:ntok], r=[f"psf{pa_}"], w=["i_ev0"])
                        p.tt("dve", evb[1][:, 0:ntok], ev[0][:, 0:ntok], ev[0][:, 0:ntok], ALU.mult, r=["i_ev0"], w=["i_evb1"])
                        p.mm(psf[pb_][:, 0:ntok], ones_b, evb[1][:, 0:ntok], True, True, r=["cstb", "i_evb1"], w=[f"psf{pb_}"])
                        p.act(ev[2][:, 0:ntok], psf[pb_][:, 0:ntok], AF.Sqrt, r=[f"psf{pb_}"], w=["i_ev2"], scale=1.0 / 128, bias=EPS)
                        p.op("dve", lambda e: e.reciprocal(ev[3][:, 0:ntok], ev[2][:, 0:ntok]), r=["i_ev2"], w=["i_ev3"])
                        p.stt(evb[0][:, 0:ntok], ev[0][:, 0:ntok], nqk[:, qk:qk + 1], ev[3][:, 0:ntok], ALU.mult, ALU.mult,
                              r=["i_ev0", "i_ev3", "i_nqk"], w=["i_evb0"])
                        p.dma(dst[hd * 128:(hd + 1) * 128, t0:t0 + ntok], evb[0][:, 0:ntok], r=["i_evb0"], w=["nqk"])

            if "ip3" in dbg:
                return finish(["nqT", "nkT"])
            load_w(C_LX, 1024)
            for si in range(len(supers)):
                h, hkey, t0, ntok = load_h(si)
                for c in range(8):
                    ps, pk = psf[c % 4], f"psf{c % 4}"
                    fm_mm(ps, pk, wb, "i_w", c * 128, 128, h, hkey, ntok)
                    e_, ek = ev[c % 4], f"i_ev{c % 4}"
                    p.copy("act" if c % 2 else "dve", e_[:, 0:ntok], ps[:, 0:ntok], r=[pk], w=[ek])
                    dst = S["lxT"] if c < 4 else S["lyT"]
                    p.dma(dst[(c % 4) * 128:(c % 4 + 1) * 128, t0:t0 + ntok], e_[:, 0:ntok], r=[ek], w=["lxy"])
            p.barrier()

        if "inproj" in dbg:
            return finish(["gqT", "gkT", "gg", "gv", "gr", "nqT", "nkT", "nv", "lxT", "lyT"])

        nct = cfg.ctx // 128
        with ExitStack() as st:
            BL = min(1024, cfg.seq)
            blocks = [(0, cfg.ctx)] + [(cfg.ctx + i * BL, BL) for i in range(cfg.seq // BL)]
            qb = sb("g_q", [128, BL], BF16, stack=st)
            kb = sb("g_k", [128, BL], BF16, stack=st)
            gb = sb("g_g", [128, BL], stack=st)
            vb = sb("g_v", [128, BL // 128, 256], BF16, stack=st)
            gc = sb("g_gc", [128, 128], stack=st)
            Hh = sb("g_H", [128, 128], stack=st)
            ex = sb("g_ex", [128, 3, 128], stack=st)
            qkb = sb("g_qkb", [128, 3, 128], BF16, stack=st)
            kdT = sb("g_kdT", [128, 128], BF16, stack=st)
            attm = [sb(f"g_att{i}", [128, 128], BF16, stack=st) for i in range(2)]
            Sst = sb("g_S", [128, 128], stack=st)
            Sbf = sb("g_Sb", [128, 128], BF16, stack=st)
            sm = sb("g_sm", [128, 8], stack=st)
            ob = sb("g_o", [128, 256], stack=st)
            of = sb("g_of", [128, 256], stack=st)
            rr = sb("g_r", [128, 256], stack=st)
            gn = sb("g_gn", [128, 128], stack=st)
            outb = sb("g_outb", [128, 256], BF16, stack=st)
            mT = sb("g_mT", [128, 256], BF16, stack=st)
            junk = sb("g_junk", [128, 128], stack=st)
            p.dma(gn[:], I["gng"][l].partition_broadcast(128), w=["g_gn"])
            for hp in range(2):
                for d_ in range(2):
                    p.op("dve", lambda e: e.memset(Sst[:], 0.0), w=["g_S"])
                    p.op("dve", lambda e: e.memset(Sbf[:], 0.0), w=["g_Sb"])
                    order = blocks if d_ == 0 else [blocks[0]] + blocks[:0:-1]
                    msk = maskF if d_ == 0 else maskB
                    for (t0, ntok) in order:
                        p.dma(qb[:, 0:ntok], S["gqT"][hp * 128:(hp + 1) * 128, t0:t0 + ntok], r=["gqk"], w=["g_q"])
                        p.dma(kb[:, 0:ntok], S["gkT"][hp * 128:(hp + 1) * 128, t0:t0 + ntok], r=["gqk"], w=["g_k"])
                        p.dma(gb[:, 0:ntok], S["gg"][d_, hp * 128:(hp + 1) * 128, t0:t0 + ntok], r=["gg"], w=["g_g"])
                        p.dma(vb[:, 0:ntok // 128, :],
                              S["gv"][t0:t0 + ntok, hp * 256:(hp + 1) * 256].rearrange("(c p) f -> p c f", p=128),
                              r=["tokdst"], w=["g_v"])
                        cs_ = list(range(ntok // 128))
                        if d_ == 1:
                            cs_ = cs_[::-1]
                        for c in cs_:
                            sl = slice(c * 128, (c + 1) * 128)
                            tok = t0 + c * 128
                            p.op("dve", lambda e: e.tensor_tensor_scan(out=gc[:], data0=ones_f, data1=gb[:, sl], initial=0.0,
                                                                         op0=ALU.mult, op1=ALU.add), r=["g_g", "cst"], w=["g_gc"])
                            gl = gc[:, 127:128]
                            p.act(sm[:, 2:3], gl, AF.Exp, r=["g_gc"], w=["g_dec"])
                            if d_ == 0:
                                p.act(ex[:, 0, :], gc[:], AF.Exp, r=["g_gc"], w=["g_ex"])
                                p.act(ex[:, 1, :], gc[:], AF.Exp, r=["g_gc"], w=["g_ex"], scale=-1.0)
                                p.act(ex[:, 2, :], gc[:], AF.Exp, r=["g_gc"], w=["g_ex"], scale=-1.0, bias=gl)
                            else:
                                p.tt("dve", Hh[:], gc[:], gb[:, sl], ALU.subtract, r=["g_gc", "g_g"], w=["g_H"])
                                p.ts("dve", sm[:, 1:2], gl, -1.0, None, ALU.mult, r=["g_gc"], w=["g_ngl"])
                                p.act(ex[:, 0, :], Hh[:], AF.Exp, r=["g_H", "g_gc"], w=["g_ex"], scale=-1.0, bias=gl)
                                p.act(ex[:, 1, :], Hh[:], AF.Exp, r=["g_H", "g_ngl"], w=["g_ex"], bias=sm[:, 1:2])
                                p.act(ex[:, 2, :], Hh[:], AF.Exp, r=["g_H"], w=["g_ex"])
                            p.tt("dve", qkb[:, 0, :], qb[:, sl], ex[:, 0, :], ALU.mult, r=["g_q", "g_ex"], w=["g_qe"])
                            p.tt("pool", qkb[:, 1, :], kb[:, sl], ex[:, 1, :], ALU.mult, r=["g_k", "g_ex"], w=["g_ke"])
                            p.tt("dve", qkb[:, 2, :], kb[:, sl], ex[:, 2, :], ALU.mult, r=["g_k", "g_ex"], w=["g_kd"])
                            p.tr(pst[0][:, 0:128], qkb[:, 2, :], ident_b, r=["g_kd", "cstb"], w=["pst0"])
                            p.copy("act", kdT[:], pst[0][:, 0:128], r=["pst0"], w=["g_kdT"])
                            for hh in range(2):
                                b0 = 64 * hh
                                p.mm(psf[0][:, hh * 128:(hh + 1) * 128], qkb[b0:b0 + 64, 1, :], qkb[b0:b0 + 64, 0, :], True, True,
                                     r=["g_ke", "g_qe"], w=["psf0"])
                                p.tt("dve", attm[hh][:], psf[0][:, hh * 128:(hh + 1) * 128], msk, ALU.mult,
                                     r=["psf0", "cst"], w=[f"g_att{hh}"])
                                p.mm(psf[1][:, hh * 128:(hh + 1) * 128], attm[hh][:], vb[:, c, hh * 128:(hh + 1) * 128], True, False,
                                     r=[f"g_att{hh}", "g_v"], w=["psf1"])
                                p.mm(psf[1][:, hh * 128:(hh + 1) * 128], qkb[b0:b0 + 64, 0, :], Sbf[b0:b0 + 64, :], False, True,
                                     r=["g_qe", "g_Sb"], w=["psf1"])
                            p.mm(psf[2][:, 0:256], kdT[:], vb[:, c, :], True, True, r=["g_kdT", "g_v"], w=["psf2"])
                            if d_ == 0:
                                p.copy("act", ob[:], psf[1][:, 0:256], r=["psf1"], w=["g_o"])
                                p.dma(S["go"][tok:tok + 128, hp * 256:(hp + 1) * 256], ob[:], r=["g_o"], w=["go"])
                            else:
                                p.dma(of[:], S["go"][tok:tok + 128, hp * 256:(hp + 1) * 256], r=["go"], w=["g_of"])
                                p.dma(rr[:], S["gr"][tok:tok + 128, hp * 256:(hp + 1) * 256], r=["tokdst"], w=["g_r"])
                                p.tt("dve", ob[:], psf[1][:, 0:256], of[:], ALU.add, r=["psf1", "g_of"], w=["g_o"])
                                for hh in range(2):
                                    oh = ob[:, hh * 128:(hh + 1) * 128]
                                    p.act(junk[:], oh, AF.Square, r=["g_o"], w=["g_junk", "g_ss"], accum=sm[:, 4 + hh:5 + hh])
                                p.act(sm[:, 6:8], sm[:, 4:6], AF.Sqrt, r=["g_ss"], w=["g_sd"], scale=1.0 / 128, bias=EPS)
                                p.op("dve", lambda e: e.reciprocal(sm[:, 4:6], sm[:, 6:8]), r=["g_sd"], w=["g_rs"])
                                for hh in range(2):
                                    oh = ob[:, hh * 128:(hh + 1) * 128]
                                    p.stt(oh, oh, sm[:, 4 + hh:5 + hh], gn[:], ALU.mult, ALU.mult, r=["g_o", "g_rs", "g_gn"], w=["g_o"])
                                p.tt("dve", outb[:], ob[:], rr[:], ALU.mult, r=["g_o", "g_r"], w=["g_outb"])
                                for hh in range(2):
                                    p.tr(pst[1][:, hh * 128:(hh + 1) * 128], outb[:, hh * 128:(hh + 1) * 128], ident_b,
                                         r=["g_outb", "cstb"], w=["pst1"])
                                p.copy("act", mT[:], pst[1][:, 0:256], r=["pst1"], w=["g_mT"])
                                for hh in range(2):
                                    f0 = (hp * 2 + hh) * 128
                                    p.dma(S["mixT"][f0:f0 + 128, tok:tok + 128], mT[:, hh * 128:(hh + 1) * 128], r=["g_mT"], w=["mixT"])
                            for hh in range(2):
                                b0 = 64 * hh
                                p.stt(Sst[b0:b0 + 64, :], Sst[b0:b0 + 64, :], sm[b0:b0 + 64, 2:3], psf[2][b0:b0 + 64, hh * 128:(hh + 1) * 128],
                                      ALU.mult, ALU.add, r=["g_S", "g_dec", "psf2"], w=["g_S"])
                            p.copy("act", Sbf[:], Sst[:], r=["g_S"], w=["g_Sb"])
            p.barrier()
        if "gla" in dbg:
            return finish(["mixT"])

        with ExitStack() as st:
            BL = min(2048, cfg.seq)
            blocks = [(0, cfg.ctx)] + [(cfg.ctx + i * BL, BL) for i in range(cfg.seq // BL)]
            xp = sb("l_xp", [128, BL + 3], stack=st)
            xc = sb("l_xc", [128, BL], stack=st)
            xcb = sb("l_xcb", [128, BL], BF16, stack=st)
            aa = sb("l_aa", [128, BL], stack=st)
            bb = sb("l_bb", [128, BL], stack=st)
            hh_ = sb("l_hh", [128, BL], stack=st)
            yb = sb("l_yb", [128, BL], stack=st)
            hfb = sb("l_hfb", [128, BL], stack=st)
            lob = sb("l_ob", [128, BL], BF16, stack=st)
            tmp = [sb(f"l_t{i}", [128, 512], stack=st) for i in range(4)]
            hprev = sb("l_hp", [128, 1], stack=st)
            cw_s = sb("l_cw", [128, 4, 4], stack=st)
            cb_s = sb("l_cb", [128, 4], stack=st)
            ba_s = sb("l_ba", [128, 8], stack=st)
            bi_s = sb("l_bi", [128, 8], stack=st)
            c8 = sb("l_c8", [128, 8], stack=st)
            wa_s = sb("l_wa", [128, 2, 4, 128], BF16, stack=st)
            wi_s = sb("l_wi", [128, 2, 4, 128], BF16, stack=st)
            p.dma(cw_s[:], I["cw"][l], w=["l_cw"])
            p.dma(cb_s[:], I["cb"][l], w=["l_cb"])
            p.dma(ba_s[:], I["ba"][l], w=["l_ba"])
            p.dma(bi_s[:], I["bi"][l], w=["l_bi"])
            p.dma(c8[:], I["lam"][l], w=["l_c8"])
            p.dma(wa_s[:], I["wa"][l].rearrange("d n c o -> c d n o"), w=["l_wa"], q="pool")
            p.dma(wi_s[:], I["wi"][l].rearrange("d n c o -> c d n o"), w=["l_wi"], q="pool")
            p.act(c8[:], c8[:], AF.Sigmoid, r=["l_c8"], w=["l_c8"])
            p.act(c8[:], c8[:], AF.Ln, r=["l_c8"], w=["l_c8"])
            p.ts("dve", c8[:], c8[:], 8.0, None, ALU.mult, r=["l_c8"], w=["l_c8"])
            for n in range(4):
                for d_ in range(2):
                    order = blocks if d_ == 0 else [blocks[0]] + blocks[:0:-1]
                    p.op("dve", lambda e: e.memset(hprev[:], 0.0), w=["l_hp"])
                    for (t0, ntok) in order:
                        seg_lo, seg_hi = (0, cfg.ctx) if t0 < cfg.ctx else (cfg.ctx, NT)
                        p.op("pool", lambda e: e.memset(xp[:], 0.0), w=["l_xp"])
                        lo, hi = max(t0 - 1, seg_lo), min(t0 + ntok + 2, seg_hi)
                        p.dma(xp[:, lo - (t0 - 1):hi - (t0 - 1)], S["lxT"][n * 128:(n + 1) * 128, lo:hi], r=["lxy"], w=["l_xp"])
                        p.ts("dve", xc[:, 0:ntok], xp[:, 0:ntok], cw_s[:, n, 0:1], cb_s[:, n:n + 1], ALU.mult, ALU.add,
                             r=["l_xp", "l_cw", "l_cb"], w=["l_xc"])
                        for j in range(1, 4):
                            p.stt(xc[:, 0:ntok], xp[:, j:j + ntok], cw_s[:, n, j:j + 1], xc[:, 0:ntok], ALU.mult, ALU.add,
                                  r=["l_xp", "l_cw", "l_xc"], w=["l_xc"])
                        p.copy("act", xcb[:, 0:ntok], xc[:, 0:ntok], r=["l_xc"], w=["l_xcb"])
                        SUB = min(512, ntok)
                        for s0 in range(0, ntok, SUB):
                            ss_ = slice(s0, s0 + SUB)
                            p.mm(psf[0][:, 0:SUB], wa_s[:, d_, n, :], xcb[:, ss_], True, True, r=["l_wa", "l_xcb"], w=["psf0"])
                            p.mm(psf[1][:, 0:SUB], wi_s[:, d_, n, :], xcb[:, ss_], True, True, r=["l_wi", "l_xcb"], w=["psf1"])
                            p.act(tmp[0][:, 0:SUB], psf[0][:, 0:SUB], AF.Sigmoid, r=["psf0", "l_ba"], w=["l_t0"],
                                  bias=ba_s[:, d_ * 4 + n:d_ * 4 + n + 1])
                            p.act(tmp[1][:, 0:SUB], psf[1][:, 0:SUB], AF.Sigmoid, r=["psf1", "l_bi"], w=["l_t1"],
                                  bias=bi_s[:, d_ * 4 + n:d_ * 4 + n + 1])
                            p.act(aa[:, ss_], tmp[0][:, 0:SUB], AF.Exp, r=["l_t0", "l_c8"], w=["l_aa"],
                                  scale=c8[:, d_ * 4 + n:d_ * 4 + n + 1])
                            p.stt(tmp[2][:, 0:SUB], aa[:, ss_], -1.0, aa[:, ss_], ALU.mult, ALU.mult, r=["l_aa"], w=["l_t2"])
                            p.act(tmp[2][:, 0:SUB], tmp[2][:, 0:SUB], AF.Sqrt, r=["l_t2"], w=["l_t2"], bias=1.0)
                            p.tt("dve", tmp[3][:, 0:SUB], tmp[1][:, 0:SUB], tmp[2][:, 0:SUB], ALU.mult, r=["l_t1", "l_t2"], w=["l_t3"])
                            p.tt("dve", bb[:, ss_], tmp[3][:, 0:SUB], xc[:, ss_], ALU.mult, r=["l_t3", "l_xc"], w=["l_bb"])
                        if d_ == 0:
                            p.op("dve", lambda e: e.tensor_tensor_scan(out=hh_[:, 0:ntok], data0=aa[:, 0:ntok], data1=bb[:, 0:ntok],
                                                                         initial=hprev[:, 0:1], op0=ALU.mult, op1=ALU.add),
                                 r=["l_aa", "l_bb", "l_hp"], w=["l_hh"])
                            p.copy("dve", hprev[:], hh_[:, ntok - 1:ntok], r=["l_hh"], w=["l_hp"])
                            p.dma(S["hf"][n * 128:(n + 1) * 128, t0:t0 + ntok], hh_[:, 0:ntok], r=["l_hh"], w=["hf"])
                        else:
                            p.op("dve", lambda e: e.tensor_tensor_scan(out=hh_[:, 0:ntok][:, ::-1], data0=aa[:, 0:ntok][:, ::-1],
                                                                         data1=bb[:, 0:ntok][:, ::-1], initial=hprev[:, 0:1],
                                                                         op0=ALU.mult, op1=ALU.add),
                                 r=["l_aa", "l_bb", "l_hp"], w=["l_hh"])
                            p.copy("dve", hprev[:], hh_[:, 0:1], r=["l_hh"], w=["l_hp"])
                            p.dma(hfb[:, 0:ntok], S["hf"][n * 128:(n + 1) * 128, t0:t0 + ntok], r=["hf"], w=["l_hfb"])
                            p.dma(yb[:, 0:ntok], S["lyT"][n * 128:(n + 1) * 128, t0:t0 + ntok], r=["lxy"], w=["l_yb"])
                            p.tt("pool", hfb[:, 0:ntok], hfb[:, 0:ntok], hh_[:, 0:ntok], ALU.add, r=["l_hfb", "l_hh"], w=["l_hfb"])
                            p.tt("dve", xc[:, 0:ntok], yb[:, 0:ntok], yb[:, 0:ntok], ALU.mult, r=["l_yb"], w=["l_xc"])
                            p.ts("dve", xc[:, 0:ntok], xc[:, 0:ntok], 0.044715, 1.0, ALU.mult, ALU.add, r=["l_xc"], w=["l_xc"])
                            p.tt("dve", xc[:, 0:ntok], xc[:, 0:ntok], yb[:, 0:ntok], ALU.mult, r=["l_xc", "l_yb"], w=["l_xc"])
                            p.act(xc[:, 0:ntok], xc[:, 0:ntok], AF.Sigmoid, r=["l_xc"], w=["l_xc"], scale=1.5957691216057308)
                            p.tt("dve", xc[:, 0:ntok], xc[:, 0:ntok], yb[:, 0:ntok], ALU.mult, r=["l_xc", "l_yb"], w=["l_xc"])
                            p.tt("dve", lob[:, 0:ntok], xc[:, 0:ntok], hfb[:, 0:ntok], ALU.mult, r=["l_xc", "l_hfb"], w=["l_ob"])
                            p.dma(S["mixT"][1536 + n * 128:1536 + (n + 1) * 128, t0:t0 + ntok], lob[:, 0:ntok], r=["l_ob"], w=["mixT"])
            p.barrier()
        if "lru" in dbg:
            return finish(["mixT"])

        with ExitStack() as st:
            kT = sb("a_k", [128, NT], BF16, stack=st)
            qT = sb("a_q", [128, NT], BF16, stack=st)
            vv = sb("a_v", [128, NTL, 128], BF16, stack=st)
            btm = sb("a_bt", [128, NCLS, 5, 128], stack=st)
            sT = sb("a_s", [128, 5 * 128], stack=st)
            pT = sb("a_p", [128, 8 * 128], BF16, stack=st)
            rs_ = sb("a_rs", [128, 128], stack=st)
            aob = [sb(f"a_ob{i}", [128, 128], BF16, stack=st) for i in range(2)]
            for hd in range(8):
                for c0 in range(0, NT, 2048):
                    c1 = min(NT, c0 + 2048)
                    p.dma(kT[:, c0:c1], S["nkT"][hd * 128:(hd + 1) * 128, c0:c1], r=["nqk"], w=["a_k"])
                    p.dma(qT[:, c0:c1], S["nqT"][hd * 128:(hd + 1) * 128, c0:c1], r=["nqk"], w=["a_q"])
                for c0 in range(0, NTL, 16):
                    c1 = min(NTL, c0 + 16)
                    p.dma(vv[:, c0:c1, :], S["nv"][c0 * 128:c1 * 128, hd * 128:(hd + 1) * 128].rearrange("(t p) f -> p t f", p=128),
                          r=["tokdst"], w=["a_v"])
                for ci in range(NCLS):
                    p.dma(btm[:, ci, :, :], I["bt"][l, ci, hd], w=["a_bt"])
                for t in range(NTL):
                    if t < nct:
                        win, ci = [], 0
                    else:
                        i = t - nct
                        win = [nct + plo_l[i] + c for c in range(nch_l[i])]
                        ci = cls_l[i]
                    keys = win + list(range(nct))
                    nw, nk = len(win), len(keys)
                    for idx, kt in enumerate(keys):
                        bk = idx // 4
                        p.mm(psf[bk][:, (idx % 4) * 128:(idx % 4 + 1) * 128], kT[:, kt * 128:(kt + 1) * 128], qT[:, t * 128:(t + 1) * 128],
                             True, True, r=["a_k", "a_q"], w=[f"psf{bk}"])
                    for bk in range(2):
                        a0, a1 = bk * 4, min(nw, bk * 4 + 4)
                        if a1 > a0:
                            n_ = (a1 - a0) * 128
                            p.tt("dve", sT[:, a0 * 128:a1 * 128], psf[bk][:, 0:n_],
                                 btm[:, ci, a0:a1, :].rearrange("p c q -> p (c q)"), ALU.add, r=[f"psf{bk}", "a_bt"], w=["a_s"])
                    if nw:
                        p.act(pT[:, 0:nw * 128], sT[:, 0:nw * 128], AF.Exp, r=["a_s"], w=["a_p"])
                    for idx in range(nw, nk):
                        bk = idx // 4
                        p.act(pT[:, idx * 128:(idx + 1) * 128], psf[bk][:, (idx % 4) * 128:(idx % 4 + 1) * 128], AF.Exp,
                              r=[f"psf{bk}"], w=["a_p"])
                    for idx, kt in enumerate(keys):
                        p.mm(psf[2][:, 0:128], vv[:, kt, :], pT[:, idx * 128:(idx + 1) * 128], idx == 0, idx == nk - 1,
                             r=["a_v", "a_p"], w=["psf2"])
                    for idx, kt in enumerate(keys):
                        p.mm(psf[3][:, 0:128], ones_b, pT[:, idx * 128:(idx + 1) * 128], idx == 0, idx == nk - 1,
                             r=["cstb", "a_p"], w=["psf3"])
                    p.op("dve", lambda e: e.reciprocal(rs_[:], psf[3][:, 0:128]), r=["psf3"], w=["a_rs"])
                    b = t % 2
                    p.tt("dve", aob[b][:], psf[2][:, 0:128], rs_[:], ALU.mult, r=["psf2", "a_rs"], w=[f"a_ob{b}"])
                    p.dma(S["mixT"][512 + hd * 128:512 + (hd + 1) * 128, t * 128:(t + 1) * 128], aob[b][:], r=[f"a_ob{b}"], w=["mixT"])
            p.barrier()
        if "mix" in dbg:
            return finish(["mixT"])

        with ExitStack() as st:
            wo = sb("o_w", [128, KD, D], BF16, stack=st)
            gbc = sb("o_g", [128, 2, D], stack=st)
            mx_ = [sb(f"o_m{i}", [128, KD, 128], BF16, stack=st) for i in range(2)]
            xt = [sb(f"o_x{i}", [128, D], stack=st) for i in range(2)]
            tq = sb("o_t", [128, 512], stack=st)
            for k0 in range(0, KD, 4):
                p.dma(wo[:, k0:k0 + 4, :], I["w_out"][l].rearrange("(k p) c -> p k c", p=128)[:, k0:k0 + 4, :], w=["o_w"], q="pool")
            for s_ in range(2):
                p.dma(gbc[:, s_, :], S["modsd"][s_:s_ + 1, 2 * D:3 * D].partition_broadcast(128), r=["modsd"], w=["o_g"])
            for t in range(NTL):
                b = t % 2
                s_ = stream_of(t)
                p.dma(mx_[b][:], S["mixT"].rearrange("(k p) n -> p k n", p=128)[:, :, t * 128:(t + 1) * 128], r=["mixT"], w=[f"o_m{b}"])
                p.dma(xt[b][:], S["xcur"][t * 128:(t + 1) * 128, :], r=["xcur"], w=[f"o_x{b}"])
                for cb in range(4):
                    ps, pk = psf[cb], f"psf{cb}"
                    for k in range(KD):
                        p.mm(ps[:, :], mx_[b][:, k, :], wo[:, k, cb * 512:(cb + 1) * 512], k == 0, k == KD - 1, r=[f"o_m{b}", "o_w"], w=[pk])
                    p.tt("dve", tq[:], ps[:, :], gbc[:, s_, cb * 512:(cb + 1) * 512], ALU.mult, r=[pk, "o_g"], w=["o_t"])
                    p.tt("pool", xt[b][:, cb * 512:(cb + 1) * 512], xt[b][:, cb * 512:(cb + 1) * 512], tq[:], ALU.add,
                         r=["o_t", f"o_x{b}"], w=[f"o_x{b}"])
                p.dma(S["xcur"][t * 128:(t + 1) * 128, :], xt[b][:], r=[f"o_x{b}"], w=["xcur"])
            p.barrier()
        if "xmid" in dbg:
            return finish(["xcur"])

        with ExitStack() as st:
            affT = sb("r_affT", [NE, NT], stack=st)
            with ExitStack() as st2:
                wr_s = sb("r_w", [128, KD, NE], BF16, stack=st2)
                lg = sb("r_lg", [128, NE], stack=st2)
                rsm = sb("r_sm", [128, 4], stack=st2)
                p.dma(wr_s[:], I["w_r"][l].rearrange("(k p) e -> p k e", p=128), w=["r_w"], q="pool")

                def router(t, hTt, hkey):
                    for k in range(KD):
                        p.mm(psf[4][:, 0:NE], hTt[:, k, :], wr_s[:, k, :], k == 0, k == KD - 1, r=[hkey, "r_w"], w=["psf4"])
                    p.op("dve", lambda e: e.reduce_max(out=rsm[:, 0:1], in_=psf[4][:, 0:NE], axis=AX.X), r=["psf4"], w=["r_mx"])
                    p.ts("dve", rsm[:, 1:2], rsm[:, 0:1], -1.0, None, ALU.mult, r=["r_mx"], w=["r_nmx"])
                    p.act(lg[:], psf[4][:, 0:NE], AF.Exp, r=["psf4", "r_nmx"], w=["r_lg", "r_sum"], bias=rsm[:, 1:2], accum=rsm[:, 2:3])
                    p.op("dve", lambda e: e.reciprocal(rsm[:, 3:4], rsm[:, 2:3]), r=["r_sum"], w=["r_rs"])
                    p.ts("dve", lg[:], lg[:], rsm[:, 3:4], None, ALU.mult, r=["r_lg", "r_rs"], w=["r_lg"])
                    p.mm(psf[5][0:NE, 0:128], lg[:], ident_f, True, True, r=["r_lg", "cst"], w=["psf5"])
                    p.copy("act", affT[:, t * 128:(t + 1) * 128], psf[5][0:NE, 0:128], r=["psf5"], w=["r_affT"])

                norm_stage(st2, 1, None, dst_xn=S["xn2"], router=router)
                p.barrier()
            with ExitStack() as st2:
                wk = sb("r_wk", [NE, cfg.seq], stack=st2)
                wts = sb("r_wts", [NE, cfg.cap_l], stack=st2)
                ixs = sb("r_ixs", [NE, cfg.cap_l], U32, stack=st2)
                for (c0, n, cap, doff, off) in [(cfg.ctx, cfg.seq, cfg.cap_l, 0, cfg.ctx), (0, cfg.ctx, cfg.cap_c, 1, 0)]:
                    p.copy("dve", wk[:, 0:n], affT[:, c0:c0 + n], r=["r_affT"], w=["r_wk"])
                    for r_ in range(cap // 8):
                        s8 = slice(r_ * 8, r_ * 8 + 8)
                        p.op("dve", lambda e: e.max(out=wts[:, s8], in_=wk[:, 0:n]), r=["r_wk"], w=["r_wts"])
                        p.op("dve", lambda e: e.max_index(out=ixs[:, s8], in_max=wts[:, s8], in_values=wk[:, 0:n]), r=["r_wk", "r_wts"], w=["r_ixs"])
                        p.op("dve", lambda e: e.match_replace(out=wk[:, 0:n], in_to_replace=wts[:, s8], in_values=wk[:, 0:n], imm_value=-1.0),
                             r=["r_wts", "r_wk"], w=["r_wk"])
                    if off:
                        p.ts("dve", ixs[:, 0:cap], ixs[:, 0:cap], float(off), None, ALU.add, r=["r_ixs"], w=["r_ixs"])
                    dI = S["idxd"] if doff == 0 else S["idxc"]
                    dW = S["wtd"] if doff == 0 else S["wtc"]
                    p.dma(dI[:, 0:cap], ixs[:, 0:cap], r=["r_ixs"], w=["idxd"])
                    p.dma(dW[:, 0:cap], wts[:, 0:cap], r=["r_wts"], w=["idxd"])
                p.barrier()
        if "route" in dbg:
            return finish(["idxd", "wtd", "idxc", "wtc"])

        with ExitStack() as st:
            GS = min(1024, cfg.cap_l)
            xsT = sb("e_xsT", [128, KD, GS], BF16, stack=st)
            hid = sb("e_hid", [128, KD, GS], BF16, stack=st)
            wd = sb("e_wd", [128, KD, D], BF16, stack=st)
            wgf = [sb(f"e_wg{i}", [128, KD, 128], BF16, stack=st) for i in range(2)]
            wuf = [sb(f"e_wu{i}", [128, KD, 128], BF16, stack=st) for i in range(2)]
            xs = sb("e_xs", [128, D], BF16, stack=st)
            xr = sb("e_xr", [128, D], stack=st)
            yt = sb("e_yt", [128, 512], stack=st)
            sg2 = [sb(f"e_sg{i}", [128, 512], stack=st) for i in range(2)]
            eit = [0]
            icol = sb("e_ic", [128, 1], U32, stack=st)
            wcol = sb("e_wc", [128, 1], stack=st)
            g2 = sb("e_g2", [128, 2, D], stack=st)
            for s_ in range(2):
                p.dma(g2[:, s_, :], S["modsd"][s_:s_ + 1, 5 * D:6 * D].partition_broadcast(128), r=["modsd"], w=["e_g2"])
            for e_ in range(NE):
                for k0 in range(0, KD, 4):
                    p.dma(wd[:, k0:k0 + 4, :], I["wed"][l, e_].rearrange("(k p) c -> p k c", p=128)[:, k0:k0 + 4, :], w=["e_wd"], q="pool")
                groups = [(0, g0, min(GS, cfg.cap_l - g0)) for g0 in range(0, cfg.cap_l, GS)] + [(1, 0, cfg.cap_c)]
                for (s_, g0, ns) in groups:
                    dI = S["idxd"] if s_ == 0 else S["idxc"]
                    dW = S["wtd"] if s_ == 0 else S["wtc"]
                    tiles = [(j0, min(128, ns - j0)) for j0 in range(0, ns, 128)]
                    for (j0, rows) in tiles:
                        p.dma(icol[0:rows, :], dI[e_, g0 + j0:g0 + j0 + rows].rearrange("(p o) -> p o", o=1), r=["idxd"], w=["e_ic"])
                        p.op("pool", lambda e: e.indirect_dma_start(out=xs[0:rows, :], out_offset=None, in_=S["xn2"][:, :],
                                                                      in_offset=bass.IndirectOffsetOnAxis(ap=icol[0:rows, 0:1], axis=0)),
                             r=["e_ic", "xn2"], w=["e_xs"], dma=True)
                        for k in range(KD):
                            pb = pst[k // 8]
                            p.tr(pb[:, (k % 8) * 128:(k % 8) * 128 + rows], xs[0:rows, k * 128:(k + 1) * 128], ident_b[0:rows, 0:rows],
                                 r=["e_xs", "cstb"], w=[f"pst{k // 8}"])
                        for k in range(KD):
                            pb = pst[k // 8]
                            src = pb[:, (k % 8) * 128:(k % 8) * 128 + rows]
                            if k % 2 == 0:
                                p.ts("dve", xsT[:, k, j0:j0 + rows], src, amul[:, 1, k, s_:s_ + 1], modp[:, 3, k, s_:s_ + 1],
                                     ALU.mult, ALU.add, r=[f"pst{k // 8}"], w=["e_xsT"])
                            else:
                                p.act(xsT[:, k, j0:j0 + rows], src, AF.Identity, r=[f"pst{k // 8}"], w=["e_xsT"],
                                      scale=amul[:, 1, k, s_:s_ + 1], bias=modp[:, 3, k, s_:s_ + 1])
                    for f in range(KD):
                        b = f % 2
                        p.dma(wgf[b][:], I["weg"][l, e_].rearrange("(k p) c -> p k c", p=128)[:, :, f * 128:(f + 1) * 128], w=[f"e_wg{b}"], q="pool")
                        p.dma(wuf[b][:], I["weu"][l, e_].rearrange("(k p) c -> p k c", p=128)[:, :, f * 128:(f + 1) * 128], w=[f"e_wu{b}"], q="pool")
                        for s0 in range(0, ns, 512):
                            n_ = min(512, ns - s0)
                            eit[0] += 1
                            ia, ib = (0, 1) if eit[0] % 2 == 0 else (4, 5)
                            sgb, sgk = sg2[eit[0] % 2], f"e_sg{eit[0] % 2}"
                            for k in range(KD):
                                p.mm(psf[ia][:, 0:n_], wgf[b][:, k, :], xsT[:, k, s0:s0 + n_], k == 0, k == KD - 1, r=[f"e_wg{b}", "e_xsT"], w=[f"psf{ia}"])
                            for k in range(KD):
                                p.mm(psf[ib][:, 0:n_], wuf[b][:, k, :], xsT[:, k, s0:s0 + n_], k == 0, k == KD - 1, r=[f"e_wu{b}", "e_xsT"], w=[f"psf{ib}"])
                            p.act(sgb[:, 0:n_], psf[ia][:, 0:n_], AF.Silu, r=[f"psf{ia}"], w=[sgk])
                            p.tt("dve", hid[:, f, s0:s0 + n_], sgb[:, 0:n_], psf[ib][:, 0:n_], ALU.mult, r=[sgk, f"psf{ib}"], w=["e_hid"])
                    for (j0, rows) in tiles:
                        p.dma(icol[0:rows, :], dI[e_, g0 + j0:g0 + j0 + rows].rearrange("(p o) -> p o", o=1), r=["idxd"], w=["e_ic"])
                        p.dma(wcol[0:rows, :], dW[e_, g0 + j0:g0 + j0 + rows].rearrange("(p o) -> p o", o=1), r=["idxd"], w=["e_wc"])
                        p.op("pool", lambda e: e.indirect_dma_start(out=xr[0:rows, :], out_offset=None, in_=S["xcur"][:, :],
                                                                      in_offset=bass.IndirectOffsetOnAxis(ap=icol[0:rows, 0:1], axis=0)),
                             r=["e_ic", "xcur"], w=["e_xr"], dma=True)
                        for cb in range(4):
                            ps, pk = psf[2 + cb % 2], f"psf{2 + cb % 2}"
                            for f in range(KD):
                                p.mm(ps[0:rows, :], hid[:, f, j0:j0 + rows], wd[:, f, cb * 512:(cb + 1) * 512], f == 0, f == KD - 1,
                                     r=["e_hid", "e_wd"], w=[pk])
                            p.stt(yt[0:rows, :], ps[0:rows, :], wcol[0:rows, 0:1], g2[0:rows, s_, cb * 512:(cb + 1) * 512], ALU.mult, ALU.mult,
                                  r=[pk, "e_wc", "e_g2"], w=["e_yt"])
                            p.tt("pool", xr[0:rows, cb * 512:(cb + 1) * 512], xr[0:rows, cb * 512:(cb + 1) * 512], yt[0:rows, :], ALU.add,
                                 r=["e_yt", "e_xr"], w=["e_xr"])
                        p.op("pool", lambda e: e.indirect_dma_start(out=S["xcur"][:, :],
                                                                      out_offset=bass.IndirectOffsetOnAxis(ap=icol[0:rows, 0:1], axis=0),
                                                                      in_=xr[0:rows, :], in_offset=None),
                             r=["e_ic", "e_xr"], w=["xcur"], dma=True)
            p.barrier()
        if "layer" in dbg:
            return finish(["xcur"])

    big_copy(yout, S["xcur"][cfg.ctx:, :], cfg.seq, key="yout")
    p.barrier()
    es.close()
    return nc


def prep_inputs(cfg, inp):
    L = cfg.depth
    f = lambda a: np.ascontiguousarray(np.asarray(a, dtype=np.float32))
    pk = lambda v: f(np.asarray(v).reshape(-1, 128).T)
    m = {}
    m["xin"] = f(np.concatenate([inp["ctx"][0], inp["x"][0]], 0))
    m["cT"] = f(np.stack([pk(inp["c"][0]), pk(inp["c_ctx"])], -1))
    m["w_mod"] = f(inp["w_mod"])
    m["b_mod"] = f(inp["b_mod"]).reshape(L, 1, -1)
    m["n1g"] = f(np.stack([pk(inp["norm1_g"][l]) for l in range(L)]))
    m["n2g"] = f(np.stack([pk(inp["norm2_g"][l]) for l in range(L)]))
    m["w_in"] = f(inp["w_in"])
    m["wg"] = f(inp["gla_w_gate"])
    m["bg"] = f(np.stack([np.concatenate([pk(inp["gla_b_gate"][l][d]) for d in range(2)], 1) for l in range(L)]))
    m["gng"] = f(inp["gla_norm_g"]).reshape(L, 1, 128)
    m["nqg"] = f(inp["na_q_norm_g"]).reshape(L, 128, 1)
    m["nkg"] = f(inp["na_k_norm_g"]).reshape(L, 128, 1)
    bts = []
    for l in range(L):
        bt, _, _, _ = _na_masks(cfg, np.asarray(inp["na_rpb"][l], np.float32))
        bts.append(bt.transpose(0, 1, 3, 2, 4))
    m["bt"] = f(np.stack(bts))
    m["cw"] = f(np.stack([np.asarray(inp["lru_conv_w"][l]).reshape(4, 4, 128).transpose(2, 1, 0) for l in range(L)]))
    m["cb"] = f(np.stack([pk(inp["lru_conv_b"][l]) for l in range(L)]))
    m["wa"] = f(inp["lru_w_a"])
    m["wi"] = f(inp["lru_w_i"])
    cat2 = lambda a, l: np.concatenate([pk(a[l][d]) for d in range(2)], 1)
    m["ba"] = f(np.stack([cat2(inp["lru_b_a"], l) for l in range(L)]))
    m["bi"] = f(np.stack([cat2(inp["lru_b_i"], l) for l in range(L)]))
    m["lam"] = f(np.stack([cat2(inp["lru_lambda"], l) for l in range(L)]))
    m["w_out"] = f(inp["w_out"])
    m["w_r"] = f(inp["w_router"])
    m["weg"] = f(inp["w_exp_gate"])
    m["weu"] = f(inp["w_exp_up"])
    m["wed"] = f(inp["w_exp_down"])
    m["rope"] = _rope_tables(cfg)
    tri = np.tril(np.ones((128, 128), np.float32))
    m["consts"] = f(np.stack([np.eye(128), np.ones((128, 128)), tri.T, tri, np.zeros((128, 128)), np.zeros((128, 128))]))
    return m


def kernel(**inputs):
    cfg = Cfg()
    nc = build_program(cfg)
    m = prep_inputs(cfg, inputs)
    res = run_bass_kernel_spmd(nc, [m], core_ids=[0])
    return np.asarray(res.results[0]["yout"], dtype=np.float32).reshape(1, cfg.seq, D)
```

```python
import numpy as np
import concourse.bass as bass
import concourse.mybir as mybir
from concourse.bass_utils import run_bass_kernel_spmd

F32 = mybir.dt.float32
BF16 = mybir.dt.bfloat16
U32 = mybir.dt.uint32
AF = mybir.ActivationFunctionType
ALU = mybir.AluOpType
AX = mybir.AxisListType

D = 2048
KD = D // 128
EPS = 1e-6
GRID_W = 64
D_IN = 5664
C_Q, C_K, C_V, C_R, C_LF, C_LB, C_NQ, C_NK, C_NV, C_LX, C_LY = 0, 256, 512, 1024, 1536, 1552, 1568, 2592, 3616, 4640, 5152


class Cfg:
    def __init__(self, seq=16384, ctx=256, depth=2, ne=16):
        self.seq, self.ctx, self.depth, self.ne = seq, ctx, depth, ne
        self.nt = seq + ctx
        self.ntl = self.nt // 128
        self.rows = seq // GRID_W
        self.cap_l = 2 * seq // ne
        self.cap_c = 2 * ctx // ne


class P:
    def __init__(self, nc):
        self.nc = nc
        self.eng = {"pe": nc.tensor, "dve": nc.vector, "act": nc.scalar, "pool": nc.gpsimd, "sp": nc.sync}
        self.sem = {}
        self.cnt = {e: 0 for e in self.eng}
        self.waited = {e: {} for e in self.eng}
        self.res = {}
        self.dq = {e: 0 for e in self.eng}
        self.NDS = 8
        for e in self.eng:
            self.sem["c_" + e] = nc.semaphore("c_" + e).__enter__()
            for i in range(self.NDS):
                self.sem[f"d_{e}_{i}"] = nc.semaphore(f"d_{e}_{i}").__enter__()

    def _need(self, eng, tok, waits):
        if tok is None:
            return
        name, val = tok
        if eng == "pe" and name == "c_pe":
            return
        if self.waited[eng].get(name, 0) >= val:
            return
        self.waited[eng][name] = val
        waits.append(tok)

    def op(self, eng, fn, r=(), w=(), dma=False, sig=True):
        waits = []
        for k in r:
            st = self.res.get(k)
            if st:
                self._need(eng, st[0], waits)
        for k in w:
            st = self.res.get(k)
            if st:
                self._need(eng, st[0], waits)
                for t in st[1].items():
                    self._need(eng, t, waits)
        if dma:
            j = self.dq[eng]
            self.dq[eng] += 1
            name = f"d_{eng}_{j % self.NDS}"
            val = 16 * (j // self.NDS + 1)
            if j >= self.NDS:
                self._need(eng, (name, val - 16), waits)
            inc = 16
        elif not sig:
            name, val, inc = "c_" + eng, self.cnt[eng] + 1, 0
        else:
            self.cnt[eng] += 1
            name, val, inc = "c_" + eng, self.cnt[eng], 1
        e = self.eng[eng]
        for (n, v) in waits:
            e.wait_ge(self.sem[n], v)
        ins = fn(e)
        if inc:
            ins.then_inc(self.sem[name], inc)
        tok = (name, val)
        for k in r:
            st = self.res.setdefault(k, [None, {}])
            st[1][name] = max(st[1].get(name, 0), val)
        for k in w:
            self.res[k] = [tok, {}]
        return tok

    def barrier(self):
        toks = []
        for e in self.eng:
            if self.cnt[e]:
                toks.append(("c_" + e, self.cnt[e]))
            j = self.dq[e]
            for i in range(self.NDS):
                n = (j - 1 - i)
                if n >= 0:
                    toks.append((f"d_{e}_{n % self.NDS}", 16 * (n // self.NDS + 1)))
        for e in self.eng:
            ws = []
            for t in toks:
                self._need(e, t, ws)
            for (n, v) in ws:
                self.eng[e].wait_ge(self.sem[n], v)

    def dma(self, out, in_, r=(), w=(), q="sp", **kw):
        return self.op(q, lambda e: e.dma_start(out=out, in_=in_, **kw), r, w, dma=True)

    def mm(self, out, lhsT, rhs, start, stop, r=(), w=()):
        return self.op("pe", lambda e: e.matmul(out, lhsT, rhs, start=start, stop=stop), r, w, sig=bool(stop))

    def tr(self, out, in_, ident, r=(), w=()):
        return self.op("pe", lambda e: e.transpose(out, in_, ident), r, w)

    def act(self, out, in_, func, r=(), w=(), bias=0.0, scale=1.0, accum=None):
        if accum is None:
            return self.op("act", lambda e: e.activation(out=out, in_=in_, func=func, bias=bias, scale=scale), r, w)
        return self.op("act", lambda e: e.activation(out=out, in_=in_, func=func, bias=bias, scale=scale,
                                                       accum_out=accum), r, w)

    def ts(self, eng, out, in0, s1, s2, op0, op1=None, r=(), w=(), accum=None):
        if op1 is None:
            return self.op(eng, lambda e: e.tensor_scalar(out, in0, s1, None, op0), r, w)
        if accum is not None:
            return self.op(eng, lambda e: e.tensor_scalar(out, in0, s1, s2, op0, op1, accum), r, w)
        return self.op(eng, lambda e: e.tensor_scalar(out, in0, s1, s2, op0, op1), r, w)

    def tt(self, eng, out, in0, in1, op, r=(), w=()):
        return self.op(eng, lambda e: e.tensor_tensor(out, in0, in1, op), r, w)

    def stt(self, out, in0, scalar, in1, op0, op1, r=(), w=()):
        return self.op("dve", lambda e: e.scalar_tensor_tensor(out, in0, scalar, in1, op0, op1), r, w)

    def copy(self, eng, out, in_, r=(), w=()):
        if eng == "act":
            return self.op("act", lambda e: e.copy(out, in_), r, w)
        return self.op(eng, lambda e: e.tensor_copy(out, in_), r, w)


def _na_tile_geom(cfg):
    rows = cfg.rows
    rs = lambda r: min(max(r - 4, 0), rows - 8)
    plo, nch, cls, defs = [], [], [], {}
    for i in range(rows // 2):
        a, b = rs(2 * i), rs(2 * i + 1)
        lo, hi = a // 2, (b + 7) // 2
        key = (a - 2 * i, b - 2 * i - 1, lo - i, hi - lo + 1)
        if key not in defs:
            defs[key] = len(defs)
        plo.append(lo)
        nch.append(hi - lo + 1)
        cls.append(defs[key])
    return plo, nch, cls, defs


def _na_masks(cfg, rpb):
    plo, nch, cls, defs = _na_tile_geom(cfg)
    ncls = len(defs)
    out = np.full((ncls, 8, 5, 128, 128), -30000.0, np.float32)
    kk = np.arange(128)
    kdr, kc = kk // 64, kk % 64
    qdr, qc = kk // 64, kk % 64
    cs = np.clip(qc - 8, 0, GRID_W - 16)
    for key, ci in defs.items():
        a_rel, b_rel, lo_rel, n = key
        for c in range(n):
            krow = 2 * (lo_rel + c) + kdr
            rs_q = np.where(qdr == 0, a_rel, b_rel + 1)
            qrow = qdr
            inr = (krow[:, None] >= rs_q[None, :]) & (krow[:, None] <= rs_q[None, :] + 7)
            inc = (kc[:, None] >= cs[None, :]) & (kc[:, None] <= cs[None, :] + 15)
            ri = np.clip(krow[:, None] - qrow[None, :] + 7, 0, 14)
            cix = np.clip(kc[:, None] - qc[None, :] + 15, 0, 30)
            m = inr & inc
            for h in range(8):
                out[ci, h, c] = np.where(m, rpb[h][ri, cix], np.float32(-30000.0))
    return out, plo, nch, cls


def _rope_tables(cfg):
    pos = np.arange(cfg.seq)
    row = (pos // GRID_W).astype(np.float32)
    col = (pos % GRID_W).astype(np.float32)
    nf = 16
    inv = (np.float32(10000.0) ** (-np.arange(nf, dtype=np.float32) / np.float32(nf))).astype(np.float32)
    ang = np.concatenate([row[:, None] * inv, col[:, None] * inv], axis=-1).astype(np.float32)
    cos, sin = np.cos(ang).astype(np.float32), np.sin(ang).astype(np.float32)
    c_full = np.concatenate([np.ones((cfg.ctx, 32), np.float32), cos], 0)
    s_full = np.concatenate([np.zeros((cfg.ctx, 32), np.float32), sin], 0)
    p = np.arange(128)
    f = p % 32
    sign = np.where((p % 64) < 32, -1.0, 1.0).astype(np.float32)
    ck = c_full[:, f].T.copy()
    sk = (s_full[:, f].T * sign[:, None]).copy()
    sc = np.float32(64 ** -0.5)
    return np.stack([ck * sc, sk * sc, ck, sk]).astype(np.float32)


def build_program(cfg, dbg=()):
    nc = bass.Bass("TRN2", target_bir_lowering=False)
    NT, NTL, L, NE = cfg.nt, cfg.ntl, cfg.depth, cfg.ne
    plo_l, nch_l, cls_l, cdefs = _na_tile_geom(cfg)
    NCLS = len(cdefs)

    def din(name, shape, dt=F32):
        return nc.dram_tensor(name, list(shape), dt, kind="ExternalInput").ap()

    def dscr(name, shape, dt=F32):
        return nc.dram_tensor(name, list(shape), dt, kind="Internal").ap()

    I = dict(
        xin=din("xin", [NT, D]), cT=din("cT", [128, KD, 2]),
        w_mod=din("w_mod", [L, D, 6 * D]), b_mod=din("b_mod", [L, 1, 6 * D]),
        n1g=din("n1g", [L, 128, KD]), n2g=din("n2g", [L, 128, KD]),
        w_in=din("w_in", [L, D, D_IN]), wg=din("wg", [L, 2, 16, 256]), bg=din("bg", [L, 128, 4]),
        gng=din("gng", [L, 1, 128]), nqg=din("nqg", [L, 128, 1]), nkg=din("nkg", [L, 128, 1]),
        bt=din("bt", [L, NCLS, 8, 128, 5, 128]), cw=din("cw", [L, 128, 4, 4]), cb=din("cb", [L, 128, 4]),
        wa=din("wa", [L, 2, 4, 128, 128]), ba=din("ba", [L, 128, 8]), wi=din("wi", [L, 2, 4, 128, 128]),
        bi=din("bi", [L, 128, 8]), lam=din("lam", [L, 128, 8]),
        w_out=din("w_out", [L, D, D]), w_r=din("w_r", [L, D, NE]),
        weg=din("weg", [L, NE, D, D]), weu=din("weu", [L, NE, D, D]), wed=din("wed", [L, NE, D, D]),
        rope=din("rope", [4, 128, NT]), consts=din("consts", [6, 128, 128]),
    )
    yout = nc.dram_tensor("yout", [cfg.seq, D], F32, kind="ExternalOutput").ap()
    dbg_out = {}
    S = dict(
        xcur=dscr("xcur", [NT, D]), modsd=dscr("modsd", [2, 6 * D]), hT=dscr("hTd", [D, NT], BF16),
        gqT=dscr("gqT", [256, NT], BF16), gkT=dscr("gkT", [256, NT], BF16), gv=dscr("gv", [NT, 512], BF16),
        gr=dscr("gr", [NT, 512]), gg=dscr("gg", [2, 256, NT]), go=dscr("go", [NT, 512]),
        nqT=dscr("nqT", [1024, NT], BF16), nkT=dscr("nkT", [1024, NT], BF16), nv=dscr("nv", [NT, 1024], BF16),
        lxT=dscr("lxT", [512, NT]), lyT=dscr("lyT", [512, NT]), hf=dscr("hf", [512, NT]),
        mixT=dscr("mixT", [D, NT], BF16), xn2=dscr("xn2", [NT, D], BF16),
        idxd=dscr("idxd", [NE, cfg.cap_l], U32), wtd=dscr("wtd", [NE, cfg.cap_l]),
        idxc=dscr("idxc", [NE, cfg.cap_c], U32), wtc=dscr("wtc", [NE, cfg.cap_c]),
    )
    p = P(nc)
    from contextlib import ExitStack
    es = ExitStack()

    uid = [0]

    def sb(name, shape, dt=F32, stack=None):
        uid[0] += 1
        return (stack or es).enter_context(nc.sbuf_tensor(f"{name}_u{uid[0]}", list(shape), dt))

    psf = [es.enter_context(nc.psum_tensor(f"psf{i}", [128, 512], F32)) for i in range(6)]
    pst = [es.enter_context(nc.psum_tensor(f"pst{i}", [128, 1024], BF16)) for i in range(2)]
    cst = sb("cst", [128, 6, 128])
    cstb = sb("cstb", [128, 2, 128], BF16)
    p.dma(cst[:], I["consts"].rearrange("c p n -> p c n"), w=["cst"])
    p.copy("dve", cstb[:], cst[:, 0:2, :], r=["cst"], w=["cstb"])
    ident_f, ones_f, maskF, maskB = cst[:, 0, :], cst[:, 1, :], cst[:, 2, :], cst[:, 3, :]
    ident_b, ones_b = cstb[:, 0, :], cstb[:, 1, :]
    modp = sb("modp", [128, 6, KD, 2])
    amul = sb("amul", [128, 2, KD, 2])
    def big_copy(dst, src, rows, step=128, key="bigcopy"):
        for r0 in range(0, rows, step):
            r1 = min(rows, r0 + step)
            p.dma(dst[r0:r1], src[r0:r1], w=[key])

    def finish(names):
        for nm in names:
            o = nc.dram_tensor("dbg_" + nm, list(S[nm].shape), S[nm].dtype, kind="ExternalOutput").ap()
            big_copy(o, S[nm], S[nm].shape[0], key="dbg")
        p.barrier()
        return nc

    big_copy(S["xcur"], I["xin"], NT, key="xcur")
    p.barrier()

    def stream_of(tile):
        return 1 if tile < cfg.ctx // 128 else 0

    supers = [(0, cfg.ctx, 1)] + [(cfg.ctx + i * 512, 512, 0) for i in range(cfg.seq // 512)]

    for l in range(L):
        with ExitStack() as st:
            sc = sb("m_sc", [128, KD, 2], stack=st)
            wm = sb("m_w", [128, KD, 512], stack=st)
            brow = sb("m_b", [2, 512], stack=st)
            mrow = sb("m_r", [2, 512], stack=st)
            p.dma(sc[:], I["cT"], w=["m_sc"])
            p.act(sc[:], sc[:], AF.Silu, r=["m_sc"], w=["m_sc"])
            for cb in range(24):
                p.dma(wm[:], I["w_mod"][l].rearrange("(k p) c -> p k c", p=128)[:, :, cb * 512:(cb + 1) * 512],
                      w=["m_w"])
                p.dma(brow[:], I["b_mod"][l][:, cb * 512:(cb + 1) * 512].partition_broadcast(2), w=["m_b"])
                for k in range(KD):
                    p.mm(psf[0][0:2, :], sc[:, k, :], wm[:, k, :], k == 0, k == KD - 1, r=["m_sc", "m_w"], w=["psf0"])
                p.tt("dve", mrow[:], psf[0][0:2, :], brow[:], ALU.add, r=["psf0", "m_b"], w=["m_r"])
                p.dma(S["modsd"][:, cb * 512:(cb + 1) * 512], mrow[:], r=["m_r"], w=["modsd"])
            p.barrier()
            for j in range(6):
                for s_ in range(2):
                    p.dma(modp[:, j, :, s_], S["modsd"][s_, j * D:(j + 1) * D].rearrange("(k p) -> p k", p=128),
                          r=["modsd"], w=["modp"], allow_slow_non_contiguous=True)
            g1 = sb("m_g1", [128, KD], stack=st)
            g2 = sb("m_g2", [128, KD], stack=st)
            p.dma(g1[:], I["n1g"][l], w=["m_g1"])
            p.dma(g2[:], I["n2g"][l], w=["m_g2"])
            for s_ in range(2):
                p.stt(amul[:, 0, :, s_], modp[:, 1, :, s_], 1.0, g1[:], ALU.add, ALU.mult, r=["modp", "m_g1"], w=["amul"])
                p.stt(amul[:, 1, :, s_], modp[:, 4, :, s_], 1.0, g2[:], ALU.add, ALU.mult, r=["modp", "m_g2"], w=["amul"])
            p.barrier()

        if "mods" in dbg:
            return finish(["modsd"])

        def norm_stage(st, which, dst_hT, dst_xn=None, router=None):
            xt = [sb(f"n_x{i}", [128, D], stack=st) for i in range(2)]
            junk = sb("n_junk", [128, D], BF16, stack=st)
            xn = [sb(f"n_xn{i}", [128, D], BF16, stack=st) for i in range(2)]
            hTt = [sb(f"n_hT{i}", [128, KD, 128], BF16, stack=st) for i in range(2)]
            stat = sb("n_stat", [128, 4], stack=st)
            for t in range(NTL):
                b = t % 2
                s_ = stream_of(t)
                p.dma(xt[b][:], S["xcur"][t * 128:(t + 1) * 128, :], r=["xcur"], w=[f"n_x{b}"])
                p.act(junk[:], xt[b][:], AF.Square, r=[f"n_x{b}"], w=["n_junk", "n_ss"], accum=stat[:, 0:1])
                p.act(stat[:, 1:2], stat[:, 0:1], AF.Sqrt, r=["n_ss"], w=["n_sd"], scale=1.0 / D, bias=EPS)
                p.op("dve", lambda e: e.reciprocal(stat[:, 2:3], stat[:, 1:2]), r=["n_sd"], w=["n_rs"])
                p.ts("dve", xn[b][:], xt[b][:], stat[:, 2:3], None, ALU.mult, r=[f"n_x{b}", "n_rs"], w=[f"n_xn{b}"])
                if dst_xn is not None:
                    p.dma(dst_xn[t * 128:(t + 1) * 128, :], xn[b][:], r=[f"n_xn{b}"], w=["xn2"])
                for k in range(KD):
                    pb = pst[k // 8]
                    p.tr(pb[:, (k % 8) * 128:(k % 8 + 1) * 128], xn[b][:, k * 128:(k + 1) * 128], ident_b,
                         r=[f"n_xn{b}", "cstb"], w=[f"pst{k // 8}"])
                for k in range(KD):
                    pb = pst[k // 8]
                    src = pb[:, (k % 8) * 128:(k % 8 + 1) * 128]
                    if k % 2 == 0:
                        p.ts("dve", hTt[b][:, k, :], src, amul[:, which, k, s_:s_ + 1], modp[:, 3 * which, k, s_:s_ + 1],
                             ALU.mult, ALU.add, r=[f"pst{k // 8}", "amul", "modp"], w=[f"n_hT{b}"])
                    else:
                        p.act(hTt[b][:, k, :], src, AF.Identity, r=[f"pst{k // 8}", "amul", "modp"], w=[f"n_hT{b}"],
                              scale=amul[:, which, k, s_:s_ + 1], bias=modp[:, 3 * which, k, s_:s_ + 1])
                if dst_hT is not None:
                    p.dma(dst_hT.rearrange("(k p) n -> p k n", p=128)[:, :, t * 128:(t + 1) * 128], hTt[b][:],
                          r=[f"n_hT{b}"], w=["hTd"])
                if router is not None:
                    router(t, hTt[b], f"n_hT{b}")

        with ExitStack() as st:
            norm_stage(st, 0, S["hT"])
            p.barrier()
        if "norm" in dbg:
            return finish(["hT"])

        with ExitStack() as st:
            wb = sb("i_w", [128, KD, 1024], BF16, stack=st)
            wsw = sb("i_wsw", [128, KD, 512], BF16, stack=st)
            hs = [sb(f"i_h{i}", [128, KD, 512], BF16, stack=st) for i in range(2)]
            rp = sb("i_rope", [128, 4, 512], stack=st)
            ev = [sb(f"i_ev{i}", [128, 512], stack=st) for i in range(4)]
            evb = [sb(f"i_evb{i}", [128, 512], BF16, stack=st) for i in range(2)]
            lrt = sb("i_lr", [16, 2, 512], BF16, stack=st)
            wgs = sb("i_wg", [16, 2, 256], BF16, stack=st)
            bgs = sb("i_bg", [128, 4], stack=st)
            nbg = sb("i_nbg", [128, 4], stack=st)
            nqk = sb("i_nqk", [128, 2], stack=st)
            p.dma(wgs[:], I["wg"][l].rearrange("d r c -> r d c"), w=["i_wg"], q="pool")
            p.dma(bgs[:], I["bg"][l], w=["i_bg"])
            p.ts("dve", nbg[:], bgs[:], -1.0, None, ALU.mult, r=["i_bg"], w=["i_nbg"])
            p.dma(nqk[:, 0:1], I["nqg"][l], w=["i_nqk"])
            p.dma(nqk[:, 1:2], I["nkg"][l], w=["i_nqk"])
            p.ts("dve", nqk[:, 0:1], nqk[:, 0:1], float(128 ** -0.5), None, ALU.mult, r=["i_nqk"], w=["i_nqk"])
            wsrc = I["w_in"][l].rearrange("(k p) c -> p k c", p=128)

            def load_w(c0, n):
                for k0 in range(0, KD, 4):
                    p.dma(wb[:, k0:k0 + 4, 0:n], wsrc[:, k0:k0 + 4, c0:c0 + n], w=["i_w"], q="pool")

            def load_h(si):
                t0, ntok, s_ = supers[si]
                b = si % 2
                p.dma(hs[b][:, :, 0:ntok], S["hT"].rearrange("(k p) n -> p k n", p=128)[:, :, t0:t0 + ntok],
                      r=["hTd"], w=[f"i_h{b}"])
                return hs[b], f"i_h{b}", t0, ntok

            def fm_mm(ps, pskey, wtile, wkey, c0, m, h, hkey, ntok):
                for k in range(KD):
                    p.mm(ps[0:m, 0:ntok], wtile[:, k, c0:c0 + m], h[:, k, 0:ntok], k == 0, k == KD - 1,
                         r=[wkey, hkey], w=[pskey])

            load_w(C_Q, 512)
            p.dma(wb[:, :, 512:544], wsrc[:, :, C_LF:C_LF + 32], w=["i_w"], q="pool")
            w5 = wb[:, :, 0:512].rearrange("p k (h two f) -> p k h two f", two=2, f=32)
            s5 = wsw[:].rearrange("p k (h two f) -> p k h two f", two=2, f=32)
            p.copy("dve", s5[:, :, :, 0, :], w5[:, :, :, 1, :], r=["i_w"], w=["i_wsw"])
            p.copy("pool", s5[:, :, :, 1, :], w5[:, :, :, 0, :], r=["i_w"], w=["i_wsw"])
            for si in range(len(supers)):
                h, hkey, t0, ntok = load_h(si)
                p.dma(rp[:, :, 0:ntok], I["rope"].rearrange("c p n -> p c n")[:, :, t0:t0 + ntok], w=["i_rope"])
                for qk in range(2):
                    dst = S["gqT"] if qk == 0 else S["gkT"]
                    for hp in range(2):
                        c0 = qk * 256 + hp * 128
                        par = hp
                        pa_, pb_ = (0, 1) if par == 0 else (4, 5)
                        ea, eb, ec = 2 * par, 2 * par + 1, par
                        fm_mm(psf[pa_], f"psf{pa_}", wb, "i_w", c0, 128, h, hkey, ntok)
                        fm_mm(psf[pb_], f"psf{pb_}", wsw, "i_wsw", c0, 128, h, hkey, ntok)
                        p.tt("dve", ev[ea][:, 0:ntok], psf[pa_][:, 0:ntok], rp[:, 2 * qk, 0:ntok], ALU.mult,
                             r=[f"psf{pa_}", "i_rope"], w=[f"i_ev{ea}"])
                        p.tt("dve", ev[eb][:, 0:ntok], psf[pb_][:, 0:ntok], rp[:, 2 * qk + 1, 0:ntok], ALU.mult,
                             r=[f"psf{pb_}", "i_rope"], w=[f"i_ev{eb}"])
                        p.tt("pool", evb[ec][:, 0:ntok], ev[ea][:, 0:ntok], ev[eb][:, 0:ntok], ALU.add,
                             r=[f"i_ev{ea}", f"i_ev{eb}"], w=[f"i_evb{ec}"])
                        p.dma(dst[hp * 128:(hp + 1) * 128, t0:t0 + ntok], evb[ec][:, 0:ntok], r=[f"i_evb{ec}"], w=["gqk"])
                for d_ in range(2):
                    fm_mm(psf[2], "psf2", wb, "i_w", 512 + 16 * d_, 16, h, hkey, ntok)
                    p.copy("act", lrt[:, d_, 0:ntok], psf[2][0:16, 0:ntok], r=["psf2"], w=["i_lr"])
                for d_ in range(2):
                    for hp in range(2):
                        p.mm(psf[3][:, 0:ntok], wgs[:, d_, hp * 128:(hp + 1) * 128], lrt[:, d_, 0:ntok], True, True,
                             r=["i_wg", "i_lr"], w=["psf3"])
                        p.act(ev[2][:, 0:ntok], psf[3][:, 0:ntok], AF.Exp, r=["psf3", "i_nbg"], w=["i_ev2"],
                              scale=-1.0, bias=nbg[:, d_ * 2 + hp:d_ * 2 + hp + 1])
                        p.act(ev[2][:, 0:ntok], ev[2][:, 0:ntok], AF.Ln, r=["i_ev2"], w=["i_ev2"], bias=1.0)
                        p.ts("dve", ev[3][:, 0:ntok], ev[2][:, 0:ntok], -1.0 / 16.0, None, ALU.mult, r=["i_ev2"], w=["i_ev3"])
                        p.dma(S["gg"][d_, hp * 128:(hp + 1) * 128, t0:t0 + ntok], ev[3][:, 0:ntok], r=["i_ev3"], w=["gg"])

            if "ip1" in dbg:
                return finish(["gqT", "gkT", "gg"])

            def tok_group(c0, ncols, dst, dst_dt_bf, silu):
                load_w(c0, ncols)
                for si in range(len(supers)):
                    h, hkey, t0, ntok = load_h(si)
                    for tt_ in range(ntok // 128):
                        for cb in range(ncols // 512):
                            ps, pk = psf[(tt_ * 2 + cb) % 4], f"psf{(tt_ * 2 + cb) % 4}"
                            for k in range(KD):
                                p.mm(ps[:, :], h[:, k, tt_ * 128:(tt_ + 1) * 128], wb[:, k, cb * 512:(cb + 1) * 512],
                                     k == 0, k == KD - 1, r=["i_w", hkey], w=[pk])
                            tok0 = t0 + tt_ * 128
                            if dst_dt_bf:
                                e_, ek = evb[cb % 2], f"i_evb{cb % 2}"
                                p.copy("act" if cb % 2 else "dve", e_[:], ps[:, :], r=[pk], w=[ek])
                            else:
                                e_, ek = ev[cb % 2], f"i_ev{cb % 2}"
                                p.act(e_[:], ps[:, :], AF.Silu if silu else AF.Copy, r=[pk], w=[ek])
                            p.dma(dst[tok0:tok0 + 128, cb * 512:(cb + 1) * 512], e_[:], r=[ek], w=["tokdst"])

            tok_group(C_V, 512, S["gv"], True, False)
            tok_group(C_R, 512, S["gr"], False, True)
            tok_group(C_NV, 1024, S["nv"], True, False)

            if "ip2" in dbg:
                return finish(["gv", "gr", "nv"])
            for qk in range(2):
                load_w(C_NQ if qk == 0 else C_NK, 1024)
                dst = S["nqT"] if qk == 0 else S["nkT"]
                for si in range(len(supers)):
                    h, hkey, t0, ntok = load_h(si)
                    for hd in range(8):
                        pa_, pb_ = (0, 1) if hd % 2 == 0 else (2, 3)
                        fm_mm(psf[pa_], f"psf{pa_}", wb, "i_w", hd * 128, 128, h, hkey, ntok)
                        p.copy("dve", ev[0][:, 0:ntok], psf[pa_][:, 0:ntok], r=[f"psf{pa_}"], w=["i_ev0"])
                        p.tt("dve", evb[1][:, 0:ntok], ev[0][:, 0:ntok], ev[0][:, 0:ntok], ALU.mult, r=["i_ev0"], w=["i_evb1"])
                        p.mm(psf[pb_][:, 0:ntok], ones_b, evb[1][:, 0:ntok], True, True, r=["cstb", "i_evb1"], w=[f"psf{pb_}"])
                        p.act(ev[2][:, 0:ntok], psf[pb_][:, 0:ntok], AF.Sqrt, r=[f"psf{pb_}"], w=["i_ev2"], scale=1.0 / 128, bias=EPS)
                        p.op("dve", lambda e: e.reciprocal(ev[3][:, 0:ntok], ev[2][:, 0:ntok]), r=["i_ev2"], w=["i_ev3"])
                        p.stt(evb[0][:, 0:ntok], ev[0][:, 0:ntok], nqk[:, qk:qk + 1], ev[3][:, 0:ntok], ALU.mult, ALU.mult,
                              r=["i_ev0", "i_ev3", "i_nqk"], w=["i_evb0"])
                        p.dma(dst[hd * 128:(hd + 1) * 128, t0:t0 + ntok], evb[0][:, 0:ntok], r=["i_evb0"], w=["nqk"])

            if "ip3" in dbg:
                return finish(["nqT", "nkT"])
            load_w(C_LX, 1024)
            for si in range(len(supers)):
                h, hkey, t0, ntok = load_h(si)
                for c in range(8):
                    ps, pk = psf[c % 4], f"psf{c % 4}"
                    fm_mm(ps, pk, wb, "i_w", c * 128, 128, h, hkey, ntok)
                    e_, ek = ev[c % 4], f"i_ev{c % 4}"
                    p.copy("act" if c % 2 else "dve", e_[:, 0:ntok], ps[:, 0:ntok], r=[pk], w=[ek])
                    dst = S["lxT"] if c < 4 else S["lyT"]
                    p.dma(dst[(c % 4) * 128:(c % 4 + 1) * 128, t0:t0 + ntok], e_[:, 0:ntok], r=[ek], w=["lxy"])
            p.barrier()

        if "inproj" in dbg:
            return finish(["gqT", "gkT", "gg", "gv", "gr", "nqT", "nkT", "nv", "lxT", "lyT"])

        nct = cfg.ctx // 128
        with ExitStack() as st:
            BL = min(1024, cfg.seq)
            blocks = [(0, cfg.ctx)] + [(cfg.ctx + i * BL, BL) for i in range(cfg.seq // BL)]
            qb = sb("g_q", [128, BL], BF16, stack=st)
            kb = sb("g_k", [128, BL], BF16, stack=st)
            gb = sb("g_g", [128, BL], stack=st)
            vb = sb("g_v", [128, BL // 128, 256], BF16, stack=st)
            gc = sb("g_gc", [128, 128], stack=st)
            Hh = sb("g_H", [128, 128], stack=st)
            ex = sb("g_ex", [128, 3, 128], stack=st)
            qkb = sb("g_qkb", [128, 3, 128], BF16, stack=st)
            kdT = sb("g_kdT", [128, 128], BF16, stack=st)
            attm = [sb(f"g_att{i}", [128, 128], BF16, stack=st) for i in range(2)]
            Sst = sb("g_S", [128, 128], stack=st)
            Sbf = sb("g_Sb", [128, 128], BF16, stack=st)
            sm = sb("g_sm", [128, 8], stack=st)
            ob = sb("g_o", [128, 256], stack=st)
            of = sb("g_of", [128, 256], stack=st)
            rr = sb("g_r", [128, 256], stack=st)
            gn = sb("g_gn", [128, 128], stack=st)
            outb = sb("g_outb", [128, 256], BF16, stack=st)
            mT = sb("g_mT", [128, 256], BF16, stack=st)
            junk = sb("g_junk", [128, 128], stack=st)
            p.dma(gn[:], I["gng"][l].partition_broadcast(128), w=["g_gn"])
            for hp in range(2):
                for d_ in range(2):
                    p.op("dve", lambda e: e.memset(Sst[:], 0.0), w=["g_S"])
                    p.op("dve", lambda e: e.memset(Sbf[:], 0.0), w=["g_Sb"])
                    order = blocks if d_ == 0 else [blocks[0]] + blocks[:0:-1]
                    msk = maskF if d_ == 0 else maskB
                    for (t0, ntok) in order:
                        p.dma(qb[:, 0:ntok], S["gqT"][hp * 128:(hp + 1) * 128, t0:t0 + ntok], r=["gqk"], w=["g_q"])
                        p.dma(kb[:, 0:ntok], S["gkT"][hp * 128:(hp + 1) * 128, t0:t0 + ntok], r=["gqk"], w=["g_k"])
                        p.dma(gb[:, 0:ntok], S["gg"][d_, hp * 128:(hp + 1) * 128, t0:t0 + ntok], r=["gg"], w=["g_g"])
                        p.dma(vb[:, 0:ntok // 128, :],
                              S["gv"][t0:t0 + ntok, hp * 256:(hp + 1) * 256].rearrange("(c p) f -> p c f", p=128),
                              r=["tokdst"], w=["g_v"])
                        cs_ = list(range(ntok // 128))
                        if d_ == 1:
                            cs_ = cs_[::-1]
                        for c in cs_:
                            sl = slice(c * 128, (c + 1) * 128)
                            tok = t0 + c * 128
                            p.op("dve", lambda e: e.tensor_tensor_scan(out=gc[:], data0=ones_f, data1=gb[:, sl], initial=0.0,
                                                                         op0=ALU.mult, op1=ALU.add), r=["g_g", "cst"], w=["g_gc"])
                            gl = gc[:, 127:128]
                            p.act(sm[:, 2:3], gl, AF.Exp, r=["g_gc"], w=["g_dec"])
                            if d_ == 0:
                                p.act(ex[:, 0, :], gc[:], AF.Exp, r=["g_gc"], w=["g_ex"])
                                p.act(ex[:, 1, :], gc[:], AF.Exp, r=["g_gc"], w=["g_ex"], scale=-1.0)
                                p.act(ex[:, 2, :], gc[:], AF.Exp, r=["g_gc"], w=["g_ex"], scale=-1.0, bias=gl)
                            else:
                                p.tt("dve", Hh[:], gc[:], gb[:, sl], ALU.subtract, r=["g_gc", "g_g"], w=["g_H"])
                                p.ts("dve", sm[:, 1:2], gl, -1.0, None, ALU.mult, r=["g_gc"], w=["g_ngl"])
                                p.act(ex[:, 0, :], Hh[:], AF.Exp, r=["g_H", "g_gc"], w=["g_ex"], scale=-1.0, bias=gl)
                                p.act(ex[:, 1, :], Hh[:], AF.Exp, r=["g_H", "g_ngl"], w=["g_ex"], bias=sm[:, 1:2])
                                p.act(ex[:, 2, :], Hh[:], AF.Exp, r=["g_H"], w=["g_ex"])
                            p.tt("dve", qkb[:, 0, :], qb[:, sl], ex[:, 0, :], ALU.mult, r=["g_q", "g_ex"], w=["g_qe"])
                            p.tt("pool", qkb[:, 1, :], kb[:, sl], ex[:, 1, :], ALU.mult, r=["g_k", "g_ex"], w=["g_ke"])
                            p.tt("dve", qkb[:, 2, :], kb[:, sl], ex[:, 2, :], ALU.mult, r=["g_k", "g_ex"], w=["g_kd"])
                            p.tr(pst[0][:, 0:128], qkb[:, 2, :], ident_b, r=["g_kd", "cstb"], w=["pst0"])
                            p.copy("act", kdT[:], pst[0][:, 0:128], r=["pst0"], w=["g_kdT"])
                            for hh in range(2):
                                b0 = 64 * hh
                                p.mm(psf[0][:, hh * 128:(hh + 1) * 128], qkb[b0:b0 + 64, 1, :], qkb[b0:b0 + 64, 0, :], True, True,
                                     r=["g_ke", "g_qe"], w=["psf0"])
                                p.tt("dve", attm[hh][:], psf[0][:, hh * 128:(hh + 1) * 128], msk, ALU.mult,
                                     r=["psf0", "cst"], w=[f"g_att{hh}"])
                                p.mm(psf[1][:, hh * 128:(hh + 1) * 128], attm[hh][:], vb[:, c, hh * 128:(hh + 1) * 128], True, False,
                                     r=[f"g_att{hh}", "g_v"], w=["psf1"])
                                p.mm(psf[1][:, hh * 128:(hh + 1) * 128], qkb[b0:b0 + 64, 0, :], Sbf[b0:b0 + 64, :], False, True,
                                     r=["g_qe", "g_Sb"], w=["psf1"])
                            p.mm(psf[2][:, 0:256], kdT[:], vb[:, c, :], True, True, r=["g_kdT", "g_v"], w=["psf2"])
                            if d_ == 0:
                                p.copy("act", ob[:], psf[1][:, 0:256], r=["psf1"], w=["g_o"])
                                p.dma(S["go"][tok:tok + 128, hp * 256:(hp + 1) * 256], ob[:], r=["g_o"], w=["go"])
                            else:
                                p.dma(of[:], S["go"][tok:tok + 128, hp * 256:(hp + 1) * 256], r=["go"], w=["g_of"])
                                p.dma(rr[:], S["gr"][tok:tok + 128, hp * 256:(hp + 1) * 256], r=["tokdst"], w=["g_r"])
                                p.tt("dve", ob[:], psf[1][:, 0:256], of[:], ALU.add, r=["psf1", "g_of"], w=["g_o"])
                                for hh in range(2):
                                    oh = ob[:, hh * 128:(hh + 1) * 128]
                                    p.act(junk[:], oh, AF.Square, r=["g_o"], w=["g_junk", "g_ss"], accum=sm[:, 4 + hh:5 + hh])
                                p.act(sm[:, 6:8], sm[:, 4:6], AF.Sqrt, r=["g_ss"], w=["g_sd"], scale=1.0 / 128, bias=EPS)
                                p.op("dve", lambda e: e.reciprocal(sm[:, 4:6], sm[:, 6:8]), r=["g_sd"], w=["g_rs"])
                                for hh in range(2):
                                    oh = ob[:, hh * 128:(hh + 1) * 128]
                                    p.stt(oh, oh, sm[:, 4 + hh:5 + hh], gn[:], ALU.mult, ALU.mult, r=["g_o", "g_rs", "g_gn"], w=["g_o"])
                                p.tt("dve", outb[:], ob[:], rr[:], ALU.mult, r=["g_o", "g_r"], w=["g_outb"])
                                for hh in range(2):
                                    p.tr(pst[1][:, hh * 128:(hh + 1) * 128], outb[:, hh * 128:(hh + 1) * 128], ident_b,
                                         r=["g_outb", "cstb"], w=["pst1"])
                                p.copy("act", mT[:], pst[1][:, 0:256], r=["pst1"], w=["g_mT"])
                                for hh in range(2):
                                    f0 = (hp * 2 + hh) * 128
                                    p.dma(S["mixT"][f0:f0 + 128, tok:tok + 128], mT[:, hh * 128:(hh + 1) * 128], r=["g_mT"], w=["mixT"])
                            for hh in range(2):
                                b0 = 64 * hh
                                p.stt(Sst[b0:b0 + 64, :], Sst[b0:b0 + 64, :], sm[b0:b0 + 64, 2:3], psf[2][b0:b0 + 64, hh * 128:(hh + 1) * 128],
                                      ALU.mult, ALU.add, r=["g_S", "g_dec", "psf2"], w=["g_S"])
                            p.copy("act", Sbf[:], Sst[:], r=["g_S"], w=["g_Sb"])
            p.barrier()
        if "gla" in dbg:
            return finish(["mixT"])

        with ExitStack() as st:
            BL = min(2048, cfg.seq)
            blocks = [(0, cfg.ctx)] + [(cfg.ctx + i * BL, BL) for i in range(cfg.seq // BL)]
            xp = sb("l_xp", [128, BL + 3], stack=st)
            xc = sb("l_xc", [128, BL], stack=st)
            xcb = sb("l_xcb", [128, BL], BF16, stack=st)
            aa = sb("l_aa", [128, BL], stack=st)
            bb = sb("l_bb", [128, BL], stack=st)
            hh_ = sb("l_hh", [128, BL], stack=st)
            yb = sb("l_yb", [128, BL], stack=st)
            hfb = sb("l_hfb", [128, BL], stack=st)
            lob = sb("l_ob", [128, BL], BF16, stack=st)
            tmp = [sb(f"l_t{i}", [128, 512], stack=st) for i in range(4)]
            hprev = sb("l_hp", [128, 1], stack=st)
            cw_s = sb("l_cw", [128, 4, 4], stack=st)
            cb_s = sb("l_cb", [128, 4], stack=st)
            ba_s = sb("l_ba", [128, 8], stack=st)
            bi_s = sb("l_bi", [128, 8], stack=st)
            c8 = sb("l_c8", [128, 8], stack=st)
            wa_s = sb("l_wa", [128, 2, 4, 128], BF16, stack=st)
            wi_s = sb("l_wi", [128, 2, 4, 128], BF16, stack=st)
            p.dma(cw_s[:], I["cw"][l], w=["l_cw"])
            p.dma(cb_s[:], I["cb"][l], w=["l_cb"])
            p.dma(ba_s[:], I["ba"][l], w=["l_ba"])
            p.dma(bi_s[:], I["bi"][l], w=["l_bi"])
            p.dma(c8[:], I["lam"][l], w=["l_c8"])
            p.dma(wa_s[:], I["wa"][l].rearrange("d n c o -> c d n o"), w=["l_wa"], q="pool")
            p.dma(wi_s[:], I["wi"][l].rearrange("d n c o -> c d n o"), w=["l_wi"], q="pool")
            p.act(c8[:], c8[:], AF.Sigmoid, r=["l_c8"], w=["l_c8"])
            p.act(c8[:], c8[:], AF.Ln, r=["l_c8"], w=["l_c8"])
            p.ts("dve", c8[:], c8[:], 8.0, None, ALU.mult, r=["l_c8"], w=["l_c8"])
            for n in range(4):
                for d_ in range(2):
                    order = blocks if d_ == 0 else [blocks[0]] + blocks[:0:-1]
                    p.op("dve", lambda e: e.memset(hprev[:], 0.0), w=["l_hp"])
                    for (t0, ntok) in order:
                        seg_lo, seg_hi = (0, cfg.ctx) if t0 < cfg.ctx else (cfg.ctx, NT)
                        p.op("pool", lambda e: e.memset(xp[:], 0.0), w=["l_xp"])
                        lo, hi = max(t0 - 1, seg_lo), min(t0 + ntok + 2, seg_hi)
                        p.dma(xp[:, lo - (t0 - 1):hi - (t0 - 1)], S["lxT"][n * 128:(n + 1) * 128, lo:hi], r=["lxy"], w=["l_xp"])
                        p.ts("dve", xc[:, 0:ntok], xp[:, 0:ntok], cw_s[:, n, 0:1], cb_s[:, n:n + 1], ALU.mult, ALU.add,
                             r=["l_xp", "l_cw", "l_cb"], w=["l_xc"])
                        for j in range(1, 4):
                            p.stt(xc[:, 0:ntok], xp[:, j:j + ntok], cw_s[:, n, j:j + 1], xc[:, 0:ntok], ALU.mult, ALU.add,
                                  r=["l_xp", "l_cw", "l_xc"], w=["l_xc"])
                        p.copy("act", xcb[:, 0:ntok], xc[:, 0:ntok], r=["l_xc"], w=["l_xcb"])
                        SUB = min(512, ntok)
                        for s0 in range(0, ntok, SUB):
                            ss_ = slice(s0, s0 + SUB)
                            p.mm(psf[0][:, 0:SUB], wa_s[:, d_, n, :], xcb[:, ss_], True, True, r=["l_wa", "l_xcb"], w=["psf0"])
                            p.mm(psf[1][:, 0:SUB], wi_s[:, d_, n, :], xcb[:, ss_], True, True, r=["l_wi", "l_xcb"], w=["psf1"])
                            p.act(tmp[0][:, 0:SUB], psf[0][:, 0:SUB], AF.Sigmoid, r=["psf0", "l_ba"], w=["l_t0"],
                                  bias=ba_s[:, d_ * 4 + n:d_ * 4 + n + 1])
                            p.act(tmp[1][:, 0:SUB], psf[1][:, 0:SUB], AF.Sigmoid, r=["psf1", "l_bi"], w=["l_t1"],
                                  bias=bi_s[:, d_ * 4 + n:d_ * 4 + n + 1])
                            p.act(aa[:, ss_], tmp[0][:, 0:SUB], AF.Exp, r=["l_t0", "l_c8"], w=["l_aa"],
                                  scale=c8[:, d_ * 4 + n:d_ * 4 + n + 1])
                            p.stt(tmp[2][:, 0:SUB], aa[:, ss_], -1.0, aa[:, ss_], ALU.mult, ALU.mult, r=["l_aa"], w=["l_t2"])
                            p.act(tmp[2][:, 0:SUB], tmp[2][:, 0:SUB], AF.Sqrt, r=["l_t2"], w=["l_t2"], bias=1.0)
                            p.tt("dve", tmp[3][:, 0:SUB], tmp[1][:, 0:SUB], tmp[2][:, 0:SUB], ALU.mult, r=["l_t1", "l_t2"], w=["l_t3"])
                            p.tt("dve", bb[:, ss_], tmp[3][:, 0:SUB], xc[:, ss_], ALU.mult, r=["l_t3", "l_xc"], w=["l_bb"])
                        if d_ == 0:
                            p.op("dve", lambda e: e.tensor_tensor_scan(out=hh_[:, 0:ntok], data0=aa[:, 0:ntok], data1=bb[:, 0:ntok],
                                                                         initial=hprev[:, 0:1], op0=ALU.mult, op1=ALU.add),
                                 r=["l_aa", "l_bb", "l_hp"], w=["l_hh"])
                            p.copy("dve", hprev[:], hh_[:, ntok - 1:ntok], r=["l_hh"], w=["l_hp"])
                            p.dma(S["hf"][n * 128:(n + 1) * 128, t0:t0 + ntok], hh_[:, 0:ntok], r=["l_hh"], w=["hf"])
                        else:
                            p.op("dve", lambda e: e.tensor_tensor_scan(out=hh_[:, 0:ntok][:, ::-1], data0=aa[:, 0:ntok][:, ::-1],
                                                                         data1=bb[:, 0:ntok][:, ::-1], initial=hprev[:, 0:1],
                                                                         op0=ALU.mult, op1=ALU.add),
                                 r=["l_aa", "l_bb", "l_hp"], w=["l_hh"])
                            p.copy("dve", hprev[:], hh_[:, 0:1], r=["l_hh"], w=["l_hp"])
                            p.dma(hfb[:, 0:ntok], S["hf"][n * 128:(n + 1) * 128, t0:t0 + ntok], r=["hf"], w=["l_hfb"])
                            p.dma(yb[:, 0:ntok], S["lyT"][n * 128:(n + 1) * 128, t0:t0 + ntok], r=["lxy"], w=["l_yb"])
                            p.tt("pool", hfb[:, 0:ntok], hfb[:, 0:ntok], hh_[:, 0:ntok], ALU.add, r=["l_hfb", "l_hh"], w=["l_hfb"])
                            p.tt("dve", xc[:, 0:ntok], yb[:, 0:ntok], yb[:, 0:ntok], ALU.mult, r=["l_yb"], w=["l_xc"])
                            p.ts("dve", xc[:, 0:ntok], xc[:, 0:ntok], 0.044715, 1.0, ALU.mult, ALU.add, r=["l_xc"], w=["l_xc"])
                            p.tt("dve", xc[:, 0:ntok], xc[:, 0:ntok], yb[:, 0:ntok], ALU.mult, r=["l_xc", "l_yb"], w=["l_xc"])
                            p.act(xc[:, 0:ntok], xc[:, 0:ntok], AF.Sigmoid, r=["l_xc"], w=["l_xc"], scale=1.5957691216057308)
                            p.tt("dve", xc[:, 0:ntok], xc[:, 0:ntok], yb[:, 0:ntok], ALU.mult, r=["l_xc", "l_yb"], w=["l_xc"])
                            p.tt("dve", lob[:, 0:ntok], xc[:, 0:ntok], hfb[:, 0:ntok], ALU.mult, r=["l_xc", "l_hfb"], w=["l_ob"])
                            p.dma(S["mixT"][1536 + n * 128:1536 + (n + 1) * 128, t0:t0 + ntok], lob[:, 0:ntok], r=["l_ob"], w=["mixT"])
            p.barrier()
        if "lru" in dbg:
            return finish(["mixT"])

        with ExitStack() as st:
            kT = sb("a_k", [128, NT], BF16, stack=st)
            qT = sb("a_q", [128, NT], BF16, stack=st)
            vv = sb("a_v", [128, NTL, 128], BF16, stack=st)
            btm = sb("a_bt", [128, NCLS, 5, 128], stack=st)
            sTs = [sb(f"a_s{i}", [128, 5 * 128], stack=st) for i in range(2)]
            pTs = [sb(f"a_p{i}", [128, 8 * 128], BF16, stack=st) for i in range(2)]
            rss = [sb(f"a_rs{i}", [128, 128], stack=st) for i in range(2)]
            aob = [sb(f"a_ob{i}", [128, 128], BF16, stack=st) for i in range(2)]
            for hd in range(8):
                for c0 in range(0, NT, 2048):
                    c1 = min(NT, c0 + 2048)
                    p.dma(kT[:, c0:c1], S["nkT"][hd * 128:(hd + 1) * 128, c0:c1], r=["nqk"], w=["a_k"])
                    p.dma(qT[:, c0:c1], S["nqT"][hd * 128:(hd + 1) * 128, c0:c1], r=["nqk"], w=["a_q"])
                for c0 in range(0, NTL, 16):
                    c1 = min(NTL, c0 + 16)
                    p.dma(vv[:, c0:c1, :], S["nv"][c0 * 128:c1 * 128, hd * 128:(hd + 1) * 128].rearrange("(t p) f -> p t f", p=128),
                          r=["tokdst"], w=["a_v"])
                for ci in range(NCLS):
                    p.dma(btm[:, ci, :, :], I["bt"][l, ci, hd], w=["a_bt"])
                for t in range(NTL):
                    b = t % 2
                    sb0 = 3 * b
                    sT, pT, rs_ = sTs[b], pTs[b], rss[b]
                    ks, kp, kr = f"a_s{b}", f"a_p{b}", f"a_rs{b}"
                    po, pok = psf[sb0 + 2], f"psf{sb0 + 2}"
                    if t < nct:
                        win, ci = [], 0
                    else:
                        i = t - nct
                        win = [nct + plo_l[i] + c for c in range(nch_l[i])]
                        ci = cls_l[i]
                    keys = win + list(range(nct))
                    nw, nk = len(win), len(keys)
                    for idx, kt in enumerate(keys):
                        bk = sb0 + idx // 4
                        p.mm(psf[bk][:, (idx % 4) * 128:(idx % 4 + 1) * 128], kT[:, kt * 128:(kt + 1) * 128], qT[:, t * 128:(t + 1) * 128],
                             True, True, r=["a_k", "a_q"], w=[f"psf{bk}"])
                    for bk_ in range(2):
                        bk = sb0 + bk_
                        a0, a1 = bk_ * 4, min(nw, bk_ * 4 + 4)
                        if a1 > a0:
                            n_ = (a1 - a0) * 128
                            p.tt("dve", sT[:, a0 * 128:a1 * 128], psf[bk][:, 0:n_],
                                 btm[:, ci, a0:a1, :].rearrange("p c q -> p (c q)"), ALU.add, r=[f"psf{bk}", "a_bt"], w=[ks])
                    if nw:
                        p.act(pT[:, 0:nw * 128], sT[:, 0:nw * 128], AF.Exp, r=[ks], w=[kp])
                    for idx in range(nw, nk):
                        bk = sb0 + idx // 4
                        p.act(pT[:, idx * 128:(idx + 1) * 128], psf[bk][:, (idx % 4) * 128:(idx % 4 + 1) * 128], AF.Exp,
                              r=[f"psf{bk}"], w=[kp])
                    for idx, kt in enumerate(keys):
                        p.mm(po[:, 0:128], vv[:, kt, :], pT[:, idx * 128:(idx + 1) * 128], idx == 0, idx == nk - 1,
                             r=["a_v", kp], w=[pok])
                    for idx, kt in enumerate(keys):
                        p.mm(po[:, 128:256], ones_b, pT[:, idx * 128:(idx + 1) * 128], idx == 0, idx == nk - 1,
                             r=["cstb", kp], w=[pok])
                    p.op("dve", lambda e: e.reciprocal(rs_[:], po[:, 128:256]), r=[pok], w=[kr])
                    p.tt("dve", aob[b][:], po[:, 0:128], rs_[:], ALU.mult, r=[pok, kr], w=[f"a_ob{b}"])
                    p.dma(S["mixT"][512 + hd * 128:512 + (hd + 1) * 128, t * 128:(t + 1) * 128], aob[b][:], r=[f"a_ob{b}"], w=["mixT"])
            p.barrier()
        if "mix" in dbg:
            return finish(["mixT"])

        with ExitStack() as st:
            wo = sb("o_w", [128, KD, D], BF16, stack=st)
            gbc = sb("o_g", [128, 2, D], stack=st)
            mx_ = [sb(f"o_m{i}", [128, KD, 128], BF16, stack=st) for i in range(2)]
            xt = [sb(f"o_x{i}", [128, D], stack=st) for i in range(2)]
            tq = sb("o_t", [128, 512], stack=st)
            for k0 in range(0, KD, 4):
                p.dma(wo[:, k0:k0 + 4, :], I["w_out"][l].rearrange("(k p) c -> p k c", p=128)[:, k0:k0 + 4, :], w=["o_w"], q="pool")
            for s_ in range(2):
                p.dma(gbc[:, s_, :], S["modsd"][s_:s_ + 1, 2 * D:3 * D].partition_broadcast(128), r=["modsd"], w=["o_g"])
            for t in range(NTL):
                b = t % 2
                s_ = stream_of(t)
                p.dma(mx_[b][:], S["mixT"].rearrange("(k p) n -> p k n", p=128)[:, :, t * 128:(t + 1) * 128], r=["mixT"], w=[f"o_m{b}"])
                p.dma(xt[b][:], S["xcur"][t * 128:(t + 1) * 128, :], r=["xcur"], w=[f"o_x{b}"])
                for cb in range(4):
                    ps, pk = psf[cb], f"psf{cb}"
                    for k in range(KD):
                        p.mm(ps[:, :], mx_[b][:, k, :], wo[:, k, cb * 512:(cb + 1) * 512], k == 0, k == KD - 1, r=[f"o_m{b}", "o_w"], w=[pk])
                    p.tt("dve", tq[:], ps[:, :], gbc[:, s_, cb * 512:(cb + 1) * 512], ALU.mult, r=[pk, "o_g"], w=["o_t"])
                    p.tt("pool", xt[b][:, cb * 512:(cb + 1) * 512], xt[b][:, cb * 512:(cb + 1) * 512], tq[:], ALU.add,
                         r=["o_t", f"o_x{b}"], w=[f"o_x{b}"])
                p.dma(S["xcur"][t * 128:(t + 1) * 128, :], xt[b][:], r=[f"o_x{b}"], w=["xcur"])
            p.barrier()
        if "xmid" in dbg:
            return finish(["xcur"])

        with ExitStack() as st:
            affT = sb("r_affT", [NE, NT], stack=st)
            with ExitStack() as st2:
                wr_s = sb("r_w", [128, KD, NE], BF16, stack=st2)
                lg = sb("r_lg", [128, NE], stack=st2)
                rsm = sb("r_sm", [128, 4], stack=st2)
                p.dma(wr_s[:], I["w_r"][l].rearrange("(k p) e -> p k e", p=128), w=["r_w"], q="pool")

                def router(t, hTt, hkey):
                    for k in range(KD):
                        p.mm(psf[4][:, 0:NE], hTt[:, k, :], wr_s[:, k, :], k == 0, k == KD - 1, r=[hkey, "r_w"], w=["psf4"])
                    p.op("dve", lambda e: e.reduce_max(out=rsm[:, 0:1], in_=psf[4][:, 0:NE], axis=AX.X), r=["psf4"], w=["r_mx"])
                    p.ts("dve", rsm[:, 1:2], rsm[:, 0:1], -1.0, None, ALU.mult, r=["r_mx"], w=["r_nmx"])
                    p.act(lg[:], psf[4][:, 0:NE], AF.Exp, r=["psf4", "r_nmx"], w=["r_lg", "r_sum"], bias=rsm[:, 1:2], accum=rsm[:, 2:3])
                    p.op("dve", lambda e: e.reciprocal(rsm[:, 3:4], rsm[:, 2:3]), r=["r_sum"], w=["r_rs"])
                    p.ts("dve", lg[:], lg[:], rsm[:, 3:4], None, ALU.mult, r=["r_lg", "r_rs"], w=["r_lg"])
                    p.mm(psf[5][0:NE, 0:128], lg[:], ident_f, True, True, r=["r_lg", "cst"], w=["psf5"])
                    p.copy("act", affT[:, t * 128:(t + 1) * 128], psf[5][0:NE, 0:128], r=["psf5"], w=["r_affT"])

                norm_stage(st2, 1, None, dst_xn=S["xn2"], router=router)
                p.barrier()
            with ExitStack() as st2:
                wk = sb("r_wk", [NE, cfg.seq], stack=st2)
                wts = sb("r_wts", [NE, cfg.cap_l], stack=st2)
                ixs = sb("r_ixs", [NE, cfg.cap_l], U32, stack=st2)
                for (c0, n, cap, doff, off) in [(cfg.ctx, cfg.seq, cfg.cap_l, 0, cfg.ctx), (0, cfg.ctx, cfg.cap_c, 1, 0)]:
                    p.copy("dve", wk[:, 0:n], affT[:, c0:c0 + n], r=["r_affT"], w=["r_wk"])
                    for r_ in range(cap // 8):
                        s8 = slice(r_ * 8, r_ * 8 + 8)
                        p.op("dve", lambda e: e.max(out=wts[:, s8], in_=wk[:, 0:n]), r=["r_wk"], w=["r_wts"])
                        p.op("dve", lambda e: e.max_index(out=ixs[:, s8], in_max=wts[:, s8], in_values=wk[:, 0:n]), r=["r_wk", "r_wts"], w=["r_ixs"])
                        p.op("dve", lambda e: e.match_replace(out=wk[:, 0:n], in_to_replace=wts[:, s8], in_values=wk[:, 0:n], imm_value=-1.0),
                             r=["r_wts", "r_wk"], w=["r_wk"])
                    if off:
                        p.ts("dve", ixs[:, 0:cap], ixs[:, 0:cap], float(off), None, ALU.add, r=["r_ixs"], w=["r_ixs"])
                    dI = S["idxd"] if doff == 0 else S["idxc"]
                    dW = S["wtd"] if doff == 0 else S["wtc"]
                    p.dma(dI[:, 0:cap], ixs[:, 0:cap], r=["r_ixs"], w=["idxd"])
                    p.dma(dW[:, 0:cap], wts[:, 0:cap], r=["r_wts"], w=["idxd"])
                p.barrier()
        if "route" in dbg:
            return finish(["idxd", "wtd", "idxc", "wtc"])

        with ExitStack() as st:
            GS = min(1024, cfg.cap_l)
            xsT = sb("e_xsT", [128, KD, GS], BF16, stack=st)
            hid = sb("e_hid", [128, KD, GS], BF16, stack=st)
            wd = sb("e_wd", [128, KD, D], BF16, stack=st)
            wgf = [sb(f"e_wg{i}", [128, KD, 128], BF16, stack=st) for i in range(2)]
            wuf = [sb(f"e_wu{i}", [128, KD, 128], BF16, stack=st) for i in range(2)]
            xs = sb("e_xs", [128, D], BF16, stack=st)
            xr = sb("e_xr", [128, D], stack=st)
            yt = sb("e_yt", [128, 512], stack=st)
            sg2 = [sb(f"e_sg{i}", [128, 512], stack=st) for i in range(2)]
            eit = [0]
            icol = sb("e_ic", [128, 1], U32, stack=st)
            wcol = sb("e_wc", [128, 1], stack=st)
            g2 = sb("e_g2", [128, 2, D], stack=st)
            for s_ in range(2):
                p.dma(g2[:, s_, :], S["modsd"][s_:s_ + 1, 5 * D:6 * D].partition_broadcast(128), r=["modsd"], w=["e_g2"])
            for e_ in range(NE):
                for k0 in range(0, KD, 4):
                    p.dma(wd[:, k0:k0 + 4, :], I["wed"][l, e_].rearrange("(k p) c -> p k c", p=128)[:, k0:k0 + 4, :], w=["e_wd"], q="pool")
                groups = [(0, g0, min(GS, cfg.cap_l - g0)) for g0 in range(0, cfg.cap_l, GS)] + [(1, 0, cfg.cap_c)]
                for (s_, g0, ns) in groups:
                    dI = S["idxd"] if s_ == 0 else S["idxc"]
                    dW = S["wtd"] if s_ == 0 else S["wtc"]
                    tiles = [(j0, min(128, ns - j0)) for j0 in range(0, ns, 128)]
                    for (j0, rows) in tiles:
                        p.dma(icol[0:rows, :], dI[e_, g0 + j0:g0 + j0 + rows].rearrange("(p o) -> p o", o=1), r=["idxd"], w=["e_ic"])
                        p.op("pool", lambda e: e.indirect_dma_start(out=xs[0:rows, :], out_offset=None, in_=S["xn2"][:, :],
                                                                      in_offset=bass.IndirectOffsetOnAxis(ap=icol[0:rows, 0:1], axis=0)),
                             r=["e_ic", "xn2"], w=["e_xs"], dma=True)
                        for k in range(KD):
                            pb = pst[k // 8]
                            p.tr(pb[:, (k % 8) * 128:(k % 8) * 128 + rows], xs[0:rows, k * 128:(k + 1) * 128], ident_b[0:rows, 0:rows],
                                 r=["e_xs", "cstb"], w=[f"pst{k // 8}"])
                        for k in range(KD):
                            pb = pst[k // 8]
                            src = pb[:, (k % 8) * 128:(k % 8) * 128 + rows]
                            if k % 2 == 0:
                                p.ts("dve", xsT[:, k, j0:j0 + rows], src, amul[:, 1, k, s_:s_ + 1], modp[:, 3, k, s_:s_ + 1],
                                     ALU.mult, ALU.add, r=[f"pst{k // 8}"], w=["e_xsT"])
                            else:
                                p.act(xsT[:, k, j0:j0 + rows], src, AF.Identity, r=[f"pst{k // 8}"], w=["e_xsT"],
                                      scale=amul[:, 1, k, s_:s_ + 1], bias=modp[:, 3, k, s_:s_ + 1])
                    for f in range(KD):
                        b = f % 2
                        p.dma(wgf[b][:], I["weg"][l, e_].rearrange("(k p) c -> p k c", p=128)[:, :, f * 128:(f + 1) * 128], w=[f"e_wg{b}"], q="pool")
                        p.dma(wuf[b][:], I["weu"][l, e_].rearrange("(k p) c -> p k c", p=128)[:, :, f * 128:(f + 1) * 128], w=[f"e_wu{b}"], q="pool")
                        for s0 in range(0, ns, 512):
                            n_ = min(512, ns - s0)
                            eit[0] += 1
                            ia, ib = (0, 1) if eit[0] % 2 == 0 else (4, 5)
                            sgb, sgk = sg2[eit[0] % 2], f"e_sg{eit[0] % 2}"
                            for k in range(KD):
                                p.mm(psf[ia][:, 0:n_], wgf[b][:, k, :], xsT[:, k, s0:s0 + n_], k == 0, k == KD - 1, r=[f"e_wg{b}", "e_xsT"], w=[f"psf{ia}"])
                            for k in range(KD):
                                p.mm(psf[ib][:, 0:n_], wuf[b][:, k, :], xsT[:, k, s0:s0 + n_], k == 0, k == KD - 1, r=[f"e_wu{b}", "e_xsT"], w=[f"psf{ib}"])
                            p.act(sgb[:, 0:n_], psf[ia][:, 0:n_], AF.Silu, r=[f"psf{ia}"], w=[sgk])
                            p.tt("dve", hid[:, f, s0:s0 + n_], sgb[:, 0:n_], psf[ib][:, 0:n_], ALU.mult, r=[sgk, f"psf{ib}"], w=["e_hid"])
                    for (j0, rows) in tiles:
                        p.dma(icol[0:rows, :], dI[e_, g0 + j0:g0 + j0 + rows].rearrange("(p o) -> p o", o=1), r=["idxd"], w=["e_ic"])
                        p.dma(wcol[0:rows, :], dW[e_, g0 + j0:g0 + j0 + rows].rearrange("(p o) -> p o", o=1), r=["idxd"], w=["e_wc"])
                        p.op("pool", lambda e: e.indirect_dma_start(out=xr[0:rows, :], out_offset=None, in_=S["xcur"][:, :],
                                                                      in_offset=bass.IndirectOffsetOnAxis(ap=icol[0:rows, 0:1], axis=0)),
                             r=["e_ic", "xcur"], w=["e_xr"], dma=True)
                        for cb in range(4):
                            ps, pk = psf[2 + cb % 2], f"psf{2 + cb % 2}"
                            for f in range(KD):
                                p.mm(ps[0:rows, :], hid[:, f, j0:j0 + rows], wd[:, f, cb * 512:(cb + 1) * 512], f == 0, f == KD - 1,
                                     r=["e_hid", "e_wd"], w=[pk])
                            p.stt(yt[0:rows, :], ps[0:rows, :], wcol[0:rows, 0:1], g2[0:rows, s_, cb * 512:(cb + 1) * 512], ALU.mult, ALU.mult,
                                  r=[pk, "e_wc", "e_g2"], w=["e_yt"])
                            p.tt("pool", xr[0:rows, cb * 512:(cb + 1) * 512], xr[0:rows, cb * 512:(cb + 1) * 512], yt[0:rows, :], ALU.add,
                                 r=["e_yt", "e_xr"], w=["e_xr"])
                        p.op("pool", lambda e: e.indirect_dma_start(out=S["xcur"][:, :],
                                                                      out_offset=bass.IndirectOffsetOnAxis(ap=icol[0:rows, 0:1], axis=0),
                                                                      in_=xr[0:rows, :], in_offset=None),
                             r=["e_ic", "e_xr"], w=["xcur"], dma=True)
            p.barrier()
        if "layer" in dbg:
            return finish(["xcur"])

    big_copy(yout, S["xcur"][cfg.ctx:, :], cfg.seq, key="yout")
    p.barrier()
    es.close()
    return nc


def prep_inputs(cfg, inp):
    L = cfg.depth
    f = lambda a: np.ascontiguousarray(np.asarray(a, dtype=np.float32))
    pk = lambda v: f(np.asarray(v).reshape(-1, 128).T)
    m = {}
    m["xin"] = f(np.concatenate([inp["ctx"][0], inp["x"][0]], 0))
    m["cT"] = f(np.stack([pk(inp["c"][0]), pk(inp["c_ctx"])], -1))
    m["w_mod"] = f(inp["w_mod"])
    m["b_mod"] = f(inp["b_mod"]).reshape(L, 1, -1)
    m["n1g"] = f(np.stack([pk(inp["norm1_g"][l]) for l in range(L)]))
    m["n2g"] = f(np.stack([pk(inp["norm2_g"][l]) for l in range(L)]))
    m["w_in"] = f(inp["w_in"])
    m["wg"] = f(inp["gla_w_gate"])
    m["bg"] = f(np.stack([np.concatenate([pk(inp["gla_b_gate"][l][d]) for d in range(2)], 1) for l in range(L)]))
    m["gng"] = f(inp["gla_norm_g"]).reshape(L, 1, 128)
    m["nqg"] = f(inp["na_q_norm_g"]).reshape(L, 128, 1)
    m["nkg"] = f(inp["na_k_norm_g"]).reshape(L, 128, 1)
    bts = []
    for l in range(L):
        bt, _, _, _ = _na_masks(cfg, np.asarray(inp["na_rpb"][l], np.float32))
        bts.append(bt.transpose(0, 1, 3, 2, 4))
    m["bt"] = f(np.stack(bts))
    m["cw"] = f(np.stack([np.asarray(inp["lru_conv_w"][l]).reshape(4, 4, 128).transpose(2, 1, 0) for l in range(L)]))
    m["cb"] = f(np.stack([pk(inp["lru_conv_b"][l]) for l in range(L)]))
    m["wa"] = f(inp["lru_w_a"])
    m["wi"] = f(inp["lru_w_i"])
    cat2 = lambda a, l: np.concatenate([pk(a[l][d]) for d in range(2)], 1)
    m["ba"] = f(np.stack([cat2(inp["lru_b_a"], l) for l in range(L)]))
    m["bi"] = f(np.stack([cat2(inp["lru_b_i"], l) for l in range(L)]))
    m["lam"] = f(np.stack([cat2(inp["lru_lambda"], l) for l in range(L)]))
    m["w_out"] = f(inp["w_out"])
    m["w_r"] = f(inp["w_router"])
    m["weg"] = f(inp["w_exp_gate"])
    m["weu"] = f(inp["w_exp_up"])
    m["wed"] = f(inp["w_exp_down"])
    m["rope"] = _rope_tables(cfg)
    tri = np.tril(np.ones((128, 128), np.float32))
    m["consts"] = f(np.stack([np.eye(128), np.ones((128, 128)), tri.T, tri, np.zeros((128, 128)), np.zeros((128, 128))]))
    return m


def kernel(**inputs):
    cfg = Cfg()
    nc = build_program(cfg)
    m = prep_inputs(cfg, inputs)
    res = run_bass_kernel_spmd(nc, [m], core_ids=[0])
    return np.asarray(res.results[0]["yout"], dtype=np.float32).reshape(1, cfg.seq, D)
```

```python
import numpy as np
import concourse.bass as bass
import concourse.mybir as mybir
from concourse.bass_utils import run_bass_kernel_spmd

F32 = mybir.dt.float32
BF16 = mybir.dt.bfloat16
U32 = mybir.dt.uint32
AF = mybir.ActivationFunctionType
ALU = mybir.AluOpType
AX = mybir.AxisListType

D = 2048
KD = D // 128
EPS = 1e-6
GRID_W = 64
D_IN = 5664
C_Q, C_K, C_V, C_R, C_LF, C_LB, C_NQ, C_NK, C_NV, C_LX, C_LY = 0, 256, 512, 1024, 1536, 1552, 1568, 2592, 3616, 4640, 5152


class Cfg:
    def __init__(self, seq=16384, ctx=256, depth=2, ne=16):
        self.seq, self.ctx, self.depth, self.ne = seq, ctx, depth, ne
        self.nt = seq + ctx
        self.ntl = self.nt // 128
        self.rows = seq // GRID_W
        self.cap_l = 2 * seq // ne
        self.cap_c = 2 * ctx // ne


class P:
    def __init__(self, nc):
        self.nc = nc
        self.eng = {"pe": nc.tensor, "dve": nc.vector, "act": nc.scalar, "pool": nc.gpsimd, "sp": nc.sync}
        self.sem = {}
        self.cnt = {e: 0 for e in self.eng}
        self.waited = {e: {} for e in self.eng}
        self.res = {}
        self.dq = {e: 0 for e in self.eng}
        self.NDS = 8
        for e in self.eng:
            self.sem["c_" + e] = nc.semaphore("c_" + e).__enter__()
            for i in range(self.NDS):
                self.sem[f"d_{e}_{i}"] = nc.semaphore(f"d_{e}_{i}").__enter__()

    def _need(self, eng, tok, waits):
        if tok is None:
            return
        name, val = tok
        if eng == "pe" and name == "c_pe":
            return
        if self.waited[eng].get(name, 0) >= val:
            return
        self.waited[eng][name] = val
        waits.append(tok)

    def op(self, eng, fn, r=(), w=(), dma=False, sig=True):
        waits = []
        for k in r:
            st = self.res.get(k)
            if st:
                self._need(eng, st[0], waits)
        for k in w:
            st = self.res.get(k)
            if st:
                self._need(eng, st[0], waits)
                for t in st[1].items():
                    self._need(eng, t, waits)
        if dma:
            j = self.dq[eng]
            self.dq[eng] += 1
            name = f"d_{eng}_{j % self.NDS}"
            val = 16 * (j // self.NDS + 1)
            if j >= self.NDS:
                self._need(eng, (name, val - 16), waits)
            inc = 16
        elif not sig:
            name, val, inc = "c_" + eng, self.cnt[eng] + 1, 0
        else:
            self.cnt[eng] += 1
            name, val, inc = "c_" + eng, self.cnt[eng], 1
        e = self.eng[eng]
        for (n, v) in waits:
            e.wait_ge(self.sem[n], v)
        ins = fn(e)
        if inc:
            ins.then_inc(self.sem[name], inc)
        tok = (name, val)
        for k in r:
            st = self.res.setdefault(k, [None, {}])
            st[1][name] = max(st[1].get(name, 0), val)
        for k in w:
            self.res[k] = [tok, {}]
        return tok

    def barrier(self):
        toks = []
        for e in self.eng:
            if self.cnt[e]:
                toks.append(("c_" + e, self.cnt[e]))
            j = self.dq[e]
            for i in range(self.NDS):
                n = (j - 1 - i)
                if n >= 0:
                    toks.append((f"d_{e}_{n % self.NDS}", 16 * (n // self.NDS + 1)))
        for e in self.eng:
            ws = []
            for t in toks:
                self._need(e, t, ws)
            for (n, v) in ws:
                self.eng[e].wait_ge(self.sem[n], v)

    def dma(self, out, in_, r=(), w=(), q="sp", **kw):
        return self.op(q, lambda e: e.dma_start(out=out, in_=in_, **kw), r, w, dma=True)

    def mm(self, out, lhsT, rhs, start, stop, r=(), w=()):
        return self.op("pe", lambda e: e.matmul(out, lhsT, rhs, start=start, stop=stop), r, w, sig=bool(stop))

    def tr(self, out, in_, ident, r=(), w=()):
        return self.op("pe", lambda e: e.transpose(out, in_, ident), r, w)

    def act(self, out, in_, func, r=(), w=(), bias=0.0, scale=1.0, accum=None):
        if accum is None:
            return self.op("act", lambda e: e.activation(out=out, in_=in_, func=func, bias=bias, scale=scale), r, w)
        return self.op("act", lambda e: e.activation(out=out, in_=in_, func=func, bias=bias, scale=scale,
                                                       accum_out=accum), r, w)

    def ts(self, eng, out, in0, s1, s2, op0, op1=None, r=(), w=(), accum=None):
        if op1 is None:
            return self.op(eng, lambda e: e.tensor_scalar(out, in0, s1, None, op0), r, w)
        if accum is not None:
            return self.op(eng, lambda e: e.tensor_scalar(out, in0, s1, s2, op0, op1, accum), r, w)
        return self.op(eng, lambda e: e.tensor_scalar(out, in0, s1, s2, op0, op1), r, w)

    def tt(self, eng, out, in0, in1, op, r=(), w=()):
        return self.op(eng, lambda e: e.tensor_tensor(out, in0, in1, op), r, w)

    def stt(self, out, in0, scalar, in1, op0, op1, r=(), w=()):
        return self.op("dve", lambda e: e.scalar_tensor_tensor(out, in0, scalar, in1, op0, op1), r, w)

    def copy(self, eng, out, in_, r=(), w=()):
        if eng == "act":
            return self.op("act", lambda e: e.copy(out, in_), r, w)
        return self.op(eng, lambda e: e.tensor_copy(out, in_), r, w)


def _na_tile_geom(cfg):
    rows = cfg.rows
    rs = lambda r: min(max(r - 4, 0), rows - 8)
    plo, nch, cls, defs = [], [], [], {}
    for i in range(rows // 2):
        a, b = rs(2 * i), rs(2 * i + 1)
        lo, hi = a // 2, (b + 7) // 2
        key = (a - 2 * i, b - 2 * i - 1, lo - i, hi - lo + 1)
        if key not in defs:
            defs[key] = len(defs)
        plo.append(lo)
        nch.append(hi - lo + 1)
        cls.append(defs[key])
    return plo, nch, cls, defs


def _na_masks(cfg, rpb):
    plo, nch, cls, defs = _na_tile_geom(cfg)
    ncls = len(defs)
    out = np.full((ncls, 8, 5, 128, 128), -30000.0, np.float32)
    kk = np.arange(128)
    kdr, kc = kk // 64, kk % 64
    qdr, qc = kk // 64, kk % 64
    cs = np.clip(qc - 8, 0, GRID_W - 16)
    for key, ci in defs.items():
        a_rel, b_rel, lo_rel, n = key
        for c in range(n):
            krow = 2 * (lo_rel + c) + kdr
            rs_q = np.where(qdr == 0, a_rel, b_rel + 1)
            qrow = qdr
            inr = (krow[:, None] >= rs_q[None, :]) & (krow[:, None] <= rs_q[None, :] + 7)
            inc = (kc[:, None] >= cs[None, :]) & (kc[:, None] <= cs[None, :] + 15)
            ri = np.clip(krow[:, None] - qrow[None, :] + 7, 0, 14)
            cix = np.clip(kc[:, None] - qc[None, :] + 15, 0, 30)
            m = inr & inc
            for h in range(8):
                out[ci, h, c] = np.where(m, rpb[h][ri, cix], np.float32(-30000.0))
    return out, plo, nch, cls


def _rope_tables(cfg):
    pos = np.arange(cfg.seq)
    row = (pos // GRID_W).astype(np.float32)
    col = (pos % GRID_W).astype(np.float32)
    nf = 16
    inv = (np.float32(10000.0) ** (-np.arange(nf, dtype=np.float32) / np.float32(nf))).astype(np.float32)
    ang = np.concatenate([row[:, None] * inv, col[:, None] * inv], axis=-1).astype(np.float32)
    cos, sin = np.cos(ang).astype(np.float32), np.sin(ang).astype(np.float32)
    c_full = np.concatenate([np.ones((cfg.ctx, 32), np.float32), cos], 0)
    s_full = np.concatenate([np.zeros((cfg.ctx, 32), np.float32), sin], 0)
    p = np.arange(128)
    f = p % 32
    sign = np.where((p % 64) < 32, -1.0, 1.0).astype(np.float32)
    ck = c_full[:, f].T.copy()
    sk = (s_full[:, f].T * sign[:, None]).copy()
    sc = np.float32(64 ** -0.5)
    return np.stack([ck * sc, sk * sc, ck, sk]).astype(np.float32)


def build_program(cfg, dbg=()):
    nc = bass.Bass("TRN2", target_bir_lowering=False)
    NT, NTL, L, NE = cfg.nt, cfg.ntl, cfg.depth, cfg.ne
    plo_l, nch_l, cls_l, cdefs = _na_tile_geom(cfg)
    NCLS = len(cdefs)

    def din(name, shape, dt=F32):
        return nc.dram_tensor(name, list(shape), dt, kind="ExternalInput").ap()

    def dscr(name, shape, dt=F32):
        return nc.dram_tensor(name, list(shape), dt, kind="Internal").ap()

    I = dict(
        xin=din("xin", [NT, D]), cT=din("cT", [128, KD, 2]),
        w_mod=din("w_mod", [L, D, 6 * D]), b_mod=din("b_mod", [L, 1, 6 * D]),
        n1g=din("n1g", [L, 128, KD]), n2g=din("n2g", [L, 128, KD]),
        w_in=din("w_in", [L, D, D_IN]), wg=din("wg", [L, 2, 16, 256]), bg=din("bg", [L, 128, 4]),
        gng=din("gng", [L, 1, 128]), nqg=din("nqg", [L, 128, 1]), nkg=din("nkg", [L, 128, 1]),
        bt=din("bt", [L, NCLS, 8, 128, 5, 128]), cw=din("cw", [L, 128, 4, 4]), cb=din("cb", [L, 128, 4]),
        wa=din("wa", [L, 2, 4, 128, 128]), ba=din("ba", [L, 128, 8]), wi=din("wi", [L, 2, 4, 128, 128]),
        bi=din("bi", [L, 128, 8]), lam=din("lam", [L, 128, 8]),
        w_out=din("w_out", [L, D, D]), w_r=din("w_r", [L, D, NE]),
        weg=din("weg", [L, NE, D, D]), weu=din("weu", [L, NE, D, D]), wed=din("wed", [L, NE, D, D]),
        rope=din("rope", [4, 128, NT]), consts=din("consts", [6, 128, 128]),
    )
    yout = nc.dram_tensor("yout", [cfg.seq, D], F32, kind="ExternalOutput").ap()
    dbg_out = {}
    S = dict(
        xcur=dscr("xcur", [NT, D]), modsd=dscr("modsd", [2, 6 * D]), hT=dscr("hTd", [D, NT], BF16),
        gqT=dscr("gqT", [256, NT], BF16), gkT=dscr("gkT", [256, NT], BF16), gv=dscr("gv", [NT, 512], BF16),
        gr=dscr("gr", [NT, 512]), gg=dscr("gg", [2, 256, NT]), go=dscr("go", [NT, 512]),
        nqT=dscr("nqT", [1024, NT], BF16), nkT=dscr("nkT", [1024, NT], BF16), nv=dscr("nv", [NT, 1024], BF16),
        lxT=dscr("lxT", [512, NT]), lyT=dscr("lyT", [512, NT]), hf=dscr("hf", [512, NT]),
        mixT=dscr("mixT", [D, NT], BF16), xn2=dscr("xn2", [NT, D], BF16),
        idxd=dscr("idxd", [NE, cfg.cap_l], U32), wtd=dscr("wtd", [NE, cfg.cap_l]),
        idxc=dscr("idxc", [NE, cfg.cap_c], U32), wtc=dscr("wtc", [NE, cfg.cap_c]),
    )
    p = P(nc)
    from contextlib import ExitStack
    es = ExitStack()

    uid = [0]

    def sb(name, shape, dt=F32, stack=None):
        uid[0] += 1
        return (stack or es).enter_context(nc.sbuf_tensor(f"{name}_u{uid[0]}", list(shape), dt))

    psf = [es.enter_context(nc.psum_tensor(f"psf{i}", [128, 512], F32)) for i in range(6)]
    pst = [es.enter_context(nc.psum_tensor(f"pst{i}", [128, 1024], BF16)) for i in range(2)]
    cst = sb("cst", [128, 6, 128])
    cstb = sb("cstb", [128, 2, 128], BF16)
    p.dma(cst[:], I["consts"].rearrange("c p n -> p c n"), w=["cst"])
    p.copy("dve", cstb[:], cst[:, 0:2, :], r=["cst"], w=["cstb"])
    ident_f, ones_f, maskF, maskB = cst[:, 0, :], cst[:, 1, :], cst[:, 2, :], cst[:, 3, :]
    ident_b, ones_b = cstb[:, 0, :], cstb[:, 1, :]
    modp = sb("modp", [128, 6, KD, 2])
    amul = sb("amul", [128, 2, KD, 2])
    def big_copy(dst, src, rows, step=128, key="bigcopy"):
        for r0 in range(0, rows, step):
            r1 = min(rows, r0 + step)
            p.dma(dst[r0:r1], src[r0:r1], w=[key])

    def finish(names):
        for nm in names:
            o = nc.dram_tensor("dbg_" + nm, list(S[nm].shape), S[nm].dtype, kind="ExternalOutput").ap()
            big_copy(o, S[nm], S[nm].shape[0], key="dbg")
        p.barrier()
        return nc

    big_copy(S["xcur"], I["xin"], NT, key="xcur")
    p.barrier()

    def stream_of(tile):
        return 1 if tile < cfg.ctx // 128 else 0

    supers = [(0, cfg.ctx, 1)] + [(cfg.ctx + i * 512, 512, 0) for i in range(cfg.seq // 512)]

    for l in range(L):
        with ExitStack() as st:
            sc = sb("m_sc", [128, KD, 2], stack=st)
            wm = sb("m_w", [128, KD, 512], stack=st)
            brow = sb("m_b", [2, 512], stack=st)
            mrow = sb("m_r", [2, 512], stack=st)
            p.dma(sc[:], I["cT"], w=["m_sc"])
            p.act(sc[:], sc[:], AF.Silu, r=["m_sc"], w=["m_sc"])
            for cb in range(24):
                p.dma(wm[:], I["w_mod"][l].rearrange("(k p) c -> p k c", p=128)[:, :, cb * 512:(cb + 1) * 512],
                      w=["m_w"])
                p.dma(brow[:], I["b_mod"][l][:, cb * 512:(cb + 1) * 512].partition_broadcast(2), w=["m_b"])
                for k in range(KD):
                    p.mm(psf[0][0:2, :], sc[:, k, :], wm[:, k, :], k == 0, k == KD - 1, r=["m_sc", "m_w"], w=["psf0"])
                p.tt("dve", mrow[:], psf[0][0:2, :], brow[:], ALU.add, r=["psf0", "m_b"], w=["m_r"])
                p.dma(S["modsd"][:, cb * 512:(cb + 1) * 512], mrow[:], r=["m_r"], w=["modsd"])
            p.barrier()
            for j in range(6):
                for s_ in range(2):
                    p.dma(modp[:, j, :, s_], S["modsd"][s_, j * D:(j + 1) * D].rearrange("(k p) -> p k", p=128),
                          r=["modsd"], w=["modp"], allow_slow_non_contiguous=True)
            g1 = sb("m_g1", [128, KD], stack=st)
            g2 = sb("m_g2", [128, KD], stack=st)
            p.dma(g1[:], I["n1g"][l], w=["m_g1"])
            p.dma(g2[:], I["n2g"][l], w=["m_g2"])
            for s_ in range(2):
                p.stt(amul[:, 0, :, s_], modp[:, 1, :, s_], 1.0, g1[:], ALU.add, ALU.mult, r=["modp", "m_g1"], w=["amul"])
                p.stt(amul[:, 1, :, s_], modp[:, 4, :, s_], 1.0, g2[:], ALU.add, ALU.mult, r=["modp", "m_g2"], w=["amul"])
            p.barrier()

        if "mods" in dbg:
            return finish(["modsd"])

        def norm_stage(st, which, dst_hT, dst_xn=None, router=None):
            xt = [sb(f"n_x{i}", [128, D], stack=st) for i in range(2)]
            junk = sb("n_junk", [128, D], BF16, stack=st)
            xn = [sb(f"n_xn{i}", [128, D], BF16, stack=st) for i in range(2)]
            hTt = [sb(f"n_hT{i}", [128, KD, 128], BF16, stack=st) for i in range(2)]
            stat = sb("n_stat", [128, 4], stack=st)
            for t in range(NTL):
                b = t % 2
                s_ = stream_of(t)
                p.dma(xt[b][:], S["xcur"][t * 128:(t + 1) * 128, :], r=["xcur"], w=[f"n_x{b}"])
                p.act(junk[:], xt[b][:], AF.Square, r=[f"n_x{b}"], w=["n_junk", "n_ss"], accum=stat[:, 0:1])
                p.act(stat[:, 1:2], stat[:, 0:1], AF.Sqrt, r=["n_ss"], w=["n_sd"], scale=1.0 / D, bias=EPS)
                p.op("dve", lambda e: e.reciprocal(stat[:, 2:3], stat[:, 1:2]), r=["n_sd"], w=["n_rs"])
                p.ts("dve", xn[b][:], xt[b][:], stat[:, 2:3], None, ALU.mult, r=[f"n_x{b}", "n_rs"], w=[f"n_xn{b}"])
                if dst_xn is not None:
                    p.dma(dst_xn[t * 128:(t + 1) * 128, :], xn[b][:], r=[f"n_xn{b}"], w=["xn2"])
                for k in range(KD):
                    pb = pst[k // 8]
                    p.tr(pb[:, (k % 8) * 128:(k % 8 + 1) * 128], xn[b][:, k * 128:(k + 1) * 128], ident_b,
                         r=[f"n_xn{b}", "cstb"], w=[f"pst{k // 8}"])
                for k in range(KD):
                    pb = pst[k // 8]
                    src = pb[:, (k % 8) * 128:(k % 8 + 1) * 128]
                    if k % 2 == 0:
                        p.ts("dve", hTt[b][:, k, :], src, amul[:, which, k, s_:s_ + 1], modp[:, 3 * which, k, s_:s_ + 1],
                             ALU.mult, ALU.add, r=[f"pst{k // 8}", "amul", "modp"], w=[f"n_hT{b}"])
                    else:
                        p.act(hTt[b][:, k, :], src, AF.Identity, r=[f"pst{k // 8}", "amul", "modp"], w=[f"n_hT{b}"],
                              scale=amul[:, which, k, s_:s_ + 1], bias=modp[:, 3 * which, k, s_:s_ + 1])
                if dst_hT is not None:
                    p.dma(dst_hT.rearrange("(k p) n -> p k n", p=128)[:, :, t * 128:(t + 1) * 128], hTt[b][:],
                          r=[f"n_hT{b}"], w=["hTd"])
                if router is not None:
                    router(t, hTt[b], f"n_hT{b}")

        with ExitStack() as st:
            norm_stage(st, 0, S["hT"])
            p.barrier()
        if "norm" in dbg:
            return finish(["hT"])

        with ExitStack() as st:
            wb = sb("i_w", [128, KD, 1024], BF16, stack=st)
            wsw = sb("i_wsw", [128, KD, 512], BF16, stack=st)
            hs = [sb(f"i_h{i}", [128, KD, 512], BF16, stack=st) for i in range(2)]
            rp = sb("i_rope", [128, 4, 512], stack=st)
            ev = [sb(f"i_ev{i}", [128, 512], stack=st) for i in range(4)]
            evb = [sb(f"i_evb{i}", [128, 512], BF16, stack=st) for i in range(2)]
            lrt = sb("i_lr", [16, 2, 512], BF16, stack=st)
            wgs = sb("i_wg", [16, 2, 256], BF16, stack=st)
            bgs = sb("i_bg", [128, 4], stack=st)
            nbg = sb("i_nbg", [128, 4], stack=st)
            nqk = sb("i_nqk", [128, 2], stack=st)
            p.dma(wgs[:], I["wg"][l].rearrange("d r c -> r d c"), w=["i_wg"], q="pool")
            p.dma(bgs[:], I["bg"][l], w=["i_bg"])
            p.ts("dve", nbg[:], bgs[:], -1.0, None, ALU.mult, r=["i_bg"], w=["i_nbg"])
            p.dma(nqk[:, 0:1], I["nqg"][l], w=["i_nqk"])
            p.dma(nqk[:, 1:2], I["nkg"][l], w=["i_nqk"])
            p.ts("dve", nqk[:, 0:1], nqk[:, 0:1], float(128 ** -0.5), None, ALU.mult, r=["i_nqk"], w=["i_nqk"])
            wsrc = I["w_in"][l].rearrange("(k p) c -> p k c", p=128)

            def load_w(c0, n):
                for k0 in range(0, KD, 4):
                    p.dma(wb[:, k0:k0 + 4, 0:n], wsrc[:, k0:k0 + 4, c0:c0 + n], w=["i_w"], q="pool")

            def load_h(si):
                t0, ntok, s_ = supers[si]
                b = si % 2
                p.dma(hs[b][:, :, 0:ntok], S["hT"].rearrange("(k p) n -> p k n", p=128)[:, :, t0:t0 + ntok],
                      r=["hTd"], w=[f"i_h{b}"])
                return hs[b], f"i_h{b}", t0, ntok

            def fm_mm(ps, pskey, wtile, wkey, c0, m, h, hkey, ntok):
                for k in range(KD):
                    p.mm(ps[0:m, 0:ntok], wtile[:, k, c0:c0 + m], h[:, k, 0:ntok], k == 0, k == KD - 1,
                         r=[wkey, hkey], w=[pskey])

            load_w(C_Q, 512)
            p.dma(wb[:, :, 512:544], wsrc[:, :, C_LF:C_LF + 32], w=["i_w"], q="pool")
            w5 = wb[:, :, 0:512].rearrange("p k (h two f) -> p k h two f", two=2, f=32)
            s5 = wsw[:].rearrange("p k (h two f) -> p k h two f", two=2, f=32)
            p.copy("dve", s5[:, :, :, 0, :], w5[:, :, :, 1, :], r=["i_w"], w=["i_wsw"])
            p.copy("pool", s5[:, :, :, 1, :], w5[:, :, :, 0, :], r=["i_w"], w=["i_wsw"])
            for si in range(len(supers)):
                h, hkey, t0, ntok = load_h(si)
                p.dma(rp[:, :, 0:ntok], I["rope"].rearrange("c p n -> p c n")[:, :, t0:t0 + ntok], w=["i_rope"])
                for qk in range(2):
                    dst = S["gqT"] if qk == 0 else S["gkT"]
                    for hp in range(2):
                        c0 = qk * 256 + hp * 128
                        par = hp
                        pa_, pb_ = (0, 1) if par == 0 else (4, 5)
                        ea, eb, ec = 2 * par, 2 * par + 1, par
                        fm_mm(psf[pa_], f"psf{pa_}", wb, "i_w", c0, 128, h, hkey, ntok)
                        fm_mm(psf[pb_], f"psf{pb_}", wsw, "i_wsw", c0, 128, h, hkey, ntok)
                        p.tt("dve", ev[ea][:, 0:ntok], psf[pa_][:, 0:ntok], rp[:, 2 * qk, 0:ntok], ALU.mult,
                             r=[f"psf{pa_}", "i_rope"], w=[f"i_ev{ea}"])
                        p.tt("dve", ev[eb][:, 0:ntok], psf[pb_][:, 0:ntok], rp[:, 2 * qk + 1, 0:ntok], ALU.mult,
                             r=[f"psf{pb_}", "i_rope"], w=[f"i_ev{eb}"])
                        p.tt("pool", evb[ec][:, 0:ntok], ev[ea][:, 0:ntok], ev[eb][:, 0:ntok], ALU.add,
                             r=[f"i_ev{ea}", f"i_ev{eb}"], w=[f"i_evb{ec}"])
                        p.dma(dst[hp * 128:(hp + 1) * 128, t0:t0 + ntok], evb[ec][:, 0:ntok], r=[f"i_evb{ec}"], w=["gqk"])
                for d_ in range(2):
                    fm_mm(psf[2], "psf2", wb, "i_w", 512 + 16 * d_, 16, h, hkey, ntok)
                    p.copy("act", lrt[:, d_, 0:ntok], psf[2][0:16, 0:ntok], r=["psf2"], w=["i_lr"])
                for d_ in range(2):
                    for hp in range(2):
                        p.mm(psf[3][:, 0:ntok], wgs[:, d_, hp * 128:(hp + 1) * 128], lrt[:, d_, 0:ntok], True, True,
                             r=["i_wg", "i_lr"], w=["psf3"])
                        p.act(ev[2][:, 0:ntok], psf[3][:, 0:ntok], AF.Exp, r=["psf3", "i_nbg"], w=["i_ev2"],
                              scale=-1.0, bias=nbg[:, d_ * 2 + hp:d_ * 2 + hp + 1])
                        p.act(ev[2][:, 0:ntok], ev[2][:, 0:ntok], AF.Ln, r=["i_ev2"], w=["i_ev2"], bias=1.0)
                        p.ts("dve", ev[3][:, 0:ntok], ev[2][:, 0:ntok], -1.0 / 16.0, None, ALU.mult, r=["i_ev2"], w=["i_ev3"])
                        p.dma(S["gg"][d_, hp * 128:(hp + 1) * 128, t0:t0 + ntok], ev[3][:, 0:ntok], r=["i_ev3"], w=["gg"])

            if "ip1" in dbg:
                return finish(["gqT", "gkT", "gg"])

            def tok_group(c0, ncols, dst, dst_dt_bf, silu):
                load_w(c0, ncols)
                for si in range(len(supers)):
                    h, hkey, t0, ntok = load_h(si)
                    for tt_ in range(ntok // 128):
                        for cb in range(ncols // 512):
                            ps, pk = psf[(tt_ * 2 + cb) % 4], f"psf{(tt_ * 2 + cb) % 4}"
                            for k in range(KD):
                                p.mm(ps[:, :], h[:, k, tt_ * 128:(tt_ + 1) * 128], wb[:, k, cb * 512:(cb + 1) * 512],
                                     k == 0, k == KD - 1, r=["i_w", hkey], w=[pk])
                            tok0 = t0 + tt_ * 128
                            if dst_dt_bf:
                                e_, ek = evb[cb % 2], f"i_evb{cb % 2}"
                                p.copy("act" if cb % 2 else "dve", e_[:], ps[:, :], r=[pk], w=[ek])
                            else:
                                e_, ek = ev[cb % 2], f"i_ev{cb % 2}"
                                p.act(e_[:], ps[:, :], AF.Silu if silu else AF.Copy, r=[pk], w=[ek])
                            p.dma(dst[tok0:tok0 + 128, cb * 512:(cb + 1) * 512], e_[:], r=[ek], w=["tokdst"])

            tok_group(C_V, 512, S["gv"], True, False)
            tok_group(C_R, 512, S["gr"], False, True)
            tok_group(C_NV, 1024, S["nv"], True, False)

            if "ip2" in dbg:
                return finish(["gv", "gr", "nv"])
            for qk in range(2):
                load_w(C_NQ if qk == 0 else C_NK, 1024)
                dst = S["nqT"] if qk == 0 else S["nkT"]
                for si in range(len(supers)):
                    h, hkey, t0, ntok = load_h(si)
                    for hd in range(8):
                        pa_, pb_ = (0, 1) if hd % 2 == 0 else (2, 3)
                        fm_mm(psf[pa_], f"psf{pa_}", wb, "i_w", hd * 128, 128, h, hkey, ntok)
                        p.copy("dve", ev[0][:, 0:ntok], psf[pa_][:, 0:ntok], r=[f"psf{pa_}"], w=["i_ev0"])
                        p.tt("dve", evb[1][:, 0:ntok], ev[0][:, 0:ntok], ev[0][:, 0:ntok], ALU.mult, r=["i_ev0"], w=["i_evb1"])
                        p.mm(psf[pb_][:, 0:ntok], ones_b, evb[1][:, 0:ntok], True, True, r=["cstb", "i_evb1"], w=[f"psf{pb_}"])
                        p.act(ev[2][:, 0:ntok], psf[pb_][:, 0:ntok], AF.Sqrt, r=[f"psf{pb_}"], w=["i_ev2"], scale=1.0 / 128, bias=EPS)
                        p.op("dve", lambda e: e.reciprocal(ev[3][:, 0:ntok], ev[2][:, 0:ntok]), r=["i_ev2"], w=["i_ev3"])
                        p.stt(evb[0][:, 0:ntok], ev[0][:, 0:ntok], nqk[:, qk:qk + 1], ev[3][:, 0:ntok], ALU.mult, ALU.mult,
                              r=["i_ev0", "i_ev3", "i_nqk"], w=["i_evb0"])
                        p.dma(dst[hd * 128:(hd + 1) * 128, t0:t0 + ntok], evb[0][:, 0:ntok], r=["i_evb0"], w=["nqk"])

            if "ip3" in dbg:
                return finish(["nqT", "nkT"])
            load_w(C_LX, 1024)
            for si in range(len(supers)):
                h, hkey, t0, ntok = load_h(si)
                for c in range(8):
                    ps, pk = psf[c % 4], f"psf{c % 4}"
                    fm_mm(ps, pk, wb, "i_w", c * 128, 128, h, hkey, ntok)
                    e_, ek = ev[c % 4], f"i_ev{c % 4}"
                    p.copy("act" if c % 2 else "dve", e_[:, 0:ntok], ps[:, 0:ntok], r=[pk], w=[ek])
                    dst = S["lxT"] if c < 4 else S["lyT"]
                    p.dma(dst[(c % 4) * 128:(c % 4 + 1) * 128, t0:t0 + ntok], e_[:, 0:ntok], r=[ek], w=["lxy"])
            p.barrier()

        if "inproj" in dbg:
            return finish(["gqT", "gkT", "gg", "gv", "gr", "nqT", "nkT", "nv", "lxT", "lyT"])

        nct = cfg.ctx // 128
        with ExitStack() as st:
            BL = min(1024, cfg.seq)
            blocks = [(0, cfg.ctx)] + [(cfg.ctx + i * BL, BL) for i in range(cfg.seq // BL)]
            qb = sb("g_q", [128, BL], BF16, stack=st)
            kb = sb("g_k", [128, BL], BF16, stack=st)
            gb = sb("g_g", [128, BL], stack=st)
            vb = sb("g_v", [128, BL // 128, 256], BF16, stack=st)
            gc = sb("g_gc", [128, 128], stack=st)
            Hh = sb("g_H", [128, 128], stack=st)
            ex = sb("g_ex", [128, 3, 128], stack=st)
            qkb = sb("g_qkb", [128, 3, 128], BF16, stack=st)
            kdT = sb("g_kdT", [128, 128], BF16, stack=st)
            attm = [sb(f"g_att{i}", [128, 128], BF16, stack=st) for i in range(2)]
            Sst = sb("g_S", [128, 128], stack=st)
            Sbf = sb("g_Sb", [128, 128], BF16, stack=st)
            sm = sb("g_sm", [128, 8], stack=st)
            ob = sb("g_o", [128, 256], stack=st)
            of = sb("g_of", [128, 256], stack=st)
            rr = sb("g_r", [128, 256], stack=st)
            gn = sb("g_gn", [128, 128], stack=st)
            outb = sb("g_outb", [128, 256], BF16, stack=st)
            mT = sb("g_mT", [128, 256], BF16, stack=st)
            junk = sb("g_junk", [128, 128], stack=st)
            p.dma(gn[:], I["gng"][l].partition_broadcast(128), w=["g_gn"])
            for hp in range(2):
                for d_ in range(2):
                    p.op("dve", lambda e: e.memset(Sst[:], 0.0), w=["g_S"])
                    p.op("dve", lambda e: e.memset(Sbf[:], 0.0), w=["g_Sb"])
                    order = blocks if d_ == 0 else [blocks[0]] + blocks[:0:-1]
                    msk = maskF if d_ == 0 else maskB
                    for (t0, ntok) in order:
                        p.dma(qb[:, 0:ntok], S["gqT"][hp * 128:(hp + 1) * 128, t0:t0 + ntok], r=["gqk"], w=["g_q"])
                        p.dma(kb[:, 0:ntok], S["gkT"][hp * 128:(hp + 1) * 128, t0:t0 + ntok], r=["gqk"], w=["g_k"])
                        p.dma(gb[:, 0:ntok], S["gg"][d_, hp * 128:(hp + 1) * 128, t0:t0 + ntok], r=["gg"], w=["g_g"])
                        p.dma(vb[:, 0:ntok // 128, :],
                              S["gv"][t0:t0 + ntok, hp * 256:(hp + 1) * 256].rearrange("(c p) f -> p c f", p=128),
                              r=["tokdst"], w=["g_v"])
                        cs_ = list(range(ntok // 128))
                        if d_ == 1:
                            cs_ = cs_[::-1]
                        for c in cs_:
                            sl = slice(c * 128, (c + 1) * 128)
                            tok = t0 + c * 128
                            p.op("dve", lambda e: e.tensor_tensor_scan(out=gc[:], data0=ones_f, data1=gb[:, sl], initial=0.0,
                                                                         op0=ALU.mult, op1=ALU.add), r=["g_g", "cst"], w=["g_gc"])
                            gl = gc[:, 127:128]
                            p.act(sm[:, 2:3], gl, AF.Exp, r=["g_gc"], w=["g_dec"])
                            if d_ == 0:
                                p.act(ex[:, 0, :], gc[:], AF.Exp, r=["g_gc"], w=["g_ex"])
                                p.act(ex[:, 1, :], gc[:], AF.Exp, r=["g_gc"], w=["g_ex"], scale=-1.0)
                                p.act(ex[:, 2, :], gc[:], AF.Exp, r=["g_gc"], w=["g_ex"], scale=-1.0, bias=gl)
                            else:
                                p.tt("dve", Hh[:], gc[:], gb[:, sl], ALU.subtract, r=["g_gc", "g_g"], w=["g_H"])
                                p.ts("dve", sm[:, 1:2], gl, -1.0, None, ALU.mult, r=["g_gc"], w=["g_ngl"])
                                p.act(ex[:, 0, :], Hh[:], AF.Exp, r=["g_H", "g_gc"], w=["g_ex"], scale=-1.0, bias=gl)
                                p.act(ex[:, 1, :], Hh[:], AF.Exp, r=["g_H", "g_ngl"], w=["g_ex"], bias=sm[:, 1:2])
                                p.act(ex[:, 2, :], Hh[:], AF.Exp, r=["g_H"], w=["g_ex"])
                            p.tt("dve", qkb[:, 0, :], qb[:, sl], ex[:, 0, :], ALU.mult, r=["g_q", "g_ex"], w=["g_qe"])
                            p.tt("pool", qkb[:, 1, :], kb[:, sl], ex[:, 1, :], ALU.mult, r=["g_k", "g_ex"], w=["g_ke"])
                            p.tt("dve", qkb[:, 2, :], kb[:, sl], ex[:, 2, :], ALU.mult, r=["g_k", "g_ex"], w=["g_kd"])
                            p.tr(pst[0][:, 0:128], qkb[:, 2, :], ident_b, r=["g_kd", "cstb"], w=["pst0"])
                            p.copy("act", kdT[:], pst[0][:, 0:128], r=["pst0"], w=["g_kdT"])
                            for hh in range(2):
                                b0 = 64 * hh
                                p.mm(psf[0][:, hh * 128:(hh + 1) * 128], qkb[b0:b0 + 64, 1, :], qkb[b0:b0 + 64, 0, :], True, True,
                                     r=["g_ke", "g_qe"], w=["psf0"])
                                p.tt("dve", attm[hh][:], psf[0][:, hh * 128:(hh + 1) * 128], msk, ALU.mult,
                                     r=["psf0", "cst"], w=[f"g_att{hh}"])
                                p.mm(psf[1][:, hh * 128:(hh + 1) * 128], attm[hh][:], vb[:, c, hh * 128:(hh + 1) * 128], True, False,
                                     r=[f"g_att{hh}", "g_v"], w=["psf1"])
                                p.mm(psf[1][:, hh * 128:(hh + 1) * 128], qkb[b0:b0 + 64, 0, :], Sbf[b0:b0 + 64, :], False, True,
                                     r=["g_qe", "g_Sb"], w=["psf1"])
                            p.mm(psf[2][:, 0:256], kdT[:], vb[:, c, :], True, True, r=["g_kdT", "g_v"], w=["psf2"])
                            if d_ == 0:
                                p.copy("act", ob[:], psf[1][:, 0:256], r=["psf1"], w=["g_o"])
                                p.dma(S["go"][tok:tok + 128, hp * 256:(hp + 1) * 256], ob[:], r=["g_o"], w=["go"])
                            else:
                                p.dma(of[:], S["go"][tok:tok + 128, hp * 256:(hp + 1) * 256], r=["go"], w=["g_of"])
                                p.dma(rr[:], S["gr"][tok:tok + 128, hp * 256:(hp + 1) * 256], r=["tokdst"], w=["g_r"])
                                p.tt("dve", ob[:], psf[1][:, 0:256], of[:], ALU.add, r=["psf1", "g_of"], w=["g_o"])
                                for hh in range(2):
                                    oh = ob[:, hh * 128:(hh + 1) * 128]
                                    p.act(junk[:], oh, AF.Square, r=["g_o"], w=["g_junk", "g_ss"], accum=sm[:, 4 + hh:5 + hh])
                                p.act(sm[:, 6:8], sm[:, 4:6], AF.Sqrt, r=["g_ss"], w=["g_sd"], scale=1.0 / 128, bias=EPS)
                                p.op("dve", lambda e: e.reciprocal(sm[:, 4:6], sm[:, 6:8]), r=["g_sd"], w=["g_rs"])
                                for hh in range(2):
                                    oh = ob[:, hh * 128:(hh + 1) * 128]
                                    p.stt(oh, oh, sm[:, 4 + hh:5 + hh], gn[:], ALU.mult, ALU.mult, r=["g_o", "g_rs", "g_gn"], w=["g_o"])
                                p.tt("dve", outb[:], ob[:], rr[:], ALU.mult, r=["g_o", "g_r"], w=["g_outb"])
                                for hh in range(2):
                                    p.tr(pst[1][:, hh * 128:(hh + 1) * 128], outb[:, hh * 128:(hh + 1) * 128], ident_b,
                                         r=["g_outb", "cstb"], w=["pst1"])
                                p.copy("act", mT[:], pst[1][:, 0:256], r=["pst1"], w=["g_mT"])
                                for hh in range(2):
                                    f0 = (hp * 2 + hh) * 128
                                    p.dma(S["mixT"][f0:f0 + 128, tok:tok + 128], mT[:, hh * 128:(hh + 1) * 128], r=["g_mT"], w=["mixT"])
                            for hh in range(2):
                                b0 = 64 * hh
                                p.stt(Sst[b0:b0 + 64, :], Sst[b0:b0 + 64, :], sm[b0:b0 + 64, 2:3], psf[2][b0:b0 + 64, hh * 128:(hh + 1) * 128],
                                      ALU.mult, ALU.add, r=["g_S", "g_dec", "psf2"], w=["g_S"])
                            p.copy("act", Sbf[:], Sst[:], r=["g_S"], w=["g_Sb"])
            p.barrier()
        if "gla" in dbg:
            return finish(["mixT"])

        with ExitStack() as st:
            BL = min(2048, cfg.seq)
            blocks = [(0, cfg.ctx)] + [(cfg.ctx + i * BL, BL) for i in range(cfg.seq // BL)]
            xp = sb("l_xp", [128, BL + 3], stack=st)
            xc = sb("l_xc", [128, BL], stack=st)
            xcb = sb("l_xcb", [128, BL], BF16, stack=st)
            aa = sb("l_aa", [128, BL], stack=st)
            bb = sb("l_bb", [128, BL], stack=st)
            hh_ = sb("l_hh", [128, BL], stack=st)
            yb = sb("l_yb", [128, BL], stack=st)
            hfb = sb("l_hfb", [128, BL], stack=st)
            lob = sb("l_ob", [128, BL], BF16, stack=st)
            tmp = [sb(f"l_t{i}", [128, 512], stack=st) for i in range(4)]
            hprev = sb("l_hp", [128, 1], stack=st)
            cw_s = sb("l_cw", [128, 4, 4], stack=st)
            cb_s = sb("l_cb", [128, 4], stack=st)
            ba_s = sb("l_ba", [128, 8], stack=st)
            bi_s = sb("l_bi", [128, 8], stack=st)
            c8 = sb("l_c8", [128, 8], stack=st)
            wa_s = sb("l_wa", [128, 2, 4, 128], BF16, stack=st)
            wi_s = sb("l_wi", [128, 2, 4, 128], BF16, stack=st)
            p.dma(cw_s[:], I["cw"][l], w=["l_cw"])
            p.dma(cb_s[:], I["cb"][l], w=["l_cb"])
            p.dma(ba_s[:], I["ba"][l], w=["l_ba"])
            p.dma(bi_s[:], I["bi"][l], w=["l_bi"])
            p.dma(c8[:], I["lam"][l], w=["l_c8"])
            p.dma(wa_s[:], I["wa"][l].rearrange("d n c o -> c d n o"), w=["l_wa"], q="pool")
            p.dma(wi_s[:], I["wi"][l].rearrange("d n c o -> c d n o"), w=["l_wi"], q="pool")
            p.act(c8[:], c8[:], AF.Sigmoid, r=["l_c8"], w=["l_c8"])
            p.act(c8[:], c8[:], AF.Ln, r=["l_c8"], w=["l_c8"])
            p.ts("dve", c8[:], c8[:], 8.0, None, ALU.mult, r=["l_c8"], w=["l_c8"])
            for n in range(4):
                for d_ in range(2):
                    order = blocks if d_ == 0 else [blocks[0]] + blocks[:0:-1]
                    p.op("dve", lambda e: e.memset(hprev[:], 0.0), w=["l_hp"])
                    for (t0, ntok) in order:
                        seg_lo, seg_hi = (0, cfg.ctx) if t0 < cfg.ctx else (cfg.ctx, NT)
                        p.op("pool", lambda e: e.memset(xp[:], 0.0), w=["l_xp"])
                        lo, hi = max(t0 - 1, seg_lo), min(t0 + ntok + 2, seg_hi)
                        p.dma(xp[:, lo - (t0 - 1):hi - (t0 - 1)], S["lxT"][n * 128:(n + 1) * 128, lo:hi], r=["lxy"], w=["l_xp"])
                        p.ts("dve", xc[:, 0:ntok], xp[:, 0:ntok], cw_s[:, n, 0:1], cb_s[:, n:n + 1], ALU.mult, ALU.add,
                             r=["l_xp", "l_cw", "l_cb"], w=["l_xc"])
                        for j in range(1, 4):
                            p.stt(xc[:, 0:ntok], xp[:, j:j + ntok], cw_s[:, n, j:j + 1], xc[:, 0:ntok], ALU.mult, ALU.add,
                                  r=["l_xp", "l_cw", "l_xc"], w=["l_xc"])
                        p.copy("act", xcb[:, 0:ntok], xc[:, 0:ntok], r=["l_xc"], w=["l_xcb"])
                        SUB = min(512, ntok)
                        for s0 in range(0, ntok, SUB):
                            ss_ = slice(s0, s0 + SUB)
                            p.mm(psf[0][:, 0:SUB], wa_s[:, d_, n, :], xcb[:, ss_], True, True, r=["l_wa", "l_xcb"], w=["psf0"])
                            p.mm(psf[1][:, 0:SUB], wi_s[:, d_, n, :], xcb[:, ss_], True, True, r=["l_wi", "l_xcb"], w=["psf1"])
                            p.act(tmp[0][:, 0:SUB], psf[0][:, 0:SUB], AF.Sigmoid, r=["psf0", "l_ba"], w=["l_t0"],
                                  bias=ba_s[:, d_ * 4 + n:d_ * 4 + n + 1])
                            p.act(tmp[1][:, 0:SUB], psf[1][:, 0:SUB], AF.Sigmoid, r=["psf1", "l_bi"], w=["l_t1"],
                                  bias=bi_s[:, d_ * 4 + n:d_ * 4 + n + 1])
                            p.act(aa[:, ss_], tmp[0][:, 0:SUB], AF.Exp, r=["l_t0", "l_c8"], w=["l_aa"],
                                  scale=c8[:, d_ * 4 + n:d_ * 4 + n + 1])
                            p.stt(tmp[2][:, 0:SUB], aa[:, ss_], -1.0, aa[:, ss_], ALU.mult, ALU.mult, r=["l_aa"], w=["l_t2"])
                            p.act(tmp[2][:, 0:SUB], tmp[2][:, 0:SUB], AF.Sqrt, r=["l_t2"], w=["l_t2"], bias=1.0)
                            p.tt("dve", tmp[3][:, 0:SUB], tmp[1][:, 0:SUB], tmp[2][:, 0:SUB], ALU.mult, r=["l_t1", "l_t2"], w=["l_t3"])
                            p.tt("dve", bb[:, ss_], tmp[3][:, 0:SUB], xc[:, ss_], ALU.mult, r=["l_t3", "l_xc"], w=["l_bb"])
                        if d_ == 0:
                            p.op("dve", lambda e: e.tensor_tensor_scan(out=hh_[:, 0:ntok], data0=aa[:, 0:ntok], data1=bb[:, 0:ntok],
                                                                         initial=hprev[:, 0:1], op0=ALU.mult, op1=ALU.add),
                                 r=["l_aa", "l_bb", "l_hp"], w=["l_hh"])
                            p.copy("dve", hprev[:], hh_[:, ntok - 1:ntok], r=["l_hh"], w=["l_hp"])
                            p.dma(S["hf"][n * 128:(n + 1) * 128, t0:t0 + ntok], hh_[:, 0:ntok], r=["l_hh"], w=["hf"])
                        else:
                            p.op("dve", lambda e: e.tensor_tensor_scan(out=hh_[:, 0:ntok][:, ::-1], data0=aa[:, 0:ntok][:, ::-1],
                                                                         data1=bb[:, 0:ntok][:, ::-1], initial=hprev[:, 0:1],
                                                                         op0=ALU.mult, op1=ALU.add),
                                 r=["l_aa", "l_bb", "l_hp"], w=["l_hh"])
                            p.copy("dve", hprev[:], hh_[:, 0:1], r=["l_hh"], w=["l_hp"])
                            p.dma(hfb[:, 0:ntok], S["hf"][n * 128:(n + 1) * 128, t0:t0 + ntok], r=["hf"], w=["l_hfb"])
                            p.dma(yb[:, 0:ntok], S["lyT"][n * 128:(n + 1) * 128, t0:t0 + ntok], r=["lxy"], w=["l_yb"])
                            p.tt("pool", hfb[:, 0:ntok], hfb[:, 0:ntok], hh_[:, 0:ntok], ALU.add, r=["l_hfb", "l_hh"], w=["l_hfb"])
                            p.tt("dve", xc[:, 0:ntok], yb[:, 0:ntok], yb[:, 0:ntok], ALU.mult, r=["l_yb"], w=["l_xc"])
                            p.ts("dve", xc[:, 0:ntok], xc[:, 0:ntok], 0.044715, 1.0, ALU.mult, ALU.add, r=["l_xc"], w=["l_xc"])
                            p.tt("dve", xc[:, 0:ntok], xc[:, 0:ntok], yb[:, 0:ntok], ALU.mult, r=["l_xc", "l_yb"], w=["l_xc"])
                            p.act(xc[:, 0:ntok], xc[:, 0:ntok], AF.Sigmoid, r=["l_xc"], w=["l_xc"], scale=1.5957691216057308)
                            p.tt("dve", xc[:, 0:ntok], xc[:, 0:ntok], yb[:, 0:ntok], ALU.mult, r=["l_xc", "l_yb"], w=["l_xc"])
                            p.tt("dve", lob[:, 0:ntok], xc[:, 0:ntok], hfb[:, 0:ntok], ALU.mult, r=["l_xc", "l_hfb"], w=["l_ob"])
                            p.dma(S["mixT"][1536 + n * 128:1536 + (n + 1) * 128, t0:t0 + ntok], lob[:, 0:ntok], r=["l_ob"], w=["mixT"])
            p.barrier()
        if "lru" in dbg:
            return finish(["mixT"])

        with ExitStack() as st:
            kT = sb("a_k", [128, NT], BF16, stack=st)
            qT = sb("a_q", [128, NT], BF16, stack=st)
            vv = sb("a_v", [128, NTL, 128], BF16, stack=st)
            btm = sb("a_bt", [128, NCLS, 5, 128], stack=st)
            sTs = [sb(f"a_s{i}", [128, 5 * 128], stack=st) for i in range(2)]
            pTs = [sb(f"a_p{i}", [128, 8 * 128], BF16, stack=st) for i in range(2)]
            rss = [sb(f"a_rs{i}", [128, 128], stack=st) for i in range(2)]
            aob = [sb(f"a_ob{i}", [128, 128], BF16, stack=st) for i in range(2)]
            for hd in range(8):
                for c0 in range(0, NT, 2048):
                    c1 = min(NT, c0 + 2048)
                    p.dma(kT[:, c0:c1], S["nkT"][hd * 128:(hd + 1) * 128, c0:c1], r=["nqk"], w=["a_k"])
                    p.dma(qT[:, c0:c1], S["nqT"][hd * 128:(hd + 1) * 128, c0:c1], r=["nqk"], w=["a_q"])
                for c0 in range(0, NTL, 16):
                    c1 = min(NTL, c0 + 16)
                    p.dma(vv[:, c0:c1, :], S["nv"][c0 * 128:c1 * 128, hd * 128:(hd + 1) * 128].rearrange("(t p) f -> p t f", p=128),
                          r=["tokdst"], w=["a_v"])
                for ci in range(NCLS):
                    p.dma(btm[:, ci, :, :], I["bt"][l, ci, hd], w=["a_bt"])
                def na_geom(t):
                    if t < nct:
                        win, ci = [], 0
                    else:
                        i = t - nct
                        win = [nct + plo_l[i] + c for c in range(nch_l[i])]
                        ci = cls_l[i]
                    return win, ci, win + list(range(nct))

                def na_S(t):
                    sb0 = 3 * (t % 2)
                    win, ci, keys = na_geom(t)
                    for idx, kt in enumerate(keys):
                        bk = sb0 + idx // 4
                        p.mm(psf[bk][:, (idx % 4) * 128:(idx % 4 + 1) * 128], kT[:, kt * 128:(kt + 1) * 128], qT[:, t * 128:(t + 1) * 128],
                             True, True, r=["a_k", "a_q"], w=[f"psf{bk}"])

                def na_soft(t):
                    b = t % 2
                    sb0 = 3 * b
                    sT, pT = sTs[b], pTs[b]
                    ks, kp = f"a_s{b}", f"a_p{b}"
                    win, ci, keys = na_geom(t)
                    nw, nk = len(win), len(keys)
                    for bk_ in range(2):
                        bk = sb0 + bk_
                        a0, a1 = bk_ * 4, min(nw, bk_ * 4 + 4)
                        if a1 > a0:
                            n_ = (a1 - a0) * 128
                            p.tt("dve", sT[:, a0 * 128:a1 * 128], psf[bk][:, 0:n_],
                                 btm[:, ci, a0:a1, :].rearrange("p c q -> p (c q)"), ALU.add, r=[f"psf{bk}", "a_bt"], w=[ks])
                    if nw:
                        p.act(pT[:, 0:nw * 128], sT[:, 0:nw * 128], AF.Exp, r=[ks], w=[kp])
                    for idx in range(nw, nk):
                        bk = sb0 + idx // 4
                        p.act(pT[:, idx * 128:(idx + 1) * 128], psf[bk][:, (idx % 4) * 128:(idx % 4 + 1) * 128], AF.Exp,
                              r=[f"psf{bk}"], w=[kp])

                def na_PV(t):
                    b = t % 2
                    pT, kp = pTs[b], f"a_p{b}"
                    po, pok = psf[3 * b + 2], f"psf{3 * b + 2}"
                    win, ci, keys = na_geom(t)
                    nk = len(keys)
                    for idx, kt in enumerate(keys):
                        p.mm(po[:, 0:128], vv[:, kt, :], pT[:, idx * 128:(idx + 1) * 128], idx == 0, idx == nk - 1,
                             r=["a_v", kp], w=[pok])
                    for idx, kt in enumerate(keys):
                        p.mm(po[:, 128:256], ones_b, pT[:, idx * 128:(idx + 1) * 128], idx == 0, idx == nk - 1,
                             r=["cstb", kp], w=[pok])

                def na_out(t):
                    b = t % 2
                    rs_, kr = rss[b], f"a_rs{b}"
                    po, pok = psf[3 * b + 2], f"psf{3 * b + 2}"
                    p.op("dve", lambda e: e.reciprocal(rs_[:], po[:, 128:256]), r=[pok], w=[kr])
                    p.tt("dve", aob[b][:], po[:, 0:128], rs_[:], ALU.mult, r=[pok, kr], w=[f"a_ob{b}"])
                    p.dma(S["mixT"][512 + hd * 128:512 + (hd + 1) * 128, t * 128:(t + 1) * 128], aob[b][:], r=[f"a_ob{b}"], w=["mixT"])

                na_S(0)
                for t in range(NTL):
                    na_soft(t)
                    if t + 1 < NTL:
                        na_S(t + 1)
                    na_PV(t)
                    if t >= 1:
                        na_out(t - 1)
                na_out(NTL - 1)
            p.barrier()
        if "mix" in dbg:
            return finish(["mixT"])

        with ExitStack() as st:
            wo = sb("o_w", [128, KD, D], BF16, stack=st)
            gbc = sb("o_g", [128, 2, D], stack=st)
            mx_ = [sb(f"o_m{i}", [128, KD, 128], BF16, stack=st) for i in range(2)]
            xt = [sb(f"o_x{i}", [128, D], stack=st) for i in range(2)]
            tq = sb("o_t", [128, 512], stack=st)
            for k0 in range(0, KD, 4):
                p.dma(wo[:, k0:k0 + 4, :], I["w_out"][l].rearrange("(k p) c -> p k c", p=128)[:, k0:k0 + 4, :], w=["o_w"], q="pool")
            for s_ in range(2):
                p.dma(gbc[:, s_, :], S["modsd"][s_:s_ + 1, 2 * D:3 * D].partition_broadcast(128), r=["modsd"], w=["o_g"])
            for t in range(NTL):
                b = t % 2
                s_ = stream_of(t)
                p.dma(mx_[b][:], S["mixT"].rearrange("(k p) n -> p k n", p=128)[:, :, t * 128:(t + 1) * 128], r=["mixT"], w=[f"o_m{b}"])
                p.dma(xt[b][:], S["xcur"][t * 128:(t + 1) * 128, :], r=["xcur"], w=[f"o_x{b}"])
                for cb in range(4):
                    ps, pk = psf[cb], f"psf{cb}"
                    for k in range(KD):
                        p.mm(ps[:, :], mx_[b][:, k, :], wo[:, k, cb * 512:(cb + 1) * 512], k == 0, k == KD - 1, r=[f"o_m{b}", "o_w"], w=[pk])
                    p.tt("dve", tq[:], ps[:, :], gbc[:, s_, cb * 512:(cb + 1) * 512], ALU.mult, r=[pk, "o_g"], w=["o_t"])
                    p.tt("pool", xt[b][:, cb * 512:(cb + 1) * 512], xt[b][:, cb * 512:(cb + 1) * 512], tq[:], ALU.add,
                         r=["o_t", f"o_x{b}"], w=[f"o_x{b}"])
                p.dma(S["xcur"][t * 128:(t + 1) * 128, :], xt[b][:], r=[f"o_x{b}"], w=["xcur"])
            p.barrier()
        if "xmid" in dbg:
            return finish(["xcur"])

        with ExitStack() as st:
            affT = sb("r_affT", [NE, NT], stack=st)
            with ExitStack() as st2:
                wr_s = sb("r_w", [128, KD, NE], BF16, stack=st2)
                lg = sb("r_lg", [128, NE], stack=st2)
                rsm = sb("r_sm", [128, 4], stack=st2)
                p.dma(wr_s[:], I["w_r"][l].rearrange("(k p) e -> p k e", p=128), w=["r_w"], q="pool")

                def router(t, hTt, hkey):
                    for k in range(KD):
                        p.mm(psf[4][:, 0:NE], hTt[:, k, :], wr_s[:, k, :], k == 0, k == KD - 1, r=[hkey, "r_w"], w=["psf4"])
                    p.op("dve", lambda e: e.reduce_max(out=rsm[:, 0:1], in_=psf[4][:, 0:NE], axis=AX.X), r=["psf4"], w=["r_mx"])
                    p.ts("dve", rsm[:, 1:2], rsm[:, 0:1], -1.0, None, ALU.mult, r=["r_mx"], w=["r_nmx"])
                    p.act(lg[:], psf[4][:, 0:NE], AF.Exp, r=["psf4", "r_nmx"], w=["r_lg", "r_sum"], bias=rsm[:, 1:2], accum=rsm[:, 2:3])
                    p.op("dve", lambda e: e.reciprocal(rsm[:, 3:4], rsm[:, 2:3]), r=["r_sum"], w=["r_rs"])
                    p.ts("dve", lg[:], lg[:], rsm[:, 3:4], None, ALU.mult, r=["r_lg", "r_rs"], w=["r_lg"])
                    p.mm(psf[5][0:NE, 0:128], lg[:], ident_f, True, True, r=["r_lg", "cst"], w=["psf5"])
                    p.copy("act", affT[:, t * 128:(t + 1) * 128], psf[5][0:NE, 0:128], r=["psf5"], w=["r_affT"])

                norm_stage(st2, 1, None, dst_xn=S["xn2"], router=router)
                p.barrier()
            with ExitStack() as st2:
                wk = sb("r_wk", [NE, cfg.seq], stack=st2)
                wts = sb("r_wts", [NE, cfg.cap_l], stack=st2)
                ixs = sb("r_ixs", [NE, cfg.cap_l], U32, stack=st2)
                for (c0, n, cap, doff, off) in [(cfg.ctx, cfg.seq, cfg.cap_l, 0, cfg.ctx), (0, cfg.ctx, cfg.cap_c, 1, 0)]:
                    p.copy("dve", wk[:, 0:n], affT[:, c0:c0 + n], r=["r_affT"], w=["r_wk"])
                    for r_ in range(cap // 8):
                        s8 = slice(r_ * 8, r_ * 8 + 8)
                        p.op("dve", lambda e: e.max(out=wts[:, s8], in_=wk[:, 0:n]), r=["r_wk"], w=["r_wts"])
                        p.op("dve", lambda e: e.max_index(out=ixs[:, s8], in_max=wts[:, s8], in_values=wk[:, 0:n]), r=["r_wk", "r_wts"], w=["r_ixs"])
                        p.op("dve", lambda e: e.match_replace(out=wk[:, 0:n], in_to_replace=wts[:, s8], in_values=wk[:, 0:n], imm_value=-1.0),
                             r=["r_wts", "r_wk"], w=["r_wk"])
                    if off:
                        p.ts("dve", ixs[:, 0:cap], ixs[:, 0:cap], float(off), None, ALU.add, r=["r_ixs"], w=["r_ixs"])
                    dI = S["idxd"] if doff == 0 else S["idxc"]
                    dW = S["wtd"] if doff == 0 else S["wtc"]
                    p.dma(dI[:, 0:cap], ixs[:, 0:cap], r=["r_ixs"], w=["idxd"])
                    p.dma(dW[:, 0:cap], wts[:, 0:cap], r=["r_wts"], w=["idxd"])
                p.barrier()
        if "route" in dbg:
            return finish(["idxd", "wtd", "idxc", "wtc"])

        with ExitStack() as st:
            GS = min(1024, cfg.cap_l)
            xsT = sb("e_xsT", [128, KD, GS + cfg.cap_c], BF16, stack=st)
            hid = sb("e_hid", [128, KD, GS + cfg.cap_c], BF16, stack=st)
            wd = sb("e_wd", [128, KD, D], BF16, stack=st)
            wgf = [sb(f"e_wg{i}", [128, KD, 128], BF16, stack=st) for i in range(2)]
            wuf = [sb(f"e_wu{i}", [128, KD, 128], BF16, stack=st) for i in range(2)]
            xs = sb("e_xs", [128, D], BF16, stack=st)
            xr = sb("e_xr", [128, D], stack=st)
            yt = sb("e_yt", [128, 512], stack=st)
            sg2 = [sb(f"e_sg{i}", [128, 512], stack=st) for i in range(2)]
            eit = [0]
            icol = sb("e_ic", [128, 1], U32, stack=st)
            wcol = sb("e_wc", [128, 1], stack=st)
            g2 = sb("e_g2", [128, 2, D], stack=st)
            for s_ in range(2):
                p.dma(g2[:, s_, :], S["modsd"][s_:s_ + 1, 5 * D:6 * D].partition_broadcast(128), r=["modsd"], w=["e_g2"])
            for e_ in range(NE):
                for k0 in range(0, KD, 4):
                    p.dma(wd[:, k0:k0 + 4, :], I["wed"][l, e_].rearrange("(k p) c -> p k c", p=128)[:, k0:k0 + 4, :], w=["e_wd"], q="pool")
                lat = [(0, g0, min(GS, cfg.cap_l - g0)) for g0 in range(0, cfg.cap_l, GS)]
                groups = [[(s_, g0, ns, 0)] for (s_, g0, ns) in lat]
                groups[-1].append((1, 0, cfg.cap_c, lat[-1][2]))
                for segs in groups:
                    ncol = sum(sg_[2] for sg_ in segs)
                    tiles = []
                    for (s_, g0, ns, col0) in segs:
                        for j0 in range(0, ns, 128):
                            tiles.append((s_, g0 + j0, min(128, ns - j0), col0 + j0))
                    for (s_, q0, rows, c0_) in tiles:
                        dI = S["idxd"] if s_ == 0 else S["idxc"]
                        p.dma(icol[0:rows, :], dI[e_, q0:q0 + rows].rearrange("(p o) -> p o", o=1), r=["idxd"], w=["e_ic"])
                        p.op("pool", lambda e: e.indirect_dma_start(out=xs[0:rows, :], out_offset=None, in_=S["xn2"][:, :],
                                                                      in_offset=bass.IndirectOffsetOnAxis(ap=icol[0:rows, 0:1], axis=0)),
                             r=["e_ic", "xn2"], w=["e_xs"], dma=True)
                        for k in range(KD):
                            pb = pst[k // 8]
                            p.tr(pb[:, (k % 8) * 128:(k % 8) * 128 + rows], xs[0:rows, k * 128:(k + 1) * 128], ident_b[0:rows, 0:rows],
                                 r=["e_xs", "cstb"], w=[f"pst{k // 8}"])
                        for k in range(KD):
                            pb = pst[k // 8]
                            src = pb[:, (k % 8) * 128:(k % 8) * 128 + rows]
                            if k % 2 == 0:
                                p.ts("dve", xsT[:, k, c0_:c0_ + rows], src, amul[:, 1, k, s_:s_ + 1], modp[:, 3, k, s_:s_ + 1],
                                     ALU.mult, ALU.add, r=[f"pst{k // 8}"], w=["e_xsT"])
                            else:
                                p.act(xsT[:, k, c0_:c0_ + rows], src, AF.Identity, r=[f"pst{k // 8}"], w=["e_xsT"],
                                      scale=amul[:, 1, k, s_:s_ + 1], bias=modp[:, 3, k, s_:s_ + 1])
                    for f in range(KD):
                        b = f % 2
                        p.dma(wgf[b][:], I["weg"][l, e_].rearrange("(k p) c -> p k c", p=128)[:, :, f * 128:(f + 1) * 128], w=[f"e_wg{b}"], q="pool")
                        p.dma(wuf[b][:], I["weu"][l, e_].rearrange("(k p) c -> p k c", p=128)[:, :, f * 128:(f + 1) * 128], w=[f"e_wu{b}"], q="pool")
                        for s0 in range(0, ncol, 512):
                            n_ = min(512, ncol - s0)
                            eit[0] += 1
                            ia, ib = (0, 1) if eit[0] % 2 == 0 else (4, 5)
                            sgb, sgk = sg2[eit[0] % 2], f"e_sg{eit[0] % 2}"
                            for k in range(KD):
                                p.mm(psf[ia][:, 0:n_], wgf[b][:, k, :], xsT[:, k, s0:s0 + n_], k == 0, k == KD - 1, r=[f"e_wg{b}", "e_xsT"], w=[f"psf{ia}"])
                            for k in range(KD):
                                p.mm(psf[ib][:, 0:n_], wuf[b][:, k, :], xsT[:, k, s0:s0 + n_], k == 0, k == KD - 1, r=[f"e_wu{b}", "e_xsT"], w=[f"psf{ib}"])
                            p.act(sgb[:, 0:n_], psf[ia][:, 0:n_], AF.Silu, r=[f"psf{ia}"], w=[sgk])
                            p.tt("dve", hid[:, f, s0:s0 + n_], sgb[:, 0:n_], psf[ib][:, 0:n_], ALU.mult, r=[sgk, f"psf{ib}"], w=["e_hid"])
                    for (s_, q0, rows, c0_) in tiles:
                        dI = S["idxd"] if s_ == 0 else S["idxc"]
                        dW = S["wtd"] if s_ == 0 else S["wtc"]
                        p.dma(icol[0:rows, :], dI[e_, q0:q0 + rows].rearrange("(p o) -> p o", o=1), r=["idxd"], w=["e_ic"])
                        p.dma(wcol[0:rows, :], dW[e_, q0:q0 + rows].rearrange("(p o) -> p o", o=1), r=["idxd"], w=["e_wc"])
                        p.op("pool", lambda e: e.indirect_dma_start(out=xr[0:rows, :], out_offset=None, in_=S["xcur"][:, :],
                                                                      in_offset=bass.IndirectOffsetOnAxis(ap=icol[0:rows, 0:1], axis=0)),
                             r=["e_ic", "xcur"], w=["e_xr"], dma=True)
                        for cb in range(4):
                            ps, pk = psf[2 + cb % 2], f"psf{2 + cb % 2}"
                            for f in range(KD):
                                p.mm(ps[0:rows, :], hid[:, f, c0_:c0_ + rows], wd[:, f, cb * 512:(cb + 1) * 512], f == 0, f == KD - 1,
                                     r=["e_hid", "e_wd"], w=[pk])
                            p.stt(yt[0:rows, :], ps[0:rows, :], wcol[0:rows, 0:1], g2[0:rows, s_, cb * 512:(cb + 1) * 512], ALU.mult, ALU.mult,
                                  r=[pk, "e_wc", "e_g2"], w=["e_yt"])
                            p.tt("pool", xr[0:rows, cb * 512:(cb + 1) * 512], xr[0:rows, cb * 512:(cb + 1) * 512], yt[0:rows, :], ALU.add,
                                 r=["e_yt", "e_xr"], w=["e_xr"])
                        p.op("pool", lambda e: e.indirect_dma_start(out=S["xcur"][:, :],
                                                                      out_offset=bass.IndirectOffsetOnAxis(ap=icol[0:rows, 0:1], axis=0),
                                                                      in_=xr[0:rows, :], in_offset=None),
                             r=["e_ic", "e_xr"], w=["xcur"], dma=True)
            p.barrier()
        if "layer" in dbg:
            return finish(["xcur"])

    big_copy(yout, S["xcur"][cfg.ctx:, :], cfg.seq, key="yout")
    p.barrier()
    es.close()
    return nc


def prep_inputs(cfg, inp):
    L = cfg.depth
    f = lambda a: np.ascontiguousarray(np.asarray(a, dtype=np.float32))
    pk = lambda v: f(np.asarray(v).reshape(-1, 128).T)
    m = {}
    m["xin"] = f(np.concatenate([inp["ctx"][0], inp["x"][0]], 0))
    m["cT"] = f(np.stack([pk(inp["c"][0]), pk(inp["c_ctx"])], -1))
    m["w_mod"] = f(inp["w_mod"])
    m["b_mod"] = f(inp["b_mod"]).reshape(L, 1, -1)
    m["n1g"] = f(np.stack([pk(inp["norm1_g"][l]) for l in range(L)]))
    m["n2g"] = f(np.stack([pk(inp["norm2_g"][l]) for l in range(L)]))
    m["w_in"] = f(inp["w_in"])
    m["wg"] = f(inp["gla_w_gate"])
    m["bg"] = f(np.stack([np.concatenate([pk(inp["gla_b_gate"][l][d]) for d in range(2)], 1) for l in range(L)]))
    m["gng"] = f(inp["gla_norm_g"]).reshape(L, 1, 128)
    m["nqg"] = f(inp["na_q_norm_g"]).reshape(L, 128, 1)
    m["nkg"] = f(inp["na_k_norm_g"]).reshape(L, 128, 1)
    bts = []
    for l in range(L):
        bt, _, _, _ = _na_masks(cfg, np.asarray(inp["na_rpb"][l], np.float32))
        bts.append(bt.transpose(0, 1, 3, 2, 4))
    m["bt"] = f(np.stack(bts))
    m["cw"] = f(np.stack([np.asarray(inp["lru_conv_w"][l]).reshape(4, 4, 128).transpose(2, 1, 0) for l in range(L)]))
    m["cb"] = f(np.stack([pk(inp["lru_conv_b"][l]) for l in range(L)]))
    m["wa"] = f(inp["lru_w_a"])
    m["wi"] = f(inp["lru_w_i"])
    cat2 = lambda a, l: np.concatenate([pk(a[l][d]) for d in range(2)], 1)
    m["ba"] = f(np.stack([cat2(inp["lru_b_a"], l) for l in range(L)]))
    m["bi"] = f(np.stack([cat2(inp["lru_b_i"], l) for l in range(L)]))
    m["lam"] = f(np.stack([cat2(inp["lru_lambda"], l) for l in range(L)]))
    m["w_out"] = f(inp["w_out"])
    m["w_r"] = f(inp["w_router"])
    m["weg"] = f(inp["w_exp_gate"])
    m["weu"] = f(inp["w_exp_up"])
    m["wed"] = f(inp["w_exp_down"])
    m["rope"] = _rope_tables(cfg)
    tri = np.tril(np.ones((128, 128), np.float32))
    m["consts"] = f(np.stack([np.eye(128), np.ones((128, 128)), tri.T, tri, np.zeros((128, 128)), np.zeros((128, 128))]))
    return m


def kernel(**inputs):
    cfg = Cfg()
    nc = build_program(cfg)
    m = prep_inputs(cfg, inputs)
    res = run_bass_kernel_spmd(nc, [m], core_ids=[0])
    return np.asarray(res.results[0]["yout"], dtype=np.float32).reshape(1, cfg.seq, D)
```

```python
import numpy as np
import concourse.bass as bass
import concourse.mybir as mybir
from concourse.bass_utils import run_bass_kernel_spmd

F32 = mybir.dt.float32
BF16 = mybir.dt.bfloat16
U32 = mybir.dt.uint32
AF = mybir.ActivationFunctionType
ALU = mybir.AluOpType
AX = mybir.AxisListType

D = 2048
KD = D // 128
EPS = 1e-6
GRID_W = 64
D_IN = 5664
C_Q, C_K, C_V, C_R, C_LF, C_LB, C_NQ, C_NK, C_NV, C_LX, C_LY = 0, 256, 512, 1024, 1536, 1552, 1568, 2592, 3616, 4640, 5152


class Cfg:
    def __init__(self, seq=16384, ctx=256, depth=2, ne=16):
        self.seq, self.ctx, self.depth, self.ne = seq, ctx, depth, ne
        self.nt = seq + ctx
        self.ntl = self.nt // 128
        self.rows = seq // GRID_W
        self.cap_l = 2 * seq // ne
        self.cap_c = 2 * ctx // ne


class P:
    def __init__(self, nc):
        self.nc = nc
        self.eng = {"pe": nc.tensor, "dve": nc.vector, "act": nc.scalar, "pool": nc.gpsimd, "sp": nc.sync}
        self.sem = {}
        self.cnt = {e: 0 for e in self.eng}
        self.waited = {e: {} for e in self.eng}
        self.res = {}
        self.dq = {e: 0 for e in self.eng}
        self.NDS = 8
        for e in self.eng:
            self.sem["c_" + e] = nc.semaphore("c_" + e).__enter__()
            for i in range(self.NDS):
                self.sem[f"d_{e}_{i}"] = nc.semaphore(f"d_{e}_{i}").__enter__()

    def _need(self, eng, tok, waits):
        if tok is None:
            return
        name, val = tok
        if eng == "pe" and name == "c_pe":
            return
        if self.waited[eng].get(name, 0) >= val:
            return
        self.waited[eng][name] = val
        waits.append(tok)

    def op(self, eng, fn, r=(), w=(), dma=False, sig=True):
        waits = []
        for k in r:
            st = self.res.get(k)
            if st:
                self._need(eng, st[0], waits)
        for k in w:
            st = self.res.get(k)
            if st:
                self._need(eng, st[0], waits)
                for t in st[1].items():
                    self._need(eng, t, waits)
        if dma:
            j = self.dq[eng]
            self.dq[eng] += 1
            name = f"d_{eng}_{j % self.NDS}"
            val = 16 * (j // self.NDS + 1)
            if j >= self.NDS:
                self._need(eng, (name, val - 16), waits)
            inc = 16
        elif not sig:
            name, val, inc = "c_" + eng, self.cnt[eng] + 1, 0
        else:
            self.cnt[eng] += 1
            name, val, inc = "c_" + eng, self.cnt[eng], 1
        e = self.eng[eng]
        for (n, v) in waits:
            e.wait_ge(self.sem[n], v)
        ins = fn(e)
        if inc:
            ins.then_inc(self.sem[name], inc)
        tok = (name, val)
        for k in r:
            st = self.res.setdefault(k, [None, {}])
            st[1][name] = max(st[1].get(name, 0), val)
        for k in w:
            self.res[k] = [tok, {}]
        return tok

    def barrier(self):
        toks = []
        for e in self.eng:
            if self.cnt[e]:
                toks.append(("c_" + e, self.cnt[e]))
            j = self.dq[e]
            for i in range(self.NDS):
                n = (j - 1 - i)
                if n >= 0:
                    toks.append((f"d_{e}_{n % self.NDS}", 16 * (n // self.NDS + 1)))
        for e in self.eng:
            ws = []
            for t in toks:
                self._need(e, t, ws)
            for (n, v) in ws:
                self.eng[e].wait_ge(self.sem[n], v)

    def dma(self, out, in_, r=(), w=(), q="sp", **kw):
        return self.op(q, lambda e: e.dma_start(out=out, in_=in_, **kw), r, w, dma=True)

    def mm(self, out, lhsT, rhs, start, stop, r=(), w=()):
        return self.op("pe", lambda e: e.matmul(out, lhsT, rhs, start=start, stop=stop), r, w, sig=bool(stop))

    def tr(self, out, in_, ident, r=(), w=()):
        return self.op("pe", lambda e: e.transpose(out, in_, ident), r, w)

    def act(self, out, in_, func, r=(), w=(), bias=0.0, scale=1.0, accum=None):
        if accum is None:
            return self.op("act", lambda e: e.activation(out=out, in_=in_, func=func, bias=bias, scale=scale), r, w)
        return self.op("act", lambda e: e.activation(out=out, in_=in_, func=func, bias=bias, scale=scale,
                                                       accum_out=accum), r, w)

    def ts(self, eng, out, in0, s1, s2, op0, op1=None, r=(), w=(), accum=None):
        if op1 is None:
            return self.op(eng, lambda e: e.tensor_scalar(out, in0, s1, None, op0), r, w)
        if accum is not None:
            return self.op(eng, lambda e: e.tensor_scalar(out, in0, s1, s2, op0, op1, accum), r, w)
        return self.op(eng, lambda e: e.tensor_scalar(out, in0, s1, s2, op0, op1), r, w)

    def tt(self, eng, out, in0, in1, op, r=(), w=()):
        return self.op(eng, lambda e: e.tensor_tensor(out, in0, in1, op), r, w)

    def stt(self, out, in0, scalar, in1, op0, op1, r=(), w=()):
        return self.op("dve", lambda e: e.scalar_tensor_tensor(out, in0, scalar, in1, op0, op1), r, w)

    def copy(self, eng, out, in_, r=(), w=()):
        if eng == "act":
            return self.op("act", lambda e: e.copy(out, in_), r, w)
        return self.op(eng, lambda e: e.tensor_copy(out, in_), r, w)


def _na_tile_geom(cfg):
    rows = cfg.rows
    rs = lambda r: min(max(r - 4, 0), rows - 8)
    plo, nch, cls, defs = [], [], [], {}
    for i in range(rows // 2):
        a, b = rs(2 * i), rs(2 * i + 1)
        lo, hi = a // 2, (b + 7) // 2
        key = (a - 2 * i, b - 2 * i - 1, lo - i, hi - lo + 1)
        if key not in defs:
            defs[key] = len(defs)
        plo.append(lo)
        nch.append(hi - lo + 1)
        cls.append(defs[key])
    return plo, nch, cls, defs


def _na_masks(cfg, rpb):
    plo, nch, cls, defs = _na_tile_geom(cfg)
    ncls = len(defs)
    out = np.full((ncls, 8, 5, 128, 128), -30000.0, np.float32)
    kk = np.arange(128)
    kdr, kc = kk // 64, kk % 64
    qdr, qc = kk // 64, kk % 64
    cs = np.clip(qc - 8, 0, GRID_W - 16)
    for key, ci in defs.items():
        a_rel, b_rel, lo_rel, n = key
        for c in range(n):
            krow = 2 * (lo_rel + c) + kdr
            rs_q = np.where(qdr == 0, a_rel, b_rel + 1)
            qrow = qdr
            inr = (krow[:, None] >= rs_q[None, :]) & (krow[:, None] <= rs_q[None, :] + 7)
            inc = (kc[:, None] >= cs[None, :]) & (kc[:, None] <= cs[None, :] + 15)
            ri = np.clip(krow[:, None] - qrow[None, :] + 7, 0, 14)
            cix = np.clip(kc[:, None] - qc[None, :] + 15, 0, 30)
            m = inr & inc
            for h in range(8):
                out[ci, h, c] = np.where(m, rpb[h][ri, cix], np.float32(-30000.0))
    return out, plo, nch, cls


def _rope_tables(cfg):
    pos = np.arange(cfg.seq)
    row = (pos // GRID_W).astype(np.float32)
    col = (pos % GRID_W).astype(np.float32)
    nf = 16
    inv = (np.float32(10000.0) ** (-np.arange(nf, dtype=np.float32) / np.float32(nf))).astype(np.float32)
    ang = np.concatenate([row[:, None] * inv, col[:, None] * inv], axis=-1).astype(np.float32)
    cos, sin = np.cos(ang).astype(np.float32), np.sin(ang).astype(np.float32)
    c_full = np.concatenate([np.ones((cfg.ctx, 32), np.float32), cos], 0)
    s_full = np.concatenate([np.zeros((cfg.ctx, 32), np.float32), sin], 0)
    p = np.arange(128)
    f = p % 32
    sign = np.where((p % 64) < 32, -1.0, 1.0).astype(np.float32)
    ck = c_full[:, f].T.copy()
    sk = (s_full[:, f].T * sign[:, None]).copy()
    sc = np.float32(64 ** -0.5)
    return np.stack([ck * sc, sk * sc, ck, sk]).astype(np.float32)


def build_program(cfg, dbg=()):
    nc = bass.Bass("TRN2", target_bir_lowering=False)
    NT, NTL, L, NE = cfg.nt, cfg.ntl, cfg.depth, cfg.ne
    plo_l, nch_l, cls_l, cdefs = _na_tile_geom(cfg)
    NCLS = len(cdefs)

    def din(name, shape, dt=F32):
        return nc.dram_tensor(name, list(shape), dt, kind="ExternalInput").ap()

    def dscr(name, shape, dt=F32):
        return nc.dram_tensor(name, list(shape), dt, kind="Internal").ap()

    I = dict(
        xin=din("xin", [NT, D]), cT=din("cT", [128, KD, 2]),
        w_mod=din("w_mod", [L, D, 6 * D]), b_mod=din("b_mod", [L, 1, 6 * D]),
        n1g=din("n1g", [L, 128, KD]), n2g=din("n2g", [L, 128, KD]),
        w_in=din("w_in", [L, D, D_IN]), wg=din("wg", [L, 2, 16, 256]), bg=din("bg", [L, 128, 4]),
        gng=din("gng", [L, 1, 128]), nqg=din("nqg", [L, 128, 1]), nkg=din("nkg", [L, 128, 1]),
        bt=din("bt", [L, NCLS, 8, 128, 5, 128]), cw=din("cw", [L, 128, 4, 4]), cb=din("cb", [L, 128, 4]),
        wa=din("wa", [L, 2, 4, 128, 128]), ba=din("ba", [L, 128, 8]), wi=din("wi", [L, 2, 4, 128, 128]),
        bi=din("bi", [L, 128, 8]), lam=din("lam", [L, 128, 8]),
        w_out=din("w_out", [L, D, D]), w_r=din("w_r", [L, D, NE]),
        weg=din("weg", [L, NE, D, D]), weu=din("weu", [L, NE, D, D]), wed=din("wed", [L, NE, D, D]),
        rope=din("rope", [4, 128, NT]), consts=din("consts", [6, 128, 128]),
    )
    yout = nc.dram_tensor("yout", [cfg.seq, D], F32, kind="ExternalOutput").ap()
    dbg_out = {}
    S = dict(
        xcur=dscr("xcur", [NT, D]), modsd=dscr("modsd", [2, 6 * D]), hT=dscr("hTd", [D, NT], BF16),
        gqT=dscr("gqT", [256, NT], BF16), gkT=dscr("gkT", [256, NT], BF16), gv=dscr("gv", [NT, 512], BF16),
        gr=dscr("gr", [NT, 512]), gg=dscr("gg", [2, 256, NT]), go=dscr("go", [NT, 512]),
        nqT=dscr("nqT", [1024, NT], BF16), nkT=dscr("nkT", [1024, NT], BF16), nv=dscr("nv", [NT, 1024], BF16),
        lxT=dscr("lxT", [512, NT]), lyT=dscr("lyT", [512, NT]), hf=dscr("hf", [512, NT]),
        mixT=dscr("mixT", [D, NT], BF16), xn2=dscr("xn2", [NT, D], BF16),
        idxd=dscr("idxd", [NE, cfg.cap_l], U32), wtd=dscr("wtd", [NE, cfg.cap_l]),
        idxc=dscr("idxc", [NE, cfg.cap_c], U32), wtc=dscr("wtc", [NE, cfg.cap_c]),
    )
    p = P(nc)
    from contextlib import ExitStack
    es = ExitStack()

    uid = [0]

    def sb(name, shape, dt=F32, stack=None):
        uid[0] += 1
        return (stack or es).enter_context(nc.sbuf_tensor(f"{name}_u{uid[0]}", list(shape), dt))

    psf = [es.enter_context(nc.psum_tensor(f"psf{i}", [128, 512], F32)) for i in range(6)]
    pst = [es.enter_context(nc.psum_tensor(f"pst{i}", [128, 1024], BF16)) for i in range(2)]
    cst = sb("cst", [128, 6, 128])
    cstb = sb("cstb", [128, 2, 128], BF16)
    p.dma(cst[:], I["consts"].rearrange("c p n -> p c n"), w=["cst"])
    p.copy("dve", cstb[:], cst[:, 0:2, :], r=["cst"], w=["cstb"])
    ident_f, ones_f, maskF, maskB = cst[:, 0, :], cst[:, 1, :], cst[:, 2, :], cst[:, 3, :]
    ident_b, ones_b = cstb[:, 0, :], cstb[:, 1, :]
    modp = sb("modp", [128, 6, KD, 2])
    amul = sb("amul", [128, 2, KD, 2])
    def big_copy(dst, src, rows, step=128, key="bigcopy"):
        for r0 in range(0, rows, step):
            r1 = min(rows, r0 + step)
            p.dma(dst[r0:r1], src[r0:r1], w=[key])

    def finish(names):
        for nm in names:
            o = nc.dram_tensor("dbg_" + nm, list(S[nm].shape), S[nm].dtype, kind="ExternalOutput").ap()
            big_copy(o, S[nm], S[nm].shape[0], key="dbg")
        p.barrier()
        return nc

    big_copy(S["xcur"], I["xin"], NT, key="xcur")
    p.barrier()

    def stream_of(tile):
        return 1 if tile < cfg.ctx // 128 else 0

    supers = [(0, cfg.ctx, 1)] + [(cfg.ctx + i * 512, 512, 0) for i in range(cfg.seq // 512)]

    for l in range(L):
        with ExitStack() as st:
            sc = sb("m_sc", [128, KD, 2], stack=st)
            wm = sb("m_w", [128, KD, 512], stack=st)
            brow = sb("m_b", [2, 512], stack=st)
            mrow = sb("m_r", [2, 512], stack=st)
            p.dma(sc[:], I["cT"], w=["m_sc"])
            p.act(sc[:], sc[:], AF.Silu, r=["m_sc"], w=["m_sc"])
            for cb in range(24):
                p.dma(wm[:], I["w_mod"][l].rearrange("(k p) c -> p k c", p=128)[:, :, cb * 512:(cb + 1) * 512],
                      w=["m_w"])
                p.dma(brow[:], I["b_mod"][l][:, cb * 512:(cb + 1) * 512].partition_broadcast(2), w=["m_b"])
                for k in range(KD):
                    p.mm(psf[0][0:2, :], sc[:, k, :], wm[:, k, :], k == 0, k == KD - 1, r=["m_sc", "m_w"], w=["psf0"])
                p.tt("dve", mrow[:], psf[0][0:2, :], brow[:], ALU.add, r=["psf0", "m_b"], w=["m_r"])
                p.dma(S["modsd"][:, cb * 512:(cb + 1) * 512], mrow[:], r=["m_r"], w=["modsd"])
            p.barrier()
            for j in range(6):
                for s_ in range(2):
                    p.dma(modp[:, j, :, s_], S["modsd"][s_, j * D:(j + 1) * D].rearrange("(k p) -> p k", p=128),
                          r=["modsd"], w=["modp"], allow_slow_non_contiguous=True)
            g1 = sb("m_g1", [128, KD], stack=st)
            g2 = sb("m_g2", [128, KD], stack=st)
            p.dma(g1[:], I["n1g"][l], w=["m_g1"])
            p.dma(g2[:], I["n2g"][l], w=["m_g2"])
            for s_ in range(2):
                p.stt(amul[:, 0, :, s_], modp[:, 1, :, s_], 1.0, g1[:], ALU.add, ALU.mult, r=["modp", "m_g1"], w=["amul"])
                p.stt(amul[:, 1, :, s_], modp[:, 4, :, s_], 1.0, g2[:], ALU.add, ALU.mult, r=["modp", "m_g2"], w=["amul"])
            p.barrier()

        if "mods" in dbg:
            return finish(["modsd"])

        def norm_stage(st, which, dst_hT, dst_xn=None, router=None):
            xt = [sb(f"n_x{i}", [128, D], stack=st) for i in range(2)]
            junk = sb("n_junk", [128, D], BF16, stack=st)
            xn = [sb(f"n_xn{i}", [128, D], BF16, stack=st) for i in range(2)]
            hTt = [sb(f"n_hT{i}", [128, KD, 128], BF16, stack=st) for i in range(2)]
            stat = sb("n_stat", [128, 4], stack=st)
            for t in range(NTL):
                b = t % 2
                s_ = stream_of(t)
                p.dma(xt[b][:], S["xcur"][t * 128:(t + 1) * 128, :], r=["xcur"], w=[f"n_x{b}"])
                p.act(junk[:], xt[b][:], AF.Square, r=[f"n_x{b}"], w=["n_junk", "n_ss"], accum=stat[:, 0:1])
                p.act(stat[:, 1:2], stat[:, 0:1], AF.Sqrt, r=["n_ss"], w=["n_sd"], scale=1.0 / D, bias=EPS)
                p.op("dve", lambda e: e.reciprocal(stat[:, 2:3], stat[:, 1:2]), r=["n_sd"], w=["n_rs"])
                p.ts("dve", xn[b][:], xt[b][:], stat[:, 2:3], None, ALU.mult, r=[f"n_x{b}", "n_rs"], w=[f"n_xn{b}"])
                if dst_xn is not None:
                    p.dma(dst_xn[t * 128:(t + 1) * 128, :], xn[b][:], r=[f"n_xn{b}"], w=["xn2"])
                for k in range(KD):
                    pb = pst[k // 8]
                    p.tr(pb[:, (k % 8) * 128:(k % 8 + 1) * 128], xn[b][:, k * 128:(k + 1) * 128], ident_b,
                         r=[f"n_xn{b}", "cstb"], w=[f"pst{k // 8}"])
                for k in range(KD):
                    pb = pst[k // 8]
                    src = pb[:, (k % 8) * 128:(k % 8 + 1) * 128]
                    if k % 2 == 0:
                        p.ts("dve", hTt[b][:, k, :], src, amul[:, which, k, s_:s_ + 1], modp[:, 3 * which, k, s_:s_ + 1],
                             ALU.mult, ALU.add, r=[f"pst{k // 8}", "amul", "modp"], w=[f"n_hT{b}"])
                    else:
                        p.act(hTt[b][:, k, :], src, AF.Identity, r=[f"pst{k // 8}", "amul", "modp"], w=[f"n_hT{b}"],
                              scale=amul[:, which, k, s_:s_ + 1], bias=modp[:, 3 * which, k, s_:s_ + 1])
                if dst_hT is not None:
                    p.dma(dst_hT.rearrange("(k p) n -> p k n", p=128)[:, :, t * 128:(t + 1) * 128], hTt[b][:],
                          r=[f"n_hT{b}"], w=["hTd"])
                if router is not None:
                    router(t, hTt[b], f"n_hT{b}")

        with ExitStack() as st:
            norm_stage(st, 0, S["hT"])
            p.barrier()
        if "norm" in dbg:
            return finish(["hT"])

        with ExitStack() as st:
            wb = sb("i_w", [128, KD, 1024], BF16, stack=st)
            wsw = sb("i_wsw", [128, KD, 512], BF16, stack=st)
            hs = [sb(f"i_h{i}", [128, KD, 512], BF16, stack=st) for i in range(2)]
            rp = sb("i_rope", [128, 4, 512], stack=st)
            ev = [sb(f"i_ev{i}", [128, 512], stack=st) for i in range(4)]
            evb = [sb(f"i_evb{i}", [128, 512], BF16, stack=st) for i in range(2)]
            lrt = sb("i_lr", [16, 2, 512], BF16, stack=st)
            wgs = sb("i_wg", [16, 2, 256], BF16, stack=st)
            bgs = sb("i_bg", [128, 4], stack=st)
            nbg = sb("i_nbg", [128, 4], stack=st)
            nqk = sb("i_nqk", [128, 2], stack=st)
            p.dma(wgs[:], I["wg"][l].rearrange("d r c -> r d c"), w=["i_wg"], q="pool")
            p.dma(bgs[:], I["bg"][l], w=["i_bg"])
            p.ts("dve", nbg[:], bgs[:], -1.0, None, ALU.mult, r=["i_bg"], w=["i_nbg"])
            p.dma(nqk[:, 0:1], I["nqg"][l], w=["i_nqk"])
            p.dma(nqk[:, 1:2], I["nkg"][l], w=["i_nqk"])
            p.ts("dve", nqk[:, 0:1], nqk[:, 0:1], float(128 ** -0.5), None, ALU.mult, r=["i_nqk"], w=["i_nqk"])
            wsrc = I["w_in"][l].rearrange("(k p) c -> p k c", p=128)

            def load_w(c0, n):
                for k0 in range(0, KD, 4):
                    p.dma(wb[:, k0:k0 + 4, 0:n], wsrc[:, k0:k0 + 4, c0:c0 + n], w=["i_w"], q="pool")

            def load_h(si):
                t0, ntok, s_ = supers[si]
                b = si % 2
                p.dma(hs[b][:, :, 0:ntok], S["hT"].rearrange("(k p) n -> p k n", p=128)[:, :, t0:t0 + ntok],
                      r=["hTd"], w=[f"i_h{b}"])
                return hs[b], f"i_h{b}", t0, ntok

            def fm_mm(ps, pskey, wtile, wkey, c0, m, h, hkey, ntok):
                for k in range(KD):
                    p.mm(ps[0:m, 0:ntok], wtile[:, k, c0:c0 + m], h[:, k, 0:ntok], k == 0, k == KD - 1,
                         r=[wkey, hkey], w=[pskey])

            load_w(C_Q, 512)
            p.dma(wb[:, :, 512:544], wsrc[:, :, C_LF:C_LF + 32], w=["i_w"], q="pool")
            w5 = wb[:, :, 0:512].rearrange("p k (h two f) -> p k h two f", two=2, f=32)
            s5 = wsw[:].rearrange("p k (h two f) -> p k h two f", two=2, f=32)
            p.copy("dve", s5[:, :, :, 0, :], w5[:, :, :, 1, :], r=["i_w"], w=["i_wsw"])
            p.copy("pool", s5[:, :, :, 1, :], w5[:, :, :, 0, :], r=["i_w"], w=["i_wsw"])
            for si in range(len(supers)):
                h, hkey, t0, ntok = load_h(si)
                p.dma(rp[:, :, 0:ntok], I["rope"].rearrange("c p n -> p c n")[:, :, t0:t0 + ntok], w=["i_rope"])
                for qk in range(2):
                    dst = S["gqT"] if qk == 0 else S["gkT"]
                    for hp in range(2):
                        c0 = qk * 256 + hp * 128
                        par = hp
                        pa_, pb_ = (0, 1) if par == 0 else (4, 5)
                        ea, eb, ec = 2 * par, 2 * par + 1, par
                        fm_mm(psf[pa_], f"psf{pa_}", wb, "i_w", c0, 128, h, hkey, ntok)
                        fm_mm(psf[pb_], f"psf{pb_}", wsw, "i_wsw", c0, 128, h, hkey, ntok)
                        p.tt("dve", ev[ea][:, 0:ntok], psf[pa_][:, 0:ntok], rp[:, 2 * qk, 0:ntok], ALU.mult,
                             r=[f"psf{pa_}", "i_rope"], w=[f"i_ev{ea}"])
                        p.tt("dve", ev[eb][:, 0:ntok], psf[pb_][:, 0:ntok], rp[:, 2 * qk + 1, 0:ntok], ALU.mult,
                             r=[f"psf{pb_}", "i_rope"], w=[f"i_ev{eb}"])
                        p.tt("pool", evb[ec][:, 0:ntok], ev[ea][:, 0:ntok], ev[eb][:, 0:ntok], ALU.add,
                             r=[f"i_ev{ea}", f"i_ev{eb}"], w=[f"i_evb{ec}"])
                        p.dma(dst[hp * 128:(hp + 1) * 128, t0:t0 + ntok], evb[ec][:, 0:ntok], r=[f"i_evb{ec}"], w=["gqk"])
                for d_ in range(2):
                    fm_mm(psf[2], "psf2", wb, "i_w", 512 + 16 * d_, 16, h, hkey, ntok)
                    p.copy("act", lrt[:, d_, 0:ntok], psf[2][0:16, 0:ntok], r=["psf2"], w=["i_lr"])
                for d_ in range(2):
                    for hp in range(2):
                        p.mm(psf[3][:, 0:ntok], wgs[:, d_, hp * 128:(hp + 1) * 128], lrt[:, d_, 0:ntok], True, True,
                             r=["i_wg", "i_lr"], w=["psf3"])
                        p.act(ev[2][:, 0:ntok], psf[3][:, 0:ntok], AF.Exp, r=["psf3", "i_nbg"], w=["i_ev2"],
                              scale=-1.0, bias=nbg[:, d_ * 2 + hp:d_ * 2 + hp + 1])
                        p.act(ev[2][:, 0:ntok], ev[2][:, 0:ntok], AF.Ln, r=["i_ev2"], w=["i_ev2"], bias=1.0)
                        p.ts("dve", ev[3][:, 0:ntok], ev[2][:, 0:ntok], -1.0 / 16.0, None, ALU.mult, r=["i_ev2"], w=["i_ev3"])
                        p.dma(S["gg"][d_, hp * 128:(hp + 1) * 128, t0:t0 + ntok], ev[3][:, 0:ntok], r=["i_ev3"], w=["gg"])

            if "ip1" in dbg:
                return finish(["gqT", "gkT", "gg"])

            def tok_group(c0, ncols, dst, dst_dt_bf, silu):
                load_w(c0, ncols)
                for si in range(len(supers)):
                    h, hkey, t0, ntok = load_h(si)
                    for tt_ in range(ntok // 128):
                        for cb in range(ncols // 512):
                            ps, pk = psf[(tt_ * 2 + cb) % 4], f"psf{(tt_ * 2 + cb) % 4}"
                            for k in range(KD):
                                p.mm(ps[:, :], h[:, k, tt_ * 128:(tt_ + 1) * 128], wb[:, k, cb * 512:(cb + 1) * 512],
                                     k == 0, k == KD - 1, r=["i_w", hkey], w=[pk])
                            tok0 = t0 + tt_ * 128
                            if dst_dt_bf:
                                e_, ek = evb[cb % 2], f"i_evb{cb % 2}"
                                p.copy("act" if cb % 2 else "dve", e_[:], ps[:, :], r=[pk], w=[ek])
                            else:
                                e_, ek = ev[cb % 2], f"i_ev{cb % 2}"
                                p.act(e_[:], ps[:, :], AF.Silu if silu else AF.Copy, r=[pk], w=[ek])
                            p.dma(dst[tok0:tok0 + 128, cb * 512:(cb + 1) * 512], e_[:], r=[ek], w=["tokdst"])

            tok_group(C_V, 512, S["gv"], True, False)
            tok_group(C_R, 512, S["gr"], False, True)
            tok_group(C_NV, 1024, S["nv"], True, False)

            if "ip2" in dbg:
                return finish(["gv", "gr", "nv"])
            for qk in range(2):
                load_w(C_NQ if qk == 0 else C_NK, 1024)
                dst = S["nqT"] if qk == 0 else S["nkT"]
                for si in range(len(supers)):
                    h, hkey, t0, ntok = load_h(si)
                    for hd in range(8):
                        pa_, pb_ = (0, 1) if hd % 2 == 0 else (2, 3)
                        fm_mm(psf[pa_], f"psf{pa_}", wb, "i_w", hd * 128, 128, h, hkey, ntok)
                        p.copy("dve", ev[0][:, 0:ntok], psf[pa_][:, 0:ntok], r=[f"psf{pa_}"], w=["i_ev0"])
                        p.tt("dve", evb[1][:, 0:ntok], ev[0][:, 0:ntok], ev[0][:, 0:ntok], ALU.mult, r=["i_ev0"], w=["i_evb1"])
                        p.mm(psf[pb_][:, 0:ntok], ones_b, evb[1][:, 0:ntok], True, True, r=["cstb", "i_evb1"], w=[f"psf{pb_}"])
                        p.act(ev[2][:, 0:ntok], psf[pb_][:, 0:ntok], AF.Sqrt, r=[f"psf{pb_}"], w=["i_ev2"], scale=1.0 / 128, bias=EPS)
                        p.op("dve", lambda e: e.reciprocal(ev[3][:, 0:ntok], ev[2][:, 0:ntok]), r=["i_ev2"], w=["i_ev3"])
                        p.stt(evb[0][:, 0:ntok], ev[0][:, 0:ntok], nqk[:, qk:qk + 1], ev[3][:, 0:ntok], ALU.mult, ALU.mult,
                              r=["i_ev0", "i_ev3", "i_nqk"], w=["i_evb0"])
                        p.dma(dst[hd * 128:(hd + 1) * 128, t0:t0 + ntok], evb[0][:, 0:ntok], r=["i_evb0"], w=["nqk"])

            if "ip3" in dbg:
                return finish(["nqT", "nkT"])
            load_w(C_LX, 1024)
            for si in range(len(supers)):
                h, hkey, t0, ntok = load_h(si)
                for c in range(8):
                    ps, pk = psf[c % 4], f"psf{c % 4}"
                    fm_mm(ps, pk, wb, "i_w", c * 128, 128, h, hkey, ntok)
                    e_, ek = ev[c % 4], f"i_ev{c % 4}"
                    p.copy("act" if c % 2 else "dve", e_[:, 0:ntok], ps[:, 0:ntok], r=[pk], w=[ek])
                    dst = S["lxT"] if c < 4 else S["lyT"]
                    p.dma(dst[(c % 4) * 128:(c % 4 + 1) * 128, t0:t0 + ntok], e_[:, 0:ntok], r=[ek], w=["lxy"])
            p.barrier()

        if "inproj" in dbg:
            return finish(["gqT", "gkT", "gg", "gv", "gr", "nqT", "nkT", "nv", "lxT", "lyT"])

        nct = cfg.ctx // 128
        with ExitStack() as st:
            BL = min(1024, cfg.seq)
            blocks = [(0, cfg.ctx)] + [(cfg.ctx + i * BL, BL) for i in range(cfg.seq // BL)]
            qb = sb("g_q", [128, BL], BF16, stack=st)
            kb = sb("g_k", [128, BL], BF16, stack=st)
            gb = sb("g_g", [128, BL], stack=st)
            vb = sb("g_v", [128, BL // 128, 256], BF16, stack=st)
            gc2 = [sb(f"g_gc{i}", [128, 128], stack=st) for i in range(2)]
            Hh2 = [sb(f"g_H{i}", [128, 128], stack=st) for i in range(2)]
            ex2 = [sb(f"g_ex{i}", [128, 3, 128], stack=st) for i in range(2)]
            qkb2 = [sb(f"g_qkb{i}", [128, 3, 128], BF16, stack=st) for i in range(2)]
            kdT2 = [sb(f"g_kdT{i}", [128, 128], BF16, stack=st) for i in range(2)]
            attm4 = [sb(f"g_att{i}", [128, 128], BF16, stack=st) for i in range(4)]
            Sst = sb("g_S", [128, 128], stack=st)
            Sbf = sb("g_Sb", [128, 128], BF16, stack=st)
            sm2 = [sb(f"g_sm{i}", [128, 8], stack=st) for i in range(2)]
            ob2 = [sb(f"g_o{i}", [128, 256], stack=st) for i in range(2)]
            of2 = [sb(f"g_of{i}", [128, 256], stack=st) for i in range(2)]
            rr2 = [sb(f"g_r{i}", [128, 256], stack=st) for i in range(2)]
            gn = sb("g_gn", [128, 128], stack=st)
            outb2 = [sb(f"g_outb{i}", [128, 256], BF16, stack=st) for i in range(2)]
            mT2 = [sb(f"g_mT{i}", [128, 256], BF16, stack=st) for i in range(2)]
            junk2 = [sb(f"g_junk{i}", [128, 128], stack=st) for i in range(2)]
            git = [0]
            p.dma(gn[:], I["gng"][l].partition_broadcast(128), w=["g_gn"])
            for hp in range(2):
                for d_ in range(2):
                    p.op("dve", lambda e: e.memset(Sst[:], 0.0), w=["g_S"])
                    p.op("dve", lambda e: e.memset(Sbf[:], 0.0), w=["g_Sb"])
                    order = blocks if d_ == 0 else [blocks[0]] + blocks[:0:-1]
                    msk = maskF if d_ == 0 else maskB
                    for (t0, ntok) in order:
                        p.dma(qb[:, 0:ntok], S["gqT"][hp * 128:(hp + 1) * 128, t0:t0 + ntok], r=["gqk"], w=["g_q"])
                        p.dma(kb[:, 0:ntok], S["gkT"][hp * 128:(hp + 1) * 128, t0:t0 + ntok], r=["gqk"], w=["g_k"])
                        p.dma(gb[:, 0:ntok], S["gg"][d_, hp * 128:(hp + 1) * 128, t0:t0 + ntok], r=["gg"], w=["g_g"])
                        p.dma(vb[:, 0:ntok // 128, :],
                              S["gv"][t0:t0 + ntok, hp * 256:(hp + 1) * 256].rearrange("(c p) f -> p c f", p=128),
                              r=["tokdst"], w=["g_v"])
                        cs_ = list(range(ntok // 128))
                        if d_ == 1:
                            cs_ = cs_[::-1]
                        for c in cs_:
                            sl = slice(c * 128, (c + 1) * 128)
                            tok = t0 + c * 128
                            git[0] += 1
                            par = git[0] % 2
                            gc, Hh, ex, qkb, kdT, sm = gc2[par], Hh2[par], ex2[par], qkb2[par], kdT2[par], sm2[par]
                            ob, of, rr, outb, mT, junk = ob2[par], of2[par], rr2[par], outb2[par], mT2[par], junk2[par]
                            attm = [attm4[2 * par], attm4[2 * par + 1]]
                            PA, PO, PD = psf[3 * par], psf[3 * par + 1], psf[3 * par + 2]
                            kPA, kPO, kPD = f"psf{3 * par}", f"psf{3 * par + 1}", f"psf{3 * par + 2}"
                            TK = pst[0][:, par * 128:(par + 1) * 128]
                            TE = pst[1][:, par * 256:(par + 1) * 256]
                            kTK, kTE = f"pstk{par}", f"pste{par}"
                            p.op("dve", lambda e: e.tensor_tensor_scan(out=gc[:], data0=ones_f, data1=gb[:, sl], initial=0.0,
                                                                         op0=ALU.mult, op1=ALU.add), r=["g_g", "cst"], w=[f"g_gc{par}"])
                            gl = gc[:, 127:128]
                            p.act(sm[:, 2:3], gl, AF.Exp, r=[f"g_gc{par}"], w=[f"g_dec{par}"])
                            if d_ == 0:
                                p.act(ex[:, 0, :], gc[:], AF.Exp, r=[f"g_gc{par}"], w=[f"g_ex{par}"])
                                p.act(ex[:, 1, :], gc[:], AF.Exp, r=[f"g_gc{par}"], w=[f"g_ex{par}"], scale=-1.0)
                                p.act(ex[:, 2, :], gc[:], AF.Exp, r=[f"g_gc{par}"], w=[f"g_ex{par}"], scale=-1.0, bias=gl)
                            else:
                                p.tt("dve", Hh[:], gc[:], gb[:, sl], ALU.subtract, r=[f"g_gc{par}", "g_g"], w=[f"g_H{par}"])
                                p.ts("dve", sm[:, 1:2], gl, -1.0, None, ALU.mult, r=[f"g_gc{par}"], w=[f"g_ngl{par}"])
                                p.act(ex[:, 0, :], Hh[:], AF.Exp, r=[f"g_H{par}", f"g_gc{par}"], w=[f"g_ex{par}"], scale=-1.0, bias=gl)
                                p.act(ex[:, 1, :], Hh[:], AF.Exp, r=[f"g_H{par}", f"g_ngl{par}"], w=[f"g_ex{par}"], bias=sm[:, 1:2])
                                p.act(ex[:, 2, :], Hh[:], AF.Exp, r=[f"g_H{par}"], w=[f"g_ex{par}"])
                            p.tt("dve", qkb[:, 0, :], qb[:, sl], ex[:, 0, :], ALU.mult, r=["g_q", f"g_ex{par}"], w=[f"g_qe{par}"])
                            p.tt("pool", qkb[:, 1, :], kb[:, sl], ex[:, 1, :], ALU.mult, r=["g_k", f"g_ex{par}"], w=[f"g_ke{par}"])
                            p.tt("dve", qkb[:, 2, :], kb[:, sl], ex[:, 2, :], ALU.mult, r=["g_k", f"g_ex{par}"], w=[f"g_kd{par}"])
                            p.tr(TK, qkb[:, 2, :], ident_b, r=[f"g_kd{par}", "cstb"], w=[kTK])
                            p.copy("act", kdT[:], TK, r=[kTK], w=[f"g_kdT{par}"])
                            for hh in range(2):
                                b0 = 64 * hh
                                p.mm(PA[:, hh * 128:(hh + 1) * 128], qkb[b0:b0 + 64, 1, :], qkb[b0:b0 + 64, 0, :], True, True,
                                     r=[f"g_ke{par}", f"g_qe{par}"], w=[kPA])
                                p.tt("dve", attm[hh][:], PA[:, hh * 128:(hh + 1) * 128], msk, ALU.mult,
                                     r=[kPA, "cst"], w=[f"g_att{2 * par + hh}"])
                                p.mm(PO[:, hh * 128:(hh + 1) * 128], attm[hh][:], vb[:, c, hh * 128:(hh + 1) * 128], True, False,
                                     r=[f"g_att{2 * par + hh}", "g_v"], w=[kPO])
                                p.mm(PO[:, hh * 128:(hh + 1) * 128], qkb[b0:b0 + 64, 0, :], Sbf[b0:b0 + 64, :], False, True,
                                     r=[f"g_qe{par}", "g_Sb"], w=[kPO])
                            p.mm(PD[:, 0:256], kdT[:], vb[:, c, :], True, True, r=[f"g_kdT{par}", "g_v"], w=[kPD])
                            if d_ == 0:
                                p.copy("act", ob[:], PO[:, 0:256], r=[kPO], w=[f"g_o{par}"])
                                p.dma(S["go"][tok:tok + 128, hp * 256:(hp + 1) * 256], ob[:], r=[f"g_o{par}"], w=["go"])
                            else:
                                p.dma(of[:], S["go"][tok:tok + 128, hp * 256:(hp + 1) * 256], r=["go"], w=[f"g_of{par}"])
                                p.dma(rr[:], S["gr"][tok:tok + 128, hp * 256:(hp + 1) * 256], r=["tokdst"], w=[f"g_r{par}"])
                                p.tt("dve", ob[:], PO[:, 0:256], of[:], ALU.add, r=[kPO, f"g_of{par}"], w=[f"g_o{par}"])
                                for hh in range(2):
                                    oh = ob[:, hh * 128:(hh + 1) * 128]
                                    p.act(junk[:], oh, AF.Square, r=[f"g_o{par}"], w=[f"g_junk{par}", f"g_ss{par}"], accum=sm[:, 4 + hh:5 + hh])
                                p.act(sm[:, 6:8], sm[:, 4:6], AF.Sqrt, r=[f"g_ss{par}"], w=[f"g_sd{par}"], scale=1.0 / 128, bias=EPS)
                                p.op("dve", lambda e: e.reciprocal(sm[:, 4:6], sm[:, 6:8]), r=[f"g_sd{par}"], w=[f"g_rs{par}"])
                                for hh in range(2):
                                    oh = ob[:, hh * 128:(hh + 1) * 128]
                                    p.stt(oh, oh, sm[:, 4 + hh:5 + hh], gn[:], ALU.mult, ALU.mult, r=[f"g_o{par}", f"g_rs{par}", "g_gn"], w=[f"g_o{par}"])
                                p.tt("dve", outb[:], ob[:], rr[:], ALU.mult, r=[f"g_o{par}", f"g_r{par}"], w=[f"g_outb{par}"])
                                for hh in range(2):
                                    p.tr(TE[:, hh * 128:(hh + 1) * 128], outb[:, hh * 128:(hh + 1) * 128], ident_b,
                                         r=[f"g_outb{par}", "cstb"], w=[kTE])
                                p.copy("act", mT[:], TE, r=[kTE], w=[f"g_mT{par}"])
                                for hh in range(2):
                                    f0 = (hp * 2 + hh) * 128
                                    p.dma(S["mixT"][f0:f0 + 128, tok:tok + 128], mT[:, hh * 128:(hh + 1) * 128], r=[f"g_mT{par}"], w=["mixT"])
                            for hh in range(2):
                                b0 = 64 * hh
                                p.stt(Sst[b0:b0 + 64, :], Sst[b0:b0 + 64, :], sm[b0:b0 + 64, 2:3], PD[b0:b0 + 64, hh * 128:(hh + 1) * 128],
                                      ALU.mult, ALU.add, r=["g_S", f"g_dec{par}", kPD], w=["g_S"])
                            p.copy("act", Sbf[:], Sst[:], r=["g_S"], w=["g_Sb"])
            p.barrier()
        if "gla" in dbg:
            return finish(["mixT"])

        with ExitStack() as st:
            BL = min(2048, cfg.seq)
            blocks = [(0, cfg.ctx)] + [(cfg.ctx + i * BL, BL) for i in range(cfg.seq // BL)]
            xp = sb("l_xp", [128, BL + 3], stack=st)
            xc = sb("l_xc", [128, BL], stack=st)
            xcb = sb("l_xcb", [128, BL], BF16, stack=st)
            aa = sb("l_aa", [128, BL], stack=st)
            bb = sb("l_bb", [128, BL], stack=st)
            hh_ = sb("l_hh", [128, BL], stack=st)
            yb = sb("l_yb", [128, BL], stack=st)
            hfb = sb("l_hfb", [128, BL], stack=st)
            lob = sb("l_ob", [128, BL], BF16, stack=st)
            tmp = [sb(f"l_t{i}", [128, 512], stack=st) for i in range(4)]
            hprev = sb("l_hp", [128, 1], stack=st)
            cw_s = sb("l_cw", [128, 4, 4], stack=st)
            cb_s = sb("l_cb", [128, 4], stack=st)
            ba_s = sb("l_ba", [128, 8], stack=st)
            bi_s = sb("l_bi", [128, 8], stack=st)
            c8 = sb("l_c8", [128, 8], stack=st)
            wa_s = sb("l_wa", [128, 2, 4, 128], BF16, stack=st)
            wi_s = sb("l_wi", [128, 2, 4, 128], BF16, stack=st)
            p.dma(cw_s[:], I["cw"][l], w=["l_cw"])
            p.dma(cb_s[:], I["cb"][l], w=["l_cb"])
            p.dma(ba_s[:], I["ba"][l], w=["l_ba"])
            p.dma(bi_s[:], I["bi"][l], w=["l_bi"])
            p.dma(c8[:], I["lam"][l], w=["l_c8"])
            p.dma(wa_s[:], I["wa"][l].rearrange("d n c o -> c d n o"), w=["l_wa"], q="pool")
            p.dma(wi_s[:], I["wi"][l].rearrange("d n c o -> c d n o"), w=["l_wi"], q="pool")
            p.act(c8[:], c8[:], AF.Sigmoid, r=["l_c8"], w=["l_c8"])
            p.act(c8[:], c8[:], AF.Ln, r=["l_c8"], w=["l_c8"])
            p.ts("dve", c8[:], c8[:], 8.0, None, ALU.mult, r=["l_c8"], w=["l_c8"])
            for n in range(4):
                for d_ in range(2):
                    order = blocks if d_ == 0 else [blocks[0]] + blocks[:0:-1]
                    p.op("dve", lambda e: e.memset(hprev[:], 0.0), w=["l_hp"])
                    for (t0, ntok) in order:
                        seg_lo, seg_hi = (0, cfg.ctx) if t0 < cfg.ctx else (cfg.ctx, NT)
                        p.op("pool", lambda e: e.memset(xp[:], 0.0), w=["l_xp"])
                        lo, hi = max(t0 - 1, seg_lo), min(t0 + ntok + 2, seg_hi)
                        p.dma(xp[:, lo - (t0 - 1):hi - (t0 - 1)], S["lxT"][n * 128:(n + 1) * 128, lo:hi], r=["lxy"], w=["l_xp"])
                        p.ts("dve", xc[:, 0:ntok], xp[:, 0:ntok], cw_s[:, n, 0:1], cb_s[:, n:n + 1], ALU.mult, ALU.add,
                             r=["l_xp", "l_cw", "l_cb"], w=["l_xc"])
                        for j in range(1, 4):
                            p.stt(xc[:, 0:ntok], xp[:, j:j + ntok], cw_s[:, n, j:j + 1], xc[:, 0:ntok], ALU.mult, ALU.add,
                                  r=["l_xp", "l_cw", "l_xc"], w=["l_xc"])
                        p.copy("act", xcb[:, 0:ntok], xc[:, 0:ntok], r=["l_xc"], w=["l_xcb"])
                        SUB = min(512, ntok)
                        for s0 in range(0, ntok, SUB):
                            ss_ = slice(s0, s0 + SUB)
                            p.mm(psf[0][:, 0:SUB], wa_s[:, d_, n, :], xcb[:, ss_], True, True, r=["l_wa", "l_xcb"], w=["psf0"])
                            p.mm(psf[1][:, 0:SUB], wi_s[:, d_, n, :], xcb[:, ss_], True, True, r=["l_wi", "l_xcb"], w=["psf1"])
                            p.act(tmp[0][:, 0:SUB], psf[0][:, 0:SUB], AF.Sigmoid, r=["psf0", "l_ba"], w=["l_t0"],
                                  bias=ba_s[:, d_ * 4 + n:d_ * 4 + n + 1])
                            p.act(tmp[1][:, 0:SUB], psf[1][:, 0:SUB], AF.Sigmoid, r=["psf1", "l_bi"], w=["l_t1"],
                                  bias=bi_s[:, d_ * 4 + n:d_ * 4 + n + 1])
                            p.act(aa[:, ss_], tmp[0][:, 0:SUB], AF.Exp, r=["l_t0", "l_c8"], w=["l_aa"],
                                  scale=c8[:, d_ * 4 + n:d_ * 4 + n + 1])
                            p.stt(tmp[2][:, 0:SUB], aa[:, ss_], -1.0, aa[:, ss_], ALU.mult, ALU.mult, r=["l_aa"], w=["l_t2"])
                            p.act(tmp[2][:, 0:SUB], tmp[2][:, 0:SUB], AF.Sqrt, r=["l_t2"], w=["l_t2"], bias=1.0)
                            p.tt("dve", tmp[3][:, 0:SUB], tmp[1][:, 0:SUB], tmp[2][:, 0:SUB], ALU.mult, r=["l_t1", "l_t2"], w=["l_t3"])
                            p.tt("dve", bb[:, ss_], tmp[3][:, 0:SUB], xc[:, ss_], ALU.mult, r=["l_t3", "l_xc"], w=["l_bb"])
                        if d_ == 0:
                            p.op("dve", lambda e: e.tensor_tensor_scan(out=hh_[:, 0:ntok], data0=aa[:, 0:ntok], data1=bb[:, 0:ntok],
                                                                         initial=hprev[:, 0:1], op0=ALU.mult, op1=ALU.add),
                                 r=["l_aa", "l_bb", "l_hp"], w=["l_hh"])
                            p.copy("dve", hprev[:], hh_[:, ntok - 1:ntok], r=["l_hh"], w=["l_hp"])
                            p.dma(S["hf"][n * 128:(n + 1) * 128, t0:t0 + ntok], hh_[:, 0:ntok], r=["l_hh"], w=["hf"])
                        else:
                            p.op("dve", lambda e: e.tensor_tensor_scan(out=hh_[:, 0:ntok][:, ::-1], data0=aa[:, 0:ntok][:, ::-1],
                                                                         data1=bb[:, 0:ntok][:, ::-1], initial=hprev[:, 0:1],
                                                                         op0=ALU.mult, op1=ALU.add),
                                 r=["l_aa", "l_bb", "l_hp"], w=["l_hh"])
                            p.copy("dve", hprev[:], hh_[:, 0:1], r=["l_hh"], w=["l_hp"])
                            p.dma(hfb[:, 0:ntok], S["hf"][n * 128:(n + 1) * 128, t0:t0 + ntok], r=["hf"], w=["l_hfb"])
                            p.dma(yb[:, 0:ntok], S["lyT"][n * 128:(n + 1) * 128, t0:t0 + ntok], r=["lxy"], w=["l_yb"])
                            p.tt("pool", hfb[:, 0:ntok], hfb[:, 0:ntok], hh_[:, 0:ntok], ALU.add, r=["l_hfb", "l_hh"], w=["l_hfb"])
                            p.tt("dve", xc[:, 0:ntok], yb[:, 0:ntok], yb[:, 0:ntok], ALU.mult, r=["l_yb"], w=["l_xc"])
                            p.ts("dve", xc[:, 0:ntok], xc[:, 0:ntok], 0.044715, 1.0, ALU.mult, ALU.add, r=["l_xc"], w=["l_xc"])
                            p.tt("dve", xc[:, 0:ntok], xc[:, 0:ntok], yb[:, 0:ntok], ALU.mult, r=["l_xc", "l_yb"], w=["l_xc"])
                            p.act(xc[:, 0:ntok], xc[:, 0:ntok], AF.Sigmoid, r=["l_xc"], w=["l_xc"], scale=1.5957691216057308)
                            p.tt("dve", xc[:, 0:ntok], xc[:, 0:ntok], yb[:, 0:ntok], ALU.mult, r=["l_xc", "l_yb"], w=["l_xc"])
                            p.tt("dve", lob[:, 0:ntok], xc[:, 0:ntok], hfb[:, 0:ntok], ALU.mult, r=["l_xc", "l_hfb"], w=["l_ob"])
                            p.dma(S["mixT"][1536 + n * 128:1536 + (n + 1) * 128, t0:t0 + ntok], lob[:, 0:ntok], r=["l_ob"], w=["mixT"])
            p.barrier()
        if "lru" in dbg:
            return finish(["mixT"])

        with ExitStack() as st:
            kT = sb("a_k", [128, NT], BF16, stack=st)
            qT = sb("a_q", [128, NT], BF16, stack=st)
            vv = sb("a_v", [128, NTL, 128], BF16, stack=st)
            btm = sb("a_bt", [128, NCLS, 5, 128], stack=st)
            sTs = [sb(f"a_s{i}", [128, 5 * 128], stack=st) for i in range(2)]
            pTs = [sb(f"a_p{i}", [128, 8 * 128], BF16, stack=st) for i in range(2)]
            rss = [sb(f"a_rs{i}", [128, 128], stack=st) for i in range(2)]
            aob = [sb(f"a_ob{i}", [128, 128], BF16, stack=st) for i in range(2)]
            for hd in range(8):
                for c0 in range(0, NT, 2048):
                    c1 = min(NT, c0 + 2048)
                    p.dma(kT[:, c0:c1], S["nkT"][hd * 128:(hd + 1) * 128, c0:c1], r=["nqk"], w=["a_k"])
                    p.dma(qT[:, c0:c1], S["nqT"][hd * 128:(hd + 1) * 128, c0:c1], r=["nqk"], w=["a_q"])
                for c0 in range(0, NTL, 16):
                    c1 = min(NTL, c0 + 16)
                    p.dma(vv[:, c0:c1, :], S["nv"][c0 * 128:c1 * 128, hd * 128:(hd + 1) * 128].rearrange("(t p) f -> p t f", p=128),
                          r=["tokdst"], w=["a_v"])
                for ci in range(NCLS):
                    p.dma(btm[:, ci, :, :], I["bt"][l, ci, hd], w=["a_bt"])
                def na_geom(t):
                    if t < nct:
                        win, ci = [], 0
                    else:
                        i = t - nct
                        win = [nct + plo_l[i] + c for c in range(nch_l[i])]
                        ci = cls_l[i]
                    return win, ci, win + list(range(nct))

                def na_S(t):
                    sb0 = 3 * (t % 2)
                    win, ci, keys = na_geom(t)
                    for idx, kt in enumerate(keys):
                        bk = sb0 + idx // 4
                        p.mm(psf[bk][:, (idx % 4) * 128:(idx % 4 + 1) * 128], kT[:, kt * 128:(kt + 1) * 128], qT[:, t * 128:(t + 1) * 128],
                             True, True, r=["a_k", "a_q"], w=[f"psf{bk}"])

                def na_soft(t):
                    b = t % 2
                    sb0 = 3 * b
                    sT, pT = sTs[b], pTs[b]
                    ks, kp = f"a_s{b}", f"a_p{b}"
                    win, ci, keys = na_geom(t)
                    nw, nk = len(win), len(keys)
                    for bk_ in range(2):
                        bk = sb0 + bk_
                        a0, a1 = bk_ * 4, min(nw, bk_ * 4 + 4)
                        if a1 > a0:
                            n_ = (a1 - a0) * 128
                            p.tt("dve", sT[:, a0 * 128:a1 * 128], psf[bk][:, 0:n_],
                                 btm[:, ci, a0:a1, :].rearrange("p c q -> p (c q)"), ALU.add, r=[f"psf{bk}", "a_bt"], w=[ks])
                    if nw:
                        p.act(pT[:, 0:nw * 128], sT[:, 0:nw * 128], AF.Exp, r=[ks], w=[kp])
                    for idx in range(nw, nk):
                        bk = sb0 + idx // 4
                        p.act(pT[:, idx * 128:(idx + 1) * 128], psf[bk][:, (idx % 4) * 128:(idx % 4 + 1) * 128], AF.Exp,
                              r=[f"psf{bk}"], w=[kp])

                def na_PV(t):
                    b = t % 2
                    pT, kp = pTs[b], f"a_p{b}"
                    po, pok = psf[3 * b + 2], f"psf{3 * b + 2}"
                    win, ci, keys = na_geom(t)
                    nk = len(keys)
                    for idx, kt in enumerate(keys):
                        p.mm(po[:, 0:128], vv[:, kt, :], pT[:, idx * 128:(idx + 1) * 128], idx == 0, idx == nk - 1,
                             r=["a_v", kp], w=[pok])
                    for idx, kt in enumerate(keys):
                        p.mm(po[:, 128:256], ones_b, pT[:, idx * 128:(idx + 1) * 128], idx == 0, idx == nk - 1,
                             r=["cstb", kp], w=[pok])

                def na_out(t):
                    b = t % 2
                    rs_, kr = rss[b], f"a_rs{b}"
                    po, pok = psf[3 * b + 2], f"psf{3 * b + 2}"
                    p.op("dve", lambda e: e.reciprocal(rs_[:], po[:, 128:256]), r=[pok], w=[kr])
                    p.tt("dve", aob[b][:], po[:, 0:128], rs_[:], ALU.mult, r=[pok, kr], w=[f"a_ob{b}"])
                    p.dma(S["mixT"][512 + hd * 128:512 + (hd + 1) * 128, t * 128:(t + 1) * 128], aob[b][:], r=[f"a_ob{b}"], w=["mixT"])

                na_S(0)
                for t in range(NTL):
                    na_soft(t)
                    if t + 1 < NTL:
                        na_S(t + 1)
                    na_PV(t)
                    if t >= 1:
                        na_out(t - 1)
                na_out(NTL - 1)
            p.barrier()
        if "mix" in dbg:
            return finish(["mixT"])

        with ExitStack() as st:
            wo = sb("o_w", [128, KD, D], BF16, stack=st)
            gbc = sb("o_g", [128, 2, D], stack=st)
            mx_ = [sb(f"o_m{i}", [128, KD, 128], BF16, stack=st) for i in range(2)]
            xt = [sb(f"o_x{i}", [128, D], stack=st) for i in range(2)]
            tq = sb("o_t", [128, 512], stack=st)
            for k0 in range(0, KD, 4):
                p.dma(wo[:, k0:k0 + 4, :], I["w_out"][l].rearrange("(k p) c -> p k c", p=128)[:, k0:k0 + 4, :], w=["o_w"], q="pool")
            for s_ in range(2):
                p.dma(gbc[:, s_, :], S["modsd"][s_:s_ + 1, 2 * D:3 * D].partition_broadcast(128), r=["modsd"], w=["o_g"])
            for t in range(NTL):
                b = t % 2
                s_ = stream_of(t)
                p.dma(mx_[b][:], S["mixT"].rearrange("(k p) n -> p k n", p=128)[:, :, t * 128:(t + 1) * 128], r=["mixT"], w=[f"o_m{b}"])
                p.dma(xt[b][:], S["xcur"][t * 128:(t + 1) * 128, :], r=["xcur"], w=[f"o_x{b}"])
                for cb in range(4):
                    ps, pk = psf[cb], f"psf{cb}"
                    for k in range(KD):
                        p.mm(ps[:, :], mx_[b][:, k, :], wo[:, k, cb * 512:(cb + 1) * 512], k == 0, k == KD - 1, r=[f"o_m{b}", "o_w"], w=[pk])
                    p.tt("dve", tq[:], ps[:, :], gbc[:, s_, cb * 512:(cb + 1) * 512], ALU.mult, r=[pk, "o_g"], w=["o_t"])
                    p.tt("pool", xt[b][:, cb * 512:(cb + 1) * 512], xt[b][:, cb * 512:(cb + 1) * 512], tq[:], ALU.add,
                         r=["o_t", f"o_x{b}"], w=[f"o_x{b}"])
                p.dma(S["xcur"][t * 128:(t + 1) * 128, :], xt[b][:], r=[f"o_x{b}"], w=["xcur"])
            p.barrier()
        if "xmid" in dbg:
            return finish(["xcur"])

        with ExitStack() as st:
            affT = sb("r_affT", [NE, NT], stack=st)
            with ExitStack() as st2:
                wr_s = sb("r_w", [128, KD, NE], BF16, stack=st2)
                lg = sb("r_lg", [128, NE], stack=st2)
                rsm = sb("r_sm", [128, 4], stack=st2)
                p.dma(wr_s[:], I["w_r"][l].rearrange("(k p) e -> p k e", p=128), w=["r_w"], q="pool")

                def router(t, hTt, hkey):
                    for k in range(KD):
                        p.mm(psf[4][:, 0:NE], hTt[:, k, :], wr_s[:, k, :], k == 0, k == KD - 1, r=[hkey, "r_w"], w=["psf4"])
                    p.op("dve", lambda e: e.reduce_max(out=rsm[:, 0:1], in_=psf[4][:, 0:NE], axis=AX.X), r=["psf4"], w=["r_mx"])
                    p.ts("dve", rsm[:, 1:2], rsm[:, 0:1], -1.0, None, ALU.mult, r=["r_mx"], w=["r_nmx"])
                    p.act(lg[:], psf[4][:, 0:NE], AF.Exp, r=["psf4", "r_nmx"], w=["r_lg", "r_sum"], bias=rsm[:, 1:2], accum=rsm[:, 2:3])
                    p.op("dve", lambda e: e.reciprocal(rsm[:, 3:4], rsm[:, 2:3]), r=["r_sum"], w=["r_rs"])
                    p.ts("dve", lg[:], lg[:], rsm[:, 3:4], None, ALU.mult, r=["r_lg", "r_rs"], w=["r_lg"])
                    p.mm(psf[5][0:NE, 0:128], lg[:], ident_f, True, True, r=["r_lg", "cst"], w=["psf5"])
                    p.copy("act", affT[:, t * 128:(t + 1) * 128], psf[5][0:NE, 0:128], r=["psf5"], w=["r_affT"])

                norm_stage(st2, 1, None, dst_xn=S["xn2"], router=router)
                p.barrier()
            with ExitStack() as st2:
                wk = sb("r_wk", [NE, cfg.seq], stack=st2)
                wts = sb("r_wts", [NE, cfg.cap_l], stack=st2)
                ixs = sb("r_ixs", [NE, cfg.cap_l], U32, stack=st2)
                for (c0, n, cap, doff, off) in [(cfg.ctx, cfg.seq, cfg.cap_l, 0, cfg.ctx), (0, cfg.ctx, cfg.cap_c, 1, 0)]:
                    p.copy("dve", wk[:, 0:n], affT[:, c0:c0 + n], r=["r_affT"], w=["r_wk"])
                    for r_ in range(cap // 8):
                        s8 = slice(r_ * 8, r_ * 8 + 8)
                        p.op("dve", lambda e: e.max(out=wts[:, s8], in_=wk[:, 0:n]), r=["r_wk"], w=["r_wts"])
                        p.op("dve", lambda e: e.max_index(out=ixs[:, s8], in_max=wts[:, s8], in_values=wk[:, 0:n]), r=["r_wk", "r_wts"], w=["r_ixs"])
                        p.op("dve", lambda e: e.match_replace(out=wk[:, 0:n], in_to_replace=wts[:, s8], in_values=wk[:, 0:n], imm_value=-1.0),
                             r=["r_wts", "r_wk"], w=["r_wk"])
                    if off:
                        p.ts("dve", ixs[:, 0:cap], ixs[:, 0:cap], float(off), None, ALU.add, r=["r_ixs"], w=["r_ixs"])
                    dI = S["idxd"] if doff == 0 else S["idxc"]
                    dW = S["wtd"] if doff == 0 else S["wtc"]
                    p.dma(dI[:, 0:cap], ixs[:, 0:cap], r=["r_ixs"], w=["idxd"])
                    p.dma(dW[:, 0:cap], wts[:, 0:cap], r=["r_wts"], w=["idxd"])
                p.barrier()
        if "route" in dbg:
            return finish(["idxd", "wtd", "idxc", "wtc"])

        with ExitStack() as st:
            GS = min(1024, cfg.cap_l)
            xsT = sb("e_xsT", [128, KD, GS + cfg.cap_c], BF16, stack=st)
            hid = sb("e_hid", [128, KD, GS + cfg.cap_c], BF16, stack=st)
            wd = sb("e_wd", [128, KD, D], BF16, stack=st)
            wgf = [sb(f"e_wg{i}", [128, KD, 256], BF16, stack=st) for i in range(2)]
            wuf = [sb(f"e_wu{i}", [128, KD, 256], BF16, stack=st) for i in range(2)]
            xs = sb("e_xs", [128, D], BF16, stack=st)
            xr = sb("e_xr", [128, D], stack=st)
            yt = sb("e_yt", [128, 512], stack=st)
            sg2 = [sb(f"e_sg{i}", [128, 512], stack=st) for i in range(2)]
            eit = [0]
            icol = sb("e_ic", [128, 1], U32, stack=st)
            wcol = sb("e_wc", [128, 1], stack=st)
            g2 = sb("e_g2", [128, 2, D], stack=st)
            for s_ in range(2):
                p.dma(g2[:, s_, :], S["modsd"][s_:s_ + 1, 5 * D:6 * D].partition_broadcast(128), r=["modsd"], w=["e_g2"])
            for e_ in range(NE):
                for k0 in range(0, KD, 4):
                    p.dma(wd[:, k0:k0 + 4, :], I["wed"][l, e_].rearrange("(k p) c -> p k c", p=128)[:, k0:k0 + 4, :], w=["e_wd"], q="pool")
                lat = [(0, g0, min(GS, cfg.cap_l - g0)) for g0 in range(0, cfg.cap_l, GS)]
                groups = [[(s_, g0, ns, 0)] for (s_, g0, ns) in lat]
                groups[-1].append((1, 0, cfg.cap_c, lat[-1][2]))
                for segs in groups:
                    ncol = sum(sg_[2] for sg_ in segs)
                    tiles = []
                    for (s_, g0, ns, col0) in segs:
                        for j0 in range(0, ns, 128):
                            tiles.append((s_, g0 + j0, min(128, ns - j0), col0 + j0))
                    for (s_, q0, rows, c0_) in tiles:
                        dI = S["idxd"] if s_ == 0 else S["idxc"]
                        p.dma(icol[0:rows, :], dI[e_, q0:q0 + rows].rearrange("(p o) -> p o", o=1), r=["idxd"], w=["e_ic"])
                        p.op("pool", lambda e: e.indirect_dma_start(out=xs[0:rows, :], out_offset=None, in_=S["xn2"][:, :],
                                                                      in_offset=bass.IndirectOffsetOnAxis(ap=icol[0:rows, 0:1], axis=0)),
                             r=["e_ic", "xn2"], w=["e_xs"], dma=True)
                        for k in range(KD):
                            pb = pst[k // 8]
                            p.tr(pb[:, (k % 8) * 128:(k % 8) * 128 + rows], xs[0:rows, k * 128:(k + 1) * 128], ident_b[0:rows, 0:rows],
                                 r=["e_xs", "cstb"], w=[f"pst{k // 8}"])
                        for k in range(KD):
                            pb = pst[k // 8]
                            src = pb[:, (k % 8) * 128:(k % 8) * 128 + rows]
                            if k % 2 == 0:
                                p.ts("dve", xsT[:, k, c0_:c0_ + rows], src, amul[:, 1, k, s_:s_ + 1], modp[:, 3, k, s_:s_ + 1],
                                     ALU.mult, ALU.add, r=[f"pst{k // 8}"], w=["e_xsT"])
                            else:
                                p.act(xsT[:, k, c0_:c0_ + rows], src, AF.Identity, r=[f"pst{k // 8}"], w=["e_xsT"],
                                      scale=amul[:, 1, k, s_:s_ + 1], bias=modp[:, 3, k, s_:s_ + 1])
                    for f in range(KD):
                        b = (f // 2) % 2
                        fo = (f % 2) * 128
                        if f % 2 == 0:
                            p.dma(wgf[b][:], I["weg"][l, e_].rearrange("(k p) c -> p k c", p=128)[:, :, f * 128:(f + 2) * 128], w=[f"e_wg{b}"], q="pool")
                            p.dma(wuf[b][:], I["weu"][l, e_].rearrange("(k p) c -> p k c", p=128)[:, :, f * 128:(f + 2) * 128], w=[f"e_wu{b}"], q="pool")
                        for s0 in range(0, ncol, 512):
                            n_ = min(512, ncol - s0)
                            eit[0] += 1
                            ia, ib = (0, 1) if eit[0] % 2 == 0 else (4, 5)
                            sgb, sgk = sg2[eit[0] % 2], f"e_sg{eit[0] % 2}"
                            for k in range(KD):
                                p.mm(psf[ia][:, 0:n_], wgf[b][:, k, fo:fo + 128], xsT[:, k, s0:s0 + n_], k == 0, k == KD - 1, r=[f"e_wg{b}", "e_xsT"], w=[f"psf{ia}"])
                            for k in range(KD):
                                p.mm(psf[ib][:, 0:n_], wuf[b][:, k, fo:fo + 128], xsT[:, k, s0:s0 + n_], k == 0, k == KD - 1, r=[f"e_wu{b}", "e_xsT"], w=[f"psf{ib}"])
                            p.act(sgb[:, 0:n_], psf[ia][:, 0:n_], AF.Silu, r=[f"psf{ia}"], w=[sgk])
                            p.tt("dve", hid[:, f, s0:s0 + n_], sgb[:, 0:n_], psf[ib][:, 0:n_], ALU.mult, r=[sgk, f"psf{ib}"], w=["e_hid"])
                    for (s_, q0, rows, c0_) in tiles:
                        dI = S["idxd"] if s_ == 0 else S["idxc"]
                        dW = S["wtd"] if s_ == 0 else S["wtc"]
                        p.dma(icol[0:rows, :], dI[e_, q0:q0 + rows].rearrange("(p o) -> p o", o=1), r=["idxd"], w=["e_ic"])
                        p.dma(wcol[0:rows, :], dW[e_, q0:q0 + rows].rearrange("(p o) -> p o", o=1), r=["idxd"], w=["e_wc"])
                        p.op("pool", lambda e: e.indirect_dma_start(out=xr[0:rows, :], out_offset=None, in_=S["xcur"][:, :],
                                                                      in_offset=bass.IndirectOffsetOnAxis(ap=icol[0:rows, 0:1], axis=0)),
                             r=["e_ic", "xcur"], w=["e_xr"], dma=True)
                        for cb in range(4):
                            ps, pk = psf[2 + cb % 2], f"psf{2 + cb % 2}"
                            for f in range(KD):
                                p.mm(ps[0:rows, :], hid[:, f, c0_:c0_ + rows], wd[:, f, cb * 512:(cb + 1) * 512], f == 0, f == KD - 1,
                                     r=["e_hid", "e_wd"], w=[pk])
                            p.stt(yt[0:rows, :], ps[0:rows, :], wcol[0:rows, 0:1], g2[0:rows, s_, cb * 512:(cb + 1) * 512], ALU.mult, ALU.mult,
                                  r=[pk, "e_wc", "e_g2"], w=["e_yt"])
                            p.tt("pool", xr[0:rows, cb * 512:(cb + 1) * 512], xr[0:rows, cb * 512:(cb + 1) * 512], yt[0:rows, :], ALU.add,
                                 r=["e_yt", "e_xr"], w=["e_xr"])
                        p.op("pool", lambda e: e.indirect_dma_start(out=S["xcur"][:, :],
                                                                      out_offset=bass.IndirectOffsetOnAxis(ap=icol[0:rows, 0:1], axis=0),
                                                                      in_=xr[0:rows, :], in_offset=None),
                             r=["e_ic", "e_xr"], w=["xcur"], dma=True)
            p.barrier()
        if "layer" in dbg:
            return finish(["xcur"])

    big_copy(yout, S["xcur"][cfg.ctx:, :], cfg.seq, key="yout")
    p.barrier()
    es.close()
    return nc


def prep_inputs(cfg, inp):
    L = cfg.depth
    f = lambda a: np.ascontiguousarray(np.asarray(a, dtype=np.float32))
    pk = lambda v: f(np.asarray(v).reshape(-1, 128).T)
    m = {}
    m["xin"] = f(np.concatenate([inp["ctx"][0], inp["x"][0]], 0))
    m["cT"] = f(np.stack([pk(inp["c"][0]), pk(inp["c_ctx"])], -1))
    m["w_mod"] = f(inp["w_mod"])
    m["b_mod"] = f(inp["b_mod"]).reshape(L, 1, -1)
    m["n1g"] = f(np.stack([pk(inp["norm1_g"][l]) for l in range(L)]))
    m["n2g"] = f(np.stack([pk(inp["norm2_g"][l]) for l in range(L)]))
    m["w_in"] = f(inp["w_in"])
    m["wg"] = f(inp["gla_w_gate"])
    m["bg"] = f(np.stack([np.concatenate([pk(inp["gla_b_gate"][l][d]) for d in range(2)], 1) for l in range(L)]))
    m["gng"] = f(inp["gla_norm_g"]).reshape(L, 1, 128)
    m["nqg"] = f(inp["na_q_norm_g"]).reshape(L, 128, 1)
    m["nkg"] = f(inp["na_k_norm_g"]).reshape(L, 128, 1)
    bts = []
    for l in range(L):
        bt, _, _, _ = _na_masks(cfg, np.asarray(inp["na_rpb"][l], np.float32))
        bts.append(bt.transpose(0, 1, 3, 2, 4))
    m["bt"] = f(np.stack(bts))
    m["cw"] = f(np.stack([np.asarray(inp["lru_conv_w"][l]).reshape(4, 4, 128).transpose(2, 1, 0) for l in range(L)]))
    m["cb"] = f(np.stack([pk(inp["lru_conv_b"][l]) for l in range(L)]))
    m["wa"] = f(inp["lru_w_a"])
    m["wi"] = f(inp["lru_w_i"])
    cat2 = lambda a, l: np.concatenate([pk(a[l][d]) for d in range(2)], 1)
    m["ba"] = f(np.stack([cat2(inp["lru_b_a"], l) for l in range(L)]))
    m["bi"] = f(np.stack([cat2(inp["lru_b_i"], l) for l in range(L)]))
    m["lam"] = f(np.stack([cat2(inp["lru_lambda"], l) for l in range(L)]))
    m["w_out"] = f(inp["w_out"])
    m["w_r"] = f(inp["w_router"])
    m["weg"] = f(inp["w_exp_gate"])
    m["weu"] = f(inp["w_exp_up"])
    m["wed"] = f(inp["w_exp_down"])
    m["rope"] = _rope_tables(cfg)
    tri = np.tril(np.ones((128, 128), np.float32))
    m["consts"] = f(np.stack([np.eye(128), np.ones((128, 128)), tri.T, tri, np.zeros((128, 128)), np.zeros((128, 128))]))
    return m


def kernel(**inputs):
    cfg = Cfg()
    nc = build_program(cfg)
    m = prep_inputs(cfg, inputs)
    res = run_bass_kernel_spmd(nc, [m], core_ids=[0])
    return np.asarray(res.results[0]["yout"], dtype=np.float32).reshape(1, cfg.seq, D)
```

```python
import numpy as np
import concourse.bass as bass
import concourse.mybir as mybir
from concourse.bass_utils import run_bass_kernel_spmd

F32 = mybir.dt.float32
BF16 = mybir.dt.bfloat16
U32 = mybir.dt.uint32
AF = mybir.ActivationFunctionType
ALU = mybir.AluOpType
AX = mybir.AxisListType

D = 2048
KD = D // 128
EPS = 1e-6
GRID_W = 64
D_IN = 5664
C_Q, C_K, C_V, C_R, C_LF, C_LB, C_NQ, C_NK, C_NV, C_LX, C_LY = 0, 256, 512, 1024, 1536, 1552, 1568, 2592, 3616, 4640, 5152


class Cfg:
    def __init__(self, seq=16384, ctx=256, depth=2, ne=16):
        self.seq, self.ctx, self.depth, self.ne = seq, ctx, depth, ne
        self.nt = seq + ctx
        self.ntl = self.nt // 128
        self.rows = seq // GRID_W
        self.cap_l = 2 * seq // ne
        self.cap_c = 2 * ctx // ne


class P:
    def __init__(self, nc):
        self.nc = nc
        self.eng = {"pe": nc.tensor, "dve": nc.vector, "act": nc.scalar, "pool": nc.gpsimd, "sp": nc.sync}
        self.sem = {}
        self.cnt = {e: 0 for e in self.eng}
        self.waited = {e: {} for e in self.eng}
        self.res = {}
        self.dq = {e: 0 for e in self.eng}
        self.NDS = 16
        for e in self.eng:
            self.sem["c_" + e] = nc.semaphore("c_" + e).__enter__()
            for i in range(self.NDS):
                self.sem[f"d_{e}_{i}"] = nc.semaphore(f"d_{e}_{i}").__enter__()

    def _need(self, eng, tok, waits):
        if tok is None:
            return
        name, val = tok
        if eng == "pe" and name == "c_pe":
            return
        if self.waited[eng].get(name, 0) >= val:
            return
        self.waited[eng][name] = val
        waits.append(tok)

    def op(self, eng, fn, r=(), w=(), dma=False, sig=True):
        waits = []
        for k in r:
            st = self.res.get(k)
            if st:
                self._need(eng, st[0], waits)
        for k in w:
            st = self.res.get(k)
            if st:
                self._need(eng, st[0], waits)
                for t in st[1].items():
                    self._need(eng, t, waits)
        if dma:
            j = self.dq[eng]
            self.dq[eng] += 1
            name = f"d_{eng}_{j % self.NDS}"
            val = 16 * (j // self.NDS + 1)
            if j >= self.NDS:
                self._need(eng, (name, val - 16), waits)
            inc = 16
        elif not sig:
            name, val, inc = "c_" + eng, self.cnt[eng] + 1, 0
        else:
            self.cnt[eng] += 1
            name, val, inc = "c_" + eng, self.cnt[eng], 1
        e = self.eng[eng]
        for (n, v) in waits:
            e.wait_ge(self.sem[n], v)
        ins = fn(e)
        if inc:
            ins.then_inc(self.sem[name], inc)
        tok = (name, val)
        for k in r:
            st = self.res.setdefault(k, [None, {}])
            st[1][name] = max(st[1].get(name, 0), val)
        for k in w:
            self.res[k] = [tok, {}]
        return tok

    def barrier(self):
        toks = []
        for e in self.eng:
            if self.cnt[e]:
                toks.append(("c_" + e, self.cnt[e]))
            j = self.dq[e]
            for i in range(self.NDS):
                n = (j - 1 - i)
                if n >= 0:
                    toks.append((f"d_{e}_{n % self.NDS}", 16 * (n // self.NDS + 1)))
        for e in self.eng:
            ws = []
            for t in toks:
                self._need(e, t, ws)
            for (n, v) in ws:
                self.eng[e].wait_ge(self.sem[n], v)

    def dma(self, out, in_, r=(), w=(), q="sp", **kw):
        return self.op(q, lambda e: e.dma_start(out=out, in_=in_, **kw), r, w, dma=True)

    def mm(self, out, lhsT, rhs, start, stop, r=(), w=()):
        return self.op("pe", lambda e: e.matmul(out, lhsT, rhs, start=start, stop=stop), r, w, sig=bool(stop))

    def tr(self, out, in_, ident, r=(), w=()):
        return self.op("pe", lambda e: e.transpose(out, in_, ident), r, w)

    def act(self, out, in_, func, r=(), w=(), bias=0.0, scale=1.0, accum=None):
        if accum is None:
            return self.op("act", lambda e: e.activation(out=out, in_=in_, func=func, bias=bias, scale=scale), r, w)
        return self.op("act", lambda e: e.activation(out=out, in_=in_, func=func, bias=bias, scale=scale,
                                                       accum_out=accum), r, w)

    def ts(self, eng, out, in0, s1, s2, op0, op1=None, r=(), w=(), accum=None):
        if op1 is None:
            return self.op(eng, lambda e: e.tensor_scalar(out, in0, s1, None, op0), r, w)
        if accum is not None:
            return self.op(eng, lambda e: e.tensor_scalar(out, in0, s1, s2, op0, op1, accum), r, w)
        return self.op(eng, lambda e: e.tensor_scalar(out, in0, s1, s2, op0, op1), r, w)

    def tt(self, eng, out, in0, in1, op, r=(), w=()):
        return self.op(eng, lambda e: e.tensor_tensor(out, in0, in1, op), r, w)

    def stt(self, out, in0, scalar, in1, op0, op1, r=(), w=()):
        return self.op("dve", lambda e: e.scalar_tensor_tensor(out, in0, scalar, in1, op0, op1), r, w)

    def copy(self, eng, out, in_, r=(), w=()):
        if eng == "act":
            return self.op("act", lambda e: e.copy(out, in_), r, w)
        return self.op(eng, lambda e: e.tensor_copy(out, in_), r, w)


def _na_tile_geom(cfg):
    rows = cfg.rows
    rs = lambda r: min(max(r - 4, 0), rows - 8)
    plo, nch, cls, defs = [], [], [], {}
    for i in range(rows // 2):
        a, b = rs(2 * i), rs(2 * i + 1)
        lo, hi = a // 2, (b + 7) // 2
        key = (a - 2 * i, b - 2 * i - 1, lo - i, hi - lo + 1)
        if key not in defs:
            defs[key] = len(defs)
        plo.append(lo)
        nch.append(hi - lo + 1)
        cls.append(defs[key])
    return plo, nch, cls, defs


def _na_masks(cfg, rpb):
    plo, nch, cls, defs = _na_tile_geom(cfg)
    ncls = len(defs)
    out = np.full((ncls, 8, 5, 128, 128), -30000.0, np.float32)
    kk = np.arange(128)
    kdr, kc = kk // 64, kk % 64
    qdr, qc = kk // 64, kk % 64
    cs = np.clip(qc - 8, 0, GRID_W - 16)
    for key, ci in defs.items():
        a_rel, b_rel, lo_rel, n = key
        for c in range(n):
            krow = 2 * (lo_rel + c) + kdr
            rs_q = np.where(qdr == 0, a_rel, b_rel + 1)
            qrow = qdr
            inr = (krow[:, None] >= rs_q[None, :]) & (krow[:, None] <= rs_q[None, :] + 7)
            inc = (kc[:, None] >= cs[None, :]) & (kc[:, None] <= cs[None, :] + 15)
            ri = np.clip(krow[:, None] - qrow[None, :] + 7, 0, 14)
            cix = np.clip(kc[:, None] - qc[None, :] + 15, 0, 30)
            m = inr & inc
            for h in range(8):
                out[ci, h, c] = np.where(m, rpb[h][ri, cix], np.float32(-30000.0))
    return out, plo, nch, cls


def _rope_tables(cfg):
    pos = np.arange(cfg.seq)
    row = (pos // GRID_W).astype(np.float32)
    col = (pos % GRID_W).astype(np.float32)
    nf = 16
    inv = (np.float32(10000.0) ** (-np.arange(nf, dtype=np.float32) / np.float32(nf))).astype(np.float32)
    ang = np.concatenate([row[:, None] * inv, col[:, None] * inv], axis=-1).astype(np.float32)
    cos, sin = np.cos(ang).astype(np.float32), np.sin(ang).astype(np.float32)
    c_full = np.concatenate([np.ones((cfg.ctx, 32), np.float32), cos], 0)
    s_full = np.concatenate([np.zeros((cfg.ctx, 32), np.float32), sin], 0)
    p = np.arange(128)
    f = p % 32
    sign = np.where((p % 64) < 32, -1.0, 1.0).astype(np.float32)
    ck = c_full[:, f].T.copy()
    sk = (s_full[:, f].T * sign[:, None]).copy()
    sc = np.float32(64 ** -0.5)
    return np.stack([ck * sc, sk * sc, ck, sk]).astype(np.float32)


def build_program(cfg, dbg=()):
    nc = bass.Bass("TRN2", target_bir_lowering=False)
    NT, NTL, L, NE = cfg.nt, cfg.ntl, cfg.depth, cfg.ne
    plo_l, nch_l, cls_l, cdefs = _na_tile_geom(cfg)
    NCLS = len(cdefs)

    def din(name, shape, dt=F32):
        return nc.dram_tensor(name, list(shape), dt, kind="ExternalInput").ap()

    def dscr(name, shape, dt=F32):
        return nc.dram_tensor(name, list(shape), dt, kind="Internal").ap()

    I = dict(
        xin=din("xin", [NT, D]), cT=din("cT", [128, KD, 2]),
        w_mod=din("w_mod", [L, D, 6 * D]), b_mod=din("b_mod", [L, 1, 6 * D]),
        n1g=din("n1g", [L, 128, KD]), n2g=din("n2g", [L, 128, KD]),
        w_in=din("w_in", [L, D, D_IN]), wg=din("wg", [L, 2, 16, 256]), bg=din("bg", [L, 128, 4]),
        gng=din("gng", [L, 1, 128]), nqg=din("nqg", [L, 128, 1]), nkg=din("nkg", [L, 128, 1]),
        bt=din("bt", [L, NCLS, 8, 128, 5, 128]), cw=din("cw", [L, 128, 4, 4]), cb=din("cb", [L, 128, 4]),
        wa=din("wa", [L, 2, 4, 128, 128]), ba=din("ba", [L, 128, 8]), wi=din("wi", [L, 2, 4, 128, 128]),
        bi=din("bi", [L, 128, 8]), lam=din("lam", [L, 128, 8]),
        w_out=din("w_out", [L, D, D]), w_r=din("w_r", [L, D, NE]),
        weg=din("weg", [L, NE, D, D]), weu=din("weu", [L, NE, D, D]), wed=din("wed", [L, NE, D, D]),
        rope=din("rope", [4, 128, NT]), consts=din("consts", [6, 128, 128]),
    )
    yout = nc.dram_tensor("yout", [cfg.seq, D], F32, kind="ExternalOutput").ap()
    dbg_out = {}
    S = dict(
        xcur=dscr("xcur", [NT, D]), modsd=dscr("modsd", [2, 6 * D]), hT=dscr("hTd", [D, NT], BF16),
        gqT=dscr("gqT", [256, NT], BF16), gkT=dscr("gkT", [256, NT], BF16), gv=dscr("gv", [NT, 512], BF16),
        gr=dscr("gr", [NT, 512]), gg=dscr("gg", [2, 256, NT]), go=dscr("go", [NT, 512]),
        nqT=dscr("nqT", [1024, NT], BF16), nkT=dscr("nkT", [1024, NT], BF16), nv=dscr("nv", [NT, 1024], BF16),
        lxT=dscr("lxT", [512, NT]), lyT=dscr("lyT", [512, NT]), hf=dscr("hf", [512, NT]),
        mixT=dscr("mixT", [D, NT], BF16), xn2=dscr("xn2", [NT, D], BF16),
        idxd=dscr("idxd", [NE, cfg.cap_l], U32), wtd=dscr("wtd", [NE, cfg.cap_l]),
        idxc=dscr("idxc", [NE, cfg.cap_c], U32), wtc=dscr("wtc", [NE, cfg.cap_c]),
    )
    p = P(nc)
    from contextlib import ExitStack
    es = ExitStack()

    uid = [0]

    def sb(name, shape, dt=F32, stack=None):
        uid[0] += 1
        return (stack or es).enter_context(nc.sbuf_tensor(f"{name}_u{uid[0]}", list(shape), dt))

    psf = [es.enter_context(nc.psum_tensor(f"psf{i}", [128, 512], F32)) for i in range(6)]
    pst = [es.enter_context(nc.psum_tensor(f"pst{i}", [128, 1024], BF16)) for i in range(2)]
    cst = sb("cst", [128, 6, 128])
    cstb = sb("cstb", [128, 2, 128], BF16)
    p.dma(cst[:], I["consts"].rearrange("c p n -> p c n"), w=["cst"])
    p.copy("dve", cstb[:], cst[:, 0:2, :], r=["cst"], w=["cstb"])
    ident_f, ones_f, maskF, maskB = cst[:, 0, :], cst[:, 1, :], cst[:, 2, :], cst[:, 3, :]
    ident_b, ones_b = cstb[:, 0, :], cstb[:, 1, :]
    modp = sb("modp", [128, 6, KD, 2])
    amul = sb("amul", [128, 2, KD, 2])
    def big_copy(dst, src, rows, step=128, key="bigcopy"):
        for r0 in range(0, rows, step):
            r1 = min(rows, r0 + step)
            p.dma(dst[r0:r1], src[r0:r1], w=[key])

    def finish(names):
        for nm in names:
            o = nc.dram_tensor("dbg_" + nm, list(S[nm].shape), S[nm].dtype, kind="ExternalOutput").ap()
            big_copy(o, S[nm], S[nm].shape[0], key="dbg")
        p.barrier()
        return nc

    big_copy(S["xcur"], I["xin"], NT, key="xcur")
    p.barrier()

    def stream_of(tile):
        return 1 if tile < cfg.ctx // 128 else 0

    supers = [(0, cfg.ctx, 1)] + [(cfg.ctx + i * 512, 512, 0) for i in range(cfg.seq // 512)]

    for l in range(L):
        with ExitStack() as st:
            sc = sb("m_sc", [128, KD, 2], stack=st)
            wm = sb("m_w", [128, KD, 512], stack=st)
            brow = sb("m_b", [2, 512], stack=st)
            mrow = sb("m_r", [2, 512], stack=st)
            p.dma(sc[:], I["cT"], w=["m_sc"])
            p.act(sc[:], sc[:], AF.Silu, r=["m_sc"], w=["m_sc"])
            for cb in range(24):
                p.dma(wm[:], I["w_mod"][l].rearrange("(k p) c -> p k c", p=128)[:, :, cb * 512:(cb + 1) * 512],
                      w=["m_w"])
                p.dma(brow[:], I["b_mod"][l][:, cb * 512:(cb + 1) * 512].partition_broadcast(2), w=["m_b"])
                for k in range(KD):
                    p.mm(psf[0][0:2, :], sc[:, k, :], wm[:, k, :], k == 0, k == KD - 1, r=["m_sc", "m_w"], w=["psf0"])
                p.tt("dve", mrow[:], psf[0][0:2, :], brow[:], ALU.add, r=["psf0", "m_b"], w=["m_r"])
                p.dma(S["modsd"][:, cb * 512:(cb + 1) * 512], mrow[:], r=["m_r"], w=["modsd"])
            p.barrier()
            for j in range(6):
                for s_ in range(2):
                    p.dma(modp[:, j, :, s_], S["modsd"][s_, j * D:(j + 1) * D].rearrange("(k p) -> p k", p=128),
                          r=["modsd"], w=["modp"], allow_slow_non_contiguous=True)
            g1 = sb("m_g1", [128, KD], stack=st)
            g2 = sb("m_g2", [128, KD], stack=st)
            p.dma(g1[:], I["n1g"][l], w=["m_g1"])
            p.dma(g2[:], I["n2g"][l], w=["m_g2"])
            for s_ in range(2):
                p.stt(amul[:, 0, :, s_], modp[:, 1, :, s_], 1.0, g1[:], ALU.add, ALU.mult, r=["modp", "m_g1"], w=["amul"])
                p.stt(amul[:, 1, :, s_], modp[:, 4, :, s_], 1.0, g2[:], ALU.add, ALU.mult, r=["modp", "m_g2"], w=["amul"])
            p.barrier()

        if "mods" in dbg:
            return finish(["modsd"])

        def norm_stage(st, which, dst_hT, dst_xn=None, router=None):
            xt = [sb(f"n_x{i}", [128, D], stack=st) for i in range(2)]
            junk = sb("n_junk", [128, D], BF16, stack=st)
            xn = [sb(f"n_xn{i}", [128, D], BF16, stack=st) for i in range(2)]
            hTt = [sb(f"n_hT{i}", [128, KD, 128], BF16, stack=st) for i in range(2)]
            stat = sb("n_stat", [128, 4], stack=st)
            for t in range(NTL):
                b = t % 2
                s_ = stream_of(t)
                p.dma(xt[b][:], S["xcur"][t * 128:(t + 1) * 128, :], r=["xcur"], w=[f"n_x{b}"])
                p.act(junk[:], xt[b][:], AF.Square, r=[f"n_x{b}"], w=["n_junk", "n_ss"], accum=stat[:, 0:1])
                p.act(stat[:, 1:2], stat[:, 0:1], AF.Sqrt, r=["n_ss"], w=["n_sd"], scale=1.0 / D, bias=EPS)
                p.op("dve", lambda e: e.reciprocal(stat[:, 2:3], stat[:, 1:2]), r=["n_sd"], w=["n_rs"])
                p.ts("dve", xn[b][:], xt[b][:], stat[:, 2:3], None, ALU.mult, r=[f"n_x{b}", "n_rs"], w=[f"n_xn{b}"])
                if dst_xn is not None:
                    p.dma(dst_xn[t * 128:(t + 1) * 128, :], xn[b][:], r=[f"n_xn{b}"], w=["xn2"])
                for k in range(KD):
                    pb = pst[k // 8]
                    p.tr(pb[:, (k % 8) * 128:(k % 8 + 1) * 128], xn[b][:, k * 128:(k + 1) * 128], ident_b,
                         r=[f"n_xn{b}", "cstb"], w=[f"pst{k // 8}"])
                for k in range(KD):
                    pb = pst[k // 8]
                    src = pb[:, (k % 8) * 128:(k % 8 + 1) * 128]
                    if k % 2 == 0:
                        p.ts("dve", hTt[b][:, k, :], src, amul[:, which, k, s_:s_ + 1], modp[:, 3 * which, k, s_:s_ + 1],
                             ALU.mult, ALU.add, r=[f"pst{k // 8}", "amul", "modp"], w=[f"n_hT{b}"])
                    else:
                        p.act(hTt[b][:, k, :], src, AF.Identity, r=[f"pst{k // 8}", "amul", "modp"], w=[f"n_hT{b}"],
                              scale=amul[:, which, k, s_:s_ + 1], bias=modp[:, 3 * which, k, s_:s_ + 1])
                if dst_hT is not None:
                    p.dma(dst_hT.rearrange("(k p) n -> p k n", p=128)[:, :, t * 128:(t + 1) * 128], hTt[b][:],
                          r=[f"n_hT{b}"], w=["hTd"])
                if router is not None:
                    router(t, hTt[b], f"n_hT{b}")

        with ExitStack() as st:
            norm_stage(st, 0, S["hT"])
            p.barrier()
        if "norm" in dbg:
            return finish(["hT"])

        with ExitStack() as st:
            wb = sb("i_w", [128, KD, 1024], BF16, stack=st)
            wsw = sb("i_wsw", [128, KD, 512], BF16, stack=st)
            hs = [sb(f"i_h{i}", [128, KD, 512], BF16, stack=st) for i in range(2)]
            rp = sb("i_rope", [128, 4, 512], stack=st)
            ev = [sb(f"i_ev{i}", [128, 512], stack=st) for i in range(4)]
            evb = [sb(f"i_evb{i}", [128, 512], BF16, stack=st) for i in range(2)]
            lrt = sb("i_lr", [16, 2, 512], BF16, stack=st)
            wgs = sb("i_wg", [16, 2, 256], BF16, stack=st)
            bgs = sb("i_bg", [128, 4], stack=st)
            nbg = sb("i_nbg", [128, 4], stack=st)
            nqk = sb("i_nqk", [128, 2], stack=st)
            p.dma(wgs[:], I["wg"][l].rearrange("d r c -> r d c"), w=["i_wg"], q="pool")
            p.dma(bgs[:], I["bg"][l], w=["i_bg"])
            p.ts("dve", nbg[:], bgs[:], -1.0, None, ALU.mult, r=["i_bg"], w=["i_nbg"])
            p.dma(nqk[:, 0:1], I["nqg"][l], w=["i_nqk"])
            p.dma(nqk[:, 1:2], I["nkg"][l], w=["i_nqk"])
            p.ts("dve", nqk[:, 0:1], nqk[:, 0:1], float(128 ** -0.5), None, ALU.mult, r=["i_nqk"], w=["i_nqk"])
            wsrc = I["w_in"][l].rearrange("(k p) c -> p k c", p=128)

            def load_w(c0, n):
                for k0 in range(0, KD, 4):
                    p.dma(wb[:, k0:k0 + 4, 0:n], wsrc[:, k0:k0 + 4, c0:c0 + n], w=["i_w"], q="pool")

            def load_h(si):
                t0, ntok, s_ = supers[si]
                b = si % 2
                p.dma(hs[b][:, :, 0:ntok], S["hT"].rearrange("(k p) n -> p k n", p=128)[:, :, t0:t0 + ntok],
                      r=["hTd"], w=[f"i_h{b}"])
                return hs[b], f"i_h{b}", t0, ntok

            def fm_mm(ps, pskey, wtile, wkey, c0, m, h, hkey, ntok):
                for k in range(KD):
                    p.mm(ps[0:m, 0:ntok], wtile[:, k, c0:c0 + m], h[:, k, 0:ntok], k == 0, k == KD - 1,
                         r=[wkey, hkey], w=[pskey])

            load_w(C_Q, 512)
            p.dma(wb[:, :, 512:544], wsrc[:, :, C_LF:C_LF + 32], w=["i_w"], q="pool")
            w5 = wb[:, :, 0:512].rearrange("p k (h two f) -> p k h two f", two=2, f=32)
            s5 = wsw[:].rearrange("p k (h two f) -> p k h two f", two=2, f=32)
            p.copy("dve", s5[:, :, :, 0, :], w5[:, :, :, 1, :], r=["i_w"], w=["i_wsw"])
            p.copy("pool", s5[:, :, :, 1, :], w5[:, :, :, 0, :], r=["i_w"], w=["i_wsw"])
            for si in range(len(supers)):
                h, hkey, t0, ntok = load_h(si)
                p.dma(rp[:, :, 0:ntok], I["rope"].rearrange("c p n -> p c n")[:, :, t0:t0 + ntok], w=["i_rope"])
                for qk in range(2):
                    dst = S["gqT"] if qk == 0 else S["gkT"]
                    for hp in range(2):
                        c0 = qk * 256 + hp * 128
                        par = hp
                        pa_, pb_ = (0, 1) if par == 0 else (4, 5)
                        ea, eb, ec = 2 * par, 2 * par + 1, par
                        fm_mm(psf[pa_], f"psf{pa_}", wb, "i_w", c0, 128, h, hkey, ntok)
                        fm_mm(psf[pb_], f"psf{pb_}", wsw, "i_wsw", c0, 128, h, hkey, ntok)
                        p.tt("dve", ev[ea][:, 0:ntok], psf[pa_][:, 0:ntok], rp[:, 2 * qk, 0:ntok], ALU.mult,
                             r=[f"psf{pa_}", "i_rope"], w=[f"i_ev{ea}"])
                        p.tt("dve", ev[eb][:, 0:ntok], psf[pb_][:, 0:ntok], rp[:, 2 * qk + 1, 0:ntok], ALU.mult,
                             r=[f"psf{pb_}", "i_rope"], w=[f"i_ev{eb}"])
                        p.tt("pool", evb[ec][:, 0:ntok], ev[ea][:, 0:ntok], ev[eb][:, 0:ntok], ALU.add,
                             r=[f"i_ev{ea}", f"i_ev{eb}"], w=[f"i_evb{ec}"])
                        p.dma(dst[hp * 128:(hp + 1) * 128, t0:t0 + ntok], evb[ec][:, 0:ntok], r=[f"i_evb{ec}"], w=["gqk"])
                for d_ in range(2):
                    fm_mm(psf[2], "psf2", wb, "i_w", 512 + 16 * d_, 16, h, hkey, ntok)
                    p.copy("act", lrt[:, d_, 0:ntok], psf[2][0:16, 0:ntok], r=["psf2"], w=["i_lr"])
                for d_ in range(2):
                    for hp in range(2):
                        p.mm(psf[3][:, 0:ntok], wgs[:, d_, hp * 128:(hp + 1) * 128], lrt[:, d_, 0:ntok], True, True,
                             r=["i_wg", "i_lr"], w=["psf3"])
                        p.act(ev[2][:, 0:ntok], psf[3][:, 0:ntok], AF.Exp, r=["psf3", "i_nbg"], w=["i_ev2"],
                              scale=-1.0, bias=nbg[:, d_ * 2 + hp:d_ * 2 + hp + 1])
                        p.act(ev[2][:, 0:ntok], ev[2][:, 0:ntok], AF.Ln, r=["i_ev2"], w=["i_ev2"], bias=1.0)
                        p.ts("dve", ev[3][:, 0:ntok], ev[2][:, 0:ntok], -1.0 / 16.0, None, ALU.mult, r=["i_ev2"], w=["i_ev3"])
                        p.dma(S["gg"][d_, hp * 128:(hp + 1) * 128, t0:t0 + ntok], ev[3][:, 0:ntok], r=["i_ev3"], w=["gg"])

            if "ip1" in dbg:
                return finish(["gqT", "gkT", "gg"])

            def tok_group(c0, ncols, dst, dst_dt_bf, silu):
                load_w(c0, ncols)
                for si in range(len(supers)):
                    h, hkey, t0, ntok = load_h(si)
                    for tt_ in range(ntok // 128):
                        for cb in range(ncols // 512):
                            ps, pk = psf[(tt_ * 2 + cb) % 4], f"psf{(tt_ * 2 + cb) % 4}"
                            for k in range(KD):
                                p.mm(ps[:, :], h[:, k, tt_ * 128:(tt_ + 1) * 128], wb[:, k, cb * 512:(cb + 1) * 512],
                                     k == 0, k == KD - 1, r=["i_w", hkey], w=[pk])
                            tok0 = t0 + tt_ * 128
                            if dst_dt_bf:
                                e_, ek = evb[cb % 2], f"i_evb{cb % 2}"
                                p.copy("act" if cb % 2 else "dve", e_[:], ps[:, :], r=[pk], w=[ek])
                            else:
                                e_, ek = ev[cb % 2], f"i_ev{cb % 2}"
                                p.act(e_[:], ps[:, :], AF.Silu if silu else AF.Copy, r=[pk], w=[ek])
                            p.dma(dst[tok0:tok0 + 128, cb * 512:(cb + 1) * 512], e_[:], r=[ek], w=["tokdst"])

            tok_group(C_V, 512, S["gv"], True, False)
            tok_group(C_R, 512, S["gr"], False, True)
            tok_group(C_NV, 1024, S["nv"], True, False)

            if "ip2" in dbg:
                return finish(["gv", "gr", "nv"])
            for qk in range(2):
                load_w(C_NQ if qk == 0 else C_NK, 1024)
                dst = S["nqT"] if qk == 0 else S["nkT"]
                for si in range(len(supers)):
                    h, hkey, t0, ntok = load_h(si)
                    for hd in range(8):
                        pa_, pb_ = (0, 1) if hd % 2 == 0 else (2, 3)
                        fm_mm(psf[pa_], f"psf{pa_}", wb, "i_w", hd * 128, 128, h, hkey, ntok)
                        p.copy("dve", ev[0][:, 0:ntok], psf[pa_][:, 0:ntok], r=[f"psf{pa_}"], w=["i_ev0"])
                        p.tt("dve", evb[1][:, 0:ntok], ev[0][:, 0:ntok], ev[0][:, 0:ntok], ALU.mult, r=["i_ev0"], w=["i_evb1"])
                        p.mm(psf[pb_][:, 0:ntok], ones_b, evb[1][:, 0:ntok], True, True, r=["cstb", "i_evb1"], w=[f"psf{pb_}"])
                        p.act(ev[2][:, 0:ntok], psf[pb_][:, 0:ntok], AF.Sqrt, r=[f"psf{pb_}"], w=["i_ev2"], scale=1.0 / 128, bias=EPS)
                        p.op("dve", lambda e: e.reciprocal(ev[3][:, 0:ntok], ev[2][:, 0:ntok]), r=["i_ev2"], w=["i_ev3"])
                        p.stt(evb[0][:, 0:ntok], ev[0][:, 0:ntok], nqk[:, qk:qk + 1], ev[3][:, 0:ntok], ALU.mult, ALU.mult,
                              r=["i_ev0", "i_ev3", "i_nqk"], w=["i_evb0"])
                        p.dma(dst[hd * 128:(hd + 1) * 128, t0:t0 + ntok], evb[0][:, 0:ntok], r=["i_evb0"], w=["nqk"])

            if "ip3" in dbg:
                return finish(["nqT", "nkT"])
            load_w(C_LX, 1024)
            for si in range(len(supers)):
                h, hkey, t0, ntok = load_h(si)
                for c in range(8):
                    ps, pk = psf[c % 4], f"psf{c % 4}"
                    fm_mm(ps, pk, wb, "i_w", c * 128, 128, h, hkey, ntok)
                    e_, ek = ev[c % 4], f"i_ev{c % 4}"
                    p.copy("act" if c % 2 else "dve", e_[:, 0:ntok], ps[:, 0:ntok], r=[pk], w=[ek])
                    dst = S["lxT"] if c < 4 else S["lyT"]
                    p.dma(dst[(c % 4) * 128:(c % 4 + 1) * 128, t0:t0 + ntok], e_[:, 0:ntok], r=[ek], w=["lxy"])
            p.barrier()

        if "inproj" in dbg:
            return finish(["gqT", "gkT", "gg", "gv", "gr", "nqT", "nkT", "nv", "lxT", "lyT"])

        nct = cfg.ctx // 128
        with ExitStack() as st:
            BL = min(1024, cfg.seq)
            blocks = [(0, cfg.ctx)] + [(cfg.ctx + i * BL, BL) for i in range(cfg.seq // BL)]
            qb = sb("g_q", [128, BL], BF16, stack=st)
            kb = sb("g_k", [128, BL], BF16, stack=st)
            gb = sb("g_g", [128, BL], stack=st)
            vb = sb("g_v", [128, BL // 128, 256], BF16, stack=st)
            gc = sb("g_gc", [128, 128], stack=st)
            Hh = sb("g_H", [128, 128], stack=st)
            ex = sb("g_ex", [128, 3, 128], stack=st)
            qkb = sb("g_qkb", [128, 3, 128], BF16, stack=st)
            kdT = sb("g_kdT", [128, 128], BF16, stack=st)
            attm = [sb(f"g_att{i}", [128, 128], BF16, stack=st) for i in range(2)]
            Sst = sb("g_S", [128, 128], stack=st)
            Sbf = sb("g_Sb", [128, 128], BF16, stack=st)
            sm = sb("g_sm", [128, 8], stack=st)
            ob = sb("g_o", [128, 256], stack=st)
            of = sb("g_of", [128, 256], stack=st)
            rr = sb("g_r", [128, 256], stack=st)
            gn = sb("g_gn", [128, 128], stack=st)
            outb = sb("g_outb", [128, 256], BF16, stack=st)
            mT = sb("g_mT", [128, 256], BF16, stack=st)
            junk = sb("g_junk", [128, 128], stack=st)
            p.dma(gn[:], I["gng"][l].partition_broadcast(128), w=["g_gn"])
            for hp in range(2):
                for d_ in range(2):
                    p.op("dve", lambda e: e.memset(Sst[:], 0.0), w=["g_S"])
                    p.op("dve", lambda e: e.memset(Sbf[:], 0.0), w=["g_Sb"])
                    order = blocks if d_ == 0 else [blocks[0]] + blocks[:0:-1]
                    msk = maskF if d_ == 0 else maskB
                    for (t0, ntok) in order:
                        p.dma(qb[:, 0:ntok], S["gqT"][hp * 128:(hp + 1) * 128, t0:t0 + ntok], r=["gqk"], w=["g_q"])
                        p.dma(kb[:, 0:ntok], S["gkT"][hp * 128:(hp + 1) * 128, t0:t0 + ntok], r=["gqk"], w=["g_k"])
                        p.dma(gb[:, 0:ntok], S["gg"][d_, hp * 128:(hp + 1) * 128, t0:t0 + ntok], r=["gg"], w=["g_g"])
                        p.dma(vb[:, 0:ntok // 128, :],
                              S["gv"][t0:t0 + ntok, hp * 256:(hp + 1) * 256].rearrange("(c p) f -> p c f", p=128),
                              r=["tokdst"], w=["g_v"])
                        cs_ = list(range(ntok // 128))
                        if d_ == 1:
                            cs_ = cs_[::-1]
                        for c in cs_:
                            sl = slice(c * 128, (c + 1) * 128)
                            tok = t0 + c * 128
                            p.op("dve", lambda e: e.tensor_tensor_scan(out=gc[:], data0=ones_f, data1=gb[:, sl], initial=0.0,
                                                                         op0=ALU.mult, op1=ALU.add), r=["g_g", "cst"], w=["g_gc"])
                            gl = gc[:, 127:128]
                            p.act(sm[:, 2:3], gl, AF.Exp, r=["g_gc"], w=["g_dec"])
                            if d_ == 0:
                                p.act(ex[:, 0, :], gc[:], AF.Exp, r=["g_gc"], w=["g_ex"])
                                p.act(ex[:, 1, :], gc[:], AF.Exp, r=["g_gc"], w=["g_ex"], scale=-1.0)
                                p.act(ex[:, 2, :], gc[:], AF.Exp, r=["g_gc"], w=["g_ex"], scale=-1.0, bias=gl)
                            else:
                                p.tt("dve", Hh[:], gc[:], gb[:, sl], ALU.subtract, r=["g_gc", "g_g"], w=["g_H"])
                                p.ts("dve", sm[:, 1:2], gl, -1.0, None, ALU.mult, r=["g_gc"], w=["g_ngl"])
                                p.act(ex[:, 0, :], Hh[:], AF.Exp, r=["g_H", "g_gc"], w=["g_ex"], scale=-1.0, bias=gl)
                                p.act(ex[:, 1, :], Hh[:], AF.Exp, r=["g_H", "g_ngl"], w=["g_ex"], bias=sm[:, 1:2])
                                p.act(ex[:, 2, :], Hh[:], AF.Exp, r=["g_H"], w=["g_ex"])
                            p.tt("dve", qkb[:, 0, :], qb[:, sl], ex[:, 0, :], ALU.mult, r=["g_q", "g_ex"], w=["g_qe"])
                            p.tt("pool", qkb[:, 1, :], kb[:, sl], ex[:, 1, :], ALU.mult, r=["g_k", "g_ex"], w=["g_ke"])
                            p.tt("dve", qkb[:, 2, :], kb[:, sl], ex[:, 2, :], ALU.mult, r=["g_k", "g_ex"], w=["g_kd"])
                            p.tr(pst[0][:, 0:128], qkb[:, 2, :], ident_b, r=["g_kd", "cstb"], w=["pst0"])
                            p.copy("act", kdT[:], pst[0][:, 0:128], r=["pst0"], w=["g_kdT"])
                            for hh in range(2):
                                b0 = 64 * hh
                                p.mm(psf[0][:, hh * 128:(hh + 1) * 128], qkb[b0:b0 + 64, 1, :], qkb[b0:b0 + 64, 0, :], True, True,
                                     r=["g_ke", "g_qe"], w=["psf0"])
                                p.tt("dve", attm[hh][:], psf[0][:, hh * 128:(hh + 1) * 128], msk, ALU.mult,
                                     r=["psf0", "cst"], w=[f"g_att{hh}"])
                                p.mm(psf[1][:, hh * 128:(hh + 1) * 128], attm[hh][:], vb[:, c, hh * 128:(hh + 1) * 128], True, False,
                                     r=[f"g_att{hh}", "g_v"], w=["psf1"])
                                p.mm(psf[1][:, hh * 128:(hh + 1) * 128], qkb[b0:b0 + 64, 0, :], Sbf[b0:b0 + 64, :], False, True,
                                     r=["g_qe", "g_Sb"], w=["psf1"])
                            p.mm(psf[2][:, 0:256], kdT[:], vb[:, c, :], True, True, r=["g_kdT", "g_v"], w=["psf2"])
                            if d_ == 0:
                                p.copy("act", ob[:], psf[1][:, 0:256], r=["psf1"], w=["g_o"])
                                p.dma(S["go"][tok:tok + 128, hp * 256:(hp + 1) * 256], ob[:], r=["g_o"], w=["go"])
                            else:
                                p.dma(of[:], S["go"][tok:tok + 128, hp * 256:(hp + 1) * 256], r=["go"], w=["g_of"])
                                p.dma(rr[:], S["gr"][tok:tok + 128, hp * 256:(hp + 1) * 256], r=["tokdst"], w=["g_r"])
                                p.tt("dve", ob[:], psf[1][:, 0:256], of[:], ALU.add, r=["psf1", "g_of"], w=["g_o"])
                                for hh in range(2):
                                    oh = ob[:, hh * 128:(hh + 1) * 128]
                                    p.act(junk[:], oh, AF.Square, r=["g_o"], w=["g_junk", "g_ss"], accum=sm[:, 4 + hh:5 + hh])
                                p.act(sm[:, 6:8], sm[:, 4:6], AF.Sqrt, r=["g_ss"], w=["g_sd"], scale=1.0 / 128, bias=EPS)
                                p.op("dve", lambda e: e.reciprocal(sm[:, 4:6], sm[:, 6:8]), r=["g_sd"], w=["g_rs"])
                                for hh in range(2):
                                    oh = ob[:, hh * 128:(hh + 1) * 128]
                                    p.stt(oh, oh, sm[:, 4 + hh:5 + hh], gn[:], ALU.mult, ALU.mult, r=["g_o", "g_rs", "g_gn"], w=["g_o"])
                                p.tt("dve", outb[:], ob[:], rr[:], ALU.mult, r=["g_o", "g_r"], w=["g_outb"])
                                for hh in range(2):
                                    p.tr(pst[1][:, hh * 128:(hh + 1) * 128], outb[:, hh * 128:(hh + 1) * 128], ident_b,
                                         r=["g_outb", "cstb"], w=["pst1"])
                                p.copy("act", mT[:], pst[1][:, 0:256], r=["pst1"], w=["g_mT"])
                                for hh in range(2):
                                    f0 = (hp * 2 + hh) * 128
                                    p.dma(S["mixT"][f0:f0 + 128, tok:tok + 128], mT[:, hh * 128:(hh + 1) * 128], r=["g_mT"], w=["mixT"])
                            for hh in range(2):
                                b0 = 64 * hh
                                p.stt(Sst[b0:b0 + 64, :], Sst[b0:b0 + 64, :], sm[b0:b0 + 64, 2:3], psf[2][b0:b0 + 64, hh * 128:(hh + 1) * 128],
                                      ALU.mult, ALU.add, r=["g_S", "g_dec", "psf2"], w=["g_S"])
                            p.copy("act", Sbf[:], Sst[:], r=["g_S"], w=["g_Sb"])
            p.barrier()
        if "gla" in dbg:
            return finish(["mixT"])

        with ExitStack() as st:
            BL = min(2048, cfg.seq)
            blocks = [(0, cfg.ctx)] + [(cfg.ctx + i * BL, BL) for i in range(cfg.seq // BL)]
            xp = sb("l_xp", [128, BL + 3], stack=st)
            xc = sb("l_xc", [128, BL], stack=st)
            xcb = sb("l_xcb", [128, BL], BF16, stack=st)
            aa = sb("l_aa", [128, BL], stack=st)
            bb = sb("l_bb", [128, BL], stack=st)
            hh_ = sb("l_hh", [128, BL], stack=st)
            yb = sb("l_yb", [128, BL], stack=st)
            hfb = sb("l_hfb", [128, BL], stack=st)
            lob = sb("l_ob", [128, BL], BF16, stack=st)
            tmp = [sb(f"l_t{i}", [128, 512], stack=st) for i in range(4)]
            hprev = sb("l_hp", [128, 1], stack=st)
            cw_s = sb("l_cw", [128, 4, 4], stack=st)
            cb_s = sb("l_cb", [128, 4], stack=st)
            ba_s = sb("l_ba", [128, 8], stack=st)
            bi_s = sb("l_bi", [128, 8], stack=st)
            c8 = sb("l_c8", [128, 8], stack=st)
            wa_s = sb("l_wa", [128, 2, 4, 128], BF16, stack=st)
            wi_s = sb("l_wi", [128, 2, 4, 128], BF16, stack=st)
            p.dma(cw_s[:], I["cw"][l], w=["l_cw"])
            p.dma(cb_s[:], I["cb"][l], w=["l_cb"])
            p.dma(ba_s[:], I["ba"][l], w=["l_ba"])
            p.dma(bi_s[:], I["bi"][l], w=["l_bi"])
            p.dma(c8[:], I["lam"][l], w=["l_c8"])
            p.dma(wa_s[:], I["wa"][l].rearrange("d n c o -> c d n o"), w=["l_wa"], q="pool")
            p.dma(wi_s[:], I["wi"][l].rearrange("d n c o -> c d n o"), w=["l_wi"], q="pool")
            p.act(c8[:], c8[:], AF.Sigmoid, r=["l_c8"], w=["l_c8"])
            p.act(c8[:], c8[:], AF.Ln, r=["l_c8"], w=["l_c8"])
            p.ts("dve", c8[:], c8[:], 8.0, None, ALU.mult, r=["l_c8"], w=["l_c8"])
            for n in range(4):
                for d_ in range(2):
                    order = blocks if d_ == 0 else [blocks[0]] + blocks[:0:-1]
                    p.op("dve", lambda e: e.memset(hprev[:], 0.0), w=["l_hp"])
                    for (t0, ntok) in order:
                        seg_lo, seg_hi = (0, cfg.ctx) if t0 < cfg.ctx else (cfg.ctx, NT)
                        p.op("pool", lambda e: e.memset(xp[:], 0.0), w=["l_xp"])
                        lo, hi = max(t0 - 1, seg_lo), min(t0 + ntok + 2, seg_hi)
                        p.dma(xp[:, lo - (t0 - 1):hi - (t0 - 1)], S["lxT"][n * 128:(n + 1) * 128, lo:hi], r=["lxy"], w=["l_xp"])
                        p.ts("dve", xc[:, 0:ntok], xp[:, 0:ntok], cw_s[:, n, 0:1], cb_s[:, n:n + 1], ALU.mult, ALU.add,
                             r=["l_xp", "l_cw", "l_cb"], w=["l_xc"])
                        for j in range(1, 4):
                            p.stt(xc[:, 0:ntok], xp[:, j:j + ntok], cw_s[:, n, j:j + 1], xc[:, 0:ntok], ALU.mult, ALU.add,
                                  r=["l_xp", "l_cw", "l_xc"], w=["l_xc"])
                        p.copy("act", xcb[:, 0:ntok], xc[:, 0:ntok], r=["l_xc"], w=["l_xcb"])
                        SUB = min(512, ntok)
                        for s0 in range(0, ntok, SUB):
                            ss_ = slice(s0, s0 + SUB)
                            p.mm(psf[0][:, 0:SUB], wa_s[:, d_, n, :], xcb[:, ss_], True, True, r=["l_wa", "l_xcb"], w=["psf0"])
                            p.mm(psf[1][:, 0:SUB], wi_s[:, d_, n, :], xcb[:, ss_], True, True, r=["l_wi", "l_xcb"], w=["psf1"])
                            p.act(tmp[0][:, 0:SUB], psf[0][:, 0:SUB], AF.Sigmoid, r=["psf0", "l_ba"], w=["l_t0"],
                                  bias=ba_s[:, d_ * 4 + n:d_ * 4 + n + 1])
                            p.act(tmp[1][:, 0:SUB], psf[1][:, 0:SUB], AF.Sigmoid, r=["psf1", "l_bi"], w=["l_t1"],
                                  bias=bi_s[:, d_ * 4 + n:d_ * 4 + n + 1])
                            p.act(aa[:, ss_], tmp[0][:, 0:SUB], AF.Exp, r=["l_t0", "l_c8"], w=["l_aa"],
                                  scale=c8[:, d_ * 4 + n:d_ * 4 + n + 1])
                            p.stt(tmp[2][:, 0:SUB], aa[:, ss_], -1.0, aa[:, ss_], ALU.mult, ALU.mult, r=["l_aa"], w=["l_t2"])
                            p.act(tmp[2][:, 0:SUB], tmp[2][:, 0:SUB], AF.Sqrt, r=["l_t2"], w=["l_t2"], bias=1.0)
                            p.tt("dve", tmp[3][:, 0:SUB], tmp[1][:, 0:SUB], tmp[2][:, 0:SUB], ALU.mult, r=["l_t1", "l_t2"], w=["l_t3"])
                            p.tt("dve", bb[:, ss_], tmp[3][:, 0:SUB], xc[:, ss_], ALU.mult, r=["l_t3", "l_xc"], w=["l_bb"])
                        if d_ == 0:
                            p.op("dve", lambda e: e.tensor_tensor_scan(out=hh_[:, 0:ntok], data0=aa[:, 0:ntok], data1=bb[:, 0:ntok],
                                                                         initial=hprev[:, 0:1], op0=ALU.mult, op1=ALU.add),
                                 r=["l_aa", "l_bb", "l_hp"], w=["l_hh"])
                            p.copy("dve", hprev[:], hh_[:, ntok - 1:ntok], r=["l_hh"], w=["l_hp"])
                            p.dma(S["hf"][n * 128:(n + 1) * 128, t0:t0 + ntok], hh_[:, 0:ntok], r=["l_hh"], w=["hf"])
                        else:
                            p.op("dve", lambda e: e.tensor_tensor_scan(out=hh_[:, 0:ntok][:, ::-1], data0=aa[:, 0:ntok][:, ::-1],
                                                                         data1=bb[:, 0:ntok][:, ::-1], initial=hprev[:, 0:1],
                                                                         op0=ALU.mult, op1=ALU.add),
                                 r=["l_aa", "l_bb", "l_hp"], w=["l_hh"])
                            p.copy("dve", hprev[:], hh_[:, 0:1], r=["l_hh"], w=["l_hp"])
                            p.dma(hfb[:, 0:ntok], S["hf"][n * 128:(n + 1) * 128, t0:t0 + ntok], r=["hf"], w=["l_hfb"])
                            p.dma(yb[:, 0:ntok], S["lyT"][n * 128:(n + 1) * 128, t0:t0 + ntok], r=["lxy"], w=["l_yb"])
                            p.tt("pool", hfb[:, 0:ntok], hfb[:, 0:ntok], hh_[:, 0:ntok], ALU.add, r=["l_hfb", "l_hh"], w=["l_hfb"])
                            p.tt("dve", xc[:, 0:ntok], yb[:, 0:ntok], yb[:, 0:ntok], ALU.mult, r=["l_yb"], w=["l_xc"])
                            p.ts("dve", xc[:, 0:ntok], xc[:, 0:ntok], 0.044715, 1.0, ALU.mult, ALU.add, r=["l_xc"], w=["l_xc"])
                            p.tt("dve", xc[:, 0:ntok], xc[:, 0:ntok], yb[:, 0:ntok], ALU.mult, r=["l_xc", "l_yb"], w=["l_xc"])
                            p.act(xc[:, 0:ntok], xc[:, 0:ntok], AF.Sigmoid, r=["l_xc"], w=["l_xc"], scale=1.5957691216057308)
                            p.tt("dve", xc[:, 0:ntok], xc[:, 0:ntok], yb[:, 0:ntok], ALU.mult, r=["l_xc", "l_yb"], w=["l_xc"])
                            p.tt("dve", lob[:, 0:ntok], xc[:, 0:ntok], hfb[:, 0:ntok], ALU.mult, r=["l_xc", "l_hfb"], w=["l_ob"])
                            p.dma(S["mixT"][1536 + n * 128:1536 + (n + 1) * 128, t0:t0 + ntok], lob[:, 0:ntok], r=["l_ob"], w=["mixT"])
            p.barrier()
        if "lru" in dbg:
            return finish(["mixT"])

        with ExitStack() as st:
            kT = sb("a_k", [128, NT], BF16, stack=st)
            qT = sb("a_q", [128, NT], BF16, stack=st)
            vv = sb("a_v", [128, NTL, 128], BF16, stack=st)
            btm = sb("a_bt", [128, NCLS, 5, 128], stack=st)
            sTs = [sb(f"a_s{i}", [128, 5 * 128], stack=st) for i in range(2)]
            pTs = [sb(f"a_p{i}", [128, 8 * 128], BF16, stack=st) for i in range(2)]
            rss = [sb(f"a_rs{i}", [128, 128], stack=st) for i in range(2)]
            aob = [sb(f"a_ob{i}", [128, 128], BF16, stack=st) for i in range(2)]
            for hd in range(8):
                for c0 in range(0, NT, 2048):
                    c1 = min(NT, c0 + 2048)
                    p.dma(kT[:, c0:c1], S["nkT"][hd * 128:(hd + 1) * 128, c0:c1], r=["nqk"], w=["a_k"])
                    p.dma(qT[:, c0:c1], S["nqT"][hd * 128:(hd + 1) * 128, c0:c1], r=["nqk"], w=["a_q"])
                for c0 in range(0, NTL, 16):
                    c1 = min(NTL, c0 + 16)
                    p.dma(vv[:, c0:c1, :], S["nv"][c0 * 128:c1 * 128, hd * 128:(hd + 1) * 128].rearrange("(t p) f -> p t f", p=128),
                          r=["tokdst"], w=["a_v"])
                for ci in range(NCLS):
                    p.dma(btm[:, ci, :, :], I["bt"][l, ci, hd], w=["a_bt"])
                def na_geom(t):
                    if t < nct:
                        win, ci = [], 0
                    else:
                        i = t - nct
                        win = [nct + plo_l[i] + c for c in range(nch_l[i])]
                        ci = cls_l[i]
                    return win, ci, win + list(range(nct))

                def na_S(t):
                    sb0 = 3 * (t % 2)
                    win, ci, keys = na_geom(t)
                    for idx, kt in enumerate(keys):
                        bk = sb0 + idx // 4
                        p.mm(psf[bk][:, (idx % 4) * 128:(idx % 4 + 1) * 128], kT[:, kt * 128:(kt + 1) * 128], qT[:, t * 128:(t + 1) * 128],
                             True, True, r=["a_k", "a_q"], w=[f"psf{bk}"])

                def na_soft(t):
                    b = t % 2
                    sb0 = 3 * b
                    sT, pT = sTs[b], pTs[b]
                    ks, kp = f"a_s{b}", f"a_p{b}"
                    win, ci, keys = na_geom(t)
                    nw, nk = len(win), len(keys)
                    for bk_ in range(2):
                        bk = sb0 + bk_
                        a0, a1 = bk_ * 4, min(nw, bk_ * 4 + 4)
                        if a1 > a0:
                            n_ = (a1 - a0) * 128
                            p.tt("dve", sT[:, a0 * 128:a1 * 128], psf[bk][:, 0:n_],
                                 btm[:, ci, a0:a1, :].rearrange("p c q -> p (c q)"), ALU.add, r=[f"psf{bk}", "a_bt"], w=[ks])
                    if nw:
                        p.act(pT[:, 0:nw * 128], sT[:, 0:nw * 128], AF.Exp, r=[ks], w=[kp])
                    for idx in range(nw, nk):
                        bk = sb0 + idx // 4
                        p.act(pT[:, idx * 128:(idx + 1) * 128], psf[bk][:, (idx % 4) * 128:(idx % 4 + 1) * 128], AF.Exp,
                              r=[f"psf{bk}"], w=[kp])

                def na_PV(t):
                    b = t % 2
                    pT, kp = pTs[b], f"a_p{b}"
                    po, pok = psf[3 * b + 2], f"psf{3 * b + 2}"
                    win, ci, keys = na_geom(t)
                    nk = len(keys)
                    for idx, kt in enumerate(keys):
                        p.mm(po[:, 0:128], vv[:, kt, :], pT[:, idx * 128:(idx + 1) * 128], idx == 0, idx == nk - 1,
                             r=["a_v", kp], w=[pok])
                    for idx, kt in enumerate(keys):
                        p.mm(po[:, 128:256], ones_b, pT[:, idx * 128:(idx + 1) * 128], idx == 0, idx == nk - 1,
                             r=["cstb", kp], w=[pok])

                def na_out(t):
                    b = t % 2
                    rs_, kr = rss[b], f"a_rs{b}"
                    po, pok = psf[3 * b + 2], f"psf{3 * b + 2}"
                    p.op("dve", lambda e: e.reciprocal(rs_[:], po[:, 128:256]), r=[pok], w=[kr])
                    p.tt("dve", aob[b][:], po[:, 0:128], rs_[:], ALU.mult, r=[pok, kr], w=[f"a_ob{b}"])
                    p.dma(S["mixT"][512 + hd * 128:512 + (hd + 1) * 128, t * 128:(t + 1) * 128], aob[b][:], r=[f"a_ob{b}"], w=["mixT"])

                na_S(0)
                for t in range(NTL):
                    na_soft(t)
                    if t + 1 < NTL:
                        na_S(t + 1)
                    na_PV(t)
                    if t >= 1:
                        na_out(t - 1)
                na_out(NTL - 1)
            p.barrier()
        if "mix" in dbg:
            return finish(["mixT"])

        with ExitStack() as st:
            wo = sb("o_w", [128, KD, D], BF16, stack=st)
            gbc = sb("o_g", [128, 2, D], stack=st)
            mx_ = [sb(f"o_m{i}", [128, KD, 128], BF16, stack=st) for i in range(2)]
            xt = [sb(f"o_x{i}", [128, D], stack=st) for i in range(2)]
            tq = sb("o_t", [128, 512], stack=st)
            for k0 in range(0, KD, 4):
                p.dma(wo[:, k0:k0 + 4, :], I["w_out"][l].rearrange("(k p) c -> p k c", p=128)[:, k0:k0 + 4, :], w=["o_w"], q="pool")
            for s_ in range(2):
                p.dma(gbc[:, s_, :], S["modsd"][s_:s_ + 1, 2 * D:3 * D].partition_broadcast(128), r=["modsd"], w=["o_g"])
            for t in range(NTL):
                b = t % 2
                s_ = stream_of(t)
                p.dma(mx_[b][:], S["mixT"].rearrange("(k p) n -> p k n", p=128)[:, :, t * 128:(t + 1) * 128], r=["mixT"], w=[f"o_m{b}"])
                p.dma(xt[b][:], S["xcur"][t * 128:(t + 1) * 128, :], r=["xcur"], w=[f"o_x{b}"])
                for cb in range(4):
                    ps, pk = psf[cb], f"psf{cb}"
                    for k in range(KD):
                        p.mm(ps[:, :], mx_[b][:, k, :], wo[:, k, cb * 512:(cb + 1) * 512], k == 0, k == KD - 1, r=[f"o_m{b}", "o_w"], w=[pk])
                    p.tt("dve", tq[:], ps[:, :], gbc[:, s_, cb * 512:(cb + 1) * 512], ALU.mult, r=[pk, "o_g"], w=["o_t"])
                    p.tt("pool", xt[b][:, cb * 512:(cb + 1) * 512], xt[b][:, cb * 512:(cb + 1) * 512], tq[:], ALU.add,
                         r=["o_t", f"o_x{b}"], w=[f"o_x{b}"])
                p.dma(S["xcur"][t * 128:(t + 1) * 128, :], xt[b][:], r=[f"o_x{b}"], w=["xcur"])
            p.barrier()
        if "xmid" in dbg:
            return finish(["xcur"])

        with ExitStack() as st:
            affT = sb("r_affT", [NE, NT], stack=st)
            with ExitStack() as st2:
                wr_s = sb("r_w", [128, KD, NE], BF16, stack=st2)
                lg = sb("r_lg", [128, NE], stack=st2)
                rsm = sb("r_sm", [128, 4], stack=st2)
                p.dma(wr_s[:], I["w_r"][l].rearrange("(k p) e -> p k e", p=128), w=["r_w"], q="pool")

                def router(t, hTt, hkey):
                    for k in range(KD):
                        p.mm(psf[4][:, 0:NE], hTt[:, k, :], wr_s[:, k, :], k == 0, k == KD - 1, r=[hkey, "r_w"], w=["psf4"])
                    p.op("dve", lambda e: e.reduce_max(out=rsm[:, 0:1], in_=psf[4][:, 0:NE], axis=AX.X), r=["psf4"], w=["r_mx"])
                    p.ts("dve", rsm[:, 1:2], rsm[:, 0:1], -1.0, None, ALU.mult, r=["r_mx"], w=["r_nmx"])
                    p.act(lg[:], psf[4][:, 0:NE], AF.Exp, r=["psf4", "r_nmx"], w=["r_lg", "r_sum"], bias=rsm[:, 1:2], accum=rsm[:, 2:3])
                    p.op("dve", lambda e: e.reciprocal(rsm[:, 3:4], rsm[:, 2:3]), r=["r_sum"], w=["r_rs"])
                    p.ts("dve", lg[:], lg[:], rsm[:, 3:4], None, ALU.mult, r=["r_lg", "r_rs"], w=["r_lg"])
                    p.mm(psf[5][0:NE, 0:128], lg[:], ident_f, True, True, r=["r_lg", "cst"], w=["psf5"])
                    p.copy("act", affT[:, t * 128:(t + 1) * 128], psf[5][0:NE, 0:128], r=["psf5"], w=["r_affT"])

                norm_stage(st2, 1, None, dst_xn=S["xn2"], router=router)
                p.barrier()
            with ExitStack() as st2:
                wk = sb("r_wk", [NE, cfg.seq], stack=st2)
                wts = sb("r_wts", [NE, cfg.cap_l], stack=st2)
                ixs = sb("r_ixs", [NE, cfg.cap_l], U32, stack=st2)
                for (c0, n, cap, doff, off) in [(cfg.ctx, cfg.seq, cfg.cap_l, 0, cfg.ctx), (0, cfg.ctx, cfg.cap_c, 1, 0)]:
                    p.copy("dve", wk[:, 0:n], affT[:, c0:c0 + n], r=["r_affT"], w=["r_wk"])
                    for r_ in range(cap // 8):
                        s8 = slice(r_ * 8, r_ * 8 + 8)
                        p.op("dve", lambda e: e.max(out=wts[:, s8], in_=wk[:, 0:n]), r=["r_wk"], w=["r_wts"])
                        p.op("dve", lambda e: e.max_index(out=ixs[:, s8], in_max=wts[:, s8], in_values=wk[:, 0:n]), r=["r_wk", "r_wts"], w=["r_ixs"])
                        p.op("dve", lambda e: e.match_replace(out=wk[:, 0:n], in_to_replace=wts[:, s8], in_values=wk[:, 0:n], imm_value=-1.0),
                             r=["r_wts", "r_wk"], w=["r_wk"])
                    if off:
                        p.ts("dve", ixs[:, 0:cap], ixs[:, 0:cap], float(off), None, ALU.add, r=["r_ixs"], w=["r_ixs"])
                    dI = S["idxd"] if doff == 0 else S["idxc"]
                    dW = S["wtd"] if doff == 0 else S["wtc"]
                    p.dma(dI[:, 0:cap], ixs[:, 0:cap], r=["r_ixs"], w=["idxd"])
                    p.dma(dW[:, 0:cap], wts[:, 0:cap], r=["r_wts"], w=["idxd"])
                p.barrier()
        if "route" in dbg:
            return finish(["idxd", "wtd", "idxc", "wtc"])

        with ExitStack() as st:
            GS = min(1024, cfg.cap_l)
            xsT = sb("e_xsT", [128, KD, GS + cfg.cap_c], BF16, stack=st)
            hid = sb("e_hid", [128, KD, GS + cfg.cap_c], BF16, stack=st)
            wd = sb("e_wd", [128, KD, D], BF16, stack=st)
            wgf = [sb(f"e_wg{i}", [128, KD, 128], BF16, stack=st) for i in range(2)]
            wuf = [sb(f"e_wu{i}", [128, KD, 128], BF16, stack=st) for i in range(2)]
            xs = sb("e_xs", [128, D], BF16, stack=st)
            xr = sb("e_xr", [128, D], stack=st)
            yt = sb("e_yt", [128, 512], stack=st)
            sg2 = [sb(f"e_sg{i}", [128, 512], stack=st) for i in range(2)]
            eit = [0]
            icol = sb("e_ic", [128, 1], U32, stack=st)
            wcol = sb("e_wc", [128, 1], stack=st)
            g2 = sb("e_g2", [128, 2, D], stack=st)
            for s_ in range(2):
                p.dma(g2[:, s_, :], S["modsd"][s_:s_ + 1, 5 * D:6 * D].partition_broadcast(128), r=["modsd"], w=["e_g2"])
            for e_ in range(NE):
                for k0 in range(0, KD, 4):
                    p.dma(wd[:, k0:k0 + 4, :], I["wed"][l, e_].rearrange("(k p) c -> p k c", p=128)[:, k0:k0 + 4, :], w=["e_wd"], q="pool")
                lat = [(0, g0, min(GS, cfg.cap_l - g0)) for g0 in range(0, cfg.cap_l, GS)]
                groups = [[(s_, g0, ns, 0)] for (s_, g0, ns) in lat]
                groups[-1].append((1, 0, cfg.cap_c, lat[-1][2]))
                for segs in groups:
                    ncol = sum(sg_[2] for sg_ in segs)
                    tiles = []
                    for (s_, g0, ns, col0) in segs:
                        for j0 in range(0, ns, 128):
                            tiles.append((s_, g0 + j0, min(128, ns - j0), col0 + j0))
                    for (s_, q0, rows, c0_) in tiles:
                        dI = S["idxd"] if s_ == 0 else S["idxc"]
                        p.dma(icol[0:rows, :], dI[e_, q0:q0 + rows].rearrange("(p o) -> p o", o=1), r=["idxd"], w=["e_ic"])
                        p.op("pool", lambda e: e.indirect_dma_start(out=xs[0:rows, :], out_offset=None, in_=S["xn2"][:, :],
                                                                      in_offset=bass.IndirectOffsetOnAxis(ap=icol[0:rows, 0:1], axis=0)),
                             r=["e_ic", "xn2"], w=["e_xs"], dma=True)
                        for k in range(KD):
                            pb = pst[k // 8]
                            p.tr(pb[:, (k % 8) * 128:(k % 8) * 128 + rows], xs[0:rows, k * 128:(k + 1) * 128], ident_b[0:rows, 0:rows],
                                 r=["e_xs", "cstb"], w=[f"pst{k // 8}"])
                        for k in range(KD):
                            pb = pst[k // 8]
                            src = pb[:, (k % 8) * 128:(k % 8) * 128 + rows]
                            if k % 2 == 0:
                                p.ts("dve", xsT[:, k, c0_:c0_ + rows], src, amul[:, 1, k, s_:s_ + 1], modp[:, 3, k, s_:s_ + 1],
                                     ALU.mult, ALU.add, r=[f"pst{k // 8}"], w=["e_xsT"])
                            else:
                                p.act(xsT[:, k, c0_:c0_ + rows], src, AF.Identity, r=[f"pst{k // 8}"], w=["e_xsT"],
                                      scale=amul[:, 1, k, s_:s_ + 1], bias=modp[:, 3, k, s_:s_ + 1])
                    for f in range(KD):
                        b = f % 2
                        p.dma(wgf[b][:], I["weg"][l, e_].rearrange("(k p) c -> p k c", p=128)[:, :, f * 128:(f + 1) * 128], w=[f"e_wg{b}"], q="pool")
                        p.dma(wuf[b][:], I["weu"][l, e_].rearrange("(k p) c -> p k c", p=128)[:, :, f * 128:(f + 1) * 128], w=[f"e_wu{b}"], q="pool")
                        for s0 in range(0, ncol, 512):
                            n_ = min(512, ncol - s0)
                            eit[0] += 1
                            ia, ib = (0, 1) if eit[0] % 2 == 0 else (4, 5)
                            sgb, sgk = sg2[eit[0] % 2], f"e_sg{eit[0] % 2}"
                            for k in range(KD):
                                p.mm(psf[ia][:, 0:n_], wgf[b][:, k, :], xsT[:, k, s0:s0 + n_], k == 0, k == KD - 1, r=[f"e_wg{b}", "e_xsT"], w=[f"psf{ia}"])
                            for k in range(KD):
                                p.mm(psf[ib][:, 0:n_], wuf[b][:, k, :], xsT[:, k, s0:s0 + n_], k == 0, k == KD - 1, r=[f"e_wu{b}", "e_xsT"], w=[f"psf{ib}"])
                            p.act(sgb[:, 0:n_], psf[ia][:, 0:n_], AF.Silu, r=[f"psf{ia}"], w=[sgk])
                            p.tt("dve", hid[:, f, s0:s0 + n_], sgb[:, 0:n_], psf[ib][:, 0:n_], ALU.mult, r=[sgk, f"psf{ib}"], w=["e_hid"])
                    for (s_, q0, rows, c0_) in tiles:
                        dI = S["idxd"] if s_ == 0 else S["idxc"]
                        dW = S["wtd"] if s_ == 0 else S["wtc"]
                        p.dma(icol[0:rows, :], dI[e_, q0:q0 + rows].rearrange("(p o) -> p o", o=1), r=["idxd"], w=["e_ic"])
                        p.dma(wcol[0:rows, :], dW[e_, q0:q0 + rows].rearrange("(p o) -> p o", o=1), r=["idxd"], w=["e_wc"])
                        p.op("pool", lambda e: e.indirect_dma_start(out=xr[0:rows, :], out_offset=None, in_=S["xcur"][:, :],
                                                                      in_offset=bass.IndirectOffsetOnAxis(ap=icol[0:rows, 0:1], axis=0)),
                             r=["e_ic", "xcur"], w=["e_xr"], dma=True)
                        for cb in range(4):
                            ps, pk = psf[2 + cb % 2], f"psf{2 + cb % 2}"
                            for f in range(KD):
                                p.mm(ps[0:rows, :], hid[:, f, c0_:c0_ + rows], wd[:, f, cb * 512:(cb + 1) * 512], f == 0, f == KD - 1,
                                     r=["e_hid", "e_wd"], w=[pk])
                            p.stt(yt[0:rows, :], ps[0:rows, :], wcol[0:rows, 0:1], g2[0:rows, s_, cb * 512:(cb + 1) * 512], ALU.mult, ALU.mult,
                                  r=[pk, "e_wc", "e_g2"], w=["e_yt"])
                            p.tt("dve", xr[0:rows, cb * 512:(cb + 1) * 512], xr[0:rows, cb * 512:(cb + 1) * 512], yt[0:rows, :], ALU.add,
                                 r=["e_yt", "e_xr"], w=["e_xr"])
                        p.op("pool", lambda e: e.indirect_dma_start(out=S["xcur"][:, :],
                                                                      out_offset=bass.IndirectOffsetOnAxis(ap=icol[0:rows, 0:1], axis=0),
                                                                      in_=xr[0:rows, :], in_offset=None),
                             r=["e_ic", "e_xr"], w=["xcur"], dma=True)
            p.barrier()
        if "layer" in dbg:
            return finish(["xcur"])

    big_copy(yout, S["xcur"][cfg.ctx:, :], cfg.seq, key="yout")
    p.barrier()
    es.close()
    return nc


def prep_inputs(cfg, inp):
    L = cfg.depth
    f = lambda a: np.ascontiguousarray(np.asarray(a, dtype=np.float32))
    pk = lambda v: f(np.asarray(v).reshape(-1, 128).T)
    m = {}
    m["xin"] = f(np.concatenate([inp["ctx"][0], inp["x"][0]], 0))
    m["cT"] = f(np.stack([pk(inp["c"][0]), pk(inp["c_ctx"])], -1))
    m["w_mod"] = f(inp["w_mod"])
    m["b_mod"] = f(inp["b_mod"]).reshape(L, 1, -1)
    m["n1g"] = f(np.stack([pk(inp["norm1_g"][l]) for l in range(L)]))
    m["n2g"] = f(np.stack([pk(inp["norm2_g"][l]) for l in range(L)]))
    m["w_in"] = f(inp["w_in"])
    m["wg"] = f(inp["gla_w_gate"])
    m["bg"] = f(np.stack([np.concatenate([pk(inp["gla_b_gate"][l][d]) for d in range(2)], 1) for l in range(L)]))
    m["gng"] = f(inp["gla_norm_g"]).reshape(L, 1, 128)
    m["nqg"] = f(inp["na_q_norm_g"]).reshape(L, 128, 1)
    m["nkg"] = f(inp["na_k_norm_g"]).reshape(L, 128, 1)
    bts = []
    for l in range(L):
        bt, _, _, _ = _na_masks(cfg, np.asarray(inp["na_rpb"][l], np.float32))
        bts.append(bt.transpose(0, 1, 3, 2, 4))
    m["bt"] = f(np.stack(bts))
    m["cw"] = f(np.stack([np.asarray(inp["lru_conv_w"][l]).reshape(4, 4, 128).transpose(2, 1, 0) for l in range(L)]))
    m["cb"] = f(np.stack([pk(inp["lru_conv_b"][l]) for l in range(L)]))
    m["wa"] = f(inp["lru_w_a"])
    m["wi"] = f(inp["lru_w_i"])
    cat2 = lambda a, l: np.concatenate([pk(a[l][d]) for d in range(2)], 1)
    m["ba"] = f(np.stack([cat2(inp["lru_b_a"], l) for l in range(L)]))
    m["bi"] = f(np.stack([cat2(inp["lru_b_i"], l) for l in range(L)]))
    m["lam"] = f(np.stack([cat2(inp["lru_lambda"], l) for l in range(L)]))
    m["w_out"] = f(inp["w_out"])
    m["w_r"] = f(inp["w_router"])
    m["weg"] = f(inp["w_exp_gate"])
    m["weu"] = f(inp["w_exp_up"])
    m["wed"] = f(inp["w_exp_down"])
    m["rope"] = _rope_tables(cfg)
    tri = np.tril(np.ones((128, 128), np.float32))
    m["consts"] = f(np.stack([np.eye(128), np.ones((128, 128)), tri.T, tri, np.zeros((128, 128)), np.zeros((128, 128))]))
    return m


def kernel(**inputs):
    cfg = Cfg()
    nc = build_program(cfg)
    m = prep_inputs(cfg, inputs)
    res = run_bass_kernel_spmd(nc, [m], core_ids=[0])
    return np.asarray(res.results[0]["yout"], dtype=np.float32).reshape(1, cfg.seq, D)
```
